# Optimizing a Trainium2 kernel written in Bass

```python
import math
import jax, jax.numpy as jnp
from jax import lax
import numpy as np

D_MODEL = 1024
BATCH = 8
SEQ = 2048
DEPTH = 4

N_MIXERS = 2
N_RWKV_LAYERS = (DEPTH + 1) // 2
N_ATTN_LAYERS = DEPTH // 2
RMS_EPS = 1e-6
N_MOD = 6

RWKV_HEAD = 64
RWKV_HEADS = D_MODEL // RWKV_HEAD
D_DECAY_LORA = 64
D_AAA_LORA = 64
D_GATE_LORA = 128
RWKV_GN_EPS = 64e-5
N_DIRS = 2
N_SHIFT_MIX = 6

ATTN_GROUPS = ((128, 1), (512, 4), (2048, 16))
N_GROUPS = len(ATTN_GROUPS)
ATTN_HEADS = 8
ATTN_HEAD_DIM = D_MODEL // ATTN_HEADS
QKV_WIDTH = N_GROUPS * 3 * ATTN_HEADS * ATTN_HEAD_DIM
Q_BLOCK = 64
NEG_INF = -1e30

N_EXPERT_GROUPS = 4
EXPERTS_PER_GROUP = 8
TOP_K_INNER = 2
D_EXPERT = D_MODEL // 4

kernel_name = "hybrid_rwkv7_dilated_alibi_hmoe_encoder"


def rms_norm(x, g):
    x32 = x.astype(jnp.float32)
    y = x32 * lax.rsqrt(jnp.mean(x32 * x32, axis=-1, keepdims=True) + RMS_EPS)
    return (y * g.astype(jnp.float32)).astype(x.dtype)


def modulate(h, shift, scale):
    return h * (1.0 + scale[:, None, :]) + shift[:, None, :]


def _wkv7_scan(r, w, k, v, a, b, reverse):
    bsz, _, h, n = r.shape

    def step(state, inp):
        r_t, w_t, k_t, v_t, a_t, b_t = inp
        sa = jnp.einsum('bhvk,bhk->bhv', state, a_t)
        state = (state * w_t[:, :, None, :]
                 + sa[:, :, :, None] * b_t[:, :, None, :]
                 + v_t[:, :, :, None] * k_t[:, :, None, :])
        y_t = jnp.einsum('bhvk,bhk->bhv', state, r_t)
        return state, y_t

    s0 = jnp.zeros((bsz, h, n, n), jnp.float32)
    xs = tuple(jnp.moveaxis(t, 1, 0) for t in (r, w, k, v, a, b))
    _, ys = lax.scan(step, s0, xs, reverse=reverse)
    return jnp.moveaxis(ys, 0, 1)


def rwkv7_mix(h, mu, w_rkv, w0, w1, w2, a0, a1, a2, g1, g2, k_k, k_a, r_k, ln_w, ln_b, w_o):
    bsz, s, d = h.shape
    f32 = jnp.float32
    x_prev = jnp.pad(h, ((0, 0), (1, 0), (0, 0)))[:, :-1]
    x_next = jnp.pad(h, ((0, 0), (0, 1), (0, 0)))[:, 1:]
    xx = 0.5 * (x_prev + x_next) - h
    xs = h[:, :, None, :] + xx[:, :, None, :] * mu
    rkv = jnp.einsum('bsjd,jde->bsje', xs[:, :, :3], w_rkv)
    r, k, v = rkv[:, :, 0], rkv[:, :, 1], rkv[:, :, 2]
    xw, xa, xg = xs[:, :, 3], xs[:, :, 4], xs[:, :, 5]

    def lora(xin, A, Bm, act):
        return jnp.einsum('zbsr,zre->zbse', act(jnp.einsum('bsd,zdr->zbsr', xin, A)), Bm)

    w_log = -jax.nn.softplus(-(w0[:, None, None, :] + lora(xw, w1, w2, jnp.tanh)).astype(f32)) - 0.5
    decay = jnp.exp(-jnp.exp(w_log))
    a = jax.nn.sigmoid((a0[:, None, None, :] + lora(xa, a1, a2, lambda t: t)).astype(f32))
    g = lora(xg, g1, g2, jax.nn.sigmoid).astype(f32)

    def heads(t):
        return t.reshape(t.shape[:-1] + (RWKV_HEADS, RWKV_HEAD))

    r32, k32, v32 = (heads(t.astype(f32)) for t in (r, k, v))
    kk = k32 * heads(k_k.astype(f32))
    kk = kk / jnp.maximum(jnp.sqrt(jnp.sum(kk * kk, axis=-1, keepdims=True)), 1e-12)
    a_h, decay_h = heads(a), heads(decay)
    k_dir = k32[None] * (1.0 + (a_h - 1.0) * heads(k_a.astype(f32)))
    b_dir = kk[None] * a_h
    y_f = _wkv7_scan(r32, decay_h[0], k_dir[0], v32, -kk, b_dir[0], reverse=False)
    y_b = _wkv7_scan(r32, decay_h[1], k_dir[1], v32, -kk, b_dir[1], reverse=True)
    y = jnp.stack([y_f, y_b])
    mean = jnp.mean(y, axis=-1, keepdims=True)
    var = jnp.mean(jnp.square(y - mean), axis=-1, keepdims=True)
    y = (y - mean) * lax.rsqrt(var + RWKV_GN_EPS) * heads(ln_w.astype(f32)) + heads(ln_b.astype(f32))
    bonus = jnp.sum(r32[None] * k_dir * r_k.astype(f32), axis=-1, keepdims=True) * v32[None]
    o = jnp.sum(heads(g) * (y + bonus), axis=0).reshape(bsz, s, d)
    return jnp.einsum('bsd,de->bse', o.astype(h.dtype), w_o)


def alibi_slopes(n):
    return 2.0 ** (-8.0 * jnp.arange(1, n + 1, dtype=jnp.float32) / n)


def _dilated_group(q, k, v, slopes, window, dilation):
    bsz, s, h, dh = q.shape
    half = window // (2 * dilation)
    L = s // dilation
    nb = -(-L // Q_BLOCK)
    lp = nb * Q_BLOCK
    kb_len = Q_BLOCK + 2 * half

    def strided(t):
        return t.reshape(bsz, L, dilation, h, dh).transpose(0, 2, 3, 1, 4)

    qd = jnp.pad(strided(q), ((0, 0), (0, 0), (0, 0), (0, lp - L), (0, 0)))
    qd = qd.reshape(bsz, dilation, h, nb, Q_BLOCK, dh)
    pad_kv = ((0, 0), (0, 0), (0, 0), (half, lp - L + half), (0, 0))
    kd = jnp.pad(strided(k), pad_kv)
    vd = jnp.pad(strided(v), pad_kv)
    key_idx = jnp.arange(nb)[:, None] * Q_BLOCK + jnp.arange(kb_len)[None, :]
    kblk = kd[:, :, :, key_idx]
    vblk = vd[:, :, :, key_idx]
    scores = jnp.einsum('brhnqd,brhnkd->brhnqk', qd, kblk) * (dh ** -0.5)
    q_pos = jnp.arange(lp).reshape(nb, Q_BLOCK)
    k_pos = key_idx - half
    rel = jnp.abs(k_pos[:, None, :] - q_pos[:, :, None])
    valid = (rel <= half) & (k_pos[:, None, :] >= 0) & (k_pos[:, None, :] < L)
    alibi = -slopes[:, None, None, None] * (rel * dilation).astype(jnp.float32)
    scores = jnp.where(valid, scores + alibi[None, None], NEG_INF)
    m = jnp.max(scores, axis=-1, keepdims=True)
    e = jnp.exp(scores - m)
    den = jnp.sum(e, axis=-1, keepdims=True)
    o = jnp.einsum('brhnqk,brhnkd->brhnqd', e, vblk) / den
    lse = (m + jnp.log(den))[..., 0]

    def unstride(t):
        t = t.reshape((bsz, dilation, h, lp) + t.shape[5:])[:, :, :, :L]
        t = jnp.moveaxis(t, 3, 1)
        return t.reshape((bsz, s, h) + t.shape[4:])

    return unstride(o), unstride(lse)


def dilated_attention_mix(h, w_qkv, w_o):
    bsz, s, d = h.shape
    qkv = jnp.einsum('bsd,de->bse', h, w_qkv).astype(jnp.float32)
    qkv = qkv.reshape(bsz, s, N_GROUPS, 3, ATTN_HEADS, ATTN_HEAD_DIM)
    slopes = alibi_slopes(N_GROUPS * ATTN_HEADS).reshape(N_GROUPS, ATTN_HEADS)
    outs, lses = [], []
    for gi, (window, dil) in enumerate(ATTN_GROUPS):
        o, lse = _dilated_group(qkv[:, :, gi, 0], qkv[:, :, gi, 1], qkv[:, :, gi, 2], slopes[gi], window, dil)
        outs.append(o)
        lses.append(lse)
    o = jnp.stack(outs)
    alpha = jax.nn.softmax(jnp.stack(lses), axis=0)
    merged = jnp.sum(alpha[..., None] * o, axis=0).reshape(bsz, s, d)
    return jnp.einsum('bsd,de->bse', merged.astype(h.dtype), w_o)


def hier_moe(h, router_g, router_g_b, router_e, router_e_b, w_gate, w_up, w_down):
    bsz, s, d = h.shape
    xt = h.reshape(-1, d)
    f32 = jnp.float32
    g_logits = (xt @ router_g + router_g_b).astype(f32)
    g_prob = jax.nn.softmax(g_logits, axis=-1)
    _, g_top = lax.top_k(g_logits, 1)
    p_group = jnp.take_along_axis(g_prob, g_top, axis=-1)
    e_logits = (jnp.einsum('td,gde->tge', xt, router_e) + router_e_b).astype(f32)
    e_sel = jnp.take_along_axis(e_logits, g_top[:, :, None], axis=1)[:, 0]
    top_v, top_i = lax.top_k(e_sel, TOP_K_INNER)
    top_w = jax.nn.softmax(top_v, axis=-1) * p_group
    inner = jnp.sum(jax.nn.one_hot(top_i, EXPERTS_PER_GROUP, dtype=f32) * top_w[..., None], axis=1)
    gates = jax.nn.one_hot(g_top[:, 0], N_EXPERT_GROUPS, dtype=f32)[:, :, None] * inner[:, None, :]
    out = jnp.zeros((xt.shape[0], d), f32)
    for gi in range(N_EXPERT_GROUPS):
        hid = jax.nn.silu(jnp.einsum('td,edf->tef', xt, w_gate[gi])) * jnp.einsum('td,edf->tef', xt, w_up[gi])
        hid = hid * gates[:, gi, :, None].astype(hid.dtype)
        out = out + jnp.einsum('tef,efd->td', hid, w_down[gi]).astype(f32)
    return out.reshape(bsz, s, d).astype(h.dtype)


def setup_inputs(seed: int = 0) -> dict:
    key = jax.random.key(seed)
    ks = iter(jax.random.split(key, 48))
    f32 = jnp.float32
    D, NR, NA = D_MODEL, N_RWKV_LAYERS, N_ATTN_LAYERS
    G, E, F = N_EXPERT_GROUPS, EXPERTS_PER_GROUP, D_EXPERT

    def nrm(shape, scale):
        return jax.random.normal(next(ks), shape, f32) * scale

    def uni(shape, lo, hi):
        return jax.random.uniform(next(ks), shape, f32, minval=lo, maxval=hi)

    return {
        "x": nrm((BATCH, SEQ, D), 1.0),
        "c": nrm((BATCH, D), 1.0),
        "ada_w": nrm((DEPTH, D, N_MOD * D), 0.5 * D ** -0.5),
        "ada_b": nrm((DEPTH, N_MOD * D), 0.01),
        "norm_tm_g": 1.0 + nrm((DEPTH, D), 0.02),
        "norm_cm_g": 1.0 + nrm((DEPTH, D), 0.02),
        "rw_mu": uni((NR, N_SHIFT_MIX, D), 0.0, 1.0),
        "rw_w_rkv": nrm((NR, 3, D, D), D ** -0.5),
        "rw_w0": uni((NR, N_DIRS, D), -4.5, -0.5),
        "rw_w1": nrm((NR, N_DIRS, D, D_DECAY_LORA), D ** -0.5),
        "rw_w2": nrm((NR, N_DIRS, D_DECAY_LORA, D), 0.5 * D_DECAY_LORA ** -0.5),
        "rw_a0": nrm((NR, N_DIRS, D), 0.5),
        "rw_a1": nrm((NR, N_DIRS, D, D_AAA_LORA), D ** -0.5),
        "rw_a2": nrm((NR, N_DIRS, D_AAA_LORA, D), 0.5 * D_AAA_LORA ** -0.5),
        "rw_g1": nrm((NR, N_DIRS, D, D_GATE_LORA), D ** -0.5),
        "rw_g2": nrm((NR, N_DIRS, D_GATE_LORA, D), D_GATE_LORA ** -0.5),
        "rw_k_k": 0.85 + nrm((NR, D), 0.05),
        "rw_k_a": 1.0 + nrm((NR, D), 0.05),
        "rw_r_k": nrm((NR, RWKV_HEADS, RWKV_HEAD), 0.1),
        "rw_ln_w": 1.0 + nrm((NR, D), 0.02),
        "rw_ln_b": nrm((NR, D), 0.01),
        "rw_w_o": nrm((NR, D, D), D ** -0.5),
        "at_w_qkv": nrm((NA, D, QKV_WIDTH), D ** -0.5),
        "at_w_o": nrm((NA, D, D), D ** -0.5),
        "moe_router_g": nrm((DEPTH, D, G), D ** -0.5),
        "moe_router_g_b": nrm((DEPTH, G), 0.01),
        "moe_router_e": nrm((DEPTH, G, D, E), D ** -0.5),
        "moe_router_e_b": nrm((DEPTH, G, E), 0.01),
        "moe_w_gate": nrm((DEPTH, G, E, D, F), D ** -0.5),
        "moe_w_up": nrm((DEPTH, G, E, D, F), D ** -0.5),
        "moe_w_down": nrm((DEPTH, G, E, F, D), F ** -0.5),
        "final_g": 1.0 + nrm((D,), 0.02),
    }


def reference(x, c, ada_w, ada_b, norm_tm_g, norm_cm_g,
              rw_mu, rw_w_rkv, rw_w0, rw_w1, rw_w2, rw_a0, rw_a1, rw_a2, rw_g1, rw_g2,
              rw_k_k, rw_k_a, rw_r_k, rw_ln_w, rw_ln_b, rw_w_o,
              at_w_qkv, at_w_o,
              moe_router_g, moe_router_g_b, moe_router_e, moe_router_e_b,
              moe_w_gate, moe_w_up, moe_w_down, final_g):
    silu_c = jax.nn.silu(c)
    for i in range(DEPTH):
        mod = silu_c @ ada_w[i] + ada_b[i]
        sh_t, sc_t, ga_t, sh_c, sc_c, ga_c = jnp.split(mod, N_MOD, axis=-1)
        hn = modulate(rms_norm(x, norm_tm_g[i]), sh_t, sc_t)
        j = i // N_MIXERS
        if i % N_MIXERS == 0:
            tm = rwkv7_mix(hn, rw_mu[j], rw_w_rkv[j], rw_w0[j], rw_w1[j], rw_w2[j],
                           rw_a0[j], rw_a1[j], rw_a2[j], rw_g1[j], rw_g2[j],
                           rw_k_k[j], rw_k_a[j], rw_r_k[j], rw_ln_w[j], rw_ln_b[j], rw_w_o[j])
        else:
            tm = dilated_attention_mix(hn, at_w_qkv[j], at_w_o[j])
        x = x + ga_t[:, None, :] * tm
        hn = modulate(rms_norm(x, norm_cm_g[i]), sh_c, sc_c)
        cm = hier_moe(hn, moe_router_g[i], moe_router_g_b[i], moe_router_e[i], moe_router_e_b[i],
                      moe_w_gate[i], moe_w_up[i], moe_w_down[i])
        x = x + ga_c[:, None, :] * cm
    return rms_norm(x, final_g)
```

```python
import numpy as np
from contextlib import ExitStack

import concourse.bass as bass
import concourse.mybir as mybir
from concourse.bass_utils import run_bass_kernel_spmd

F32 = mybir.dt.float32
BF16 = mybir.dt.bfloat16
AF = mybir.ActivationFunctionType
ALU = mybir.AluOpType
AX = mybir.AxisListType

D = 1024
S = 2048
DEPTH = 4
NKC = 8
NTB = 4
NTT = 16
RMS_EPS = 1e-6
SEM_LIM = 24000


class Res:
    __slots__ = ("name", "w", "rs", "excl")

    def __init__(self, name):
        self.name = name
        self.w = None
        self.rs = {}
        self.excl = False

    def add_reader(self, tag):
        st, ep, v = tag
        if self.rs.get(st, (-1, 0)) < (ep, v):
            self.rs[st] = (ep, v)


class KB:
    COMPUTE = ("pe", "act", "dve", "pool")

    def __init__(self, nc):
        self.nc = nc
        self.es = ExitStack()
        self.eng = {"pe": nc.tensor, "act": nc.scalar, "dve": nc.vector, "pool": nc.gpsimd, "sp": nc.sync}
        self.sems = {}
        self.cnt = {}
        self.seen = {e: {} for e in self.eng}
        self.nres = 0
        self.epoch_final = {}
        self.ninst = {e: 0 for e in self.eng}

    def res(self, name=None):
        self.nres += 1
        return Res(name or f"r{self.nres}")

    def sb(self, name, shape, dt):
        return self.es.enter_context(self.nc.sbuf_tensor(name, list(shape), dt))

    def _sem(self, stream, epoch):
        key = (stream, epoch)
        if key not in self.sems:
            self.sems[key] = self.es.enter_context(self.nc.semaphore(f"s_{stream}_{epoch}"))
        return self.sems[key]

    def _bump(self, stream, inc):
        ep, v = self.cnt.get(stream, (0, 0))
        if v + inc > SEM_LIM:
            self.epoch_final[(stream, ep)] = v
            ep, v = ep + 1, 0
        v += inc
        self.cnt[stream] = (ep, v)
        return (stream, ep, v), self._sem(stream, ep)

    def _wait(self, eng, tags):
        best = {}
        for t in tags:
            if t is None:
                continue
            st, ep, v = t
            if st == "pe" and eng == "pe":
                continue
            if (st not in best) or (ep, v) > best[st]:
                best[st] = (ep, v)
        for st, (ep, v) in best.items():
            if st.startswith("d_"):
                cur_ep, cur_v = self.cnt[st]
                v = cur_v if cur_ep == ep else self.epoch_final[(st, ep)]
            if self.seen[eng].get(st, (-1, 0)) >= (ep, v):
                continue
            self.eng[eng].wait_ge(self._sem(st, ep), v)
            self.seen[eng][st] = (ep, v)

    def _deps(self, r, w, same_stream=None):
        tags = []
        for x in r:
            tags.append(x.w)
            if x.excl:
                for st, (ep, v) in x.rs.items():
                    if same_stream is not None and st == same_stream:
                        continue
                    tags.append((st, ep, v))
        for x in w:
            tags.append(x.w)
            for st, (ep, v) in x.rs.items():
                if same_stream is not None and st == same_stream:
                    continue
                tags.append((st, ep, v))
        return tags

    def op(self, eng, fn, r=(), w=()):
        self._wait(eng, self._deps(r, w, same_stream=eng))
        ins = fn(self.eng[eng])
        tag, sem = self._bump(eng, 1)
        ins.then_inc(sem, 1)
        self.ninst[eng] += 1
        for x in r:
            x.add_reader(tag)
        for x in w:
            x.w = tag
            x.rs = {}
        return tag

    def dma(self, q, chan, out, in_, r=(), w=(), **kw):
        self._wait(q, self._deps(r, w))
        ins = self.eng[q].dma_start(out=out, in_=in_, **kw)
        tag, sem = self._bump("d_" + chan, 16)
        ins.then_inc(sem, 16)
        self.ninst[q] += 1
        for x in r:
            x.add_reader(tag)
        for x in w:
            x.w = tag
            x.rs = {}
        return tag

    def barrier(self, engines=("pe", "act", "dve", "pool", "sp")):
        tags = [(st, ep, v) for st, (ep, v) in self.cnt.items()]
        for e in engines:
            self._wait(e, tags)

    def wait_all(self, eng):
        self._wait(eng, [(st, ep, v) for st, (ep, v) in self.cnt.items()])


class Prog:
    def __init__(self, nc, cfg):
        self.nc = nc
        self.cfg = cfg
        self.k = KB(nc)
        self.dram = {}
        self._uid = 0

    def sbuf(self, name, shape, dt):
        self._uid += 1
        return self.nc.sbuf_tensor(f"{name}_u{self._uid}", list(shape), dt)

    def dump(self, name, ap, shape, dt, rs):
        if name not in self.cfg.get("dump", ()):
            return
        o = self.dout("dbg_" + name, shape, dt)
        self.k.dma("sp", "dbg", o, ap, r=rs)
        self.k.barrier()

    def din(self, name, shape, dt=F32):
        self.dram[name] = self.nc.dram_tensor(name, list(shape), dt, kind="ExternalInput").ap()
        return self.dram[name]

    def dout(self, name, shape, dt=F32):
        self.dram[name] = self.nc.dram_tensor(name, list(shape), dt, kind="ExternalOutput").ap()
        return self.dram[name]

    def build(self):
        nc, k, cfg = self.nc, self.k, self.cfg
        layers = cfg.get("layers", list(range(DEPTH)))
        d = self.dram
        self.din("x", [S, D])
        self.din("cT", [128, NKC])
        self.din("ada_w", [DEPTH, D, 6 * D])
        self.din("ada_bT", [DEPTH, 128, 48])
        self.din("gtmT", [DEPTH, 128, NKC])
        self.din("gcmT", [DEPTH, 128, NKC])
        self.din("gfinT", [128, NKC])
        self.din("wr", [DEPTH, 128, NKC, 36])
        self.din("br", [DEPTH, 1, 36])
        self.din("moe_w_gate", [DEPTH, 4, 8, D, 256])
        self.din("moe_w_up", [DEPTH, 4, 8, D, 256])
        self.din("moe_w_down", [DEPTH, 4, 8, 256, D])
        self.din("rw_muT", [2, 128, 6, NKC])
        self.din("rw_w_rkv", [2, 3, D, D])
        self.din("rw_w1cat", [2, D, 128])
        self.din("rw_a1cat", [2, D, 128])
        self.din("rw_g1", [2, 2, D, 128])
        self.din("rw_w2cat", [2, 128, D])
        self.din("rw_a2cat", [2, 128, D])
        self.din("rw_g2", [2, 2, 128, D])
        self.din("rw_w0", [2, 2, D])
        self.din("rw_a0", [2, 2, D])
        for n_ in ("rw_k_k", "rw_k_a", "rw_r_k", "rw_ln_w", "rw_ln_b"):
            self.din(n_, [2, D])
        self.din("rw_w_o", [2, D, D])
        self.din("c_tri", [6, 128, 128])
        self.din("c_csel", [128, 2])
        self.din("c_mask", [6, 64, 64])
        itn = lambda n_, shp, dt=F32: nc.dram_tensor(n_, list(shp), dt, kind="Internal").ap()
        self.scr = {"RAW": itn("scr_raw", [2, S, D]), "RAWV": itn("scr_rawv", [S, D], BF16),
                    "FM": itn("scr_fm", [2, 4, NTT, 64, 16, 128], BF16),
                    "TM": itn("scr_tm", [2, 2, S, D], BF16), "GG": itn("scr_gg", [2, S, D]), "RK": itn("scr_rk", [2, S, 16])}
        self.r_scr = {"RAW": [[k.res() for _ in range(NTT)] for _ in range(3)],
                      "FM": [[[k.res() for _ in range(NTT)] for _ in range(4)] for _ in range(2)],
                      "TM": [[[k.res() for _ in range(NTT)] for _ in range(2)] for _ in range(2)],
                      "GG": [[k.res() for _ in range(NTT)] for _ in range(2)],
                      "RK": [[k.res() for _ in range(NTT)] for _ in range(2)]}
        self.din("at_w_qkv", [2, D, 9216])
        self.din("at_w_o", [2, D, D])
        self.din("c_abias", [24, 128, 384])
        self.din("c_ident", [128, 128])
        self.din("c_sel", [32, 32 * 128])
        self.dout("out", [S, D])

        self.xT = k.sb("xT", [128, NKC, S], F32)
        self.hT = k.sb("hT", [128, NKC, S], BF16)
        self.r_xT = [[k.res(f"xT{kc}_{tb}") for tb in range(NTB)] for kc in range(NKC)]
        self.r_hT = [[k.res(f"hT{kc}_{tb}") for tb in range(NTB)] for kc in range(NKC)]
        self.ident = k.sb("ident", [128, 128], F32)
        self.ones = k.sb("ones", [128, 128], F32)
        self.epsT = k.sb("epsT", [128, 1], F32)
        self.onesb = k.sb("onesb", [128, 128], BF16)
        self.r_const = k.res("const")
        self.modT = k.sb("modT", [128, DEPTH, 48], F32)
        self.mod1T = k.sb("mod1T", [128, DEPTH, 48], F32)
        self.r_mod = k.res("mod")
        self.gT = k.sb("gT", [128, 2 * DEPTH + 1, NKC], F32)
        self.ps = [self.k.es.enter_context(nc.psum_tensor(f"ps{i}", [128, 512], F32)) for i in range(8)]
        self.r_ps = [k.res(f"ps{i}") for i in range(8)]
        for r_ in self.r_ps:
            r_.excl = True

        k.dma("sp", "c0", self.ident[:], d["c_ident"][:, :], w=[self.r_const])
        k.op("dve", lambda e: e.memset(self.ones[:], 1.0), w=[self.r_const])
        k.op("dve", lambda e: e.memset(self.epsT[:], RMS_EPS), w=[self.r_const])
        k.op("dve", lambda e: e.memset(self.onesb[:], 1.0), w=[self.r_const])
        k.dma("sp", "c0", self.gT[:, 0:DEPTH, :], d["gtmT"].rearrange("l p k -> p l k"), w=[self.r_const])
        k.dma("sp", "c0", self.gT[:, DEPTH:2 * DEPTH, :], d["gcmT"].rearrange("l p k -> p l k"), w=[self.r_const])
        k.dma("sp", "c0", self.gT[:, 2 * DEPTH, :], d["gfinT"][:, :], w=[self.r_const])

        self.load_x()
        self.ada_all()
        k.barrier()
        for i in layers:
            if cfg.get("mixers", True):
                self.norm_mod(i, 0)
                if i % 2 == 0:
                    self.rwkv(i)
                else:
                    self.attn(i)
            if cfg.get("moe", True):
                self.moe(i)
        self.final()
        k.barrier()
        k.wait_all("sp")
        k.wait_all("pool")

    def load_x(self):
        nc, k, d = self.nc, self.k, self.dram
        with ExitStack() as es:
            st = [es.enter_context(self.sbuf(f"xst{i}", [128, D], F32)) for i in range(2)]
            r_st = [k.res() for _ in range(2)]
            for tt in range(NTT):
                b = tt % 2
                k.dma("sp", f"xst{b}", st[b][:], d["x"][tt * 128:(tt + 1) * 128, :], w=[r_st[b]])
                for half in range(2):
                    pi = (tt * 2 + half) % 4
                    for j in range(4):
                        kc = half * 4 + j
                        k.op("pe", lambda e, kc=kc, j=j, pi=pi, b=b: e.transpose(
                            self.ps[pi][:, j * 128:(j + 1) * 128], st[b][:, kc * 128:(kc + 1) * 128], self.ident[:]),
                            r=[r_st[b], self.r_const], w=[self.r_ps[pi]])
                    tb = tt // 4
                    ws = [self.r_xT[half * 4 + j][tb] for j in range(4)]
                    eng = "act" if half else "dve"
                    if eng == "dve":
                        k.op("dve", lambda e, half=half, pi=pi, tt=tt: e.tensor_copy(
                            out=self.xT[:, half * 4:half * 4 + 4, tt * 128:(tt + 1) * 128],
                            in_=self.ps[pi][:, :].rearrange("p (j t) -> p j t", j=4)),
                            r=[self.r_ps[pi]], w=ws)
                    else:
                        k.op("act", lambda e, half=half, pi=pi, tt=tt: e.copy(
                            out=self.xT[:, half * 4:half * 4 + 4, tt * 128:(tt + 1) * 128],
                            in_=self.ps[pi][:, :].rearrange("p (j t) -> p j t", j=4)),
                            r=[self.r_ps[pi]], w=ws)
            k.barrier()

    def ada_all(self):
        nc, k, d = self.nc, self.k, self.dram
        NP = 8
        PW = 6 * D // NP
        with ExitStack() as es:
            scT = es.enter_context(self.sbuf("scT", [128, NKC], F32))
            cT = es.enter_context(self.sbuf("cTs", [128, NKC], F32))
            abT = es.enter_context(self.sbuf("abT", [128, DEPTH, 48], F32))
            wst = [es.enter_context(self.sbuf(f"adaw{i}", [128, NKC, PW], F32)) for i in range(2)]
            r_w = [k.res() for _ in range(2)]
            r_sc = k.res()
            r_ab = k.res()
            k.dma("sp", "c1", cT[:], d["cT"][:, :], w=[r_sc])
            k.dma("sp", "c1", abT[:], d["ada_bT"].rearrange("l p j -> p l j"), w=[r_ab])
            k.op("act", lambda e: e.activation(out=scT[:], in_=cT[:], func=AF.Silu), r=[r_sc], w=[r_sc])
            q = 0
            for i in range(DEPTH):
                pi = i % 2
                for pc in range(NP):
                    b = q % 2
                    q += 1
                    k.dma("sp", f"adaw{b}", wst[b][:],
                          d["ada_w"][i, :, pc * PW:(pc + 1) * PW].rearrange("(kc p) n -> p kc n", p=128),
                          w=[r_w[b]])
                    for jj in range(PW // 128):
                        j = pc * (PW // 128) + jj
                        for kc in range(NKC):
                            k.op("pe", lambda e, b=b, jj=jj, kc=kc, j=j, pi=pi: e.matmul(
                                self.ps[pi][:, j:j + 1], wst[b][:, kc, jj * 128:(jj + 1) * 128], scT[:, kc:kc + 1],
                                start=(kc == 0), stop=(kc == NKC - 1)),
                                r=[r_w[b], r_sc], w=[self.r_ps[pi]])
                k.op("dve", lambda e, i=i, pi=pi: e.tensor_tensor(
                    out=self.modT[:, i, :], in0=self.ps[pi][:, 0:48], in1=abT[:, i, :], op=ALU.add),
                    r=[self.r_ps[pi], r_ab], w=[self.r_mod])
                k.op("dve", lambda e, i=i: e.tensor_scalar_add(
                    out=self.mod1T[:, i, :], in0=self.modT[:, i, :], scalar1=1.0),
                    r=[self.r_mod], w=[self.r_mod])
            k.barrier()

    def norm_core(self, es, tb, geff, shift, r_par, sq, r_sq, rstd, r_rstd, psi):
        nc, k = self.nc, self.k
        tsl = slice(tb * 512, (tb + 1) * 512)
        k.op("act", lambda e: e.activation(out=sq[:, :, :], in_=self.xT[:, :, tsl], func=AF.Square),
             r=[self.r_xT[kc][tb] for kc in range(NKC)], w=[r_sq])
        for kc in range(NKC):
            k.op("pe", lambda e, kc=kc: e.matmul(self.ps[psi][:, :], self.onesb[:, :], sq[:, kc, :],
                                                 start=(kc == 0), stop=(kc == NKC - 1)),
                 r=[r_sq, self.r_const], w=[self.r_ps[psi]])
        k.op("act", lambda e: e.activation(out=rstd[:, :], in_=self.ps[psi][:, :], func=AF.Sqrt,
                                           scale=1.0 / D, bias=self.epsT[:, 0:1]),
             r=[self.r_ps[psi], self.r_const], w=[r_rstd])
        k.op("dve", lambda e: e.reciprocal(out=rstd[:, :], in_=rstd[:, :]), r=[r_rstd], w=[r_rstd])

    def norm_mod(self, i, which, router=None):
        nc, k = self.nc, self.k
        gi = i if which == 0 else DEPTH + i
        so = 0 if which == 0 else 24
        with ExitStack() as es:
            geff = es.enter_context(self.sbuf("geff", [128, NKC], F32))
            sq = [es.enter_context(self.sbuf(f"nsq{j}", [128, NKC, 512], BF16)) for j in range(2)]
            rstd = [es.enter_context(self.sbuf(f"nrstd{j}", [128, 512], F32)) for j in range(2)]
            t1 = [es.enter_context(self.sbuf(f"nt1_{j}", [128, 512], F32)) for j in range(2)]
            r_t1 = [k.res() for _ in range(2)]
            r_g = k.res()
            r_sq = [k.res() for _ in range(2)]
            r_rstd = [k.res() for _ in range(2)]
            if router is not None:
                h32 = es.enter_context(self.sbuf("h32", [128, NKC, 512], F32))
                r_h32 = k.res()
            k.op("dve", lambda e: e.tensor_tensor(out=geff[:], in0=self.gT[:, gi, :], in1=self.mod1T[:, i, so + 8:so + 16],
                                                  op=ALU.mult), r=[self.r_const, self.r_mod], w=[r_g])
            ncore = lambda tb: self.norm_core(es, tb, None, None, None, sq[tb % 2], r_sq[tb % 2], rstd[tb % 2], r_rstd[tb % 2],
                                              7 if tb % 2 == 0 else 5)
            ncore(0)
            for tb in range(NTB):
                tsl = slice(tb * 512, (tb + 1) * 512)
                if tb + 1 < NTB:
                    ncore(tb + 1)
                rs_, r_rs = rstd[tb % 2], r_rstd[tb % 2]
                for kc in range(NKC):
                    b = kc % 2
                    k.op("dve", lambda e, kc=kc, b=b: e.tensor_tensor(out=t1[b][:, :], in0=self.xT[:, kc, tsl], in1=rs_[:, :],
                                                                      op=ALU.mult),
                         r=[self.r_xT[kc][tb], r_rs], w=[r_t1[b]])
                    if router is None:
                        k.op("act", lambda e, kc=kc, b=b: e.activation(
                            out=self.hT[:, kc, tsl], in_=t1[b][:, :], func=AF.Identity, scale=geff[:, kc:kc + 1],
                            bias=self.modT[:, i, so + kc:so + kc + 1]),
                            r=[r_t1[b], r_g, self.r_mod], w=[self.r_hT[kc][tb]])
                    else:
                        k.op("act", lambda e, kc=kc, b=b: e.activation(
                            out=h32[:, kc, :], in_=t1[b][:, :], func=AF.Identity, scale=geff[:, kc:kc + 1],
                            bias=self.modT[:, i, so + kc:so + kc + 1]),
                            r=[r_t1[b], r_g, self.r_mod], w=[r_h32])
                        k.op("act", lambda e, kc=kc, b=b: e.activation(
                            out=self.hT[:, kc, tsl], in_=t1[b][:, :], func=AF.Identity, scale=geff[:, kc:kc + 1],
                            bias=self.modT[:, i, so + kc:so + kc + 1]),
                             r=[r_t1[b], r_g, self.r_mod], w=[self.r_hT[kc][tb]])
                if router is not None:
                    router(tb, h32, r_h32)
            k.barrier()

    def moe(self, i):
        nc, k, d = self.nc, self.k, self.dram
        with ExitStack() as es0:
            gT_all = es0.enter_context(self.sbuf("gT_all", [32, S], F32))
            r_gT = [k.res() for _ in range(NTT)]
            with ExitStack() as es:
                wr = es.enter_context(self.sbuf("wr", [128, NKC, 36], F32))
                brs = es.enter_context(self.sbuf("brs", [1, 36], F32))
                r_wr = k.res()
                k.dma("sp", "c2", wr[:], d["wr"][i], w=[r_wr])
                k.dma("sp", "c2", brs[:], d["br"][i], w=[r_wr])
                L = es.enter_context(self.sbuf("rL", [128, 36], F32))
                sm = es.enter_context(self.sbuf("rsm", [128, 64], F32))
                gates = es.enter_context(self.sbuf("rgates", [128, 32], F32))
                r_L, r_sm, r_gates = k.res(), k.res(), k.res()

                def router(tb, h32, r_h32):
                    for t4 in range(4):
                        tt = tb * 4 + t4
                        pi = 6
                        for kc in range(NKC):
                            k.op("pe", lambda e, kc=kc: e.matmul(self.ps[pi][:, 0:36], h32[:, kc, t4 * 128:(t4 + 1) * 128],
                                                                 wr[:, kc, :], start=(kc == 0), stop=False),
                                 r=[r_h32, r_wr], w=[self.r_ps[pi]])
                        k.op("pe", lambda e: e.matmul(self.ps[pi][:, 0:36], self.ones[0:1, :], brs[0:1, :],
                                                      start=False, stop=True),
                             r=[r_wr, self.r_const], w=[self.r_ps[pi]])
                        k.op("dve", lambda e: e.tensor_copy(out=L[:, :], in_=self.ps[pi][:, 0:36]),
                             r=[self.r_ps[pi]], w=[r_L])
                        self.route_math(L, r_L, sm, r_sm, gates, r_gates)
                        k.op("pe", lambda e: e.transpose(self.ps[pi][0:32, 128:256], gates[:, :], self.ident[:]),
                             r=[r_gates, self.r_const], w=[self.r_ps[pi]])
                        k.op("act", lambda e, tt=tt: e.copy(out=gT_all[:, tt * 128:(tt + 1) * 128],
                                                            in_=self.ps[pi][0:32, 128:256]),
                             r=[self.r_ps[pi]], w=[r_gT[tt]])

                self.norm_mod(i, 1, router=router)
                self.dump("modT", self.modT[:], [128, DEPTH, 48], F32, [self.r_mod])
                self.dump("hT", self.hT[:], [128, NKC, S], BF16, [x for y in self.r_hT for x in y])
                self.dump("gT_all", gT_all[:], [32, S], F32, r_gT)
            with ExitStack() as es:
                sel = es.enter_context(self.sbuf("sel", [32, 32 * 128], F32))
                r_sel = k.res()
                k.dma("sp", "c2", sel[:], d["c_sel"][:, :], w=[r_sel])
                NS = 16
                wg = [es.enter_context(self.sbuf(f"wg{b}", [128, 2, NKC, 256], BF16)) for b in range(2)]
                wu = [es.enter_context(self.sbuf(f"wu{b}", [128, 2, NKC, 256], BF16)) for b in range(2)]
                wd = [es.enter_context(self.sbuf(f"wd{b}", [128, 2, 2, D], BF16)) for b in range(2)]
                r_w = [k.res() for _ in range(2)]
                gbc = [es.enter_context(self.sbuf(f"gbc{b}", [128, 2, 512], F32)) for b in range(2)]
                r_gbc = [k.res() for _ in range(2)]
                hid = [es.enter_context(self.sbuf(f"hid{b}", [128, 4, 512], BF16)) for b in range(2)]
                r_hid = [k.res() for _ in range(2)]
                sg = [es.enter_context(self.sbuf(f"sg{b}", [128, 512], F32)) for b in range(2)]
                r_sg = [k.res() for _ in range(2)]
                tm = [es.enter_context(self.sbuf(f"tm{b}", [128, 512], F32)) for b in range(2)]
                r_tm = [k.res() for _ in range(2)]

                def load_w(s):
                    b = s % 2
                    for ee in range(2):
                        eg = 2 * s + ee
                        g_, e_ = eg // 8, eg % 8
                        k.dma("pool", f"moew{b}", wg[b][:, ee, :, :],
                              d["moe_w_gate"][i, g_, e_].rearrange("(kc p) f -> p kc f", p=128), w=[r_w[b]])
                        k.dma("pool", f"moew{b}", wu[b][:, ee, :, :],
                              d["moe_w_up"][i, g_, e_].rearrange("(kc p) f -> p kc f", p=128), w=[r_w[b]])
                        k.dma("pool", f"moew{b}", wd[b][:, ee, :, :],
                              d["moe_w_down"][i, g_, e_].rearrange("(fc p) n -> p fc n", p=128), w=[r_w[b]])

                pending = [None]
                it = [0]

                def down(s, tb, hb):
                    b = s % 2
                    tsl = slice(tb * 512, (tb + 1) * 512)
                    for dc in range(NKC):
                        pi = 4 + dc % 2
                        for u in range(4):
                            ee, fc = u // 2, u % 2
                            k.op("pe", lambda e, u=u, ee=ee, fc=fc, dc=dc, pi=pi: e.matmul(
                                self.ps[pi][:, :], wd[b][:, ee, fc, dc * 128:(dc + 1) * 128], hid[hb][:, u, :],
                                start=(u == 0), stop=(u == 3)),
                                r=[r_w[b], r_hid[hb]], w=[self.r_ps[pi]])
                        k.op("dve", lambda e, dc=dc, pi=pi: e.scalar_tensor_tensor(
                            out=self.xT[:, dc, tsl], in0=self.ps[pi][:, :], scalar=self.modT[:, i, 40 + dc:41 + dc],
                            in1=self.xT[:, dc, tsl], op0=ALU.mult, op1=ALU.add),
                            r=[self.r_ps[pi], self.r_mod], w=[self.r_xT[dc][tb]])

                load_w(0)
                for s in range(NS):
                    b = s % 2
                    for tb in range(NTB):
                        tsl = slice(tb * 512, (tb + 1) * 512)
                        hb = it[0] % 2
                        gb = it[0] % 2
                        it[0] += 1
                        for ee in range(2):
                            eg = 2 * s + ee
                            k.op("pe", lambda e, eg=eg: e.matmul(self.ps[6][:, :], sel[:, eg * 128:(eg + 1) * 128],
                                                                 gT_all[:, tsl], start=True, stop=True),
                                 r=[r_sel] + r_gT[tb * 4:tb * 4 + 4], w=[self.r_ps[6]])
                            k.op("act", lambda e, ee=ee, gb=gb: e.copy(out=gbc[gb][:, ee, :], in_=self.ps[6][:, :]),
                                 r=[self.r_ps[6]], w=[r_gbc[gb]])
                        for u in range(4):
                            ee, fc = u // 2, u % 2
                            pg, pu = (0, 1) if u % 2 == 0 else (2, 3)
                            for kc in range(NKC):
                                k.op("pe", lambda e, kc=kc, ee=ee, fc=fc, pg=pg: e.matmul(
                                    self.ps[pg][:, :], wg[b][:, ee, kc, fc * 128:(fc + 1) * 128], self.hT[:, kc, tsl],
                                    start=(kc == 0), stop=(kc == NKC - 1)),
                                    r=[r_w[b], self.r_hT[kc][tb]], w=[self.r_ps[pg]])
                            for kc in range(NKC):
                                k.op("pe", lambda e, kc=kc, ee=ee, fc=fc, pu=pu: e.matmul(
                                    self.ps[pu][:, :], wu[b][:, ee, kc, fc * 128:(fc + 1) * 128], self.hT[:, kc, tsl],
                                    start=(kc == 0), stop=(kc == NKC - 1)),
                                    r=[r_w[b], self.r_hT[kc][tb]], w=[self.r_ps[pu]])
                            sb_ = u % 2
                            k.op("act", lambda e, pg=pg, sb_=sb_: e.activation(out=sg[sb_][:, :], in_=self.ps[pg][:, :],
                                                                               func=AF.Silu),
                                 r=[self.r_ps[pg]], w=[r_sg[sb_]])
                            k.op("dve", lambda e, pu=pu, sb_=sb_: e.tensor_tensor(out=tm[sb_][:, :], in0=sg[sb_][:, :],
                                                                                  in1=self.ps[pu][:, :], op=ALU.mult),
                                 r=[r_sg[sb_], self.r_ps[pu]], w=[r_tm[sb_]])
                            k.op("dve", lambda e, u=u, ee=ee, sb_=sb_, hb=hb, gb=gb: e.tensor_tensor(
                                out=hid[hb][:, u, :], in0=tm[sb_][:, :], in1=gbc[gb][:, ee, :], op=ALU.mult),
                                r=[r_tm[sb_], r_gbc[gb]], w=[r_hid[hb]])
                        if pending[0] is not None:
                            down(*pending[0])
                        pending[0] = (s, tb, hb)
                        if tb == 0 and s + 1 < NS:
                            load_w(s + 1)
                down(*pending[0])
                k.barrier()

    def route_math(self, L, r_L, sm, r_sm, gates, r_gates):
        k = self.k
        GMAX, NGMAX, GSUM, PG, M1, M2, DD, EE, W1, W2 = range(10)
        GOH = slice(10, 14)
        GE = slice(14, 18)
        ESEL = slice(18, 26)
        OH1 = slice(26, 34)
        MSK = slice(34, 42)
        OH2 = slice(42, 50)
        INN = slice(50, 58)
        c = lambda j: slice(j, j + 1)

        def dv(fn, r=(), w=()):
            k.op("dve", fn, r=r, w=w)

        rs = [r_L, r_sm]
        dv(lambda e: e.reduce_max(out=sm[:, c(GMAX)], in_=L[:, 0:4], axis=AX.X), r=[r_L], w=[r_sm])
        dv(lambda e: e.tensor_scalar(out=sm[:, GOH], in0=L[:, 0:4], scalar1=sm[:, c(GMAX)], scalar2=None,
                                     op0=ALU.is_equal), r=rs, w=[r_sm])
        dv(lambda e: e.tensor_scalar_mul(out=sm[:, c(NGMAX)], in0=sm[:, c(GMAX)], scalar1=-1.0), r=[r_sm], w=[r_sm])
        k.op("act", lambda e: e.activation(out=sm[:, GE], in_=L[:, 0:4], func=AF.Exp, bias=sm[:, c(NGMAX)],
                                           scale=1.0, accum_out=sm[:, c(GSUM)]), r=rs, w=[r_sm])
        dv(lambda e: e.reciprocal(out=sm[:, c(PG)], in_=sm[:, c(GSUM)]), r=[r_sm], w=[r_sm])
        dv(lambda e: e.tensor_scalar_mul(out=sm[:, ESEL], in0=L[:, 4:12], scalar1=sm[:, c(10)]), r=rs, w=[r_sm])
        for g in range(1, 4):
            dv(lambda e, g=g: e.scalar_tensor_tensor(out=sm[:, ESEL], in0=L[:, 4 + 8 * g:12 + 8 * g],
                                                     scalar=sm[:, c(10 + g)], in1=sm[:, ESEL],
                                                     op0=ALU.mult, op1=ALU.add), r=rs, w=[r_sm])
        dv(lambda e: e.reduce_max(out=sm[:, c(M1)], in_=sm[:, ESEL], axis=AX.X), r=[r_sm], w=[r_sm])
        dv(lambda e: e.tensor_scalar(out=sm[:, OH1], in0=sm[:, ESEL], scalar1=sm[:, c(M1)], scalar2=None,
                                     op0=ALU.is_equal), r=[r_sm], w=[r_sm])
        dv(lambda e: e.scalar_tensor_tensor(out=sm[:, MSK], in0=sm[:, OH1], scalar=-1e30, in1=sm[:, ESEL],
                                            op0=ALU.mult, op1=ALU.add), r=[r_sm], w=[r_sm])
        dv(lambda e: e.reduce_max(out=sm[:, c(M2)], in_=sm[:, MSK], axis=AX.X), r=[r_sm], w=[r_sm])
        dv(lambda e: e.tensor_scalar(out=sm[:, OH2], in0=sm[:, MSK], scalar1=sm[:, c(M2)], scalar2=None,
                                     op0=ALU.is_equal), r=[r_sm], w=[r_sm])
        dv(lambda e: e.tensor_tensor(out=sm[:, c(DD)], in0=sm[:, c(M2)], in1=sm[:, c(M1)], op=ALU.subtract),
           r=[r_sm], w=[r_sm])
        k.op("act", lambda e: e.activation(out=sm[:, c(EE)], in_=sm[:, c(DD)], func=AF.Exp), r=[r_sm], w=[r_sm])
        dv(lambda e: e.tensor_scalar_add(out=sm[:, c(W1)], in0=sm[:, c(EE)], scalar1=1.0), r=[r_sm], w=[r_sm])
        dv(lambda e: e.reciprocal(out=sm[:, c(W1)], in_=sm[:, c(W1)]), r=[r_sm], w=[r_sm])
        dv(lambda e: e.tensor_tensor(out=sm[:, c(W2)], in0=sm[:, c(EE)], in1=sm[:, c(W1)], op=ALU.mult),
           r=[r_sm], w=[r_sm])
        dv(lambda e: e.tensor_tensor(out=sm[:, c(W1)], in0=sm[:, c(W1)], in1=sm[:, c(PG)], op=ALU.mult),
           r=[r_sm], w=[r_sm])
        dv(lambda e: e.tensor_tensor(out=sm[:, c(W2)], in0=sm[:, c(W2)], in1=sm[:, c(PG)], op=ALU.mult),
           r=[r_sm], w=[r_sm])
        dv(lambda e: e.tensor_scalar_mul(out=sm[:, INN], in0=sm[:, OH1], scalar1=sm[:, c(W1)]), r=[r_sm], w=[r_sm])
        dv(lambda e: e.scalar_tensor_tensor(out=sm[:, INN], in0=sm[:, OH2], scalar=sm[:, c(W2)], in1=sm[:, INN],
                                            op0=ALU.mult, op1=ALU.add), r=[r_sm], w=[r_sm])
        for g in range(4):
            dv(lambda e, g=g: e.tensor_scalar_mul(out=gates[:, 8 * g:8 * g + 8], in0=sm[:, INN],
                                                  scalar1=sm[:, c(10 + g)]), r=[r_sm], w=[r_gates])

    def rwkv(self, i):
        nc, k, d = self.nc, self.k, self.dram
        j = i // 2
        with ExitStack() as es:
            PCt = es.enter_context(self.sbuf("PCt", [64, 2, NTT, 16, 2], F32))
            r_PC = k.res()
            self.rwkv_A(i, j, PCt, r_PC)
            self.rwkv_B(i, j, PCt, r_PC)

    def rwkv_A(self, i, j, PCt, r_PC):
        nc, k, d = self.nc, self.k, self.dram
        all_hT = [x for y in self.r_hT for x in y]
        sc = self.scr
        with ExitStack() as esA:
            sb = lambda es, n, s, dt=F32: es.enter_context(self.sbuf(n, s, dt))
            h1w = sb(esA, "h1w", [128, S], BF16)
            h1a = sb(esA, "h1a", [128, S], BF16)
            r_h1w, r_h1a = k.res(), k.res()
            muT = sb(esA, "muT", [128, 6, NKC])
            c1 = sb(esA, "c1", [128, 6, NKC])
            c2 = sb(esA, "c2", [128, 6, NKC])
            r_c = k.res()
            k.dma("sp", "rwc", muT[:], d["rw_muT"][j], w=[r_c])
            k.op("dve", lambda e: e.tensor_scalar(out=c1[:], in0=muT[:], scalar1=-1.0, scalar2=1.0, op0=ALU.mult, op1=ALU.add),
                 r=[r_c], w=[r_c])
            k.op("dve", lambda e: e.tensor_scalar_mul(out=c2[:], in0=muT[:], scalar1=0.5), r=[r_c], w=[r_c])

            def scale_w(stg, r_stg, W1, W2, r_W, jm, ncol, col0=0):
                for kc in range(NKC):
                    k.op("act", lambda e, kc=kc: e.mul(out=W1[:, kc, col0:col0 + ncol], in_=stg[:, kc, 0:ncol],
                                                       mul=c1[:, jm, kc:kc + 1]), r=[r_stg, r_c], w=[r_W])
                    k.op("dve", lambda e, kc=kc: e.tensor_scalar_mul(out=W2[:, kc, col0:col0 + ncol], in0=stg[:, kc, 0:ncol],
                                                                     scalar1=c2[:, jm, kc:kc + 1]), r=[r_stg, r_c], w=[r_W])

            with ExitStack() as es12:
                hsT = sb(es12, "hsT", [128, NKC, S], BF16)
                r_hs = k.res()
                for kc in range(NKC):
                    k.op("dve", lambda e, kc=kc: e.tensor_tensor(out=hsT[:, kc, 1:S - 1], in0=self.hT[:, kc, 0:S - 2],
                                                                 in1=self.hT[:, kc, 2:S], op=ALU.add), r=all_hT, w=[r_hs])
                    k.op("act", lambda e, kc=kc: e.copy(out=hsT[:, kc, 0:1], in_=self.hT[:, kc, 1:2]), r=all_hT, w=[r_hs])
                    k.op("act", lambda e, kc=kc: e.copy(out=hsT[:, kc, S - 1:S], in_=self.hT[:, kc, S - 2:S - 1]), r=all_hT, w=[r_hs])
                with ExitStack() as es1:
                    h1g = [sb(es1, f"h1g{dd}", [128, S], BF16) for dd in range(2)]
                    r_h1g = [k.res() for _ in range(2)]
                    stg = [sb(es1, f"lstg{b}", [128, NKC, 128]) for b in range(2)]
                    r_stg = [k.res() for _ in range(2)]
                    W1 = [sb(es1, f"lW1_{b}", [128, NKC, 128], BF16) for b in range(2)]
                    W2 = [sb(es1, f"lW2_{b}", [128, NKC, 128], BF16) for b in range(2)]
                    r_W = [k.res() for _ in range(2)]
                    groups = [(d["rw_w1cat"][j], 3, AF.Tanh, h1w, r_h1w), (d["rw_a1cat"][j], 4, AF.Copy, h1a, r_h1a),
                              (d["rw_g1"][j, 0], 5, AF.Sigmoid, h1g[0], r_h1g[0]), (d["rw_g1"][j, 1], 5, AF.Sigmoid, h1g[1], r_h1g[1])]
                    for gi, (src, jm, fn, dst, r_dst) in enumerate(groups):
                        b = gi % 2
                        k.dma("sp", f"lstg{b}", stg[b][:], src.rearrange("(kc p) n -> p kc n", p=128), w=[r_stg[b]])
                        scale_w(stg[b], r_stg[b], W1[b], W2[b], r_W[b], jm, 128)
                        for tb in range(NTB):
                            tsl = slice(tb * 512, (tb + 1) * 512)
                            pi = tb % 2
                            for kc in range(NKC):
                                k.op("pe", lambda e, kc=kc: e.matmul(self.ps[pi][:, :], W1[b][:, kc, :], self.hT[:, kc, tsl],
                                                                     start=(kc == 0), stop=False),
                                     r=[r_W[b], self.r_hT[kc][tb]], w=[self.r_ps[pi]])
                            for kc in range(NKC):
                                k.op("pe", lambda e, kc=kc: e.matmul(self.ps[pi][:, :], W2[b][:, kc, :], hsT[:, kc, tsl],
                                                                     start=False, stop=(kc == NKC - 1)),
                                     r=[r_W[b], r_hs], w=[self.r_ps[pi]])
                            k.op("act", lambda e: e.activation(out=dst[:, tsl], in_=self.ps[pi][:, :], func=fn),
                                 r=[self.r_ps[pi]], w=[r_dst])
                    g2 = [sb(es1, f"g2_{dd}", [128, D], BF16) for dd in range(2)]
                    r_g2 = k.res()
                    for dd in range(2):
                        k.dma("pool", "g2", g2[dd][:], d["rw_g2"][j, dd], w=[r_g2])
                    gst = [sb(es1, f"gst{b}", [128, D]) for b in range(2)]
                    r_gst = [k.res() for _ in range(2)]
                    n = 0
                    for dd in range(2):
                        for tt in range(NTT):
                            b = n % 2
                            n += 1
                            for half in range(2):
                                pi = 2 + half
                                k.op("pe", lambda e, half=half, pi=pi: e.matmul(
                                    self.ps[pi][:, :], h1g[dd][:, tt * 128:(tt + 1) * 128], g2[dd][:, half * 512:(half + 1) * 512],
                                    start=True, stop=True), r=[r_h1g[dd], r_g2], w=[self.r_ps[pi]])
                                if half == 0:
                                    k.op("act", lambda e, pi=pi: e.copy(out=gst[b][:, 0:512], in_=self.ps[pi][:, :]),
                                         r=[self.r_ps[pi]], w=[r_gst[b]])
                                else:
                                    k.op("dve", lambda e, pi=pi: e.tensor_copy(out=gst[b][:, 512:1024], in_=self.ps[pi][:, :]),
                                         r=[self.r_ps[pi]], w=[r_gst[b]])
                            k.dma("sp", f"gst{b}", sc["GG"][dd, tt * 128:(tt + 1) * 128, :], gst[b][:], r=[r_gst[b]],
                                  w=[self.r_scr["GG"][dd][tt]])
                    k.barrier()
                with ExitStack() as es2:
                    stg = [sb(es2, f"pstg{b}", [128, NKC, 256]) for b in range(2)]
                    r_stg = [k.res() for _ in range(2)]
                    W1 = sb(es2, "pW1", [128, NKC, D], BF16)
                    W2 = sb(es2, "pW2", [128, NKC, D], BF16)
                    r_W = k.res()
                    rst = [sb(es2, f"rst{b}", [128, D]) for b in range(2)]
                    rstb = [rst[b][:, :].bitcast(BF16)[:, 0:D] for b in range(2)]
                    r_rst = [k.res() for _ in range(2)]
                    n = 0
                    for pj in range(3):
                        for qq in range(4):
                            sb_ = qq % 2
                            k.dma("sp", f"pstg{sb_}", stg[sb_][:],
                                  d["rw_w_rkv"][j, pj, :, qq * 256:(qq + 1) * 256].rearrange("(kc p) n -> p kc n", p=128),
                                  w=[r_stg[sb_]])
                            scale_w(stg[sb_], r_stg[sb_], W1, W2, r_W, pj, 256, col0=qq * 256)
                        for tt in range(NTT):
                            b = n % 2
                            n += 1
                            tb = tt // 4
                            tsl = slice(tt * 128, (tt + 1) * 128)
                            for half in range(2):
                                pi = half
                                for kc in range(NKC):
                                    k.op("pe", lambda e, kc=kc, half=half, pi=pi: e.matmul(
                                        self.ps[pi][:, :], self.hT[:, kc, tsl], W1[:, kc, half * 512:(half + 1) * 512],
                                        start=(kc == 0), stop=False), r=[r_W, self.r_hT[kc][tb]], w=[self.r_ps[pi]])
                                for kc in range(NKC):
                                    k.op("pe", lambda e, kc=kc, half=half, pi=pi: e.matmul(
                                        self.ps[pi][:, :], hsT[:, kc, tsl], W2[:, kc, half * 512:(half + 1) * 512],
                                        start=False, stop=(kc == NKC - 1)), r=[r_W, r_hs], w=[self.r_ps[pi]])
                                dst_t = rst[b] if pj < 2 else rstb[b]
                                if half == 0:
                                    k.op("act", lambda e, pi=pi: e.copy(out=dst_t[:, 0:512], in_=self.ps[pi][:, :]),
                                         r=[self.r_ps[pi]], w=[r_rst[b]])
                                else:
                                    k.op("dve", lambda e, pi=pi: e.tensor_copy(out=dst_t[:, 512:1024], in_=self.ps[pi][:, :]),
                                         r=[self.r_ps[pi]], w=[r_rst[b]])
                            if pj < 2:
                                k.dma("sp", f"rst{b}", sc["RAW"][pj, tt * 128:(tt + 1) * 128, :], rst[b][:], r=[r_rst[b]],
                                      w=[self.r_scr["RAW"][pj][tt]])
                            else:
                                k.dma("sp", f"rst{b}", sc["RAWV"][tt * 128:(tt + 1) * 128, :], rstb[b], r=[r_rst[b]],
                                      w=[self.r_scr["RAW"][pj][tt]])
                    k.barrier()
            with ExitStack() as es3:
                w2c = sb(es3, "w2c", [128, D], BF16)
                a2c = sb(es3, "a2c", [128, D], BF16)
                r_l2 = k.res()
                k.dma("pool", "l2w", w2c[:], d["rw_w2cat"][j], w=[r_l2])
                k.dma("pool", "l2w", a2c[:], d["rw_a2cat"][j], w=[r_l2])
                KKb = sb(es3, "KKb", [128, D]); KAb = sb(es3, "KAb", [128, D]); RKb = sb(es3, "RKb", [128, D])
                r_par = k.res()
                k.dma("sp", "rwp", KKb[:], d["rw_k_k"][j:j + 1, :].partition_broadcast(128), w=[r_par])
                k.dma("sp", "rwp", KAb[:], d["rw_k_a"][j:j + 1, :].partition_broadcast(128), w=[r_par])
                k.dma("sp", "rwp", RKb[:], d["rw_r_k"][j:j + 1, :].partition_broadcast(128), w=[r_par])
                b32 = sb(es3, "b32", [1, 4, D])
                r_b = k.res()
                k.dma("sp", "rwp2", b32[0:1, 0:2, :], d["rw_w0"][j:j + 1, :, :], w=[r_b])
                k.dma("sp", "rwp2", b32[0:1, 2:4, :], d["rw_a0"][j:j + 1, :, :], w=[r_b])
                tri = sb(es3, "tri", [128, 6, 128])
                csel = sb(es3, "csel", [128, 2])
                k.dma("sp", "rwp", tri[:], d["c_tri"].rearrange("q s t -> s q t"), w=[r_par])
                k.dma("sp", "rwp", csel[:], d["c_csel"][:, :], w=[r_par])
                hsc = [self.hT[:, kc, :].bitcast(F32) for kc in range(NKC)]
                mk2 = lambda n_, extra: [sb(es3, f"{n_}{q}", [128, D]) if extra[q] is None else extra[q] for q in range(2)]
                Rr_s = mk2("Rr", [None, hsc[0]]); Rk_s = mk2("Rk", [None, hsc[1]]); kk_s = mk2("kk", [None, hsc[2]])
                RRK_s = mk2("RRK", [None, hsc[3]]); T0_s = mk2("T0", [None, hsc[4]])
                SIG_s = mk2("SIG", [None, hsc[5]]); A_s = mk2("A_", [None, hsc[6]]); KD_s = mk2("KD", [None, hsc[7]])
                E1_s = mk2("E1", [None, None]); E2_s = mk2("E2", [None, None])
                O = [sb(es3, f"O{b}", [128, D], BF16) for b in range(2)]
                FMst = [sb(es3, f"FMst{b}", [64, 8, 128], BF16) for b in range(2)]
                identb = sb(es3, "identb3", [128, 128], BF16)
                r_idb = k.res()
                k.op("act", lambda e: e.copy(out=identb[:], in_=self.ident[:]), r=[self.r_const], w=[r_idb])
                psb = {4: self.ps[4][0:64, :].bitcast(BF16), 5: self.ps[5][0:64, :].bitcast(BF16)}
                sm = sb(es3, "a3sm", [128, 64])
                rkt_s = [sb(es3, f"rkt{q}", [128, 16]) for q in range(2)]
                r_rkt_s = [k.res(), k.res()]
                r2 = lambda: [k.res(), k.res()]
                r_Rr_s, r_Rk_s, r_kk_s, r_RRK_s, r_T0_s = r2(), r2(), r2(), r2(), r2()
                r_SIG_s, r_A_s, r_KD_s, r_E1_s, r_E2_s = r2(), r2(), r2(), r2(), r2()
                r_sm = k.res()
                r_O = [k.res() for _ in range(2)]
                r_FM = [k.res() for _ in range(2)]
                on = [0]
                v3 = lambda t: t[:, :].rearrange("p (h n) -> p h n", h=16)

                fmn = [0]

                def emit_fm(src, r_src, dd, q, tt):
                    on[0] += 1
                    for h8 in range(2):
                        fb = fmn[0] % 2
                        fmn[0] += 1
                        for h4 in range(2):
                            pi = 4 + h4 % 2
                            for hh in range(4):
                                h = h8 * 8 + h4 * 4 + hh
                                k.op("pe", lambda e, h=h, hh=hh, pi=pi: e.transpose(
                                    psb[pi][:, hh * 128:(hh + 1) * 128], src[:, h * 64:(h + 1) * 64], identb[:]),
                                    r=[r_src, r_idb], w=[self.r_ps[pi]])
                            if h4 == 0:
                                k.op("act", lambda e, h4=h4, pi=pi: e.copy(
                                    out=FMst[fb][:, h4 * 4:h4 * 4 + 4, :], in_=psb[pi][:, 0:512].rearrange("p (a t) -> p a t", a=4)),
                                    r=[self.r_ps[pi]], w=[r_FM[fb]])
                            else:
                                k.op("dve", lambda e, h4=h4, pi=pi: e.tensor_copy(
                                    out=FMst[fb][:, h4 * 4:h4 * 4 + 4, :], in_=psb[pi][:, 0:512].rearrange("p (a t) -> p a t", a=4)),
                                    r=[self.r_ps[pi]], w=[r_FM[fb]])
                        k.dma("sp", f"FMst{fb}", sc["FM"][dd, q, tt, :, h8 * 8:(h8 + 1) * 8, :], FMst[fb][:], r=[r_FM[fb]],
                              w=[self.r_scr["FM"][dd][q][tt]])

                def a3_load(tt):
                    q = tt % 2
                    rws = slice(tt * 128, (tt + 1) * 128)
                    k.dma("pool", f"a3r{q}", Rr_s[q][:], sc["RAW"][0, rws, :], r=[self.r_scr["RAW"][0][tt]], w=[r_Rr_s[q]])
                    k.dma("pool", f"a3k{q}", Rk_s[q][:], sc["RAW"][1, rws, :], r=[self.r_scr["RAW"][1][tt]], w=[r_Rk_s[q]])

                a3_load(0)
                for tt in range(NTT):
                    rows = slice(tt * 128, (tt + 1) * 128)
                    if tt + 1 < NTT:
                        a3_load(tt + 1)
                    q_ = tt % 2
                    Rr, Rk, kk, RRK = Rr_s[q_], Rk_s[q_], kk_s[q_], RRK_s[q_]
                    r_Rr, r_Rk, r_kk, r_RRK = r_Rr_s[q_], r_Rk_s[q_], r_kk_s[q_], r_RRK_s[q_]
                    T0, r_T0 = T0_s[0], r_T0_s[0]
                    k.op("dve", lambda e: e.tensor_tensor(out=kk[:], in0=Rk[:], in1=KKb[:], op=ALU.mult), r=[r_Rk, r_par], w=[r_kk])
                    k.op("dve", lambda e: e.tensor_tensor(out=T0[:], in0=kk[:], in1=kk[:], op=ALU.mult), r=[r_kk], w=[r_T0])
                    k.op("dve", lambda e: e.reduce_sum(out=sm[:, 0:16], in_=v3(T0), axis=AX.X), r=[r_T0], w=[r_sm])
                    k.op("act", lambda e: e.activation(out=sm[:, 0:16], in_=sm[:, 0:16], func=AF.Sqrt), r=[r_sm], w=[r_sm])
                    k.op("dve", lambda e: e.tensor_scalar_max(out=sm[:, 0:16], in0=sm[:, 0:16], scalar1=1e-12), r=[r_sm], w=[r_sm])
                    k.op("dve", lambda e: e.reciprocal(out=sm[:, 16:32], in_=sm[:, 0:16]), r=[r_sm], w=[r_sm])
                    k.op("dve", lambda e: e.tensor_tensor(out=v3(kk), in0=v3(kk),
                                                          in1=sm[:, 16:32].unsqueeze(2).to_broadcast([128, 16, 64]), op=ALU.mult),
                         r=[r_kk, r_sm], w=[r_kk])
                    k.op("dve", lambda e: e.tensor_tensor(out=RRK[:], in0=Rr[:], in1=RKb[:], op=ALU.mult), r=[r_Rr, r_par], w=[r_RRK])
                    for dd in range(2):
                        T0, SIG, A_, KD, E1, E2 = T0_s[dd], SIG_s[dd], A_s[dd], KD_s[dd], E1_s[dd], E2_s[dd]
                        r_T0, r_SIG, r_A, r_KD, r_E1, r_E2 = r_T0_s[dd], r_SIG_s[dd], r_A_s[dd], r_KD_s[dd], r_E1_s[dd], r_E2_s[dd]
                        for (h1, r_h1, w2t, bi, dst, r_dst) in ((h1w, r_h1w, w2c, dd, SIG, r_SIG), (h1a, r_h1a, a2c, 2 + dd, A_, r_A)):
                            for half in range(2):
                                pi = half
                                csl = slice(half * 512, (half + 1) * 512)
                                k.op("pe", lambda e: e.matmul(self.ps[pi][:, :], h1[dd * 64:(dd + 1) * 64, rows],
                                                              w2t[dd * 64:(dd + 1) * 64, csl], start=True, stop=False),
                                     r=[r_h1, r_l2], w=[self.r_ps[pi]])
                                k.op("pe", lambda e: e.matmul(self.ps[pi][:, :], self.ones[0:1, :], b32[0:1, bi, csl],
                                                              start=False, stop=True),
                                     r=[r_b, self.r_const], w=[self.r_ps[pi]])
                                k.op("act", lambda e: e.activation(out=dst[:, csl], in_=self.ps[pi][:, :], func=AF.Sigmoid),
                                     r=[self.r_ps[pi]], w=[r_dst])
                        k.op("dve", lambda e: e.scalar_tensor_tensor(out=KD[:], in0=A_[:], scalar=-1.0, in1=KAb[:],
                                                                     op0=ALU.add, op1=ALU.mult), r=[r_A, r_par], w=[r_KD])
                        k.op("dve", lambda e: e.scalar_tensor_tensor(out=KD[:], in0=KD[:], scalar=1.0, in1=Rk[:],
                                                                     op0=ALU.add, op1=ALU.mult), r=[r_KD, r_Rk], w=[r_KD])
                        k.op("dve", lambda e: e.tensor_tensor(out=A_[:], in0=A_[:], in1=kk[:], op=ALU.mult), r=[r_A, r_kk], w=[r_A])
                        k.op("dve", lambda e: e.tensor_tensor(out=T0[:], in0=RRK[:], in1=KD[:], op=ALU.mult), r=[r_RRK, r_KD], w=[r_T0])
                        rkt, r_rkt = rkt_s[dd], r_rkt_s[dd]
                        k.op("dve", lambda e: e.reduce_sum(out=rkt[:, :], in_=v3(T0), axis=AX.X), r=[r_T0], w=[r_rkt])
                        k.dma("sp", f"rkt{dd}", sc["RK"][dd, rows, :], rkt[:], r=[r_rkt], w=[self.r_scr["RK"][dd][tt]])
                        for h in range(16):
                            k.op("pe", lambda e, h=h: e.matmul(self.ps[6][0:64, h * 2:h * 2 + 2], SIG[:, h * 64:(h + 1) * 64], csel[:, :],
                                                               start=True, stop=True), r=[r_SIG, r_par], w=[self.r_ps[6]])
                        k.op("act", lambda e: e.activation(out=PCt[:, dd, tt, :, :], in_=self.ps[6][0:64, 0:32].rearrange("p (h c) -> p h c", c=2),
                                                           func=AF.Exp), r=[self.r_ps[6]], w=[r_PC])
                        def cums(kind, outs):
                            for half in range(2):
                                pi = 2 + half
                                csl = slice(half * 512, (half + 1) * 512)
                                k.op("pe", lambda e: e.matmul(self.ps[pi][:, :], tri[:, dd * 3 + kind, :], SIG[:, csl],
                                                              start=True, stop=True), r=[r_SIG, r_par], w=[self.r_ps[pi]])
                                for (dst, r_dst, scl) in outs:
                                    k.op("act", lambda e, dst=dst, scl=scl: e.activation(out=dst[:, csl], in_=self.ps[pi][:, :],
                                                                                         func=AF.Exp, scale=scl),
                                         r=[self.r_ps[pi]], w=[r_dst])
                        cums(0, [(E1, r_E1, 1.0), (E2, r_E2, -1.0)])
                        ob = on[0] % 2
                        k.op("dve", lambda e: e.tensor_tensor(out=O[ob][:], in0=Rr[:], in1=E1[:], op=ALU.mult), r=[r_Rr, r_E1], w=[r_O[ob]])
                        emit_fm(O[ob], r_O[ob], dd, 0, tt)
                        ob = on[0] % 2
                        k.op("dve", lambda e: e.tensor_tensor(out=O[ob][:], in0=A_[:], in1=E2[:], op=ALU.mult), r=[r_A, r_E2], w=[r_O[ob]])
                        emit_fm(O[ob], r_O[ob], dd, 2, tt)
                        ob = on[0] % 2
                        k.op("dve", lambda e: e.tensor_tensor(out=O[ob][:], in0=KD[:], in1=E2[:], op=ALU.mult), r=[r_KD, r_E2], w=[r_O[ob]])
                        emit_fm(O[ob], r_O[ob], dd, 3, tt)
                        cums(1, [(E1, r_E1, 1.0)])
                        ob = on[0] % 2
                        k.op("dve", lambda e: e.scalar_tensor_tensor(out=O[ob][:], in0=kk[:], scalar=-1.0, in1=E1[:],
                                                                     op0=ALU.mult, op1=ALU.mult), r=[r_kk, r_E1], w=[r_O[ob]])
                        emit_fm(O[ob], r_O[ob], dd, 1, tt)
                        cums(2, [(E2, r_E2, 1.0)])
                        for q, (src, r_src) in enumerate(((A_, r_A), (KD, r_KD))):
                            ob = on[0] % 2
                            on[0] += 1
                            k.op("dve" if q == 0 else "pool", lambda e, src=src: e.tensor_tensor(out=O[ob][:], in0=src[:], in1=E2[:], op=ALU.mult),
                                 r=[r_src, r_E2], w=[r_O[ob]])
                            k.dma("sp", f"Otm{ob}", sc["TM"][dd, q, rows, :], O[ob][:], r=[r_O[ob]], w=[self.r_scr["TM"][dd][q][tt]])
                k.barrier()

    def rwkv_B(self, i, j, PCt, r_PC):
        nc, k, d = self.nc, self.k, self.dram
        sc = self.scr
        oT = self.hT
        r_oT = self.r_hT
        NH = 8
        with ExitStack() as es:
            sb = lambda n, s_, dt=F32: es.enter_context(self.sbuf(n, s_, dt))
            for kc in range(NKC):
                k.op("pool", lambda e, kc=kc: e.memset(oT[:, kc, :], 0.0), w=r_oT[kc])
            msk = sb("msk", [64, 6, 64])
            LNW = sb("LNW", [64, D]); LNB = sb("LNB", [64, D])
            epsg = sb("epsg", [64, 1])
            r_cb = k.res()
            k.dma("sp", "rbc", msk[:], d["c_mask"].rearrange("q s t -> s q t"), w=[r_cb])
            k.dma("sp", "rbc", LNW[:], d["rw_ln_w"][j:j + 1, :].partition_broadcast(64), w=[r_cb])
            k.dma("sp", "rbc", LNB[:], d["rw_ln_b"][j:j + 1, :].partition_broadcast(64), w=[r_cb])
            k.op("dve", lambda e: e.memset(epsg[:], 64e-5), w=[r_cb])
            i64 = self.ident[0:64, 0:64]
            bcm = lambda q: msk[:, q, :].unsqueeze(1).to_broadcast([64, NH, 64])
            bci = i64.unsqueeze(1).to_broadcast([64, NH, 64])

            class Chain:
                pass
            chains = []
            for ci, (dd, hg) in enumerate(((0, 0), (1, 0), (0, 1), (1, 1))):
                c = Chain()
                c.dd, c.hg = dd, hg
                nm = f"c{ci}"
                c.nm = nm
                c.ld = {n_: sb(f"{nm}_{n_}", [64, NH, 64], BF16) for n_ in ("RT", "AT", "BT", "KT", "Bh", "Kh", "Vt")}
                c.ld["Gt"] = sb(f"{nm}_Gt", [64, NH, 64])
                c.r_ld = {n_: k.res() for n_ in c.ld}
                c.rk = sb(f"{nm}_rk", [64, NH]); c.r_rk = k.res()
                c.sl = [sb(f"{nm}_s{q}", [64, NH, 64], BF16) for q in range(6)]
                c.r_sl = [k.res() for _ in range(6)]
                c.ST = sb(f"{nm}_ST", [64, NH, 64]); c.r_ST = k.res()
                c.STb = sb(f"{nm}_STb", [64, NH, 64], BF16); c.r_STb = k.res()
                c.y = sb(f"{nm}_y", [64, NH, 64]); c.r_y = k.res()
                c.sq = sb(f"{nm}_sq", [64, NH, 64]); c.r_sq = k.res()
                c.sm = sb(f"{nm}_sm", [64, 8 * NH]); c.r_sm = k.res()
                c.pb = [ci * 2, ci * 2 + 1]
                c.pbi = 0
                chains.append(c)

            def p3(pi):
                return self.ps[pi][0:64, :].rearrange("p (h n) -> p h n", h=NH)

            def mm(c, pi, pairs, rs):
                for h in range(NH):
                    for q, (lt, rt) in enumerate(pairs):
                        k.op("pe", lambda e, h=h, lt=lt, rt=rt, q=q: e.matmul(
                            self.ps[pi][0:64, h * 64:(h + 1) * 64], lt[:, h, :], rt[:, h, :],
                            start=(q == 0), stop=(q == len(pairs) - 1)), r=rs, w=[self.r_ps[pi]])

            def nb(c):
                c.pbi = (c.pbi + 1) % 2
                return c.pb[c.pbi]

            def chunk_steps(c, n):
                dd, hg = c.dd, c.hg
                ct = n if dd == 0 else 31 - n
                tt, half = ct // 2, ct % 2
                rows = slice(ct * 64, (ct + 1) * 64)
                hs = slice(hg * NH, (hg + 1) * NH)
                cs = slice(hg * 512, (hg + 1) * 512)
                L, R = c.ld, c.r_ld
                for q, n_ in enumerate(("RT", "AT", "BT", "KT")):
                    k.dma("sp", f"{c.nm}{n_}", L[n_][:], sc["FM"][dd, q, tt, :, hs, half * 64:(half + 1) * 64],
                          r=[self.r_scr["FM"][dd][q][tt]], w=[R[n_]])
                for q, n_ in enumerate(("Bh", "Kh")):
                    k.dma("sp", f"{c.nm}{n_}", L[n_][:].rearrange("p h n -> p (h n)"), sc["TM"][dd, q, rows, cs],
                          r=[self.r_scr["TM"][dd][q][ct // 2]], w=[R[n_]])
                k.dma("sp", f"{c.nm}Vt", L["Vt"][:].rearrange("p h n -> p (h n)"), sc["RAWV"][rows, cs],
                      r=[self.r_scr["RAW"][2][ct // 2]], w=[R["Vt"]])
                k.dma("sp", f"{c.nm}Gt", L["Gt"][:].rearrange("p h n -> p (h n)"), sc["GG"][dd, rows, cs],
                      r=[self.r_scr["GG"][dd][ct // 2]], w=[R["Gt"]])
                k.dma("sp", f"{c.nm}rk", c.rk[:], sc["RK"][dd, rows, hs], r=[self.r_scr["RK"][dd][ct // 2]], w=[c.r_rk])
                yield
                P1, P1T, P2, P2T, T, U6 = range(6)
                S_, RS = c.sl, c.r_sl
                pi = nb(c)
                mm(c, pi, [(L["BT"], L["AT"])], [R["BT"], R["AT"]])
                k.op("dve", lambda e: e.tensor_tensor(out=S_[P1][:], in0=p3(pi), in1=bcm(dd * 3 + 0), op=ALU.mult),
                     r=[self.r_ps[pi], r_cb], w=[RS[P1]])
                yield
                pi = nb(c)
                mm(c, pi, [(L["AT"], L["BT"])], [R["BT"], R["AT"]])
                k.op("dve", lambda e: e.tensor_tensor(out=S_[P1T][:], in0=p3(pi), in1=bcm(dd * 3 + 1), op=ALU.mult),
                     r=[self.r_ps[pi], r_cb], w=[RS[P1T]])
                k.op("dve", lambda e: e.tensor_tensor(out=S_[T][:], in0=S_[P1][:], in1=bci, op=ALU.add),
                     r=[RS[P1], self.r_const], w=[RS[T]])
                yield
                a, aT, b_, bT = P1, P1T, P2, P2T
                for lvl in range(5):
                    pi = nb(c)
                    mm(c, pi, [(S_[a], S_[aT])], [RS[a], RS[aT]])
                    k.op("act", lambda e, pi=pi, bT=bT: e.copy(out=S_[bT][:], in_=p3(pi)), r=[self.r_ps[pi]], w=[RS[bT]])
                    if lvl < 4:
                        pi2 = nb(c)
                        mm(c, pi2, [(S_[aT], S_[a])], [RS[a], RS[aT]])
                        k.op("act", lambda e, pi2=pi2, b_=b_: e.copy(out=S_[b_][:], in_=p3(pi2)), r=[self.r_ps[pi2]], w=[RS[b_]])
                    yield
                    pi = nb(c)
                    mm(c, pi, [(S_[bT], S_[T])], [RS[bT], RS[T]])
                    k.op("dve", lambda e, pi=pi: e.tensor_tensor(out=S_[T][:], in0=S_[T][:], in1=p3(pi), op=ALU.add),
                         r=[self.r_ps[pi], RS[T]], w=[RS[T]])
                    yield
                    a, aT, b_, bT = b_, bT, a, aT
                Aak, Abr, Akr, WT = P1, P1T, P2, P2T
                for (dst, lt, rt, mq, eng) in ((Aak, "KT", "AT", 0, "dve"), (Abr, "BT", "RT", 2, "pool"), (Akr, "KT", "RT", 2, "dve")):
                    pi = nb(c)
                    mm(c, pi, [(L[lt], L[rt])], [R[lt], R[rt]])
                    k.op("dve", lambda e, pi=pi, dst=dst, mq=mq: e.tensor_tensor(out=S_[dst][:], in0=p3(pi), in1=bcm(dd * 3 + mq), op=ALU.mult),
                         r=[self.r_ps[pi], r_cb], w=[RS[dst]])
                    yield
                pi = nb(c)
                mm(c, pi, [(L["AT"], c.STb), (S_[Aak], L["Vt"])], [R["AT"], c.r_STb, RS[Aak], R["Vt"]])
                k.op("act", lambda e: e.copy(out=S_[WT][:], in_=p3(pi)), r=[self.r_ps[pi]], w=[RS[WT]])
                yield
                pi = nb(c)
                mm(c, pi, [(S_[T], S_[WT])], [RS[T], RS[WT]])
                k.op("act", lambda e: e.copy(out=S_[U6][:], in_=p3(pi)), r=[self.r_ps[pi]], w=[RS[U6]])
                yield
                piy = nb(c)
                mm(c, piy, [(L["RT"], c.STb), (S_[Abr], S_[U6]), (S_[Akr], L["Vt"])],
                   [R["RT"], c.r_STb, RS[Abr], RS[U6], RS[Akr], R["Vt"]])
                k.op("act", lambda e: e.copy(out=c.y[:], in_=p3(piy)), r=[self.r_ps[piy]], w=[c.r_y])
                pis = nb(c)
                mm(c, pis, [(L["Bh"], S_[U6]), (L["Kh"], L["Vt"])], [R["Bh"], RS[U6], R["Kh"], R["Vt"]])
                pcb = PCt[:, dd, tt, hs, half].unsqueeze(2).to_broadcast([64, NH, 64])
                k.op("dve", lambda e: e.tensor_tensor(out=c.ST[:], in0=c.ST[:], in1=pcb, op=ALU.mult), r=[c.r_ST, r_PC], w=[c.r_ST])
                k.op("dve", lambda e: e.tensor_tensor(out=c.ST[:], in0=c.ST[:], in1=p3(pis), op=ALU.add), r=[c.r_ST, self.r_ps[pis]], w=[c.r_ST])
                k.op("act", lambda e: e.copy(out=c.STb[:], in_=c.ST[:]), r=[c.r_ST], w=[c.r_STb])
                yield
                sm, r_sm = c.sm, c.r_sm
                bl = lambda a0: sm[:, a0:a0 + NH].unsqueeze(2).to_broadcast([64, NH, 64])
                dv = lambda fn, r, w: k.op("dve", fn, r=r, w=w)
                dv(lambda e: e.reduce_sum(out=sm[:, 0:NH], in_=c.y[:], axis=AX.X), [c.r_y], [r_sm])
                k.op("act", lambda e: e.activation(out=c.sq[:], in_=c.y[:], func=AF.Square), r=[c.r_y], w=[c.r_sq])
                dv(lambda e: e.reduce_sum(out=sm[:, NH:2 * NH], in_=c.sq[:], axis=AX.X), [c.r_sq], [r_sm])
                dv(lambda e: e.tensor_scalar_mul(out=sm[:, 0:2 * NH], in0=sm[:, 0:2 * NH], scalar1=1.0 / 64), [r_sm], [r_sm])
                dv(lambda e: e.tensor_tensor(out=sm[:, 2 * NH:3 * NH], in0=sm[:, 0:NH], in1=sm[:, 0:NH], op=ALU.mult), [r_sm], [r_sm])
                dv(lambda e: e.tensor_tensor(out=sm[:, 3 * NH:4 * NH], in0=sm[:, NH:2 * NH], in1=sm[:, 2 * NH:3 * NH], op=ALU.subtract), [r_sm], [r_sm])
                k.op("act", lambda e: e.activation(out=sm[:, 4 * NH:5 * NH], in_=sm[:, 3 * NH:4 * NH], func=AF.Sqrt, bias=epsg[:, 0:1], scale=1.0),
                     r=[r_sm, r_cb], w=[r_sm])
                dv(lambda e: e.reciprocal(out=sm[:, 5 * NH:6 * NH], in_=sm[:, 4 * NH:5 * NH]), [r_sm], [r_sm])
                yield
                z, r_z = c.y, c.r_y
                dv(lambda e: e.tensor_tensor(out=z[:], in0=c.y[:], in1=bl(0), op=ALU.subtract), [c.r_y, r_sm], [r_z])
                dv(lambda e: e.tensor_tensor(out=z[:], in0=z[:], in1=bl(5 * NH), op=ALU.mult), [r_z, r_sm], [r_z])
                zf = z[:].rearrange("p h n -> p (h n)")
                k.op("dve", lambda e: e.tensor_tensor(out=zf, in0=zf, in1=LNW[:, cs], op=ALU.mult), r=[r_z, r_cb], w=[r_z])
                k.op("dve", lambda e: e.tensor_tensor(out=zf, in0=zf, in1=LNB[:, cs], op=ALU.add), r=[r_z, r_cb], w=[r_z])
                dv(lambda e: e.tensor_tensor(out=c.sq[:], in0=L["Vt"][:], in1=c.rk[:, :].unsqueeze(2).to_broadcast([64, NH, 64]), op=ALU.mult),
                   [R["Vt"], c.r_rk], [c.r_sq])
                k.op("dve", lambda e: e.tensor_tensor(out=z[:], in0=z[:], in1=c.sq[:], op=ALU.add), r=[r_z, c.r_sq], w=[r_z])
                k.op("dve", lambda e: e.tensor_tensor(out=z[:], in0=z[:], in1=L["Gt"][:], op=ALU.mult), r=[r_z, R["Gt"]], w=[r_z])
                yield
                pt = nb(c)
                for q in range(4):
                    k.op("pe", lambda e, q=q: e.transpose(self.ps[pt][:, q * 64:(q + 1) * 64], zf[:, q * 128:(q + 1) * 128], i64),
                         r=[r_z, self.r_const], w=[self.r_ps[pt]])
                tb = ct // 8
                osl = oT[:, hg * 4:hg * 4 + 4, ct * 64:(ct + 1) * 64]
                k.op("dve", lambda e: e.tensor_tensor(out=osl, in0=osl, in1=self.ps[pt][:, 0:256].rearrange("p (q t) -> p q t", q=4), op=ALU.add),
                     r=[self.r_ps[pt]] + [r_oT[hg * 4 + q][tb] for q in range(4)], w=[r_oT[hg * 4 + q][tb] for q in range(4)])
                yield

            nchunks = self.cfg.get("rw_chunks", 32)
            for c in chains:
                k.op("pool", lambda e, c=c: e.memset(c.ST[:], 0.0), w=[c.r_ST])
                k.op("pool", lambda e, c=c: e.memset(c.STb[:], 0.0), w=[c.r_STb])
            for n in range(nchunks):
                gens = [chunk_steps(c, n) for c in chains]
                live = list(gens)
                while live:
                    for g_ in list(live):
                        try:
                            next(g_)
                        except StopIteration:
                            live.remove(g_)
            k.barrier()
        with ExitStack() as es:
            wo = es.enter_context(self.sbuf("rwo", [128, NKC, D], BF16))
            r_wo = k.res()
            k.dma("pool", "rwo", wo[:], d["rw_w_o"][j].rearrange("(kc p) n -> p kc n", p=128), w=[r_wo])
            for tb in range(NTB):
                tsl = slice(tb * 512, (tb + 1) * 512)
                for dc in range(NKC):
                    pi = dc % 2
                    for kc in range(NKC):
                        k.op("pe", lambda e, kc=kc, dc=dc, pi=pi: e.matmul(self.ps[pi][:, :], wo[:, kc, dc * 128:(dc + 1) * 128], oT[:, kc, tsl],
                                                                           start=(kc == 0), stop=(kc == NKC - 1)),
                             r=[r_wo, r_oT[kc][tb]], w=[self.r_ps[pi]])
                    k.op("dve", lambda e, dc=dc, pi=pi: e.scalar_tensor_tensor(
                        out=self.xT[:, dc, tsl], in0=self.ps[pi][:, :], scalar=self.modT[:, i, 16 + dc:17 + dc],
                        in1=self.xT[:, dc, tsl], op0=ALU.mult, op1=ALU.add),
                        r=[self.r_ps[pi], self.r_mod], w=[self.r_xT[dc][tb]])
            k.barrier()

    def attn(self, i):
        nc, k, d = self.nc, self.k, self.dram
        j = i // 2
        DIL = [1, 4, 16]
        ss = lambda start, n, step: slice(start, start + (n - 1) * step + 1, step)
        all_hT = [x for y in self.r_hT for x in y]
        with ExitStack() as es:
            identb = es.enter_context(self.sbuf("identb", [128, 128], BF16))
            r_idb = k.res()
            k.op("act", lambda e: e.copy(out=identb[:], in_=self.ident[:]), r=[self.r_const], w=[r_idb])
            w3 = [es.enter_context(self.sbuf(f"w3_{b}", [128, 3, NKC, 128], BF16)) for b in range(2)]
            r_w3 = [k.res() for _ in range(2)]
            wo = [es.enter_context(self.sbuf(f"wo_{b}", [128, D], BF16)) for b in range(2)]
            r_wo = [k.res() for _ in range(2)]
            bias = [es.enter_context(self.sbuf(f"ab_{b}", [128, 384], F32)) for b in range(2)]
            r_bias = [k.res() for _ in range(2)]
            QT = es.enter_context(self.sbuf("QT", [128, S], BF16))
            KT = es.enter_context(self.sbuf("KT", [128, S], BF16))
            V = es.enter_context(self.sbuf("Vb", [128, 16, 128], BF16))
            r_QT, r_KT, r_V = k.res(), k.res(), k.res()
            Og = [es.enter_context(self.sbuf(f"Og{g}", [128, S], F32)) for g in range(3)]
            r_Og = [k.res() for _ in range(3)]
            LSE = es.enter_context(self.sbuf("LSE", [1, 3, S], F32))
            r_LSE = k.res()
            rowA = es.enter_context(self.sbuf("rowA", [1, S], F32))
            r_rowA = k.res()
            NB = 4
            sc = [es.enter_context(self.sbuf(f"sc{b}", [128, 384], F32)) for b in range(NB)]
            pn = [es.enter_context(self.sbuf(f"pn{b}", [128, 384], BF16)) for b in range(NB)]
            st = [es.enter_context(self.sbuf(f"ast{b}", [128, 8], F32)) for b in range(NB)]
            r_blk = [k.res() for _ in range(NB)]
            PT = [es.enter_context(self.sbuf(f"PT{b}", [128, 384], BF16)) for b in range(2)]
            r_PT = [k.res() for _ in range(2)]
            mrg = es.enter_context(self.sbuf("mrg", [128, S], BF16))
            r_mrg = k.res()
            tmpM = es.enter_context(self.sbuf("tmpM", [128, 512], F32))
            t2 = es.enter_context(self.sbuf("t2M", [128, 512], F32))
            r_tmpM, r_t2 = k.res(), k.res()
            psT = self.ps[4][:, :].bitcast(BF16)

            def load_unit(u):
                g, h = u % 3, u // 3
                b = u % 2
                for t in range(3):
                    c0 = ((g * 3 + t) * 8 + h) * 128
                    k.dma("pool", f"w3_{b}", w3[b][:, t, :, :],
                          d["at_w_qkv"][j, :, c0:c0 + 128].rearrange("(kc p) n -> p kc n", p=128), w=[r_w3[b]])
                k.dma("sp", f"ab_{b}", bias[b][:, :], d["c_abias"][g * 8 + h], w=[r_bias[b]])

            def load_wo(h):
                b = h % 2
                k.dma("pool", f"wo_{b}", wo[b][:, :], d["at_w_o"][j, h * 128:(h + 1) * 128, :], w=[r_wo[b]])

            nblk_it = [0]

            def stageA(u, blk):
                g, h = u % 3, u // 3
                dil = DIL[g]
                nblk = 16 // dil
                r_, ib = blk // nblk, blk % nblk
                kb0, kb1 = max(ib - 1, 0), min(ib + 1, nblk - 1)
                nkb = kb1 - kb0 + 1
                nk = nkb * 128
                bc0 = (kb0 - (ib - 1)) * 128
                qsl = ss(r_ + dil * ib * 128, 128, dil)
                ksl = ss(r_ + dil * kb0 * 128, nk, dil)
                n = nblk_it[0]
                nblk_it[0] += 1
                sb_ = n % NB
                psc = 3 if n % 2 == 0 else 6
                bb = u % 2
                k.op("pe", lambda e: e.matmul(self.ps[psc][:, 0:nk], QT[:, qsl], KT[:, ksl], start=True, stop=True),
                     r=[r_QT, r_KT], w=[self.r_ps[psc]])
                k.op("dve", lambda e: e.tensor_tensor(out=sc[sb_][:, 0:nk], in0=self.ps[psc][:, 0:nk],
                                                      in1=bias[bb][:, bc0:bc0 + nk], op=ALU.add),
                     r=[self.r_ps[psc], r_bias[bb]], w=[r_blk[sb_]])
                k.op("dve", lambda e: e.reduce_max(out=st[sb_][:, 0:1], in_=sc[sb_][:, 0:nk], axis=AX.X),
                     r=[r_blk[sb_]], w=[r_blk[sb_]])
                k.op("dve", lambda e: e.tensor_scalar_mul(out=st[sb_][:, 1:2], in0=st[sb_][:, 0:1], scalar1=-1.0),
                     r=[r_blk[sb_]], w=[r_blk[sb_]])
                k.op("act", lambda e: e.activation(out=sc[sb_][:, 0:nk], in_=sc[sb_][:, 0:nk], func=AF.Exp,
                                                   bias=st[sb_][:, 1:2], scale=1.0, accum_out=st[sb_][:, 2:3]),
                     r=[r_blk[sb_]], w=[r_blk[sb_]])
                k.op("act", lambda e: e.activation(out=st[sb_][:, 4:5], in_=st[sb_][:, 2:3], func=AF.Ln),
                     r=[r_blk[sb_]], w=[r_blk[sb_]])
                k.op("dve", lambda e: e.reciprocal(out=st[sb_][:, 3:4], in_=st[sb_][:, 2:3]),
                     r=[r_blk[sb_]], w=[r_blk[sb_]])
                k.op("dve", lambda e: e.tensor_scalar_mul(out=pn[sb_][:, 0:nk], in0=sc[sb_][:, 0:nk],
                                                          scalar1=st[sb_][:, 3:4]),
                     r=[r_blk[sb_]], w=[r_blk[sb_]])
                k.op("dve", lambda e: e.tensor_tensor(out=st[sb_][:, 5:6], in0=st[sb_][:, 4:5], in1=st[sb_][:, 0:1],
                                                      op=ALU.add),
                     r=[r_blk[sb_]], w=[r_blk[sb_]])
                return (g, r_, kb0, nkb, nblk, qsl, sb_, n)

            def stageB(desc):
                g, r_, kb0, nkb, nblk, qsl, sb_, n = desc
                nk = nkb * 128
                pb = n % 2
                for c in range(nkb):
                    k.op("pe", lambda e, c=c: e.transpose(psT[:, c * 128:(c + 1) * 128], pn[sb_][:, c * 128:(c + 1) * 128],
                                                          identb[:]),
                         r=[r_blk[sb_], r_idb], w=[self.r_ps[4]])
                k.op("act", lambda e: e.copy(out=PT[pb][:, 0:nk], in_=psT[:, 0:nk]),
                     r=[self.r_ps[4]], w=[r_PT[pb]])
                k.op("pe", lambda e: e.transpose(self.ps[5][0:1, 128:256], st[sb_][:, 5:6], self.ident[:]),
                     r=[r_blk[sb_], self.r_const], w=[self.r_ps[5]])
                for c in range(nkb):
                    vb = r_ * nblk + kb0 + c
                    k.op("pe", lambda e, c=c, vb=vb: e.matmul(self.ps[5][:, 0:128], V[:, vb, :], PT[pb][:, c * 128:(c + 1) * 128],
                                                              start=(c == 0), stop=(c == nkb - 1)),
                         r=[r_V, r_PT[pb]], w=[self.r_ps[5]])
                k.op("dve", lambda e: e.tensor_copy(out=Og[g][:, qsl], in_=self.ps[5][:, 0:128]),
                     r=[self.r_ps[5]], w=[r_Og[g]])
                k.op("dve", lambda e: e.tensor_copy(out=LSE[0:1, g, qsl], in_=self.ps[5][0:1, 128:256]),
                     r=[self.r_ps[5]], w=[r_LSE])

            def proj_unit(u):
                g, h = u % 3, u // 3
                b = u % 2
                dil = DIL[g]
                nblk = 16 // dil
                for tb in range(NTB):
                    tsl = slice(tb * 512, (tb + 1) * 512)
                    for kc in range(NKC):
                        k.op("pe", lambda e, kc=kc: e.matmul(self.ps[0][:, :], w3[b][:, 0, kc, :], self.hT[:, kc, tsl],
                                                             start=(kc == 0), stop=(kc == NKC - 1)),
                             r=[r_w3[b], self.r_hT[kc][tb]], w=[self.r_ps[0]])
                    k.op("act", lambda e: e.mul(out=QT[:, tsl], in_=self.ps[0][:, :], mul=float(128 ** -0.5)),
                         r=[self.r_ps[0]], w=[r_QT])
                    for kc in range(NKC):
                        k.op("pe", lambda e, kc=kc: e.matmul(self.ps[1][:, :], w3[b][:, 1, kc, :], self.hT[:, kc, tsl],
                                                             start=(kc == 0), stop=(kc == NKC - 1)),
                             r=[r_w3[b], self.r_hT[kc][tb]], w=[self.r_ps[1]])
                    k.op("dve", lambda e: e.tensor_copy(out=KT[:, tsl], in_=self.ps[1][:, :]),
                         r=[self.r_ps[1]], w=[r_KT])
                for b4 in range(4):
                    for q in range(4):
                        blk = b4 * 4 + q
                        r_, ib = blk // nblk, blk % nblk
                        tsl = ss(r_ + dil * ib * 128, 128, dil)
                        for kc in range(NKC):
                            k.op("pe", lambda e, kc=kc, q=q, tsl=tsl: e.matmul(
                                self.ps[2][:, q * 128:(q + 1) * 128], self.hT[:, kc, tsl], w3[b][:, 2, kc, :],
                                start=(kc == 0), stop=(kc == NKC - 1)),
                                r=[r_w3[b]] + all_hT, w=[self.r_ps[2]])
                    k.op("act", lambda e, b4=b4: e.copy(out=V[:, b4 * 4:b4 * 4 + 4, :],
                                                        in_=self.ps[2][:, :].rearrange("p (q n) -> p q n", q=4)),
                         r=[self.r_ps[2]], w=[r_V])

            def merge_head(h):
                b = h % 2
                row = lambda g: LSE[0:1, g, :]
                dv = lambda fn, r, w: k.op("dve", fn, r=r, w=w)
                dv(lambda e: e.tensor_tensor(out=rowA[0:1, :], in0=row(0), in1=row(1), op=ALU.max), [r_LSE], [r_rowA])
                dv(lambda e: e.tensor_tensor(out=rowA[0:1, :], in0=rowA[0:1, :], in1=row(2), op=ALU.max), [r_LSE, r_rowA], [r_rowA])
                for g in range(3):
                    dv(lambda e, g=g: e.tensor_tensor(out=row(g), in0=row(g), in1=rowA[0:1, :], op=ALU.subtract),
                       [r_LSE, r_rowA], [r_LSE])
                k.op("act", lambda e: e.activation(out=LSE[0:1, :, :], in_=LSE[0:1, :, :], func=AF.Exp), r=[r_LSE], w=[r_LSE])
                dv(lambda e: e.tensor_tensor(out=rowA[0:1, :], in0=row(0), in1=row(1), op=ALU.add), [r_LSE], [r_rowA])
                dv(lambda e: e.tensor_tensor(out=rowA[0:1, :], in0=rowA[0:1, :], in1=row(2), op=ALU.add), [r_LSE, r_rowA], [r_rowA])
                dv(lambda e: e.reciprocal(out=rowA[0:1, :], in_=rowA[0:1, :]), [r_rowA], [r_rowA])
                for g in range(3):
                    dv(lambda e, g=g: e.tensor_tensor(out=row(g), in0=row(g), in1=rowA[0:1, :], op=ALU.mult),
                       [r_LSE, r_rowA], [r_LSE])
                for tb in range(NTB):
                    tsl = slice(tb * 512, (tb + 1) * 512)
                    for g in range(3):
                        pi = 6 if g % 2 == 0 else 3
                        k.op("pe", lambda e, g=g, pi=pi: e.matmul(self.ps[pi][:, :], self.ones[0:1, :], LSE[0:1, g, tsl],
                                                                  start=True, stop=True),
                             r=[r_LSE, self.r_const], w=[self.r_ps[pi]])
                        if g == 0:
                            dv(lambda e, pi=pi: e.tensor_tensor(out=tmpM[:, :], in0=Og[0][:, tsl], in1=self.ps[pi][:, :], op=ALU.mult),
                               [r_Og[0], self.r_ps[pi]], [r_tmpM])
                        else:
                            dv(lambda e, g=g, pi=pi: e.tensor_tensor(out=t2[:, :], in0=Og[g][:, tsl], in1=self.ps[pi][:, :], op=ALU.mult),
                               [r_Og[g], self.r_ps[pi]], [r_t2])
                            if g == 1:
                                k.op("dve", lambda e: e.tensor_tensor(out=tmpM[:, :], in0=tmpM[:, :], in1=t2[:, :], op=ALU.add),
                                     r=[r_tmpM, r_t2], w=[r_tmpM])
                            else:
                                k.op("dve", lambda e: e.tensor_tensor(out=mrg[:, tsl], in0=tmpM[:, :], in1=t2[:, :], op=ALU.add),
                                     r=[r_tmpM, r_t2], w=[r_mrg])
                for tb in range(NTB):
                    tsl = slice(tb * 512, (tb + 1) * 512)
                    for dc in range(NKC):
                        pi = 7 if dc % 2 == 0 else 0
                        k.op("pe", lambda e, dc=dc, pi=pi: e.matmul(self.ps[pi][:, :], wo[b][:, dc * 128:(dc + 1) * 128], mrg[:, tsl],
                                                                    start=True, stop=True),
                             r=[r_wo[b], r_mrg], w=[self.r_ps[pi]])
                        k.op("dve", lambda e, dc=dc, pi=pi: e.scalar_tensor_tensor(
                            out=self.xT[:, dc, tsl], in0=self.ps[pi][:, :], scalar=self.modT[:, i, 16 + dc:17 + dc],
                            in1=self.xT[:, dc, tsl], op0=ALU.mult, op1=ALU.add),
                            r=[self.r_ps[pi], self.r_mod], w=[self.r_xT[dc][tb]])

            LA = 2
            load_unit(0)
            for u in range(24):
                g, h = u % 3, u // 3
                if g == 0:
                    load_wo(h)
                if u + 1 < 24:
                    load_unit(u + 1)
                proj_unit(u)
                pend = []
                for blk in range(16):
                    pend.append(stageA(u, blk))
                    if len(pend) > LA:
                        stageB(pend.pop(0))
                while pend:
                    stageB(pend.pop(0))
                if g == 2:
                    merge_head(h)
            k.barrier()

    def final(self):
        nc, k, d = self.nc, self.k, self.dram
        plain = self.cfg.get("final_plain", False)
        with ExitStack() as es:
            sq = es.enter_context(self.sbuf("fsq", [128, NKC, 512], BF16))
            rstd = es.enter_context(self.sbuf("frstd", [128, 512], F32))
            y = es.enter_context(self.sbuf("fy", [128, NKC, 512], F32))
            ost = [es.enter_context(self.sbuf(f"fo{j}", [128, D], F32)) for j in range(2)]
            r_o = [k.res() for _ in range(2)]
            r_sq, r_rstd, r_y = k.res(), k.res(), k.res()
            gfin = self.gT[:, 2 * DEPTH, :]
            for tb in range(NTB):
                tsl = slice(tb * 512, (tb + 1) * 512)
                if not plain:
                    self.norm_core(es, tb, None, None, None, sq, r_sq, rstd, r_rstd, 7)
                for kc in range(NKC):
                    if plain:
                        k.op("dve", lambda e, kc=kc: e.tensor_copy(out=y[:, kc, :], in_=self.xT[:, kc, tsl]),
                             r=[self.r_xT[kc][tb]], w=[r_y])
                    else:
                        k.op("dve", lambda e, kc=kc: e.scalar_tensor_tensor(
                            out=y[:, kc, :], in0=self.xT[:, kc, tsl], scalar=gfin[:, kc:kc + 1], in1=rstd[:, :],
                            op0=ALU.mult, op1=ALU.mult),
                            r=[self.r_xT[kc][tb], r_rstd, self.r_const], w=[r_y])
                for t4 in range(4):
                    tt = tb * 4 + t4
                    ob = tt % 2
                    for half in range(2):
                        pi = (tt * 2 + half) % 4
                        for j in range(4):
                            kc = half * 4 + j
                            k.op("pe", lambda e, kc=kc, j=j, pi=pi: e.transpose(
                                self.ps[pi][:, j * 128:(j + 1) * 128], y[:, kc, t4 * 128:(t4 + 1) * 128], self.ident[:]),
                                r=[r_y, self.r_const], w=[self.r_ps[pi]])
                        if half == 0:
                            k.op("act", lambda e, pi=pi, ob=ob: e.copy(out=ost[ob][:, 0:512], in_=self.ps[pi][:, :]),
                                 r=[self.r_ps[pi]], w=[r_o[ob]])
                        else:
                            k.op("dve", lambda e, pi=pi, ob=ob: e.tensor_copy(out=ost[ob][:, 512:1024], in_=self.ps[pi][:, :]),
                                 r=[self.r_ps[pi]], w=[r_o[ob]])
                    k.dma("sp", f"fo{ob}", d["out"][tt * 128:(tt + 1) * 128, :], ost[ob][:], r=[r_o[ob]])


def host_consts():
    ident = np.eye(128, dtype=np.float32)
    sel = np.zeros((32, 32 * 128), np.float32)
    for e in range(32):
        sel[e, e * 128:(e + 1) * 128] = 1.0
    ab = np.zeros((24, 128, 384), np.float32)
    ii = np.arange(128)[:, None]
    jj = np.arange(384)[None, :]
    rel = np.abs((jj - 128) - ii).astype(np.float32)
    for g, dil in enumerate((1, 4, 16)):
        for h in range(8):
            slope = 2.0 ** (-8.0 * (g * 8 + h + 1) / 24.0)
            ab[g * 8 + h] = np.where(rel <= 64, -slope * rel * dil, -1e30)
    CW = -float(np.exp(-0.5))
    tri = np.zeros((6, 128, 128), np.float32)
    msk = np.zeros((6, 64, 64), np.float32)
    sidx = np.arange(128)[:, None]
    tidx = np.arange(128)[None, :]
    same = (sidx // 64) == (tidx // 64)
    s64 = np.arange(64)[:, None]
    t64 = np.arange(64)[None, :]
    for dd in range(2):
        before = (sidx < tidx) if dd == 0 else (sidx > tidx)
        after = (sidx > tidx) if dd == 0 else (sidx < tidx)
        tri[dd * 3 + 0] = np.where(same & (before | (sidx == tidx)), CW, 0.0)
        tri[dd * 3 + 1] = np.where(same & before, CW, 0.0)
        tri[dd * 3 + 2] = np.where(same & after, CW, 0.0)
        b64 = (s64 < t64) if dd == 0 else (s64 > t64)
        msk[dd * 3 + 0] = b64
        msk[dd * 3 + 1] = b64.T
        msk[dd * 3 + 2] = b64 | (s64 == t64)
    csel = np.zeros((128, 2), np.float32)
    csel[:64, 0] = CW
    csel[64:, 1] = CW
    return {"c_ident": ident, "c_sel": sel, "c_abias": ab, "c_tri": tri, "c_csel": csel, "c_mask": msk}


def prep_shared(inp):
    f = lambda a: np.ascontiguousarray(a, dtype=np.float32)
    sh = {}
    sh["ada_w"] = f(inp["ada_w"])
    sh["ada_bT"] = f(np.asarray(inp["ada_b"]).reshape(DEPTH, 48, 128).transpose(0, 2, 1))
    sh["gtmT"] = f(np.asarray(inp["norm_tm_g"]).reshape(DEPTH, NKC, 128).transpose(0, 2, 1))
    sh["gcmT"] = f(np.asarray(inp["norm_cm_g"]).reshape(DEPTH, NKC, 128).transpose(0, 2, 1))
    sh["gfinT"] = f(np.asarray(inp["final_g"]).reshape(NKC, 128).T)
    rg = np.asarray(inp["moe_router_g"])
    re_ = np.asarray(inp["moe_router_e"])
    wr = np.concatenate([rg] + [re_[:, g] for g in range(4)], axis=-1)
    sh["wr"] = f(wr.reshape(DEPTH, NKC, 128, 36).transpose(0, 2, 1, 3))
    br = np.concatenate([np.asarray(inp["moe_router_g_b"]), np.asarray(inp["moe_router_e_b"]).reshape(DEPTH, 32)], axis=-1)
    sh["br"] = f(br.reshape(DEPTH, 1, 36))
    sh["rw_muT"] = f(np.asarray(inp["rw_mu"]).reshape(2, 6, NKC, 128).transpose(0, 3, 1, 2))
    sh["rw_w_rkv"] = f(inp["rw_w_rkv"])
    sh["rw_w1cat"] = f(np.concatenate([np.asarray(inp["rw_w1"])[:, 0], np.asarray(inp["rw_w1"])[:, 1]], axis=-1))
    sh["rw_a1cat"] = f(np.concatenate([np.asarray(inp["rw_a1"])[:, 0], np.asarray(inp["rw_a1"])[:, 1]], axis=-1))
    sh["rw_g1"] = f(inp["rw_g1"])
    sh["rw_w2cat"] = f(np.asarray(inp["rw_w2"]).reshape(2, 128, D))
    sh["rw_a2cat"] = f(np.asarray(inp["rw_a2"]).reshape(2, 128, D))
    sh["rw_g2"] = f(inp["rw_g2"])
    sh["rw_w0"] = f(inp["rw_w0"])
    sh["rw_a0"] = f(inp["rw_a0"])
    for n_ in ("rw_k_k", "rw_k_a", "rw_ln_w", "rw_ln_b"):
        sh[n_] = f(inp[n_])
    sh["rw_r_k"] = f(np.asarray(inp["rw_r_k"]).reshape(2, D))
    sh["rw_w_o"] = f(inp["rw_w_o"])
    sh["at_w_qkv"] = f(inp["at_w_qkv"])
    sh["at_w_o"] = f(inp["at_w_o"])
    sh["moe_w_gate"] = f(inp["moe_w_gate"])
    sh["moe_w_up"] = f(inp["moe_w_up"])
    sh["moe_w_down"] = f(inp["moe_w_down"])
    sh.update(host_consts())
    return sh


def prep_core(inp, b):
    return {
        "x": np.ascontiguousarray(np.asarray(inp["x"])[b], dtype=np.float32),
        "cT": np.ascontiguousarray(np.asarray(inp["c"])[b].reshape(NKC, 128).T, dtype=np.float32),
    }


def build_prog(cfg):
    nc = bass.Bass("TRN2", target_bir_lowering=False)
    p = Prog(nc, cfg)
    with p.k.es:
        p.build()
    return nc, p


def run(inp, cfg, cores):
    nc, p = build_prog(cfg)
    sh = prep_shared(inp)
    in_maps = []
    for b in cores:
        m = dict(sh)
        m.update(prep_core(inp, b))
        m = {kk: v for kk, v in m.items() if kk in p.dram}
        in_maps.append(m)
    res = run_bass_kernel_spmd(nc, in_maps, core_ids=list(range(len(cores))))
    return res


def kernel(**inputs):
    res = run(inputs, {}, list(range(8)))
    out = np.stack([np.asarray(r["out"]) for r in res.results], axis=0)
    return out.astype(np.float32)
```

```python
import numpy as np
from contextlib import ExitStack

import concourse.bass as bass
import concourse.mybir as mybir
from concourse.bass_utils import run_bass_kernel_spmd

F32 = mybir.dt.float32
BF16 = mybir.dt.bfloat16
AF = mybir.ActivationFunctionType
ALU = mybir.AluOpType
AX = mybir.AxisListType

D = 1024
S = 2048
DEPTH = 4
NKC = 8
NTB = 4
NTT = 16
RMS_EPS = 1e-6
SEM_LIM = 24000


class Res:
    __slots__ = ("name", "w", "rs", "excl")

    def __init__(self, name):
        self.name = name
        self.w = None
        self.rs = {}
        self.excl = False

    def add_reader(self, tag):
        st, ep, v = tag
        if self.rs.get(st, (-1, 0)) < (ep, v):
            self.rs[st] = (ep, v)


class KB:
    COMPUTE = ("pe", "act", "dve", "pool")

    def __init__(self, nc):
        self.nc = nc
        self.es = ExitStack()
        self.eng = {"pe": nc.tensor, "act": nc.scalar, "dve": nc.vector, "pool": nc.gpsimd, "sp": nc.sync}
        self.sems = {}
        self.cnt = {}
        self.seen = {e: {} for e in self.eng}
        self.nres = 0
        self.epoch_final = {}
        self.ninst = {e: 0 for e in self.eng}

    def res(self, name=None):
        self.nres += 1
        return Res(name or f"r{self.nres}")

    def sb(self, name, shape, dt):
        return self.es.enter_context(self.nc.sbuf_tensor(name, list(shape), dt))

    def _sem(self, stream, epoch):
        key = (stream, epoch)
        if key not in self.sems:
            self.sems[key] = self.es.enter_context(self.nc.semaphore(f"s_{stream}_{epoch}"))
        return self.sems[key]

    def _bump(self, stream, inc):
        ep, v = self.cnt.get(stream, (0, 0))
        if v + inc > SEM_LIM:
            self.epoch_final[(stream, ep)] = v
            ep, v = ep + 1, 0
        v += inc
        self.cnt[stream] = (ep, v)
        return (stream, ep, v), self._sem(stream, ep)

    def _wait(self, eng, tags):
        best = {}
        for t in tags:
            if t is None:
                continue
            st, ep, v = t
            if st == "pe" and eng == "pe":
                continue
            if (st not in best) or (ep, v) > best[st]:
                best[st] = (ep, v)
        for st, (ep, v) in best.items():
            if st.startswith("d_"):
                cur_ep, cur_v = self.cnt[st]
                v = cur_v if cur_ep == ep else self.epoch_final[(st, ep)]
            if self.seen[eng].get(st, (-1, 0)) >= (ep, v):
                continue
            self.eng[eng].wait_ge(self._sem(st, ep), v)
            self.seen[eng][st] = (ep, v)

    def _deps(self, r, w, same_stream=None):
        tags = []
        for x in r:
            tags.append(x.w)
            if x.excl:
                for st, (ep, v) in x.rs.items():
                    if same_stream is not None and st == same_stream:
                        continue
                    tags.append((st, ep, v))
        for x in w:
            tags.append(x.w)
            for st, (ep, v) in x.rs.items():
                if same_stream is not None and st == same_stream:
                    continue
                tags.append((st, ep, v))
        return tags

    def op(self, eng, fn, r=(), w=()):
        self._wait(eng, self._deps(r, w, same_stream=eng))
        ins = fn(self.eng[eng])
        tag, sem = self._bump(eng, 1)
        ins.then_inc(sem, 1)
        self.ninst[eng] += 1
        for x in r:
            x.add_reader(tag)
        for x in w:
            x.w = tag
            x.rs = {}
        return tag

    def dma(self, q, chan, out, in_, r=(), w=(), **kw):
        self._wait(q, self._deps(r, w))
        ins = self.eng[q].dma_start(out=out, in_=in_, **kw)
        tag, sem = self._bump("d_" + chan, 16)
        ins.then_inc(sem, 16)
        self.ninst[q] += 1
        for x in r:
            x.add_reader(tag)
        for x in w:
            x.w = tag
            x.rs = {}
        return tag

    def barrier(self, engines=("pe", "act", "dve", "pool", "sp")):
        tags = [(st, ep, v) for st, (ep, v) in self.cnt.items()]
        for e in engines:
            self._wait(e, tags)

    def wait_all(self, eng):
        self._wait(eng, [(st, ep, v) for st, (ep, v) in self.cnt.items()])


class Prog:
    def __init__(self, nc, cfg):
        self.nc = nc
        self.cfg = cfg
        self.k = KB(nc)
        self.dram = {}
        self._uid = 0

    def sbuf(self, name, shape, dt):
        self._uid += 1
        return self.nc.sbuf_tensor(f"{name}_u{self._uid}", list(shape), dt)

    def dump(self, name, ap, shape, dt, rs):
        if name not in self.cfg.get("dump", ()):
            return
        o = self.dout("dbg_" + name, shape, dt)
        self.k.dma("sp", "dbg", o, ap, r=rs)
        self.k.barrier()

    def din(self, name, shape, dt=F32):
        self.dram[name] = self.nc.dram_tensor(name, list(shape), dt, kind="ExternalInput").ap()
        return self.dram[name]

    def dout(self, name, shape, dt=F32):
        self.dram[name] = self.nc.dram_tensor(name, list(shape), dt, kind="ExternalOutput").ap()
        return self.dram[name]

    def build(self):
        nc, k, cfg = self.nc, self.k, self.cfg
        layers = cfg.get("layers", list(range(DEPTH)))
        d = self.dram
        self.din("x", [S, D])
        self.din("cT", [128, NKC])
        self.din("ada_w", [DEPTH, D, 6 * D])
        self.din("ada_bT", [DEPTH, 128, 48])
        self.din("gtmT", [DEPTH, 128, NKC])
        self.din("gcmT", [DEPTH, 128, NKC])
        self.din("gfinT", [128, NKC])
        self.din("wr", [DEPTH, 128, NKC, 36])
        self.din("br", [DEPTH, 1, 36])
        self.din("moe_w_gate", [DEPTH, 4, 8, D, 256])
        self.din("moe_w_up", [DEPTH, 4, 8, D, 256])
        self.din("moe_w_down", [DEPTH, 4, 8, 256, D])
        self.din("rw_muT", [2, 128, 6, NKC])
        self.din("rw_w_rkv", [2, 3, D, D])
        self.din("rw_w1cat", [2, D, 128])
        self.din("rw_a1cat", [2, D, 128])
        self.din("rw_g1", [2, 2, D, 128])
        self.din("rw_w2cat", [2, 128, D])
        self.din("rw_a2cat", [2, 128, D])
        self.din("rw_g2", [2, 2, 128, D])
        self.din("rw_w0", [2, 2, D])
        self.din("rw_a0", [2, 2, D])
        for n_ in ("rw_k_k", "rw_k_a", "rw_r_k", "rw_ln_w", "rw_ln_b"):
            self.din(n_, [2, D])
        self.din("rw_w_o", [2, D, D])
        self.din("c_tri", [6, 128, 128])
        self.din("c_csel", [128, 2])
        self.din("c_mask", [6, 64, 64])
        itn = lambda n_, shp, dt=F32: nc.dram_tensor(n_, list(shp), dt, kind="Internal").ap()
        self.scr = {"RAW": itn("scr_raw", [2, S, D]), "RAWV": itn("scr_rawv", [S, D], BF16),
                    "FM": itn("scr_fm", [2, 4, NTT, 64, 16, 128], BF16),
                    "TM": itn("scr_tm", [2, 2, S, D], BF16), "GG": itn("scr_gg", [2, S, D]), "RK": itn("scr_rk", [2, S, 16])}
        self.r_scr = {"RAW": [[k.res() for _ in range(NTT)] for _ in range(3)],
                      "FM": [[[k.res() for _ in range(NTT)] for _ in range(4)] for _ in range(2)],
                      "TM": [[[k.res() for _ in range(NTT)] for _ in range(2)] for _ in range(2)],
                      "GG": [[k.res() for _ in range(NTT)] for _ in range(2)],
                      "RK": [[k.res() for _ in range(NTT)] for _ in range(2)]}
        self.din("at_w_qkv", [2, D, 9216])
        self.din("at_w_o", [2, D, D])
        self.din("c_abias", [24, 128, 384])
        self.din("c_ident", [128, 128])
        self.din("c_sel", [32, 32 * 128])
        self.dout("out", [S, D])

        self.xT = k.sb("xT", [128, NKC, S], F32)
        self.hT = k.sb("hT", [128, NKC, S], BF16)
        self.r_xT = [[k.res(f"xT{kc}_{tb}") for tb in range(NTB)] for kc in range(NKC)]
        self.r_hT = [[k.res(f"hT{kc}_{tb}") for tb in range(NTB)] for kc in range(NKC)]
        self.ident = k.sb("ident", [128, 128], F32)
        self.ones = k.sb("ones", [128, 128], F32)
        self.epsT = k.sb("epsT", [128, 1], F32)
        self.onesb = k.sb("onesb", [128, 128], BF16)
        self.r_const = k.res("const")
        self.modT = k.sb("modT", [128, DEPTH, 48], F32)
        self.mod1T = k.sb("mod1T", [128, DEPTH, 48], F32)
        self.r_mod = k.res("mod")
        self.gT = k.sb("gT", [128, 2 * DEPTH + 1, NKC], F32)
        self.ps = [self.k.es.enter_context(nc.psum_tensor(f"ps{i}", [128, 512], F32)) for i in range(8)]
        self.r_ps = [k.res(f"ps{i}") for i in range(8)]
        for r_ in self.r_ps:
            r_.excl = True

        k.dma("sp", "c0", self.ident[:], d["c_ident"][:, :], w=[self.r_const])
        k.op("dve", lambda e: e.memset(self.ones[:], 1.0), w=[self.r_const])
        k.op("dve", lambda e: e.memset(self.epsT[:], RMS_EPS), w=[self.r_const])
        k.op("dve", lambda e: e.memset(self.onesb[:], 1.0), w=[self.r_const])
        k.dma("sp", "c0", self.gT[:, 0:DEPTH, :], d["gtmT"].rearrange("l p k -> p l k"), w=[self.r_const])
        k.dma("sp", "c0", self.gT[:, DEPTH:2 * DEPTH, :], d["gcmT"].rearrange("l p k -> p l k"), w=[self.r_const])
        k.dma("sp", "c0", self.gT[:, 2 * DEPTH, :], d["gfinT"][:, :], w=[self.r_const])

        self.load_x()
        self.ada_all()
        k.barrier()
        for i in layers:
            if cfg.get("mixers", True):
                self.norm_mod(i, 0)
                if i % 2 == 0:
                    self.rwkv(i)
                else:
                    self.attn(i)
            if cfg.get("moe", True):
                self.moe(i)
        self.final()
        k.barrier()
        k.wait_all("sp")
        k.wait_all("pool")

    def load_x(self):
        nc, k, d = self.nc, self.k, self.dram
        with ExitStack() as es:
            st = [es.enter_context(self.sbuf(f"xst{i}", [128, D], F32)) for i in range(2)]
            r_st = [k.res() for _ in range(2)]
            for tt in range(NTT):
                b = tt % 2
                k.dma("sp", f"xst{b}", st[b][:], d["x"][tt * 128:(tt + 1) * 128, :], w=[r_st[b]])
                for half in range(2):
                    pi = (tt * 2 + half) % 4
                    for j in range(4):
                        kc = half * 4 + j
                        k.op("pe", lambda e, kc=kc, j=j, pi=pi, b=b: e.transpose(
                            self.ps[pi][:, j * 128:(j + 1) * 128], st[b][:, kc * 128:(kc + 1) * 128], self.ident[:]),
                            r=[r_st[b], self.r_const], w=[self.r_ps[pi]])
                    tb = tt // 4
                    ws = [self.r_xT[half * 4 + j][tb] for j in range(4)]
                    eng = "act" if half else "dve"
                    if eng == "dve":
                        k.op("dve", lambda e, half=half, pi=pi, tt=tt: e.tensor_copy(
                            out=self.xT[:, half * 4:half * 4 + 4, tt * 128:(tt + 1) * 128],
                            in_=self.ps[pi][:, :].rearrange("p (j t) -> p j t", j=4)),
                            r=[self.r_ps[pi]], w=ws)
                    else:
                        k.op("act", lambda e, half=half, pi=pi, tt=tt: e.copy(
                            out=self.xT[:, half * 4:half * 4 + 4, tt * 128:(tt + 1) * 128],
                            in_=self.ps[pi][:, :].rearrange("p (j t) -> p j t", j=4)),
                            r=[self.r_ps[pi]], w=ws)
            k.barrier()

    def ada_all(self):
        nc, k, d = self.nc, self.k, self.dram
        NP = 8
        PW = 6 * D // NP
        with ExitStack() as es:
            scT = es.enter_context(self.sbuf("scT", [128, NKC], F32))
            cT = es.enter_context(self.sbuf("cTs", [128, NKC], F32))
            abT = es.enter_context(self.sbuf("abT", [128, DEPTH, 48], F32))
            wst = [es.enter_context(self.sbuf(f"adaw{i}", [128, NKC, PW], F32)) for i in range(2)]
            r_w = [k.res() for _ in range(2)]
            r_sc = k.res()
            r_ab = k.res()
            k.dma("sp", "c1", cT[:], d["cT"][:, :], w=[r_sc])
            k.dma("sp", "c1", abT[:], d["ada_bT"].rearrange("l p j -> p l j"), w=[r_ab])
            k.op("act", lambda e: e.activation(out=scT[:], in_=cT[:], func=AF.Silu), r=[r_sc], w=[r_sc])
            q = 0
            for i in range(DEPTH):
                pi = i % 2
                for pc in range(NP):
                    b = q % 2
                    q += 1
                    k.dma("sp", f"adaw{b}", wst[b][:],
                          d["ada_w"][i, :, pc * PW:(pc + 1) * PW].rearrange("(kc p) n -> p kc n", p=128),
                          w=[r_w[b]])
                    for jj in range(PW // 128):
                        j = pc * (PW // 128) + jj
                        for kc in range(NKC):
                            k.op("pe", lambda e, b=b, jj=jj, kc=kc, j=j, pi=pi: e.matmul(
                                self.ps[pi][:, j:j + 1], wst[b][:, kc, jj * 128:(jj + 1) * 128], scT[:, kc:kc + 1],
                                start=(kc == 0), stop=(kc == NKC - 1)),
                                r=[r_w[b], r_sc], w=[self.r_ps[pi]])
                k.op("dve", lambda e, i=i, pi=pi: e.tensor_tensor(
                    out=self.modT[:, i, :], in0=self.ps[pi][:, 0:48], in1=abT[:, i, :], op=ALU.add),
                    r=[self.r_ps[pi], r_ab], w=[self.r_mod])
                k.op("dve", lambda e, i=i: e.tensor_scalar_add(
                    out=self.mod1T[:, i, :], in0=self.modT[:, i, :], scalar1=1.0),
                    r=[self.r_mod], w=[self.r_mod])
            k.barrier()

    def norm_core(self, es, tb, geff, shift, r_par, sq, r_sq, rstd, r_rstd, psi):
        nc, k = self.nc, self.k
        tsl = slice(tb * 512, (tb + 1) * 512)
        k.op("act", lambda e: e.activation(out=sq[:, :, :], in_=self.xT[:, :, tsl], func=AF.Square),
             r=[self.r_xT[kc][tb] for kc in range(NKC)], w=[r_sq])
        for kc in range(NKC):
            k.op("pe", lambda e, kc=kc: e.matmul(self.ps[psi][:, :], self.onesb[:, :], sq[:, kc, :],
                                                 start=(kc == 0), stop=(kc == NKC - 1)),
                 r=[r_sq, self.r_const], w=[self.r_ps[psi]])
        k.op("act", lambda e: e.activation(out=rstd[:, :], in_=self.ps[psi][:, :], func=AF.Sqrt,
                                           scale=1.0 / D, bias=self.epsT[:, 0:1]),
             r=[self.r_ps[psi], self.r_const], w=[r_rstd])
        k.op("dve", lambda e: e.reciprocal(out=rstd[:, :], in_=rstd[:, :]), r=[r_rstd], w=[r_rstd])

    def norm_mod(self, i, which, router=None):
        nc, k = self.nc, self.k
        gi = i if which == 0 else DEPTH + i
        so = 0 if which == 0 else 24
        with ExitStack() as es:
            geff = es.enter_context(self.sbuf("geff", [128, NKC], F32))
            sq = [es.enter_context(self.sbuf(f"nsq{j}", [128, NKC, 512], BF16)) for j in range(2)]
            rstd = [es.enter_context(self.sbuf(f"nrstd{j}", [128, 512], F32)) for j in range(2)]
            t1 = [es.enter_context(self.sbuf(f"nt1_{j}", [128, 512], F32)) for j in range(2)]
            r_t1 = [k.res() for _ in range(2)]
            r_g = k.res()
            r_sq = [k.res() for _ in range(2)]
            r_rstd = [k.res() for _ in range(2)]
            if router is not None:
                h32 = es.enter_context(self.sbuf("h32", [128, NKC, 512], F32))
                r_h32 = k.res()
            k.op("dve", lambda e: e.tensor_tensor(out=geff[:], in0=self.gT[:, gi, :], in1=self.mod1T[:, i, so + 8:so + 16],
                                                  op=ALU.mult), r=[self.r_const, self.r_mod], w=[r_g])
            ncore = lambda tb: self.norm_core(es, tb, None, None, None, sq[tb % 2], r_sq[tb % 2], rstd[tb % 2], r_rstd[tb % 2],
                                              7 if tb % 2 == 0 else 5)
            ncore(0)
            for tb in range(NTB):
                tsl = slice(tb * 512, (tb + 1) * 512)
                if tb + 1 < NTB:
                    ncore(tb + 1)
                rs_, r_rs = rstd[tb % 2], r_rstd[tb % 2]
                for kc in range(NKC):
                    b = kc % 2
                    k.op("dve", lambda e, kc=kc, b=b: e.tensor_tensor(out=t1[b][:, :], in0=self.xT[:, kc, tsl], in1=rs_[:, :],
                                                                      op=ALU.mult),
                         r=[self.r_xT[kc][tb], r_rs], w=[r_t1[b]])
                    if router is None:
                        k.op("act", lambda e, kc=kc, b=b: e.activation(
                            out=self.hT[:, kc, tsl], in_=t1[b][:, :], func=AF.Identity, scale=geff[:, kc:kc + 1],
                            bias=self.modT[:, i, so + kc:so + kc + 1]),
                            r=[r_t1[b], r_g, self.r_mod], w=[self.r_hT[kc][tb]])
                    else:
                        k.op("act", lambda e, kc=kc, b=b: e.activation(
                            out=h32[:, kc, :], in_=t1[b][:, :], func=AF.Identity, scale=geff[:, kc:kc + 1],
                            bias=self.modT[:, i, so + kc:so + kc + 1]),
                            r=[r_t1[b], r_g, self.r_mod], w=[r_h32])
                        k.op("act", lambda e, kc=kc, b=b: e.activation(
                            out=self.hT[:, kc, tsl], in_=t1[b][:, :], func=AF.Identity, scale=geff[:, kc:kc + 1],
                            bias=self.modT[:, i, so + kc:so + kc + 1]),
                             r=[r_t1[b], r_g, self.r_mod], w=[self.r_hT[kc][tb]])
                if router is not None:
                    router(tb, h32, r_h32)
            k.barrier()

    def moe(self, i):
        nc, k, d = self.nc, self.k, self.dram
        with ExitStack() as es0:
            gT_all = es0.enter_context(self.sbuf("gT_all", [32, S], F32))
            r_gT = [k.res() for _ in range(NTT)]
            with ExitStack() as es:
                wr = es.enter_context(self.sbuf("wr", [128, NKC, 36], F32))
                brs = es.enter_context(self.sbuf("brs", [1, 36], F32))
                r_wr = k.res()
                k.dma("sp", "c2", wr[:], d["wr"][i], w=[r_wr])
                k.dma("sp", "c2", brs[:], d["br"][i], w=[r_wr])
                L = es.enter_context(self.sbuf("rL", [128, 36], F32))
                sm = es.enter_context(self.sbuf("rsm", [128, 64], F32))
                gates = es.enter_context(self.sbuf("rgates", [128, 32], F32))
                r_L, r_sm, r_gates = k.res(), k.res(), k.res()

                def router(tb, h32, r_h32):
                    for t4 in range(4):
                        tt = tb * 4 + t4
                        pi = 6
                        for kc in range(NKC):
                            k.op("pe", lambda e, kc=kc: e.matmul(self.ps[pi][:, 0:36], h32[:, kc, t4 * 128:(t4 + 1) * 128],
                                                                 wr[:, kc, :], start=(kc == 0), stop=False),
                                 r=[r_h32, r_wr], w=[self.r_ps[pi]])
                        k.op("pe", lambda e: e.matmul(self.ps[pi][:, 0:36], self.ones[0:1, :], brs[0:1, :],
                                                      start=False, stop=True),
                             r=[r_wr, self.r_const], w=[self.r_ps[pi]])
                        k.op("dve", lambda e: e.tensor_copy(out=L[:, :], in_=self.ps[pi][:, 0:36]),
                             r=[self.r_ps[pi]], w=[r_L])
                        self.route_math(L, r_L, sm, r_sm, gates, r_gates)
                        k.op("pe", lambda e: e.transpose(self.ps[pi][0:32, 128:256], gates[:, :], self.ident[:]),
                             r=[r_gates, self.r_const], w=[self.r_ps[pi]])
                        k.op("act", lambda e, tt=tt: e.copy(out=gT_all[:, tt * 128:(tt + 1) * 128],
                                                            in_=self.ps[pi][0:32, 128:256]),
                             r=[self.r_ps[pi]], w=[r_gT[tt]])

                self.norm_mod(i, 1, router=router)
                self.dump("modT", self.modT[:], [128, DEPTH, 48], F32, [self.r_mod])
                self.dump("hT", self.hT[:], [128, NKC, S], BF16, [x for y in self.r_hT for x in y])
                self.dump("gT_all", gT_all[:], [32, S], F32, r_gT)
            with ExitStack() as es:
                sel = es.enter_context(self.sbuf("sel", [32, 32 * 128], F32))
                r_sel = k.res()
                k.dma("sp", "c2", sel[:], d["c_sel"][:, :], w=[r_sel])
                NS = 16
                wg = [es.enter_context(self.sbuf(f"wg{b}", [128, 2, NKC, 256], BF16)) for b in range(2)]
                wu = [es.enter_context(self.sbuf(f"wu{b}", [128, 2, NKC, 256], BF16)) for b in range(2)]
                wd = [es.enter_context(self.sbuf(f"wd{b}", [128, 2, 2, D], BF16)) for b in range(2)]
                r_w = [k.res() for _ in range(2)]
                gbc = [es.enter_context(self.sbuf(f"gbc{b}", [128, 2, 512], F32)) for b in range(2)]
                r_gbc = [k.res() for _ in range(2)]
                hid = [es.enter_context(self.sbuf(f"hid{b}", [128, 4, 512], BF16)) for b in range(2)]
                r_hid = [k.res() for _ in range(2)]
                sg = [es.enter_context(self.sbuf(f"sg{b}", [128, 512], F32)) for b in range(2)]
                r_sg = [k.res() for _ in range(2)]
                tm = [es.enter_context(self.sbuf(f"tm{b}", [128, 512], F32)) for b in range(2)]
                r_tm = [k.res() for _ in range(2)]

                def load_w(s):
                    b = s % 2
                    for ee in range(2):
                        eg = 2 * s + ee
                        g_, e_ = eg // 8, eg % 8
                        k.dma("pool", f"moew{b}", wg[b][:, ee, :, :],
                              d["moe_w_gate"][i, g_, e_].rearrange("(kc p) f -> p kc f", p=128), w=[r_w[b]])
                        k.dma("pool", f"moew{b}", wu[b][:, ee, :, :],
                              d["moe_w_up"][i, g_, e_].rearrange("(kc p) f -> p kc f", p=128), w=[r_w[b]])
                        k.dma("pool", f"moew{b}", wd[b][:, ee, :, :],
                              d["moe_w_down"][i, g_, e_].rearrange("(fc p) n -> p fc n", p=128), w=[r_w[b]])

                pending = [None]
                it = [0]

                def down(s, tb, hb):
                    b = s % 2
                    tsl = slice(tb * 512, (tb + 1) * 512)
                    for dc in range(NKC):
                        pi = 4 + dc % 2
                        for u in range(4):
                            ee, fc = u // 2, u % 2
                            k.op("pe", lambda e, u=u, ee=ee, fc=fc, dc=dc, pi=pi: e.matmul(
                                self.ps[pi][:, :], wd[b][:, ee, fc, dc * 128:(dc + 1) * 128], hid[hb][:, u, :],
                                start=(u == 0), stop=(u == 3)),
                                r=[r_w[b], r_hid[hb]], w=[self.r_ps[pi]])
                        k.op("dve", lambda e, dc=dc, pi=pi: e.scalar_tensor_tensor(
                            out=self.xT[:, dc, tsl], in0=self.ps[pi][:, :], scalar=self.modT[:, i, 40 + dc:41 + dc],
                            in1=self.xT[:, dc, tsl], op0=ALU.mult, op1=ALU.add),
                            r=[self.r_ps[pi], self.r_mod], w=[self.r_xT[dc][tb]])

                load_w(0)
                for s in range(NS):
                    b = s % 2
                    for tb in range(NTB):
                        tsl = slice(tb * 512, (tb + 1) * 512)
                        hb = it[0] % 2
                        gb = it[0] % 2
                        it[0] += 1
                        for ee in range(2):
                            eg = 2 * s + ee
                            k.op("pe", lambda e, eg=eg: e.matmul(self.ps[6][:, :], sel[:, eg * 128:(eg + 1) * 128],
                                                                 gT_all[:, tsl], start=True, stop=True),
                                 r=[r_sel] + r_gT[tb * 4:tb * 4 + 4], w=[self.r_ps[6]])
                            k.op("act", lambda e, ee=ee, gb=gb: e.copy(out=gbc[gb][:, ee, :], in_=self.ps[6][:, :]),
                                 r=[self.r_ps[6]], w=[r_gbc[gb]])
                        for u in range(4):
                            ee, fc = u // 2, u % 2
                            pg, pu = (0, 1) if u % 2 == 0 else (2, 3)
                            for kc in range(NKC):
                                k.op("pe", lambda e, kc=kc, ee=ee, fc=fc, pg=pg: e.matmul(
                                    self.ps[pg][:, :], wg[b][:, ee, kc, fc * 128:(fc + 1) * 128], self.hT[:, kc, tsl],
                                    start=(kc == 0), stop=(kc == NKC - 1)),
                                    r=[r_w[b], self.r_hT[kc][tb]], w=[self.r_ps[pg]])
                            for kc in range(NKC):
                                k.op("pe", lambda e, kc=kc, ee=ee, fc=fc, pu=pu: e.matmul(
                                    self.ps[pu][:, :], wu[b][:, ee, kc, fc * 128:(fc + 1) * 128], self.hT[:, kc, tsl],
                                    start=(kc == 0), stop=(kc == NKC - 1)),
                                    r=[r_w[b], self.r_hT[kc][tb]], w=[self.r_ps[pu]])
                            sb_ = u % 2
                            k.op("act", lambda e, pg=pg, sb_=sb_: e.activation(out=sg[sb_][:, :], in_=self.ps[pg][:, :],
                                                                               func=AF.Silu),
                                 r=[self.r_ps[pg]], w=[r_sg[sb_]])
                            k.op("dve", lambda e, pu=pu, sb_=sb_: e.tensor_tensor(out=tm[sb_][:, :], in0=sg[sb_][:, :],
                                                                                  in1=self.ps[pu][:, :], op=ALU.mult),
                                 r=[r_sg[sb_], self.r_ps[pu]], w=[r_tm[sb_]])
                            k.op("dve", lambda e, u=u, ee=ee, sb_=sb_, hb=hb, gb=gb: e.tensor_tensor(
                                out=hid[hb][:, u, :], in0=tm[sb_][:, :], in1=gbc[gb][:, ee, :], op=ALU.mult),
                                r=[r_tm[sb_], r_gbc[gb]], w=[r_hid[hb]])
                        if pending[0] is not None:
                            down(*pending[0])
                        pending[0] = (s, tb, hb)
                        if tb == 0 and s + 1 < NS:
                            load_w(s + 1)
                down(*pending[0])
                k.barrier()

    def route_math(self, L, r_L, sm, r_sm, gates, r_gates):
        k = self.k
        GMAX, NGMAX, GSUM, PG, M1, M2, DD, EE, W1, W2 = range(10)
        GOH = slice(10, 14)
        GE = slice(14, 18)
        ESEL = slice(18, 26)
        OH1 = slice(26, 34)
        MSK = slice(34, 42)
        OH2 = slice(42, 50)
        INN = slice(50, 58)
        c = lambda j: slice(j, j + 1)

        def dv(fn, r=(), w=()):
            k.op("dve", fn, r=r, w=w)

        rs = [r_L, r_sm]
        dv(lambda e: e.reduce_max(out=sm[:, c(GMAX)], in_=L[:, 0:4], axis=AX.X), r=[r_L], w=[r_sm])
        dv(lambda e: e.tensor_scalar(out=sm[:, GOH], in0=L[:, 0:4], scalar1=sm[:, c(GMAX)], scalar2=None,
                                     op0=ALU.is_equal), r=rs, w=[r_sm])
        dv(lambda e: e.tensor_scalar_mul(out=sm[:, c(NGMAX)], in0=sm[:, c(GMAX)], scalar1=-1.0), r=[r_sm], w=[r_sm])
        k.op("act", lambda e: e.activation(out=sm[:, GE], in_=L[:, 0:4], func=AF.Exp, bias=sm[:, c(NGMAX)],
                                           scale=1.0, accum_out=sm[:, c(GSUM)]), r=rs, w=[r_sm])
        dv(lambda e: e.reciprocal(out=sm[:, c(PG)], in_=sm[:, c(GSUM)]), r=[r_sm], w=[r_sm])
        dv(lambda e: e.tensor_scalar_mul(out=sm[:, ESEL], in0=L[:, 4:12], scalar1=sm[:, c(10)]), r=rs, w=[r_sm])
        for g in range(1, 4):
            dv(lambda e, g=g: e.scalar_tensor_tensor(out=sm[:, ESEL], in0=L[:, 4 + 8 * g:12 + 8 * g],
                                                     scalar=sm[:, c(10 + g)], in1=sm[:, ESEL],
                                                     op0=ALU.mult, op1=ALU.add), r=rs, w=[r_sm])
        dv(lambda e: e.reduce_max(out=sm[:, c(M1)], in_=sm[:, ESEL], axis=AX.X), r=[r_sm], w=[r_sm])
        dv(lambda e: e.tensor_scalar(out=sm[:, OH1], in0=sm[:, ESEL], scalar1=sm[:, c(M1)], scalar2=None,
                                     op0=ALU.is_equal), r=[r_sm], w=[r_sm])
        dv(lambda e: e.scalar_tensor_tensor(out=sm[:, MSK], in0=sm[:, OH1], scalar=-1e30, in1=sm[:, ESEL],
                                            op0=ALU.mult, op1=ALU.add), r=[r_sm], w=[r_sm])
        dv(lambda e: e.reduce_max(out=sm[:, c(M2)], in_=sm[:, MSK], axis=AX.X), r=[r_sm], w=[r_sm])
        dv(lambda e: e.tensor_scalar(out=sm[:, OH2], in0=sm[:, MSK], scalar1=sm[:, c(M2)], scalar2=None,
                                     op0=ALU.is_equal), r=[r_sm], w=[r_sm])
        dv(lambda e: e.tensor_tensor(out=sm[:, c(DD)], in0=sm[:, c(M2)], in1=sm[:, c(M1)], op=ALU.subtract),
           r=[r_sm], w=[r_sm])
        k.op("act", lambda e: e.activation(out=sm[:, c(EE)], in_=sm[:, c(DD)], func=AF.Exp), r=[r_sm], w=[r_sm])
        dv(lambda e: e.tensor_scalar_add(out=sm[:, c(W1)], in0=sm[:, c(EE)], scalar1=1.0), r=[r_sm], w=[r_sm])
        dv(lambda e: e.reciprocal(out=sm[:, c(W1)], in_=sm[:, c(W1)]), r=[r_sm], w=[r_sm])
        dv(lambda e: e.tensor_tensor(out=sm[:, c(W2)], in0=sm[:, c(EE)], in1=sm[:, c(W1)], op=ALU.mult),
           r=[r_sm], w=[r_sm])
        dv(lambda e: e.tensor_tensor(out=sm[:, c(W1)], in0=sm[:, c(W1)], in1=sm[:, c(PG)], op=ALU.mult),
           r=[r_sm], w=[r_sm])
        dv(lambda e: e.tensor_tensor(out=sm[:, c(W2)], in0=sm[:, c(W2)], in1=sm[:, c(PG)], op=ALU.mult),
           r=[r_sm], w=[r_sm])
        dv(lambda e: e.tensor_scalar_mul(out=sm[:, INN], in0=sm[:, OH1], scalar1=sm[:, c(W1)]), r=[r_sm], w=[r_sm])
        dv(lambda e: e.scalar_tensor_tensor(out=sm[:, INN], in0=sm[:, OH2], scalar=sm[:, c(W2)], in1=sm[:, INN],
                                            op0=ALU.mult, op1=ALU.add), r=[r_sm], w=[r_sm])
        for g in range(4):
            dv(lambda e, g=g: e.tensor_scalar_mul(out=gates[:, 8 * g:8 * g + 8], in0=sm[:, INN],
                                                  scalar1=sm[:, c(10 + g)]), r=[r_sm], w=[r_gates])

    def rwkv(self, i):
        nc, k, d = self.nc, self.k, self.dram
        j = i // 2
        with ExitStack() as es:
            PCt = es.enter_context(self.sbuf("PCt", [64, 2, NTT, 16, 2], F32))
            r_PC = k.res()
            self.rwkv_A(i, j, PCt, r_PC)
            self.rwkv_B(i, j, PCt, r_PC)

    def rwkv_A(self, i, j, PCt, r_PC):
        nc, k, d = self.nc, self.k, self.dram
        all_hT = [x for y in self.r_hT for x in y]
        sc = self.scr
        with ExitStack() as esA:
            sb = lambda es, n, s, dt=F32: es.enter_context(self.sbuf(n, s, dt))
            h1w = sb(esA, "h1w", [128, S], BF16)
            h1a = sb(esA, "h1a", [128, S], BF16)
            r_h1w, r_h1a = k.res(), k.res()
            muT = sb(esA, "muT", [128, 6, NKC])
            c1 = sb(esA, "c1", [128, 6, NKC])
            c2 = sb(esA, "c2", [128, 6, NKC])
            r_c = k.res()
            k.dma("sp", "rwc", muT[:], d["rw_muT"][j], w=[r_c])
            k.op("dve", lambda e: e.tensor_scalar(out=c1[:], in0=muT[:], scalar1=-1.0, scalar2=1.0, op0=ALU.mult, op1=ALU.add),
                 r=[r_c], w=[r_c])
            k.op("dve", lambda e: e.tensor_scalar_mul(out=c2[:], in0=muT[:], scalar1=0.5), r=[r_c], w=[r_c])

            def scale_w(stg, r_stg, W1, W2, r_W, jm, ncol, col0=0):
                for kc in range(NKC):
                    k.op("act", lambda e, kc=kc: e.mul(out=W1[:, kc, col0:col0 + ncol], in_=stg[:, kc, 0:ncol],
                                                       mul=c1[:, jm, kc:kc + 1]), r=[r_stg, r_c], w=[r_W])
                    k.op("dve", lambda e, kc=kc: e.tensor_scalar_mul(out=W2[:, kc, col0:col0 + ncol], in0=stg[:, kc, 0:ncol],
                                                                     scalar1=c2[:, jm, kc:kc + 1]), r=[r_stg, r_c], w=[r_W])

            with ExitStack() as es12:
                hsT = sb(es12, "hsT", [128, NKC, S], BF16)
                r_hs = k.res()
                for kc in range(NKC):
                    k.op("dve", lambda e, kc=kc: e.tensor_tensor(out=hsT[:, kc, 1:S - 1], in0=self.hT[:, kc, 0:S - 2],
                                                                 in1=self.hT[:, kc, 2:S], op=ALU.add), r=all_hT, w=[r_hs])
                    k.op("act", lambda e, kc=kc: e.copy(out=hsT[:, kc, 0:1], in_=self.hT[:, kc, 1:2]), r=all_hT, w=[r_hs])
                    k.op("act", lambda e, kc=kc: e.copy(out=hsT[:, kc, S - 1:S], in_=self.hT[:, kc, S - 2:S - 1]), r=all_hT, w=[r_hs])
                with ExitStack() as es1:
                    h1g = [sb(es1, f"h1g{dd}", [128, S], BF16) for dd in range(2)]
                    r_h1g = [k.res() for _ in range(2)]
                    stg = [sb(es1, f"lstg{b}", [128, NKC, 128]) for b in range(2)]
                    r_stg = [k.res() for _ in range(2)]
                    W1 = [sb(es1, f"lW1_{b}", [128, NKC, 128], BF16) for b in range(2)]
                    W2 = [sb(es1, f"lW2_{b}", [128, NKC, 128], BF16) for b in range(2)]
                    r_W = [k.res() for _ in range(2)]
                    groups = [(d["rw_w1cat"][j], 3, AF.Tanh, h1w, r_h1w), (d["rw_a1cat"][j], 4, AF.Copy, h1a, r_h1a),
                              (d["rw_g1"][j, 0], 5, AF.Sigmoid, h1g[0], r_h1g[0]), (d["rw_g1"][j, 1], 5, AF.Sigmoid, h1g[1], r_h1g[1])]
                    for gi, (src, jm, fn, dst, r_dst) in enumerate(groups):
                        b = gi % 2
                        k.dma("sp", f"lstg{b}", stg[b][:], src.rearrange("(kc p) n -> p kc n", p=128), w=[r_stg[b]])
                        scale_w(stg[b], r_stg[b], W1[b], W2[b], r_W[b], jm, 128)
                        for tb in range(NTB):
                            tsl = slice(tb * 512, (tb + 1) * 512)
                            pi = tb % 2
                            for kc in range(NKC):
                                k.op("pe", lambda e, kc=kc: e.matmul(self.ps[pi][:, :], W1[b][:, kc, :], self.hT[:, kc, tsl],
                                                                     start=(kc == 0), stop=False),
                                     r=[r_W[b], self.r_hT[kc][tb]], w=[self.r_ps[pi]])
                            for kc in range(NKC):
                                k.op("pe", lambda e, kc=kc: e.matmul(self.ps[pi][:, :], W2[b][:, kc, :], hsT[:, kc, tsl],
                                                                     start=False, stop=(kc == NKC - 1)),
                                     r=[r_W[b], r_hs], w=[self.r_ps[pi]])
                            k.op("act", lambda e: e.activation(out=dst[:, tsl], in_=self.ps[pi][:, :], func=fn),
                                 r=[self.r_ps[pi]], w=[r_dst])
                    g2 = [sb(es1, f"g2_{dd}", [128, D], BF16) for dd in range(2)]
                    r_g2 = k.res()
                    for dd in range(2):
                        k.dma("pool", "g2", g2[dd][:], d["rw_g2"][j, dd], w=[r_g2])
                    gst = [sb(es1, f"gst{b}", [128, D]) for b in range(2)]
                    r_gst = [k.res() for _ in range(2)]
                    n = 0
                    for dd in range(2):
                        for tt in range(NTT):
                            b = n % 2
                            n += 1
                            for half in range(2):
                                pi = 2 + half
                                k.op("pe", lambda e, half=half, pi=pi: e.matmul(
                                    self.ps[pi][:, :], h1g[dd][:, tt * 128:(tt + 1) * 128], g2[dd][:, half * 512:(half + 1) * 512],
                                    start=True, stop=True), r=[r_h1g[dd], r_g2], w=[self.r_ps[pi]])
                                if half == 0:
                                    k.op("act", lambda e, pi=pi: e.copy(out=gst[b][:, 0:512], in_=self.ps[pi][:, :]),
                                         r=[self.r_ps[pi]], w=[r_gst[b]])
                                else:
                                    k.op("dve", lambda e, pi=pi: e.tensor_copy(out=gst[b][:, 512:1024], in_=self.ps[pi][:, :]),
                                         r=[self.r_ps[pi]], w=[r_gst[b]])
                            k.dma("sp", f"gst{b}", sc["GG"][dd, tt * 128:(tt + 1) * 128, :], gst[b][:], r=[r_gst[b]],
                                  w=[self.r_scr["GG"][dd][tt]])
                    k.barrier()
                with ExitStack() as es2:
                    stg = [sb(es2, f"pstg{b}", [128, NKC, 256]) for b in range(2)]
                    r_stg = [k.res() for _ in range(2)]
                    W1 = sb(es2, "pW1", [128, NKC, D], BF16)
                    W2 = sb(es2, "pW2", [128, NKC, D], BF16)
                    r_W = k.res()
                    rst = [sb(es2, f"rst{b}", [128, D]) for b in range(2)]
                    rstb = [rst[b][:, :].bitcast(BF16)[:, 0:D] for b in range(2)]
                    r_rst = [k.res() for _ in range(2)]
                    n = 0
                    for pj in range(3):
                        for qq in range(4):
                            sb_ = qq % 2
                            k.dma("sp", f"pstg{sb_}", stg[sb_][:],
                                  d["rw_w_rkv"][j, pj, :, qq * 256:(qq + 1) * 256].rearrange("(kc p) n -> p kc n", p=128),
                                  w=[r_stg[sb_]])
                            scale_w(stg[sb_], r_stg[sb_], W1, W2, r_W, pj, 256, col0=qq * 256)
                        for tt in range(NTT):
                            b = n % 2
                            n += 1
                            tb = tt // 4
                            tsl = slice(tt * 128, (tt + 1) * 128)
                            for half in range(2):
                                pi = half
                                for kc in range(NKC):
                                    k.op("pe", lambda e, kc=kc, half=half, pi=pi: e.matmul(
                                        self.ps[pi][:, :], self.hT[:, kc, tsl], W1[:, kc, half * 512:(half + 1) * 512],
                                        start=(kc == 0), stop=False), r=[r_W, self.r_hT[kc][tb]], w=[self.r_ps[pi]])
                                for kc in range(NKC):
                                    k.op("pe", lambda e, kc=kc, half=half, pi=pi: e.matmul(
                                        self.ps[pi][:, :], hsT[:, kc, tsl], W2[:, kc, half * 512:(half + 1) * 512],
                                        start=False, stop=(kc == NKC - 1)), r=[r_W, r_hs], w=[self.r_ps[pi]])
                                dst_t = rst[b] if pj < 2 else rstb[b]
                                if half == 0:
                                    k.op("act", lambda e, pi=pi: e.copy(out=dst_t[:, 0:512], in_=self.ps[pi][:, :]),
                                         r=[self.r_ps[pi]], w=[r_rst[b]])
                                else:
                                    k.op("dve", lambda e, pi=pi: e.tensor_copy(out=dst_t[:, 512:1024], in_=self.ps[pi][:, :]),
                                         r=[self.r_ps[pi]], w=[r_rst[b]])
                            if pj < 2:
                                k.dma("sp", f"rst{b}", sc["RAW"][pj, tt * 128:(tt + 1) * 128, :], rst[b][:], r=[r_rst[b]],
                                      w=[self.r_scr["RAW"][pj][tt]])
                            else:
                                k.dma("sp", f"rst{b}", sc["RAWV"][tt * 128:(tt + 1) * 128, :], rstb[b], r=[r_rst[b]],
                                      w=[self.r_scr["RAW"][pj][tt]])
                    k.barrier()
            with ExitStack() as es3:
                w2c = sb(es3, "w2c", [128, D], BF16)
                a2c = sb(es3, "a2c", [128, D], BF16)
                r_l2 = k.res()
                k.dma("pool", "l2w", w2c[:], d["rw_w2cat"][j], w=[r_l2])
                k.dma("pool", "l2w", a2c[:], d["rw_a2cat"][j], w=[r_l2])
                KKb = sb(es3, "KKb", [128, D]); KAb = sb(es3, "KAb", [128, D]); RKb = sb(es3, "RKb", [128, D])
                r_par = k.res()
                k.dma("sp", "rwp", KKb[:], d["rw_k_k"][j:j + 1, :].partition_broadcast(128), w=[r_par])
                k.dma("sp", "rwp", KAb[:], d["rw_k_a"][j:j + 1, :].partition_broadcast(128), w=[r_par])
                k.dma("sp", "rwp", RKb[:], d["rw_r_k"][j:j + 1, :].partition_broadcast(128), w=[r_par])
                b32 = sb(es3, "b32", [1, 4, D])
                r_b = k.res()
                k.dma("sp", "rwp2", b32[0:1, 0:2, :], d["rw_w0"][j:j + 1, :, :], w=[r_b])
                k.dma("sp", "rwp2", b32[0:1, 2:4, :], d["rw_a0"][j:j + 1, :, :], w=[r_b])
                tri = sb(es3, "tri", [128, 6, 128])
                csel = sb(es3, "csel", [128, 2])
                k.dma("sp", "rwp", tri[:], d["c_tri"].rearrange("q s t -> s q t"), w=[r_par])
                k.dma("sp", "rwp", csel[:], d["c_csel"][:, :], w=[r_par])
                hsc = [self.hT[:, kc, :].bitcast(F32) for kc in range(NKC)]
                mk2 = lambda n_, extra: [sb(es3, f"{n_}{q}", [128, D]) if extra[q] is None else extra[q] for q in range(2)]
                Rr_s = mk2("Rr", [None, hsc[0]]); Rk_s = mk2("Rk", [None, hsc[1]]); kk_s = mk2("kk", [None, hsc[2]])
                RRK_s = mk2("RRK", [None, hsc[3]]); T0_s = mk2("T0", [None, hsc[4]])
                SIG_s = mk2("SIG", [None, hsc[5]]); A_s = mk2("A_", [None, hsc[6]]); KD_s = mk2("KD", [None, hsc[7]])
                E1_s = mk2("E1", [None, None]); E2_s = mk2("E2", [None, None])
                O = [sb(es3, f"O{b}", [128, D], BF16) for b in range(2)]
                FMst = [sb(es3, f"FMst{b}", [64, 8, 128], BF16) for b in range(2)]
                identb = sb(es3, "identb3", [128, 128], BF16)
                r_idb = k.res()
                k.op("act", lambda e: e.copy(out=identb[:], in_=self.ident[:]), r=[self.r_const], w=[r_idb])
                psb = {4: self.ps[4][0:64, :].bitcast(BF16), 5: self.ps[5][0:64, :].bitcast(BF16)}
                sm = sb(es3, "a3sm", [128, 64])
                rkt_s = [sb(es3, f"rkt{q}", [128, 16]) for q in range(2)]
                r_rkt_s = [k.res(), k.res()]
                r2 = lambda: [k.res(), k.res()]
                r_Rr_s, r_Rk_s, r_kk_s, r_RRK_s, r_T0_s = r2(), r2(), r2(), r2(), r2()
                r_SIG_s, r_A_s, r_KD_s, r_E1_s, r_E2_s = r2(), r2(), r2(), r2(), r2()
                r_sm = k.res()
                r_O = [k.res() for _ in range(2)]
                r_FM = [k.res() for _ in range(2)]
                on = [0]
                v3 = lambda t: t[:, :].rearrange("p (h n) -> p h n", h=16)

                fmn = [0]

                def emit_fm(src, r_src, dd, q, tt):
                    on[0] += 1
                    for h8 in range(2):
                        fb = fmn[0] % 2
                        fmn[0] += 1
                        for h4 in range(2):
                            pi = 4 + h4 % 2
                            for hh in range(4):
                                h = h8 * 8 + h4 * 4 + hh
                                k.op("pe", lambda e, h=h, hh=hh, pi=pi: e.transpose(
                                    psb[pi][:, hh * 128:(hh + 1) * 128], src[:, h * 64:(h + 1) * 64], identb[:]),
                                    r=[r_src, r_idb], w=[self.r_ps[pi]])
                            if h4 == 0:
                                k.op("act", lambda e, h4=h4, pi=pi: e.copy(
                                    out=FMst[fb][:, h4 * 4:h4 * 4 + 4, :], in_=psb[pi][:, 0:512].rearrange("p (a t) -> p a t", a=4)),
                                    r=[self.r_ps[pi]], w=[r_FM[fb]])
                            else:
                                k.op("dve", lambda e, h4=h4, pi=pi: e.tensor_copy(
                                    out=FMst[fb][:, h4 * 4:h4 * 4 + 4, :], in_=psb[pi][:, 0:512].rearrange("p (a t) -> p a t", a=4)),
                                    r=[self.r_ps[pi]], w=[r_FM[fb]])
                        k.dma("sp", f"FMst{fb}", sc["FM"][dd, q, tt, :, h8 * 8:(h8 + 1) * 8, :], FMst[fb][:], r=[r_FM[fb]],
                              w=[self.r_scr["FM"][dd][q][tt]])

                def a3_load(tt):
                    q = tt % 2
                    rws = slice(tt * 128, (tt + 1) * 128)
                    k.dma("pool", f"a3r{q}", Rr_s[q][:], sc["RAW"][0, rws, :], r=[self.r_scr["RAW"][0][tt]], w=[r_Rr_s[q]])
                    k.dma("pool", f"a3k{q}", Rk_s[q][:], sc["RAW"][1, rws, :], r=[self.r_scr["RAW"][1][tt]], w=[r_Rk_s[q]])

                a3_load(0)
                for tt in range(NTT):
                    rows = slice(tt * 128, (tt + 1) * 128)
                    if tt + 1 < NTT:
                        a3_load(tt + 1)
                    q_ = tt % 2
                    Rr, Rk, kk, RRK = Rr_s[q_], Rk_s[q_], kk_s[q_], RRK_s[q_]
                    r_Rr, r_Rk, r_kk, r_RRK = r_Rr_s[q_], r_Rk_s[q_], r_kk_s[q_], r_RRK_s[q_]
                    T0, r_T0 = T0_s[0], r_T0_s[0]
                    k.op("dve", lambda e: e.tensor_tensor(out=kk[:], in0=Rk[:], in1=KKb[:], op=ALU.mult), r=[r_Rk, r_par], w=[r_kk])
                    k.op("dve", lambda e: e.tensor_tensor(out=T0[:], in0=kk[:], in1=kk[:], op=ALU.mult), r=[r_kk], w=[r_T0])
                    k.op("dve", lambda e: e.reduce_sum(out=sm[:, 0:16], in_=v3(T0), axis=AX.X), r=[r_T0], w=[r_sm])
                    k.op("act", lambda e: e.activation(out=sm[:, 0:16], in_=sm[:, 0:16], func=AF.Sqrt), r=[r_sm], w=[r_sm])
                    k.op("dve", lambda e: e.tensor_scalar_max(out=sm[:, 0:16], in0=sm[:, 0:16], scalar1=1e-12), r=[r_sm], w=[r_sm])
                    k.op("dve", lambda e: e.reciprocal(out=sm[:, 16:32], in_=sm[:, 0:16]), r=[r_sm], w=[r_sm])
                    k.op("dve", lambda e: e.tensor_tensor(out=v3(kk), in0=v3(kk),
                                                          in1=sm[:, 16:32].unsqueeze(2).to_broadcast([128, 16, 64]), op=ALU.mult),
                         r=[r_kk, r_sm], w=[r_kk])
                    k.op("dve", lambda e: e.tensor_tensor(out=RRK[:], in0=Rr[:], in1=RKb[:], op=ALU.mult), r=[r_Rr, r_par], w=[r_RRK])
                    for dd in range(2):
                        T0, SIG, A_, KD, E1, E2 = T0_s[dd], SIG_s[dd], A_s[dd], KD_s[dd], E1_s[dd], E2_s[dd]
                        r_T0, r_SIG, r_A, r_KD, r_E1, r_E2 = r_T0_s[dd], r_SIG_s[dd], r_A_s[dd], r_KD_s[dd], r_E1_s[dd], r_E2_s[dd]
                        for (h1, r_h1, w2t, bi, dst, r_dst) in ((h1w, r_h1w, w2c, dd, SIG, r_SIG), (h1a, r_h1a, a2c, 2 + dd, A_, r_A)):
                            for half in range(2):
                                pi = half
                                csl = slice(half * 512, (half + 1) * 512)
                                k.op("pe", lambda e: e.matmul(self.ps[pi][:, :], h1[dd * 64:(dd + 1) * 64, rows],
                                                              w2t[dd * 64:(dd + 1) * 64, csl], start=True, stop=False),
                                     r=[r_h1, r_l2], w=[self.r_ps[pi]])
                                k.op("pe", lambda e: e.matmul(self.ps[pi][:, :], self.ones[0:1, :], b32[0:1, bi, csl],
                                                              start=False, stop=True),
                                     r=[r_b, self.r_const], w=[self.r_ps[pi]])
                                k.op("act", lambda e: e.activation(out=dst[:, csl], in_=self.ps[pi][:, :], func=AF.Sigmoid),
                                     r=[self.r_ps[pi]], w=[r_dst])
                        k.op("dve", lambda e: e.scalar_tensor_tensor(out=KD[:], in0=A_[:], scalar=-1.0, in1=KAb[:],
                                                                     op0=ALU.add, op1=ALU.mult), r=[r_A, r_par], w=[r_KD])
                        k.op("dve", lambda e: e.scalar_tensor_tensor(out=KD[:], in0=KD[:], scalar=1.0, in1=Rk[:],
                                                                     op0=ALU.add, op1=ALU.mult), r=[r_KD, r_Rk], w=[r_KD])
                        k.op("dve", lambda e: e.tensor_tensor(out=A_[:], in0=A_[:], in1=kk[:], op=ALU.mult), r=[r_A, r_kk], w=[r_A])
                        k.op("dve", lambda e: e.tensor_tensor(out=T0[:], in0=RRK[:], in1=KD[:], op=ALU.mult), r=[r_RRK, r_KD], w=[r_T0])
                        rkt, r_rkt = rkt_s[dd], r_rkt_s[dd]
                        k.op("dve", lambda e: e.reduce_sum(out=rkt[:, :], in_=v3(T0), axis=AX.X), r=[r_T0], w=[r_rkt])
                        k.dma("sp", f"rkt{dd}", sc["RK"][dd, rows, :], rkt[:], r=[r_rkt], w=[self.r_scr["RK"][dd][tt]])
                        for h in range(16):
                            k.op("pe", lambda e, h=h: e.matmul(self.ps[6][0:64, h * 2:h * 2 + 2], SIG[:, h * 64:(h + 1) * 64], csel[:, :],
                                                               start=True, stop=True), r=[r_SIG, r_par], w=[self.r_ps[6]])
                        k.op("act", lambda e: e.activation(out=PCt[:, dd, tt, :, :], in_=self.ps[6][0:64, 0:32].rearrange("p (h c) -> p h c", c=2),
                                                           func=AF.Exp), r=[self.r_ps[6]], w=[r_PC])
                        def cums(kind, outs):
                            for half in range(2):
                                pi = 2 + half
                                csl = slice(half * 512, (half + 1) * 512)
                                k.op("pe", lambda e: e.matmul(self.ps[pi][:, :], tri[:, dd * 3 + kind, :], SIG[:, csl],
                                                              start=True, stop=True), r=[r_SIG, r_par], w=[self.r_ps[pi]])
                                for (dst, r_dst, scl) in outs:
                                    k.op("act", lambda e, dst=dst, scl=scl: e.activation(out=dst[:, csl], in_=self.ps[pi][:, :],
                                                                                         func=AF.Exp, scale=scl),
                                         r=[self.r_ps[pi]], w=[r_dst])
                        cums(0, [(E1, r_E1, 1.0), (E2, r_E2, -1.0)])
                        ob = on[0] % 2
                        k.op("dve", lambda e: e.tensor_tensor(out=O[ob][:], in0=Rr[:], in1=E1[:], op=ALU.mult), r=[r_Rr, r_E1], w=[r_O[ob]])
                        emit_fm(O[ob], r_O[ob], dd, 0, tt)
                        ob = on[0] % 2
                        k.op("dve", lambda e: e.tensor_tensor(out=O[ob][:], in0=A_[:], in1=E2[:], op=ALU.mult), r=[r_A, r_E2], w=[r_O[ob]])
                        emit_fm(O[ob], r_O[ob], dd, 2, tt)
                        ob = on[0] % 2
                        k.op("dve", lambda e: e.tensor_tensor(out=O[ob][:], in0=KD[:], in1=E2[:], op=ALU.mult), r=[r_KD, r_E2], w=[r_O[ob]])
                        emit_fm(O[ob], r_O[ob], dd, 3, tt)
                        cums(1, [(E1, r_E1, 1.0)])
                        ob = on[0] % 2
                        k.op("dve", lambda e: e.scalar_tensor_tensor(out=O[ob][:], in0=kk[:], scalar=-1.0, in1=E1[:],
                                                                     op0=ALU.mult, op1=ALU.mult), r=[r_kk, r_E1], w=[r_O[ob]])
                        emit_fm(O[ob], r_O[ob], dd, 1, tt)
                        cums(2, [(E2, r_E2, 1.0)])
                        for q, (src, r_src) in enumerate(((A_, r_A), (KD, r_KD))):
                            ob = on[0] % 2
                            on[0] += 1
                            k.op("dve" if q == 0 else "pool", lambda e, src=src: e.tensor_tensor(out=O[ob][:], in0=src[:], in1=E2[:], op=ALU.mult),
                                 r=[r_src, r_E2], w=[r_O[ob]])
                            k.dma("sp", f"Otm{ob}", sc["TM"][dd, q, rows, :], O[ob][:], r=[r_O[ob]], w=[self.r_scr["TM"][dd][q][tt]])
                k.barrier()

    def rwkv_B(self, i, j, PCt, r_PC):
        nc, k, d = self.nc, self.k, self.dram
        sc = self.scr
        oT = self.hT
        r_oT = self.r_hT
        NH = 8
        with ExitStack() as es:
            sb = lambda n, s_, dt=F32: es.enter_context(self.sbuf(n, s_, dt))
            for kc in range(NKC):
                k.op("pool", lambda e, kc=kc: e.memset(oT[:, kc, :], 0.0), w=r_oT[kc])
            msk = sb("msk", [64, 6, 64])
            LNW = sb("LNW", [64, D]); LNB = sb("LNB", [64, D])
            epsg = sb("epsg", [64, 1])
            r_cb = k.res()
            k.dma("sp", "rbc", msk[:], d["c_mask"].rearrange("q s t -> s q t"), w=[r_cb])
            k.dma("sp", "rbc", LNW[:], d["rw_ln_w"][j:j + 1, :].partition_broadcast(64), w=[r_cb])
            k.dma("sp", "rbc", LNB[:], d["rw_ln_b"][j:j + 1, :].partition_broadcast(64), w=[r_cb])
            k.op("dve", lambda e: e.memset(epsg[:], 64e-5), w=[r_cb])
            i64 = self.ident[0:64, 0:64]
            bcm = lambda q: msk[:, q, :].unsqueeze(1).to_broadcast([64, NH, 64])
            bci = i64.unsqueeze(1).to_broadcast([64, NH, 64])

            class Chain:
                pass
            chains = []
            for ci, (dd, hg) in enumerate(((0, 0), (1, 0), (0, 1), (1, 1))):
                c = Chain()
                c.dd, c.hg = dd, hg
                nm = f"c{ci}"
                c.nm = nm
                c.ld = {n_: sb(f"{nm}_{n_}", [64, NH, 64], BF16) for n_ in ("RT", "AT", "BT", "KT", "Bh", "Kh", "Vt")}
                c.ld["Gt"] = sb(f"{nm}_Gt", [64, NH, 64])
                c.r_ld = {n_: k.res() for n_ in c.ld}
                c.rk = sb(f"{nm}_rk", [64, NH]); c.r_rk = k.res()
                c.sl = [sb(f"{nm}_s{q}", [64, NH, 64], BF16) for q in range(6)]
                c.r_sl = [k.res() for _ in range(6)]
                c.ST = sb(f"{nm}_ST", [64, NH, 64]); c.r_ST = k.res()
                c.STb = sb(f"{nm}_STb", [64, NH, 64], BF16); c.r_STb = k.res()
                c.y = sb(f"{nm}_y", [64, NH, 64]); c.r_y = k.res()
                c.sq = sb(f"{nm}_sq", [64, NH, 64]); c.r_sq = k.res()
                c.sm = sb(f"{nm}_sm", [64, 8 * NH]); c.r_sm = k.res()
                c.pb = [ci * 2, ci * 2 + 1]
                c.pbi = 0
                chains.append(c)

            def p3(pi):
                return self.ps[pi][0:64, :].rearrange("p (h n) -> p h n", h=NH)

            def mm(c, pi, pairs, rs):
                for h in range(NH):
                    for q, (lt, rt) in enumerate(pairs):
                        k.op("pe", lambda e, h=h, lt=lt, rt=rt, q=q: e.matmul(
                            self.ps[pi][0:64, h * 64:(h + 1) * 64], lt[:, h, :], rt[:, h, :],
                            start=(q == 0), stop=(q == len(pairs) - 1)), r=rs, w=[self.r_ps[pi]])

            def nb(c):
                c.pbi = (c.pbi + 1) % 2
                return c.pb[c.pbi]

            def chunk_steps(c, n):
                dd, hg = c.dd, c.hg
                ct = n if dd == 0 else 31 - n
                tt, half = ct // 2, ct % 2
                rows = slice(ct * 64, (ct + 1) * 64)
                hs = slice(hg * NH, (hg + 1) * NH)
                cs = slice(hg * 512, (hg + 1) * 512)
                L, R = c.ld, c.r_ld
                for q, n_ in enumerate(("RT", "AT", "BT", "KT")):
                    k.dma("sp", f"{c.nm}{n_}", L[n_][:], sc["FM"][dd, q, tt, :, hs, half * 64:(half + 1) * 64],
                          r=[self.r_scr["FM"][dd][q][tt]], w=[R[n_]])
                for q, n_ in enumerate(("Bh", "Kh")):
                    k.dma("sp", f"{c.nm}{n_}", L[n_][:].rearrange("p h n -> p (h n)"), sc["TM"][dd, q, rows, cs],
                          r=[self.r_scr["TM"][dd][q][ct // 2]], w=[R[n_]])
                yield
                P1, P1T, P2, P2T, T, U6 = range(6)
                S_, RS = c.sl, c.r_sl
                pi = nb(c)
                mm(c, pi, [(L["BT"], L["AT"])], [R["BT"], R["AT"]])
                k.op("dve", lambda e: e.tensor_tensor(out=S_[P1][:], in0=p3(pi), in1=bcm(dd * 3 + 0), op=ALU.mult),
                     r=[self.r_ps[pi], r_cb], w=[RS[P1]])
                yield
                pi = nb(c)
                mm(c, pi, [(L["AT"], L["BT"])], [R["BT"], R["AT"]])
                k.op("dve", lambda e: e.tensor_tensor(out=S_[P1T][:], in0=p3(pi), in1=bcm(dd * 3 + 1), op=ALU.mult),
                     r=[self.r_ps[pi], r_cb], w=[RS[P1T]])
                k.op("dve", lambda e: e.tensor_tensor(out=S_[T][:], in0=S_[P1][:], in1=bci, op=ALU.add),
                     r=[RS[P1], self.r_const], w=[RS[T]])
                yield
                a, aT, b_, bT = P1, P1T, P2, P2T
                for lvl in range(5):
                    pi = nb(c)
                    mm(c, pi, [(S_[a], S_[aT])], [RS[a], RS[aT]])
                    k.op("act", lambda e, pi=pi, bT=bT: e.copy(out=S_[bT][:], in_=p3(pi)), r=[self.r_ps[pi]], w=[RS[bT]])
                    if lvl < 4:
                        pi2 = nb(c)
                        mm(c, pi2, [(S_[aT], S_[a])], [RS[a], RS[aT]])
                        k.op("act", lambda e, pi2=pi2, b_=b_: e.copy(out=S_[b_][:], in_=p3(pi2)), r=[self.r_ps[pi2]], w=[RS[b_]])
                    yield
                    pi = nb(c)
                    mm(c, pi, [(S_[bT], S_[T])], [RS[bT], RS[T]])
                    k.op("dve", lambda e, pi=pi: e.tensor_tensor(out=S_[T][:], in0=S_[T][:], in1=p3(pi), op=ALU.add),
                         r=[self.r_ps[pi], RS[T]], w=[RS[T]])
                    yield
                    a, aT, b_, bT = b_, bT, a, aT
                Aak, Abr, Akr, WT = P1, P1T, P2, P2T
                for (dst, lt, rt, mq, eng) in ((Aak, "KT", "AT", 0, "dve"), (Abr, "BT", "RT", 2, "pool"), (Akr, "KT", "RT", 2, "dve")):
                    pi = nb(c)
                    mm(c, pi, [(L[lt], L[rt])], [R[lt], R[rt]])
                    k.op("dve", lambda e, pi=pi, dst=dst, mq=mq: e.tensor_tensor(out=S_[dst][:], in0=p3(pi), in1=bcm(dd * 3 + mq), op=ALU.mult),
                         r=[self.r_ps[pi], r_cb], w=[RS[dst]])
                    yield
                k.dma("pool", f"{c.nm}Vt", L["Vt"][:].rearrange("p h n -> p (h n)"), sc["RAWV"][rows, cs],
                      r=[self.r_scr["RAW"][2][ct // 2]], w=[R["Vt"]])
                k.dma("pool", f"{c.nm}Gt", L["Gt"][:].rearrange("p h n -> p (h n)"), sc["GG"][dd, rows, cs],
                      r=[self.r_scr["GG"][dd][ct // 2]], w=[R["Gt"]])
                k.dma("pool", f"{c.nm}rk", c.rk[:], sc["RK"][dd, rows, hs], r=[self.r_scr["RK"][dd][ct // 2]], w=[c.r_rk])
                pi = nb(c)
                mm(c, pi, [(L["AT"], c.STb), (S_[Aak], L["Vt"])], [R["AT"], c.r_STb, RS[Aak], R["Vt"]])
                k.op("act", lambda e: e.copy(out=S_[WT][:], in_=p3(pi)), r=[self.r_ps[pi]], w=[RS[WT]])
                yield
                pi = nb(c)
                mm(c, pi, [(S_[T], S_[WT])], [RS[T], RS[WT]])
                k.op("act", lambda e: e.copy(out=S_[U6][:], in_=p3(pi)), r=[self.r_ps[pi]], w=[RS[U6]])
                yield
                piy = nb(c)
                mm(c, piy, [(L["RT"], c.STb), (S_[Abr], S_[U6]), (S_[Akr], L["Vt"])],
                   [R["RT"], c.r_STb, RS[Abr], RS[U6], RS[Akr], R["Vt"]])
                k.op("act", lambda e: e.copy(out=c.y[:], in_=p3(piy)), r=[self.r_ps[piy]], w=[c.r_y])
                pis = nb(c)
                mm(c, pis, [(L["Bh"], S_[U6]), (L["Kh"], L["Vt"])], [R["Bh"], RS[U6], R["Kh"], R["Vt"]])
                pcb = PCt[:, dd, tt, hs, half].unsqueeze(2).to_broadcast([64, NH, 64])
                k.op("dve", lambda e: e.tensor_tensor(out=c.ST[:], in0=c.ST[:], in1=pcb, op=ALU.mult), r=[c.r_ST, r_PC], w=[c.r_ST])
                k.op("dve", lambda e: e.tensor_tensor(out=c.ST[:], in0=c.ST[:], in1=p3(pis), op=ALU.add), r=[c.r_ST, self.r_ps[pis]], w=[c.r_ST])
                k.op("act", lambda e: e.copy(out=c.STb[:], in_=c.ST[:]), r=[c.r_ST], w=[c.r_STb])
                yield

            def epi_steps(c, n):
                dd, hg = c.dd, c.hg
                ct = n if dd == 0 else 31 - n
                cs = slice(hg * 512, (hg + 1) * 512)
                L, R = c.ld, c.r_ld
                sm, r_sm = c.sm, c.r_sm
                bl = lambda a0: sm[:, a0:a0 + NH].unsqueeze(2).to_broadcast([64, NH, 64])
                dv = lambda fn, r, w: k.op("dve", fn, r=r, w=w)
                dv(lambda e: e.reduce_sum(out=sm[:, 0:NH], in_=c.y[:], axis=AX.X), [c.r_y], [r_sm])
                k.op("act", lambda e: e.activation(out=c.sq[:], in_=c.y[:], func=AF.Square), r=[c.r_y], w=[c.r_sq])
                yield
                dv(lambda e: e.reduce_sum(out=sm[:, NH:2 * NH], in_=c.sq[:], axis=AX.X), [c.r_sq], [r_sm])
                dv(lambda e: e.tensor_scalar_mul(out=sm[:, 0:2 * NH], in0=sm[:, 0:2 * NH], scalar1=1.0 / 64), [r_sm], [r_sm])
                yield
                dv(lambda e: e.tensor_tensor(out=sm[:, 2 * NH:3 * NH], in0=sm[:, 0:NH], in1=sm[:, 0:NH], op=ALU.mult), [r_sm], [r_sm])
                dv(lambda e: e.tensor_tensor(out=sm[:, 3 * NH:4 * NH], in0=sm[:, NH:2 * NH], in1=sm[:, 2 * NH:3 * NH], op=ALU.subtract), [r_sm], [r_sm])
                k.op("act", lambda e: e.activation(out=sm[:, 4 * NH:5 * NH], in_=sm[:, 3 * NH:4 * NH], func=AF.Sqrt, bias=epsg[:, 0:1], scale=1.0),
                     r=[r_sm, r_cb], w=[r_sm])
                dv(lambda e: e.reciprocal(out=sm[:, 5 * NH:6 * NH], in_=sm[:, 4 * NH:5 * NH]), [r_sm], [r_sm])
                yield
                z, r_z = c.y, c.r_y
                dv(lambda e: e.tensor_tensor(out=z[:], in0=c.y[:], in1=bl(0), op=ALU.subtract), [c.r_y, r_sm], [r_z])
                yield
                dv(lambda e: e.tensor_tensor(out=z[:], in0=z[:], in1=bl(5 * NH), op=ALU.mult), [r_z, r_sm], [r_z])
                yield
                zf = z[:].rearrange("p h n -> p (h n)")
                k.op("dve", lambda e: e.tensor_tensor(out=zf, in0=zf, in1=LNW[:, cs], op=ALU.mult), r=[r_z, r_cb], w=[r_z])
                yield
                k.op("dve", lambda e: e.tensor_tensor(out=zf, in0=zf, in1=LNB[:, cs], op=ALU.add), r=[r_z, r_cb], w=[r_z])
                yield
                dv(lambda e: e.tensor_tensor(out=c.sq[:], in0=L["Vt"][:], in1=c.rk[:, :].unsqueeze(2).to_broadcast([64, NH, 64]), op=ALU.mult),
                   [R["Vt"], c.r_rk], [c.r_sq])
                yield
                k.op("dve", lambda e: e.tensor_tensor(out=z[:], in0=z[:], in1=c.sq[:], op=ALU.add), r=[r_z, c.r_sq], w=[r_z])
                yield
                k.op("dve", lambda e: e.tensor_tensor(out=z[:], in0=z[:], in1=L["Gt"][:], op=ALU.mult), r=[r_z, R["Gt"]], w=[r_z])
                yield
                pt = nb(c)
                for q in range(4):
                    k.op("pe", lambda e, q=q: e.transpose(self.ps[pt][:, q * 64:(q + 1) * 64], zf[:, q * 128:(q + 1) * 128], i64),
                         r=[r_z, self.r_const], w=[self.r_ps[pt]])
                tb = ct // 8
                osl = oT[:, hg * 4:hg * 4 + 4, ct * 64:(ct + 1) * 64]
                k.op("dve", lambda e: e.tensor_tensor(out=osl, in0=osl, in1=self.ps[pt][:, 0:256].rearrange("p (q t) -> p q t", q=4), op=ALU.add),
                     r=[self.r_ps[pt]] + [r_oT[hg * 4 + q][tb] for q in range(4)], w=[r_oT[hg * 4 + q][tb] for q in range(4)])
                yield

            nchunks = self.cfg.get("rw_chunks", 32)
            for c in chains:
                k.op("pool", lambda e, c=c: e.memset(c.ST[:], 0.0), w=[c.r_ST])
                k.op("pool", lambda e, c=c: e.memset(c.STb[:], 0.0), w=[c.r_STb])
            for n in range(nchunks + 1):
                live = []
                for c in chains:
                    if n < nchunks:
                        live.append(chunk_steps(c, n))
                    if n >= 1:
                        live.append(epi_steps(c, n - 1))
                while live:
                    for g_ in list(live):
                        try:
                            next(g_)
                        except StopIteration:
                            live.remove(g_)
            k.barrier()
        with ExitStack() as es:
            wo = es.enter_context(self.sbuf("rwo", [128, NKC, D], BF16))
            r_wo = k.res()
            k.dma("pool", "rwo", wo[:], d["rw_w_o"][j].rearrange("(kc p) n -> p kc n", p=128), w=[r_wo])
            for tb in range(NTB):
                tsl = slice(tb * 512, (tb + 1) * 512)
                for dc in range(NKC):
                    pi = dc % 2
                    for kc in range(NKC):
                        k.op("pe", lambda e, kc=kc, dc=dc, pi=pi: e.matmul(self.ps[pi][:, :], wo[:, kc, dc * 128:(dc + 1) * 128], oT[:, kc, tsl],
                                                                           start=(kc == 0), stop=(kc == NKC - 1)),
                             r=[r_wo, r_oT[kc][tb]], w=[self.r_ps[pi]])
                    k.op("dve", lambda e, dc=dc, pi=pi: e.scalar_tensor_tensor(
                        out=self.xT[:, dc, tsl], in0=self.ps[pi][:, :], scalar=self.modT[:, i, 16 + dc:17 + dc],
                        in1=self.xT[:, dc, tsl], op0=ALU.mult, op1=ALU.add),
                        r=[self.r_ps[pi], self.r_mod], w=[self.r_xT[dc][tb]])
            k.barrier()

    def attn(self, i):
        nc, k, d = self.nc, self.k, self.dram
        j = i // 2
        DIL = [1, 4, 16]
        ss = lambda start, n, step: slice(start, start + (n - 1) * step + 1, step)
        all_hT = [x for y in self.r_hT for x in y]
        with ExitStack() as es:
            identb = es.enter_context(self.sbuf("identb", [128, 128], BF16))
            r_idb = k.res()
            k.op("act", lambda e: e.copy(out=identb[:], in_=self.ident[:]), r=[self.r_const], w=[r_idb])
            w3 = [es.enter_context(self.sbuf(f"w3_{b}", [128, 3, NKC, 128], BF16)) for b in range(2)]
            r_w3 = [k.res() for _ in range(2)]
            wo = [es.enter_context(self.sbuf(f"wo_{b}", [128, D], BF16)) for b in range(2)]
            r_wo = [k.res() for _ in range(2)]
            bias = [es.enter_context(self.sbuf(f"ab_{b}", [128, 384], F32)) for b in range(2)]
            r_bias = [k.res() for _ in range(2)]
            QT = es.enter_context(self.sbuf("QT", [128, S], BF16))
            KT = es.enter_context(self.sbuf("KT", [128, S], BF16))
            V = es.enter_context(self.sbuf("Vb", [128, 16, 128], BF16))
            r_QT, r_KT, r_V = k.res(), k.res(), k.res()
            Og = [es.enter_context(self.sbuf(f"Og{g}", [128, S], F32)) for g in range(3)]
            r_Og = [k.res() for _ in range(3)]
            LSE = es.enter_context(self.sbuf("LSE", [1, 3, S], F32))
            r_LSE = k.res()
            rowA = es.enter_context(self.sbuf("rowA", [1, S], F32))
            r_rowA = k.res()
            NB = 4
            sc = [es.enter_context(self.sbuf(f"sc{b}", [128, 384], F32)) for b in range(NB)]
            pn = [es.enter_context(self.sbuf(f"pn{b}", [128, 384], BF16)) for b in range(NB)]
            st = [es.enter_context(self.sbuf(f"ast{b}", [128, 8], F32)) for b in range(NB)]
            r_blk = [k.res() for _ in range(NB)]
            PT = [es.enter_context(self.sbuf(f"PT{b}", [128, 384], BF16)) for b in range(2)]
            r_PT = [k.res() for _ in range(2)]
            mrg = es.enter_context(self.sbuf("mrg", [128, S], BF16))
            r_mrg = k.res()
            tmpM = es.enter_context(self.sbuf("tmpM", [128, 512], F32))
            t2 = es.enter_context(self.sbuf("t2M", [128, 512], F32))
            r_tmpM, r_t2 = k.res(), k.res()
            psT = self.ps[4][:, :].bitcast(BF16)

            def load_unit(u):
                g, h = u % 3, u // 3
                b = u % 2
                for t in range(3):
                    c0 = ((g * 3 + t) * 8 + h) * 128
                    k.dma("pool", f"w3_{b}", w3[b][:, t, :, :],
                          d["at_w_qkv"][j, :, c0:c0 + 128].rearrange("(kc p) n -> p kc n", p=128), w=[r_w3[b]])
                k.dma("sp", f"ab_{b}", bias[b][:, :], d["c_abias"][g * 8 + h], w=[r_bias[b]])

            def load_wo(h):
                b = h % 2
                k.dma("pool", f"wo_{b}", wo[b][:, :], d["at_w_o"][j, h * 128:(h + 1) * 128, :], w=[r_wo[b]])

            nblk_it = [0]

            def stageA(u, blk):
                g, h = u % 3, u // 3
                dil = DIL[g]
                nblk = 16 // dil
                r_, ib = blk // nblk, blk % nblk
                kb0, kb1 = max(ib - 1, 0), min(ib + 1, nblk - 1)
                nkb = kb1 - kb0 + 1
                nk = nkb * 128
                bc0 = (kb0 - (ib - 1)) * 128
                qsl = ss(r_ + dil * ib * 128, 128, dil)
                ksl = ss(r_ + dil * kb0 * 128, nk, dil)
                n = nblk_it[0]
                nblk_it[0] += 1
                sb_ = n % NB
                psc = 3 if n % 2 == 0 else 6
                bb = u % 2
                k.op("pe", lambda e: e.matmul(self.ps[psc][:, 0:nk], QT[:, qsl], KT[:, ksl], start=True, stop=True),
                     r=[r_QT, r_KT], w=[self.r_ps[psc]])
                k.op("dve", lambda e: e.tensor_tensor(out=sc[sb_][:, 0:nk], in0=self.ps[psc][:, 0:nk],
                                                      in1=bias[bb][:, bc0:bc0 + nk], op=ALU.add),
                     r=[self.r_ps[psc], r_bias[bb]], w=[r_blk[sb_]])
                k.op("dve", lambda e: e.reduce_max(out=st[sb_][:, 0:1], in_=sc[sb_][:, 0:nk], axis=AX.X),
                     r=[r_blk[sb_]], w=[r_blk[sb_]])
                k.op("dve", lambda e: e.tensor_scalar_mul(out=st[sb_][:, 1:2], in0=st[sb_][:, 0:1], scalar1=-1.0),
                     r=[r_blk[sb_]], w=[r_blk[sb_]])
                k.op("act", lambda e: e.activation(out=sc[sb_][:, 0:nk], in_=sc[sb_][:, 0:nk], func=AF.Exp,
                                                   bias=st[sb_][:, 1:2], scale=1.0, accum_out=st[sb_][:, 2:3]),
                     r=[r_blk[sb_]], w=[r_blk[sb_]])
                k.op("act", lambda e: e.activation(out=st[sb_][:, 4:5], in_=st[sb_][:, 2:3], func=AF.Ln),
                     r=[r_blk[sb_]], w=[r_blk[sb_]])
                k.op("dve", lambda e: e.reciprocal(out=st[sb_][:, 3:4], in_=st[sb_][:, 2:3]),
                     r=[r_blk[sb_]], w=[r_blk[sb_]])
                k.op("dve", lambda e: e.tensor_scalar_mul(out=pn[sb_][:, 0:nk], in0=sc[sb_][:, 0:nk],
                                                          scalar1=st[sb_][:, 3:4]),
                     r=[r_blk[sb_]], w=[r_blk[sb_]])
                k.op("dve", lambda e: e.tensor_tensor(out=st[sb_][:, 5:6], in0=st[sb_][:, 4:5], in1=st[sb_][:, 0:1],
                                                      op=ALU.add),
                     r=[r_blk[sb_]], w=[r_blk[sb_]])
                return (g, r_, kb0, nkb, nblk, qsl, sb_, n)

            def stageB(desc):
                g, r_, kb0, nkb, nblk, qsl, sb_, n = desc
                nk = nkb * 128
                pb = n % 2
                for c in range(nkb):
                    k.op("pe", lambda e, c=c: e.transpose(psT[:, c * 128:(c + 1) * 128], pn[sb_][:, c * 128:(c + 1) * 128],
                                                          identb[:]),
                         r=[r_blk[sb_], r_idb], w=[self.r_ps[4]])
                k.op("act", lambda e: e.copy(out=PT[pb][:, 0:nk], in_=psT[:, 0:nk]),
                     r=[self.r_ps[4]], w=[r_PT[pb]])
                k.op("pe", lambda e: e.transpose(self.ps[5][0:1, 128:256], st[sb_][:, 5:6], self.ident[:]),
                     r=[r_blk[sb_], self.r_const], w=[self.r_ps[5]])
                for c in range(nkb):
                    vb = r_ * nblk + kb0 + c
                    k.op("pe", lambda e, c=c, vb=vb: e.matmul(self.ps[5][:, 0:128], V[:, vb, :], PT[pb][:, c * 128:(c + 1) * 128],
                                                              start=(c == 0), stop=(c == nkb - 1)),
                         r=[r_V, r_PT[pb]], w=[self.r_ps[5]])
                k.op("dve", lambda e: e.tensor_copy(out=Og[g][:, qsl], in_=self.ps[5][:, 0:128]),
                     r=[self.r_ps[5]], w=[r_Og[g]])
                k.op("dve", lambda e: e.tensor_copy(out=LSE[0:1, g, qsl], in_=self.ps[5][0:1, 128:256]),
                     r=[self.r_ps[5]], w=[r_LSE])

            def proj_unit(u):
                g, h = u % 3, u // 3
                b = u % 2
                dil = DIL[g]
                nblk = 16 // dil
                for tb in range(NTB):
                    tsl = slice(tb * 512, (tb + 1) * 512)
                    for kc in range(NKC):
                        k.op("pe", lambda e, kc=kc: e.matmul(self.ps[0][:, :], w3[b][:, 0, kc, :], self.hT[:, kc, tsl],
                                                             start=(kc == 0), stop=(kc == NKC - 1)),
                             r=[r_w3[b], self.r_hT[kc][tb]], w=[self.r_ps[0]])
                    k.op("act", lambda e: e.mul(out=QT[:, tsl], in_=self.ps[0][:, :], mul=float(128 ** -0.5)),
                         r=[self.r_ps[0]], w=[r_QT])
                    for kc in range(NKC):
                        k.op("pe", lambda e, kc=kc: e.matmul(self.ps[1][:, :], w3[b][:, 1, kc, :], self.hT[:, kc, tsl],
                                                             start=(kc == 0), stop=(kc == NKC - 1)),
                             r=[r_w3[b], self.r_hT[kc][tb]], w=[self.r_ps[1]])
                    k.op("dve", lambda e: e.tensor_copy(out=KT[:, tsl], in_=self.ps[1][:, :]),
                         r=[self.r_ps[1]], w=[r_KT])
                for b4 in range(4):
                    for q in range(4):
                        blk = b4 * 4 + q
                        r_, ib = blk // nblk, blk % nblk
                        tsl = ss(r_ + dil * ib * 128, 128, dil)
                        for kc in range(NKC):
                            k.op("pe", lambda e, kc=kc, q=q, tsl=tsl: e.matmul(
                                self.ps[2][:, q * 128:(q + 1) * 128], self.hT[:, kc, tsl], w3[b][:, 2, kc, :],
                                start=(kc == 0), stop=(kc == NKC - 1)),
                                r=[r_w3[b]] + all_hT, w=[self.r_ps[2]])
                    k.op("act", lambda e, b4=b4: e.copy(out=V[:, b4 * 4:b4 * 4 + 4, :],
                                                        in_=self.ps[2][:, :].rearrange("p (q n) -> p q n", q=4)),
                         r=[self.r_ps[2]], w=[r_V])

            def merge_head(h):
                b = h % 2
                row = lambda g: LSE[0:1, g, :]
                dv = lambda fn, r, w: k.op("dve", fn, r=r, w=w)
                dv(lambda e: e.tensor_tensor(out=rowA[0:1, :], in0=row(0), in1=row(1), op=ALU.max), [r_LSE], [r_rowA])
                dv(lambda e: e.tensor_tensor(out=rowA[0:1, :], in0=rowA[0:1, :], in1=row(2), op=ALU.max), [r_LSE, r_rowA], [r_rowA])
                for g in range(3):
                    dv(lambda e, g=g: e.tensor_tensor(out=row(g), in0=row(g), in1=rowA[0:1, :], op=ALU.subtract),
                       [r_LSE, r_rowA], [r_LSE])
                k.op("act", lambda e: e.activation(out=LSE[0:1, :, :], in_=LSE[0:1, :, :], func=AF.Exp), r=[r_LSE], w=[r_LSE])
                dv(lambda e: e.tensor_tensor(out=rowA[0:1, :], in0=row(0), in1=row(1), op=ALU.add), [r_LSE], [r_rowA])
                dv(lambda e: e.tensor_tensor(out=rowA[0:1, :], in0=rowA[0:1, :], in1=row(2), op=ALU.add), [r_LSE, r_rowA], [r_rowA])
                dv(lambda e: e.reciprocal(out=rowA[0:1, :], in_=rowA[0:1, :]), [r_rowA], [r_rowA])
                for g in range(3):
                    dv(lambda e, g=g: e.tensor_tensor(out=row(g), in0=row(g), in1=rowA[0:1, :], op=ALU.mult),
                       [r_LSE, r_rowA], [r_LSE])
                for tb in range(NTB):
                    tsl = slice(tb * 512, (tb + 1) * 512)
                    for g in range(3):
                        pi = 6 if g % 2 == 0 else 3
                        k.op("pe", lambda e, g=g, pi=pi: e.matmul(self.ps[pi][:, :], self.ones[0:1, :], LSE[0:1, g, tsl],
                                                                  start=True, stop=True),
                             r=[r_LSE, self.r_const], w=[self.r_ps[pi]])
                        if g == 0:
                            dv(lambda e, pi=pi: e.tensor_tensor(out=tmpM[:, :], in0=Og[0][:, tsl], in1=self.ps[pi][:, :], op=ALU.mult),
                               [r_Og[0], self.r_ps[pi]], [r_tmpM])
                        else:
                            dv(lambda e, g=g, pi=pi: e.tensor_tensor(out=t2[:, :], in0=Og[g][:, tsl], in1=self.ps[pi][:, :], op=ALU.mult),
                               [r_Og[g], self.r_ps[pi]], [r_t2])
                            if g == 1:
                                k.op("dve", lambda e: e.tensor_tensor(out=tmpM[:, :], in0=tmpM[:, :], in1=t2[:, :], op=ALU.add),
                                     r=[r_tmpM, r_t2], w=[r_tmpM])
                            else:
                                k.op("dve", lambda e: e.tensor_tensor(out=mrg[:, tsl], in0=tmpM[:, :], in1=t2[:, :], op=ALU.add),
                                     r=[r_tmpM, r_t2], w=[r_mrg])
                for tb in range(NTB):
                    tsl = slice(tb * 512, (tb + 1) * 512)
                    for dc in range(NKC):
                        pi = 7 if dc % 2 == 0 else 0
                        k.op("pe", lambda e, dc=dc, pi=pi: e.matmul(self.ps[pi][:, :], wo[b][:, dc * 128:(dc + 1) * 128], mrg[:, tsl],
                                                                    start=True, stop=True),
                             r=[r_wo[b], r_mrg], w=[self.r_ps[pi]])
                        k.op("dve", lambda e, dc=dc, pi=pi: e.scalar_tensor_tensor(
                            out=self.xT[:, dc, tsl], in0=self.ps[pi][:, :], scalar=self.modT[:, i, 16 + dc:17 + dc],
                            in1=self.xT[:, dc, tsl], op0=ALU.mult, op1=ALU.add),
                            r=[self.r_ps[pi], self.r_mod], w=[self.r_xT[dc][tb]])

            LA = 2
            load_unit(0)
            for u in range(24):
                g, h = u % 3, u // 3
                if g == 0:
                    load_wo(h)
                if u + 1 < 24:
                    load_unit(u + 1)
                proj_unit(u)
                pend = []
                for blk in range(16):
                    pend.append(stageA(u, blk))
                    if len(pend) > LA:
                        stageB(pend.pop(0))
                while pend:
                    stageB(pend.pop(0))
                if g == 2:
                    merge_head(h)
            k.barrier()

    def final(self):
        nc, k, d = self.nc, self.k, self.dram
        plain = self.cfg.get("final_plain", False)
        with ExitStack() as es:
            sq = es.enter_context(self.sbuf("fsq", [128, NKC, 512], BF16))
            rstd = es.enter_context(self.sbuf("frstd", [128, 512], F32))
            y = es.enter_context(self.sbuf("fy", [128, NKC, 512], F32))
            ost = [es.enter_context(self.sbuf(f"fo{j}", [128, D], F32)) for j in range(2)]
            r_o = [k.res() for _ in range(2)]
            r_sq, r_rstd, r_y = k.res(), k.res(), k.res()
            gfin = self.gT[:, 2 * DEPTH, :]
            for tb in range(NTB):
                tsl = slice(tb * 512, (tb + 1) * 512)
                if not plain:
                    self.norm_core(es, tb, None, None, None, sq, r_sq, rstd, r_rstd, 7)
                for kc in range(NKC):
                    if plain:
                        k.op("dve", lambda e, kc=kc: e.tensor_copy(out=y[:, kc, :], in_=self.xT[:, kc, tsl]),
                             r=[self.r_xT[kc][tb]], w=[r_y])
                    else:
                        k.op("dve", lambda e, kc=kc: e.scalar_tensor_tensor(
                            out=y[:, kc, :], in0=self.xT[:, kc, tsl], scalar=gfin[:, kc:kc + 1], in1=rstd[:, :],
                            op0=ALU.mult, op1=ALU.mult),
                            r=[self.r_xT[kc][tb], r_rstd, self.r_const], w=[r_y])
                for t4 in range(4):
                    tt = tb * 4 + t4
                    ob = tt % 2
                    for half in range(2):
                        pi = (tt * 2 + half) % 4
                        for j in range(4):
                            kc = half * 4 + j
                            k.op("pe", lambda e, kc=kc, j=j, pi=pi: e.transpose(
                                self.ps[pi][:, j * 128:(j + 1) * 128], y[:, kc, t4 * 128:(t4 + 1) * 128], self.ident[:]),
                                r=[r_y, self.r_const], w=[self.r_ps[pi]])
                        if half == 0:
                            k.op("act", lambda e, pi=pi, ob=ob: e.copy(out=ost[ob][:, 0:512], in_=self.ps[pi][:, :]),
                                 r=[self.r_ps[pi]], w=[r_o[ob]])
                        else:
                            k.op("dve", lambda e, pi=pi, ob=ob: e.tensor_copy(out=ost[ob][:, 512:1024], in_=self.ps[pi][:, :]),
                                 r=[self.r_ps[pi]], w=[r_o[ob]])
                    k.dma("sp", f"fo{ob}", d["out"][tt * 128:(tt + 1) * 128, :], ost[ob][:], r=[r_o[ob]])


def host_consts():
    ident = np.eye(128, dtype=np.float32)
    sel = np.zeros((32, 32 * 128), np.float32)
    for e in range(32):
        sel[e, e * 128:(e + 1) * 128] = 1.0
    ab = np.zeros((24, 128, 384), np.float32)
    ii = np.arange(128)[:, None]
    jj = np.arange(384)[None, :]
    rel = np.abs((jj - 128) - ii).astype(np.float32)
    for g, dil in enumerate((1, 4, 16)):
        for h in range(8):
            slope = 2.0 ** (-8.0 * (g * 8 + h + 1) / 24.0)
            ab[g * 8 + h] = np.where(rel <= 64, -slope * rel * dil, -1e30)
    CW = -float(np.exp(-0.5))
    tri = np.zeros((6, 128, 128), np.float32)
    msk = np.zeros((6, 64, 64), np.float32)
    sidx = np.arange(128)[:, None]
    tidx = np.arange(128)[None, :]
    same = (sidx // 64) == (tidx // 64)
    s64 = np.arange(64)[:, None]
    t64 = np.arange(64)[None, :]
    for dd in range(2):
        before = (sidx < tidx) if dd == 0 else (sidx > tidx)
        after = (sidx > tidx) if dd == 0 else (sidx < tidx)
        tri[dd * 3 + 0] = np.where(same & (before | (sidx == tidx)), CW, 0.0)
        tri[dd * 3 + 1] = np.where(same & before, CW, 0.0)
        tri[dd * 3 + 2] = np.where(same & after, CW, 0.0)
        b64 = (s64 < t64) if dd == 0 else (s64 > t64)
        msk[dd * 3 + 0] = b64
        msk[dd * 3 + 1] = b64.T
        msk[dd * 3 + 2] = b64 | (s64 == t64)
    csel = np.zeros((128, 2), np.float32)
    csel[:64, 0] = CW
    csel[64:, 1] = CW
    return {"c_ident": ident, "c_sel": sel, "c_abias": ab, "c_tri": tri, "c_csel": csel, "c_mask": msk}


def prep_shared(inp):
    f = lambda a: np.ascontiguousarray(a, dtype=np.float32)
    sh = {}
    sh["ada_w"] = f(inp["ada_w"])
    sh["ada_bT"] = f(np.asarray(inp["ada_b"]).reshape(DEPTH, 48, 128).transpose(0, 2, 1))
    sh["gtmT"] = f(np.asarray(inp["norm_tm_g"]).reshape(DEPTH, NKC, 128).transpose(0, 2, 1))
    sh["gcmT"] = f(np.asarray(inp["norm_cm_g"]).reshape(DEPTH, NKC, 128).transpose(0, 2, 1))
    sh["gfinT"] = f(np.asarray(inp["final_g"]).reshape(NKC, 128).T)
    rg = np.asarray(inp["moe_router_g"])
    re_ = np.asarray(inp["moe_router_e"])
    wr = np.concatenate([rg] + [re_[:, g] for g in range(4)], axis=-1)
    sh["wr"] = f(wr.reshape(DEPTH, NKC, 128, 36).transpose(0, 2, 1, 3))
    br = np.concatenate([np.asarray(inp["moe_router_g_b"]), np.asarray(inp["moe_router_e_b"]).reshape(DEPTH, 32)], axis=-1)
    sh["br"] = f(br.reshape(DEPTH, 1, 36))
    sh["rw_muT"] = f(np.asarray(inp["rw_mu"]).reshape(2, 6, NKC, 128).transpose(0, 3, 1, 2))
    sh["rw_w_rkv"] = f(inp["rw_w_rkv"])
    sh["rw_w1cat"] = f(np.concatenate([np.asarray(inp["rw_w1"])[:, 0], np.asarray(inp["rw_w1"])[:, 1]], axis=-1))
    sh["rw_a1cat"] = f(np.concatenate([np.asarray(inp["rw_a1"])[:, 0], np.asarray(inp["rw_a1"])[:, 1]], axis=-1))
    sh["rw_g1"] = f(inp["rw_g1"])
    sh["rw_w2cat"] = f(np.asarray(inp["rw_w2"]).reshape(2, 128, D))
    sh["rw_a2cat"] = f(np.asarray(inp["rw_a2"]).reshape(2, 128, D))
    sh["rw_g2"] = f(inp["rw_g2"])
    sh["rw_w0"] = f(inp["rw_w0"])
    sh["rw_a0"] = f(inp["rw_a0"])
    for n_ in ("rw_k_k", "rw_k_a", "rw_ln_w", "rw_ln_b"):
        sh[n_] = f(inp[n_])
    sh["rw_r_k"] = f(np.asarray(inp["rw_r_k"]).reshape(2, D))
    sh["rw_w_o"] = f(inp["rw_w_o"])
    sh["at_w_qkv"] = f(inp["at_w_qkv"])
    sh["at_w_o"] = f(inp["at_w_o"])
    sh["moe_w_gate"] = f(inp["moe_w_gate"])
    sh["moe_w_up"] = f(inp["moe_w_up"])
    sh["moe_w_down"] = f(inp["moe_w_down"])
    sh.update(host_consts())
    return sh


def prep_core(inp, b):
    return {
        "x": np.ascontiguousarray(np.asarray(inp["x"])[b], dtype=np.float32),
        "cT": np.ascontiguousarray(np.asarray(inp["c"])[b].reshape(NKC, 128).T, dtype=np.float32),
    }


def build_prog(cfg):
    nc = bass.Bass("TRN2", target_bir_lowering=False)
    p = Prog(nc, cfg)
    with p.k.es:
        p.build()
    return nc, p


def run(inp, cfg, cores):
    nc, p = build_prog(cfg)
    sh = prep_shared(inp)
    in_maps = []
    for b in cores:
        m = dict(sh)
        m.update(prep_core(inp, b))
        m = {kk: v for kk, v in m.items() if kk in p.dram}
        in_maps.append(m)
    res = run_bass_kernel_spmd(nc, in_maps, core_ids=list(range(len(cores))))
    return res


def kernel(**inputs):
    res = run(inputs, {}, list(range(8)))
    out = np.stack([np.asarray(r["out"]) for r in res.results], axis=0)
    return out.astype(np.float32)
```

```python
import numpy as np
from contextlib import ExitStack

import concourse.bass as bass
import concourse.mybir as mybir
from concourse.bass_utils import run_bass_kernel_spmd

F32 = mybir.dt.float32
BF16 = mybir.dt.bfloat16
AF = mybir.ActivationFunctionType
ALU = mybir.AluOpType
AX = mybir.AxisListType

D = 1024
S = 2048
DEPTH = 4
NKC = 8
NTB = 4
NTT = 16
RMS_EPS = 1e-6
SEM_LIM = 24000


class Res:
    __slots__ = ("name", "w", "rs", "excl")

    def __init__(self, name):
        self.name = name
        self.w = None
        self.rs = {}
        self.excl = False

    def add_reader(self, tag):
        st, ep, v = tag
        if self.rs.get(st, (-1, 0)) < (ep, v):
            self.rs[st] = (ep, v)


class KB:
    COMPUTE = ("pe", "act", "dve", "pool")

    def __init__(self, nc):
        self.nc = nc
        self.es = ExitStack()
        self.eng = {"pe": nc.tensor, "act": nc.scalar, "dve": nc.vector, "pool": nc.gpsimd, "sp": nc.sync}
        self.sems = {}
        self.cnt = {}
        self.seen = {e: {} for e in self.eng}
        self.nres = 0
        self.epoch_final = {}
        self.ninst = {e: 0 for e in self.eng}

    def res(self, name=None):
        self.nres += 1
        return Res(name or f"r{self.nres}")

    def sb(self, name, shape, dt):
        return self.es.enter_context(self.nc.sbuf_tensor(name, list(shape), dt))

    def _sem(self, stream, epoch):
        key = (stream, epoch)
        if key not in self.sems:
            self.sems[key] = self.es.enter_context(self.nc.semaphore(f"s_{stream}_{epoch}"))
        return self.sems[key]

    def _bump(self, stream, inc):
        ep, v = self.cnt.get(stream, (0, 0))
        if v + inc > SEM_LIM:
            self.epoch_final[(stream, ep)] = v
            ep, v = ep + 1, 0
        v += inc
        self.cnt[stream] = (ep, v)
        return (stream, ep, v), self._sem(stream, ep)

    def _wait(self, eng, tags):
        best = {}
        for t in tags:
            if t is None:
                continue
            st, ep, v = t
            if st == "pe" and eng == "pe":
                continue
            if (st not in best) or (ep, v) > best[st]:
                best[st] = (ep, v)
        for st, (ep, v) in best.items():
            if st.startswith("d_"):
                cur_ep, cur_v = self.cnt[st]
                v = cur_v if cur_ep == ep else self.epoch_final[(st, ep)]
            if self.seen[eng].get(st, (-1, 0)) >= (ep, v):
                continue
            self.eng[eng].wait_ge(self._sem(st, ep), v)
            self.seen[eng][st] = (ep, v)

    def _deps(self, r, w, same_stream=None):
        tags = []
        for x in r:
            tags.append(x.w)
            if x.excl:
                for st, (ep, v) in x.rs.items():
                    if same_stream is not None and st == same_stream:
                        continue
                    tags.append((st, ep, v))
        for x in w:
            tags.append(x.w)
            for st, (ep, v) in x.rs.items():
                if same_stream is not None and st == same_stream:
                    continue
                tags.append((st, ep, v))
        return tags

    def op(self, eng, fn, r=(), w=()):
        self._wait(eng, self._deps(r, w, same_stream=eng))
        ins = fn(self.eng[eng])
        tag, sem = self._bump(eng, 1)
        ins.then_inc(sem, 1)
        self.ninst[eng] += 1
        for x in r:
            x.add_reader(tag)
        for x in w:
            x.w = tag
            x.rs = {}
        return tag

    def dma(self, q, chan, out, in_, r=(), w=(), **kw):
        self._wait(q, self._deps(r, w))
        ins = self.eng[q].dma_start(out=out, in_=in_, **kw)
        tag, sem = self._bump("d_" + chan, 16)
        ins.then_inc(sem, 16)
        self.ninst[q] += 1
        for x in r:
            x.add_reader(tag)
        for x in w:
            x.w = tag
            x.rs = {}
        return tag

    def barrier(self, engines=("pe", "act", "dve", "pool", "sp")):
        tags = [(st, ep, v) for st, (ep, v) in self.cnt.items()]
        for e in engines:
            self._wait(e, tags)

    def wait_all(self, eng):
        self._wait(eng, [(st, ep, v) for st, (ep, v) in self.cnt.items()])


class Prog:
    def __init__(self, nc, cfg):
        self.nc = nc
        self.cfg = cfg
        self.k = KB(nc)
        self.dram = {}
        self._uid = 0

    def sbuf(self, name, shape, dt):
        self._uid += 1
        return self.nc.sbuf_tensor(f"{name}_u{self._uid}", list(shape), dt)

    def dump(self, name, ap, shape, dt, rs):
        if name not in self.cfg.get("dump", ()):
            return
        o = self.dout("dbg_" + name, shape, dt)
        self.k.dma("sp", "dbg", o, ap, r=rs)
        self.k.barrier()

    def din(self, name, shape, dt=F32):
        self.dram[name] = self.nc.dram_tensor(name, list(shape), dt, kind="ExternalInput").ap()
        return self.dram[name]

    def dout(self, name, shape, dt=F32):
        self.dram[name] = self.nc.dram_tensor(name, list(shape), dt, kind="ExternalOutput").ap()
        return self.dram[name]

    def build(self):
        nc, k, cfg = self.nc, self.k, self.cfg
        layers = cfg.get("layers", list(range(DEPTH)))
        d = self.dram
        self.din("x", [S, D])
        self.din("cT", [128, NKC])
        self.din("ada_w", [DEPTH, D, 6 * D])
        self.din("ada_bT", [DEPTH, 128, 48])
        self.din("gtmT", [DEPTH, 128, NKC])
        self.din("gcmT", [DEPTH, 128, NKC])
        self.din("gfinT", [128, NKC])
        self.din("wr", [DEPTH, 128, NKC, 36])
        self.din("br", [DEPTH, 1, 36])
        self.din("moe_w_gate", [DEPTH, 4, 8, D, 256])
        self.din("moe_w_up", [DEPTH, 4, 8, D, 256])
        self.din("moe_w_down", [DEPTH, 4, 8, 256, D])
        self.din("rw_muT", [2, 128, 6, NKC])
        self.din("rw_w_rkv", [2, 3, D, D])
        self.din("rw_w1cat", [2, D, 128])
        self.din("rw_a1cat", [2, D, 128])
        self.din("rw_g1", [2, 2, D, 128])
        self.din("rw_w2cat", [2, 128, D])
        self.din("rw_a2cat", [2, 128, D])
        self.din("rw_g2", [2, 2, 128, D])
        self.din("rw_w0", [2, 2, D])
        self.din("rw_a0", [2, 2, D])
        for n_ in ("rw_k_k", "rw_k_a", "rw_r_k", "rw_ln_w", "rw_ln_b"):
            self.din(n_, [2, D])
        self.din("rw_w_o", [2, D, D])
        self.din("c_tri", [6, 128, 128])
        self.din("c_csel", [128, 2])
        self.din("c_mask", [6, 64, 64])
        itn = lambda n_, shp, dt=F32: nc.dram_tensor(n_, list(shp), dt, kind="Internal").ap()
        self.scr = {"RAW": itn("scr_raw", [2, S, D]), "RAWV": itn("scr_rawv", [S, D], BF16),
                    "FM": itn("scr_fm", [2, 4, NTT, 64, 16, 128], BF16),
                    "TM": itn("scr_tm", [2, 2, S, D], BF16), "GG": itn("scr_gg", [2, S, D]), "RK": itn("scr_rk", [2, S, 16])}
        self.r_scr = {"RAW": [[k.res() for _ in range(NTT)] for _ in range(3)],
                      "FM": [[[k.res() for _ in range(NTT)] for _ in range(4)] for _ in range(2)],
                      "TM": [[[k.res() for _ in range(NTT)] for _ in range(2)] for _ in range(2)],
                      "GG": [[k.res() for _ in range(NTT)] for _ in range(2)],
                      "RK": [[k.res() for _ in range(NTT)] for _ in range(2)]}
        self.din("at_w_qkv", [2, D, 9216])
        self.din("at_w_o", [2, D, D])
        self.din("c_abias", [24, 128, 384])
        self.din("c_ident", [128, 128])
        self.din("c_sel", [32, 32 * 128])
        self.dout("out", [S, D])

        self.xT = k.sb("xT", [128, NKC, S], F32)
        self.hT = k.sb("hT", [128, NKC, S], BF16)
        self.r_xT = [[k.res(f"xT{kc}_{tb}") for tb in range(NTB)] for kc in range(NKC)]
        self.r_hT = [[k.res(f"hT{kc}_{tb}") for tb in range(NTB)] for kc in range(NKC)]
        self.ident = k.sb("ident", [128, 128], F32)
        self.ones = k.sb("ones", [128, 128], F32)
        self.epsT = k.sb("epsT", [128, 1], F32)
        self.onesb = k.sb("onesb", [128, 128], BF16)
        self.r_const = k.res("const")
        self.modT = k.sb("modT", [128, DEPTH, 48], F32)
        self.mod1T = k.sb("mod1T", [128, DEPTH, 48], F32)
        self.r_mod = k.res("mod")
        self.gT = k.sb("gT", [128, 2 * DEPTH + 1, NKC], F32)
        self.ps = [self.k.es.enter_context(nc.psum_tensor(f"ps{i}", [128, 512], F32)) for i in range(8)]
        self.r_ps = [k.res(f"ps{i}") for i in range(8)]
        for r_ in self.r_ps:
            r_.excl = True

        k.dma("sp", "c0", self.ident[:], d["c_ident"][:, :], w=[self.r_const])
        k.op("dve", lambda e: e.memset(self.ones[:], 1.0), w=[self.r_const])
        k.op("dve", lambda e: e.memset(self.epsT[:], RMS_EPS), w=[self.r_const])
        k.op("dve", lambda e: e.memset(self.onesb[:], 1.0), w=[self.r_const])
        k.dma("sp", "c0", self.gT[:, 0:DEPTH, :], d["gtmT"].rearrange("l p k -> p l k"), w=[self.r_const])
        k.dma("sp", "c0", self.gT[:, DEPTH:2 * DEPTH, :], d["gcmT"].rearrange("l p k -> p l k"), w=[self.r_const])
        k.dma("sp", "c0", self.gT[:, 2 * DEPTH, :], d["gfinT"][:, :], w=[self.r_const])

        self.load_x()
        self.ada_all()
        k.barrier()
        for i in layers:
            if cfg.get("mixers", True):
                self.norm_mod(i, 0)
                if i % 2 == 0:
                    self.rwkv(i)
                else:
                    self.attn(i)
            if cfg.get("moe", True):
                self.moe(i)
        self.final()
        k.barrier()
        k.wait_all("sp")
        k.wait_all("pool")

    def load_x(self):
        nc, k, d = self.nc, self.k, self.dram
        with ExitStack() as es:
            st = [es.enter_context(self.sbuf(f"xst{i}", [128, D], F32)) for i in range(2)]
            r_st = [k.res() for _ in range(2)]
            for tt in range(NTT):
                b = tt % 2
                k.dma("sp", f"xst{b}", st[b][:], d["x"][tt * 128:(tt + 1) * 128, :], w=[r_st[b]])
                for half in range(2):
                    pi = (tt * 2 + half) % 4
                    for j in range(4):
                        kc = half * 4 + j
                        k.op("pe", lambda e, kc=kc, j=j, pi=pi, b=b: e.transpose(
                            self.ps[pi][:, j * 128:(j + 1) * 128], st[b][:, kc * 128:(kc + 1) * 128], self.ident[:]),
                            r=[r_st[b], self.r_const], w=[self.r_ps[pi]])
                    tb = tt // 4
                    ws = [self.r_xT[half * 4 + j][tb] for j in range(4)]
                    eng = "act" if half else "dve"
                    if eng == "dve":
                        k.op("dve", lambda e, half=half, pi=pi, tt=tt: e.tensor_copy(
                            out=self.xT[:, half * 4:half * 4 + 4, tt * 128:(tt + 1) * 128],
                            in_=self.ps[pi][:, :].rearrange("p (j t) -> p j t", j=4)),
                            r=[self.r_ps[pi]], w=ws)
                    else:
                        k.op("act", lambda e, half=half, pi=pi, tt=tt: e.copy(
                            out=self.xT[:, half * 4:half * 4 + 4, tt * 128:(tt + 1) * 128],
                            in_=self.ps[pi][:, :].rearrange("p (j t) -> p j t", j=4)),
                            r=[self.r_ps[pi]], w=ws)
            k.barrier()

    def ada_all(self):
        nc, k, d = self.nc, self.k, self.dram
        NP = 8
        PW = 6 * D // NP
        with ExitStack() as es:
            scT = es.enter_context(self.sbuf("scT", [128, NKC], F32))
            cT = es.enter_context(self.sbuf("cTs", [128, NKC], F32))
            abT = es.enter_context(self.sbuf("abT", [128, DEPTH, 48], F32))
            wst = [es.enter_context(self.sbuf(f"adaw{i}", [128, NKC, PW], F32)) for i in range(2)]
            r_w = [k.res() for _ in range(2)]
            r_sc = k.res()
            r_ab = k.res()
            k.dma("sp", "c1", cT[:], d["cT"][:, :], w=[r_sc])
            k.dma("sp", "c1", abT[:], d["ada_bT"].rearrange("l p j -> p l j"), w=[r_ab])
            k.op("act", lambda e: e.activation(out=scT[:], in_=cT[:], func=AF.Silu), r=[r_sc], w=[r_sc])
            q = 0
            for i in range(DEPTH):
                pi = i % 2
                for pc in range(NP):
                    b = q % 2
                    q += 1
                    k.dma("sp", f"adaw{b}", wst[b][:],
                          d["ada_w"][i, :, pc * PW:(pc + 1) * PW].rearrange("(kc p) n -> p kc n", p=128),
                          w=[r_w[b]])
                    for jj in range(PW // 128):
                        j = pc * (PW // 128) + jj
                        for kc in range(NKC):
                            k.op("pe", lambda e, b=b, jj=jj, kc=kc, j=j, pi=pi: e.matmul(
                                self.ps[pi][:, j:j + 1], wst[b][:, kc, jj * 128:(jj + 1) * 128], scT[:, kc:kc + 1],
                                start=(kc == 0), stop=(kc == NKC - 1)),
                                r=[r_w[b], r_sc], w=[self.r_ps[pi]])
                k.op("dve", lambda e, i=i, pi=pi: e.tensor_tensor(
                    out=self.modT[:, i, :], in0=self.ps[pi][:, 0:48], in1=abT[:, i, :], op=ALU.add),
                    r=[self.r_ps[pi], r_ab], w=[self.r_mod])
                k.op("dve", lambda e, i=i: e.tensor_scalar_add(
                    out=self.mod1T[:, i, :], in0=self.modT[:, i, :], scalar1=1.0),
                    r=[self.r_mod], w=[self.r_mod])
            k.barrier()

    def norm_core(self, es, tb, geff, shift, r_par, sq, r_sq, rstd, r_rstd, psi):
        nc, k = self.nc, self.k
        tsl = slice(tb * 512, (tb + 1) * 512)
        k.op("act", lambda e: e.activation(out=sq[:, :, :], in_=self.xT[:, :, tsl], func=AF.Square),
             r=[self.r_xT[kc][tb] for kc in range(NKC)], w=[r_sq])
        for kc in range(NKC):
            k.op("pe", lambda e, kc=kc: e.matmul(self.ps[psi][:, :], self.onesb[:, :], sq[:, kc, :],
                                                 start=(kc == 0), stop=(kc == NKC - 1)),
                 r=[r_sq, self.r_const], w=[self.r_ps[psi]])
        k.op("act", lambda e: e.activation(out=rstd[:, :], in_=self.ps[psi][:, :], func=AF.Sqrt,
                                           scale=1.0 / D, bias=self.epsT[:, 0:1]),
             r=[self.r_ps[psi], self.r_const], w=[r_rstd])
        k.op("dve", lambda e: e.reciprocal(out=rstd[:, :], in_=rstd[:, :]), r=[r_rstd], w=[r_rstd])

    def norm_mod(self, i, which, router=None):
        nc, k = self.nc, self.k
        gi = i if which == 0 else DEPTH + i
        so = 0 if which == 0 else 24
        with ExitStack() as es:
            geff = es.enter_context(self.sbuf("geff", [128, NKC], F32))
            sq = [es.enter_context(self.sbuf(f"nsq{j}", [128, NKC, 512], BF16)) for j in range(2)]
            rstd = [es.enter_context(self.sbuf(f"nrstd{j}", [128, 512], F32)) for j in range(2)]
            t1 = [es.enter_context(self.sbuf(f"nt1_{j}", [128, 512], F32)) for j in range(2)]
            r_t1 = [k.res() for _ in range(2)]
            r_g = k.res()
            r_sq = [k.res() for _ in range(2)]
            r_rstd = [k.res() for _ in range(2)]
            if router is not None:
                h32 = es.enter_context(self.sbuf("h32", [128, NKC, 512], F32))
                r_h32 = k.res()
            k.op("dve", lambda e: e.tensor_tensor(out=geff[:], in0=self.gT[:, gi, :], in1=self.mod1T[:, i, so + 8:so + 16],
                                                  op=ALU.mult), r=[self.r_const, self.r_mod], w=[r_g])
            ncore = lambda tb: self.norm_core(es, tb, None, None, None, sq[tb % 2], r_sq[tb % 2], rstd[tb % 2], r_rstd[tb % 2],
                                              7 if tb % 2 == 0 else 5)
            ncore(0)
            for tb in range(NTB):
                tsl = slice(tb * 512, (tb + 1) * 512)
                if tb + 1 < NTB:
                    ncore(tb + 1)
                rs_, r_rs = rstd[tb % 2], r_rstd[tb % 2]
                for kc in range(NKC):
                    b = kc % 2
                    k.op("dve", lambda e, kc=kc, b=b: e.tensor_tensor(out=t1[b][:, :], in0=self.xT[:, kc, tsl], in1=rs_[:, :],
                                                                      op=ALU.mult),
                         r=[self.r_xT[kc][tb], r_rs], w=[r_t1[b]])
                    if router is None:
                        k.op("act", lambda e, kc=kc, b=b: e.activation(
                            out=self.hT[:, kc, tsl], in_=t1[b][:, :], func=AF.Identity, scale=geff[:, kc:kc + 1],
                            bias=self.modT[:, i, so + kc:so + kc + 1]),
                            r=[r_t1[b], r_g, self.r_mod], w=[self.r_hT[kc][tb]])
                    else:
                        k.op("act", lambda e, kc=kc, b=b: e.activation(
                            out=h32[:, kc, :], in_=t1[b][:, :], func=AF.Identity, scale=geff[:, kc:kc + 1],
                            bias=self.modT[:, i, so + kc:so + kc + 1]),
                            r=[r_t1[b], r_g, self.r_mod], w=[r_h32])
                        k.op("act", lambda e, kc=kc, b=b: e.activation(
                            out=self.hT[:, kc, tsl], in_=t1[b][:, :], func=AF.Identity, scale=geff[:, kc:kc + 1],
                            bias=self.modT[:, i, so + kc:so + kc + 1]),
                             r=[r_t1[b], r_g, self.r_mod], w=[self.r_hT[kc][tb]])
                if router is not None:
                    router(tb, h32, r_h32)
            k.barrier()

    def moe(self, i):
        nc, k, d = self.nc, self.k, self.dram
        with ExitStack() as es0:
            gT_all = es0.enter_context(self.sbuf("gT_all", [32, S], F32))
            r_gT = [k.res() for _ in range(NTT)]
            with ExitStack() as es:
                wr = es.enter_context(self.sbuf("wr", [128, NKC, 36], F32))
                brs = es.enter_context(self.sbuf("brs", [1, 36], F32))
                r_wr = k.res()
                k.dma("sp", "c2", wr[:], d["wr"][i], w=[r_wr])
                k.dma("sp", "c2", brs[:], d["br"][i], w=[r_wr])
                L = es.enter_context(self.sbuf("rL", [128, 36], F32))
                sm = es.enter_context(self.sbuf("rsm", [128, 64], F32))
                gates = es.enter_context(self.sbuf("rgates", [128, 32], F32))
                r_L, r_sm, r_gates = k.res(), k.res(), k.res()

                def router(tb, h32, r_h32):
                    for t4 in range(4):
                        tt = tb * 4 + t4
                        pi = 6
                        for kc in range(NKC):
                            k.op("pe", lambda e, kc=kc: e.matmul(self.ps[pi][:, 0:36], h32[:, kc, t4 * 128:(t4 + 1) * 128],
                                                                 wr[:, kc, :], start=(kc == 0), stop=False),
                                 r=[r_h32, r_wr], w=[self.r_ps[pi]])
                        k.op("pe", lambda e: e.matmul(self.ps[pi][:, 0:36], self.ones[0:1, :], brs[0:1, :],
                                                      start=False, stop=True),
                             r=[r_wr, self.r_const], w=[self.r_ps[pi]])
                        k.op("dve", lambda e: e.tensor_copy(out=L[:, :], in_=self.ps[pi][:, 0:36]),
                             r=[self.r_ps[pi]], w=[r_L])
                        self.route_math(L, r_L, sm, r_sm, gates, r_gates)
                        k.op("pe", lambda e: e.transpose(self.ps[pi][0:32, 128:256], gates[:, :], self.ident[:]),
                             r=[r_gates, self.r_const], w=[self.r_ps[pi]])
                        k.op("act", lambda e, tt=tt: e.copy(out=gT_all[:, tt * 128:(tt + 1) * 128],
                                                            in_=self.ps[pi][0:32, 128:256]),
                             r=[self.r_ps[pi]], w=[r_gT[tt]])

                self.norm_mod(i, 1, router=router)
                self.dump("modT", self.modT[:], [128, DEPTH, 48], F32, [self.r_mod])
                self.dump("hT", self.hT[:], [128, NKC, S], BF16, [x for y in self.r_hT for x in y])
                self.dump("gT_all", gT_all[:], [32, S], F32, r_gT)
            with ExitStack() as es:
                sel = es.enter_context(self.sbuf("sel", [32, 32 * 128], F32))
                r_sel = k.res()
                k.dma("sp", "c2", sel[:], d["c_sel"][:, :], w=[r_sel])
                NS = 16
                wg = [es.enter_context(self.sbuf(f"wg{b}", [128, 2, NKC, 256], BF16)) for b in range(2)]
                wu = [es.enter_context(self.sbuf(f"wu{b}", [128, 2, NKC, 256], BF16)) for b in range(2)]
                wd = [es.enter_context(self.sbuf(f"wd{b}", [128, 2, 2, D], BF16)) for b in range(2)]
                r_w = [k.res() for _ in range(2)]
                gbc = [es.enter_context(self.sbuf(f"gbc{b}", [128, 2, 512], F32)) for b in range(2)]
                r_gbc = [k.res() for _ in range(2)]
                hid = [es.enter_context(self.sbuf(f"hid{b}", [128, 4, 512], BF16)) for b in range(2)]
                r_hid = [k.res() for _ in range(2)]
                sg = [es.enter_context(self.sbuf(f"sg{b}", [128, 512], F32)) for b in range(2)]
                r_sg = [k.res() for _ in range(2)]
                tm = [es.enter_context(self.sbuf(f"tm{b}", [128, 512], F32)) for b in range(2)]
                r_tm = [k.res() for _ in range(2)]

                def load_w(s):
                    b = s % 2
                    for ee in range(2):
                        eg = 2 * s + ee
                        g_, e_ = eg // 8, eg % 8
                        k.dma("pool", f"moew{b}", wg[b][:, ee, :, :],
                              d["moe_w_gate"][i, g_, e_].rearrange("(kc p) f -> p kc f", p=128), w=[r_w[b]])
                        k.dma("pool", f"moew{b}", wu[b][:, ee, :, :],
                              d["moe_w_up"][i, g_, e_].rearrange("(kc p) f -> p kc f", p=128), w=[r_w[b]])
                        k.dma("pool", f"moew{b}", wd[b][:, ee, :, :],
                              d["moe_w_down"][i, g_, e_].rearrange("(fc p) n -> p fc n", p=128), w=[r_w[b]])

                pending = [None]
                it = [0]

                def down(s, tb, hb):
                    b = s % 2
                    tsl = slice(tb * 512, (tb + 1) * 512)
                    for dc in range(NKC):
                        pi = 4 + dc % 2
                        for u in range(4):
                            ee, fc = u // 2, u % 2
                            k.op("pe", lambda e, u=u, ee=ee, fc=fc, dc=dc, pi=pi: e.matmul(
                                self.ps[pi][:, :], wd[b][:, ee, fc, dc * 128:(dc + 1) * 128], hid[hb][:, u, :],
                                start=(u == 0), stop=(u == 3)),
                                r=[r_w[b], r_hid[hb]], w=[self.r_ps[pi]])
                        k.op("dve", lambda e, dc=dc, pi=pi: e.scalar_tensor_tensor(
                            out=self.xT[:, dc, tsl], in0=self.ps[pi][:, :], scalar=self.modT[:, i, 40 + dc:41 + dc],
                            in1=self.xT[:, dc, tsl], op0=ALU.mult, op1=ALU.add),
                            r=[self.r_ps[pi], self.r_mod], w=[self.r_xT[dc][tb]])

                load_w(0)
                for s in range(NS):
                    b = s % 2
                    for tb in range(NTB):
                        tsl = slice(tb * 512, (tb + 1) * 512)
                        hb = it[0] % 2
                        gb = it[0] % 2
                        it[0] += 1
                        for ee in range(2):
                            eg = 2 * s + ee
                            k.op("pe", lambda e, eg=eg: e.matmul(self.ps[6][:, :], sel[:, eg * 128:(eg + 1) * 128],
                                                                 gT_all[:, tsl], start=True, stop=True),
                                 r=[r_sel] + r_gT[tb * 4:tb * 4 + 4], w=[self.r_ps[6]])
                            k.op("act", lambda e, ee=ee, gb=gb: e.copy(out=gbc[gb][:, ee, :], in_=self.ps[6][:, :]),
                                 r=[self.r_ps[6]], w=[r_gbc[gb]])
                        for u in range(4):
                            ee, fc = u // 2, u % 2
                            pg, pu = (0, 1) if u % 2 == 0 else (2, 3)
                            for kc in range(NKC):
                                k.op("pe", lambda e, kc=kc, ee=ee, fc=fc, pg=pg: e.matmul(
                                    self.ps[pg][:, :], wg[b][:, ee, kc, fc * 128:(fc + 1) * 128], self.hT[:, kc, tsl],
                                    start=(kc == 0), stop=(kc == NKC - 1)),
                                    r=[r_w[b], self.r_hT[kc][tb]], w=[self.r_ps[pg]])
                            for kc in range(NKC):
                                k.op("pe", lambda e, kc=kc, ee=ee, fc=fc, pu=pu: e.matmul(
                                    self.ps[pu][:, :], wu[b][:, ee, kc, fc * 128:(fc + 1) * 128], self.hT[:, kc, tsl],
                                    start=(kc == 0), stop=(kc == NKC - 1)),
                                    r=[r_w[b], self.r_hT[kc][tb]], w=[self.r_ps[pu]])
                            sb_ = u % 2
                            k.op("act", lambda e, pg=pg, sb_=sb_: e.activation(out=sg[sb_][:, :], in_=self.ps[pg][:, :],
                                                                               func=AF.Silu),
                                 r=[self.r_ps[pg]], w=[r_sg[sb_]])
                            k.op("dve", lambda e, pu=pu, sb_=sb_: e.tensor_tensor(out=tm[sb_][:, :], in0=sg[sb_][:, :],
                                                                                  in1=self.ps[pu][:, :], op=ALU.mult),
                                 r=[r_sg[sb_], self.r_ps[pu]], w=[r_tm[sb_]])
                            k.op("dve", lambda e, u=u, ee=ee, sb_=sb_, hb=hb, gb=gb: e.tensor_tensor(
                                out=hid[hb][:, u, :], in0=tm[sb_][:, :], in1=gbc[gb][:, ee, :], op=ALU.mult),
                                r=[r_tm[sb_], r_gbc[gb]], w=[r_hid[hb]])
                        if pending[0] is not None:
                            down(*pending[0])
                        pending[0] = (s, tb, hb)
                        if tb == 0 and s + 1 < NS:
                            load_w(s + 1)
                down(*pending[0])
                k.barrier()

    def route_math(self, L, r_L, sm, r_sm, gates, r_gates):
        k = self.k
        GMAX, NGMAX, GSUM, PG, M1, M2, DD, EE, W1, W2 = range(10)
        GOH = slice(10, 14)
        GE = slice(14, 18)
        ESEL = slice(18, 26)
        OH1 = slice(26, 34)
        MSK = slice(34, 42)
        OH2 = slice(42, 50)
        INN = slice(50, 58)
        c = lambda j: slice(j, j + 1)

        def dv(fn, r=(), w=()):
            k.op("dve", fn, r=r, w=w)

        rs = [r_L, r_sm]
        dv(lambda e: e.reduce_max(out=sm[:, c(GMAX)], in_=L[:, 0:4], axis=AX.X), r=[r_L], w=[r_sm])
        dv(lambda e: e.tensor_scalar(out=sm[:, GOH], in0=L[:, 0:4], scalar1=sm[:, c(GMAX)], scalar2=None,
                                     op0=ALU.is_equal), r=rs, w=[r_sm])
        dv(lambda e: e.tensor_scalar_mul(out=sm[:, c(NGMAX)], in0=sm[:, c(GMAX)], scalar1=-1.0), r=[r_sm], w=[r_sm])
        k.op("act", lambda e: e.activation(out=sm[:, GE], in_=L[:, 0:4], func=AF.Exp, bias=sm[:, c(NGMAX)],
                                           scale=1.0, accum_out=sm[:, c(GSUM)]), r=rs, w=[r_sm])
        dv(lambda e: e.reciprocal(out=sm[:, c(PG)], in_=sm[:, c(GSUM)]), r=[r_sm], w=[r_sm])
        dv(lambda e: e.tensor_scalar_mul(out=sm[:, ESEL], in0=L[:, 4:12], scalar1=sm[:, c(10)]), r=rs, w=[r_sm])
        for g in range(1, 4):
            dv(lambda e, g=g: e.scalar_tensor_tensor(out=sm[:, ESEL], in0=L[:, 4 + 8 * g:12 + 8 * g],
                                                     scalar=sm[:, c(10 + g)], in1=sm[:, ESEL],
                                                     op0=ALU.mult, op1=ALU.add), r=rs, w=[r_sm])
        dv(lambda e: e.reduce_max(out=sm[:, c(M1)], in_=sm[:, ESEL], axis=AX.X), r=[r_sm], w=[r_sm])
        dv(lambda e: e.tensor_scalar(out=sm[:, OH1], in0=sm[:, ESEL], scalar1=sm[:, c(M1)], scalar2=None,
                                     op0=ALU.is_equal), r=[r_sm], w=[r_sm])
        dv(lambda e: e.scalar_tensor_tensor(out=sm[:, MSK], in0=sm[:, OH1], scalar=-1e30, in1=sm[:, ESEL],
                                            op0=ALU.mult, op1=ALU.add), r=[r_sm], w=[r_sm])
        dv(lambda e: e.reduce_max(out=sm[:, c(M2)], in_=sm[:, MSK], axis=AX.X), r=[r_sm], w=[r_sm])
        dv(lambda e: e.tensor_scalar(out=sm[:, OH2], in0=sm[:, MSK], scalar1=sm[:, c(M2)], scalar2=None,
                                     op0=ALU.is_equal), r=[r_sm], w=[r_sm])
        dv(lambda e: e.tensor_tensor(out=sm[:, c(DD)], in0=sm[:, c(M2)], in1=sm[:, c(M1)], op=ALU.subtract),
           r=[r_sm], w=[r_sm])
        k.op("act", lambda e: e.activation(out=sm[:, c(EE)], in_=sm[:, c(DD)], func=AF.Exp), r=[r_sm], w=[r_sm])
        dv(lambda e: e.tensor_scalar_add(out=sm[:, c(W1)], in0=sm[:, c(EE)], scalar1=1.0), r=[r_sm], w=[r_sm])
        dv(lambda e: e.reciprocal(out=sm[:, c(W1)], in_=sm[:, c(W1)]), r=[r_sm], w=[r_sm])
        dv(lambda e: e.tensor_tensor(out=sm[:, c(W2)], in0=sm[:, c(EE)], in1=sm[:, c(W1)], op=ALU.mult),
           r=[r_sm], w=[r_sm])
        dv(lambda e: e.tensor_tensor(out=sm[:, c(W1)], in0=sm[:, c(W1)], in1=sm[:, c(PG)], op=ALU.mult),
           r=[r_sm], w=[r_sm])
        dv(lambda e: e.tensor_tensor(out=sm[:, c(W2)], in0=sm[:, c(W2)], in1=sm[:, c(PG)], op=ALU.mult),
           r=[r_sm], w=[r_sm])
        dv(lambda e: e.tensor_scalar_mul(out=sm[:, INN], in0=sm[:, OH1], scalar1=sm[:, c(W1)]), r=[r_sm], w=[r_sm])
        dv(lambda e: e.scalar_tensor_tensor(out=sm[:, INN], in0=sm[:, OH2], scalar=sm[:, c(W2)], in1=sm[:, INN],
                                            op0=ALU.mult, op1=ALU.add), r=[r_sm], w=[r_sm])
        for g in range(4):
            dv(lambda e, g=g: e.tensor_scalar_mul(out=gates[:, 8 * g:8 * g + 8], in0=sm[:, INN],
                                                  scalar1=sm[:, c(10 + g)]), r=[r_sm], w=[r_gates])

    def rwkv(self, i):
        nc, k, d = self.nc, self.k, self.dram
        j = i // 2
        with ExitStack() as es:
            PCt = es.enter_context(self.sbuf("PCt", [64, 2, NTT, 16, 2], F32))
            r_PC = k.res()
            self.rwkv_A(i, j, PCt, r_PC)
            self.rwkv_B(i, j, PCt, r_PC)

    def rwkv_A(self, i, j, PCt, r_PC):
        nc, k, d = self.nc, self.k, self.dram
        all_hT = [x for y in self.r_hT for x in y]
        sc = self.scr
        with ExitStack() as esA:
            sb = lambda es, n, s, dt=F32: es.enter_context(self.sbuf(n, s, dt))
            h1w = sb(esA, "h1w", [128, S], BF16)
            h1a = sb(esA, "h1a", [128, S], BF16)
            r_h1w, r_h1a = k.res(), k.res()
            muT = sb(esA, "muT", [128, 6, NKC])
            c1 = sb(esA, "c1", [128, 6, NKC])
            c2 = sb(esA, "c2", [128, 6, NKC])
            r_c = k.res()
            k.dma("sp", "rwc", muT[:], d["rw_muT"][j], w=[r_c])
            k.op("dve", lambda e: e.tensor_scalar(out=c1[:], in0=muT[:], scalar1=-1.0, scalar2=1.0, op0=ALU.mult, op1=ALU.add),
                 r=[r_c], w=[r_c])
            k.op("dve", lambda e: e.tensor_scalar_mul(out=c2[:], in0=muT[:], scalar1=0.5), r=[r_c], w=[r_c])

            def scale_w(stg, r_stg, W1, W2, r_W, jm, ncol, col0=0):
                for kc in range(NKC):
                    k.op("act", lambda e, kc=kc: e.mul(out=W1[:, kc, col0:col0 + ncol], in_=stg[:, kc, 0:ncol],
                                                       mul=c1[:, jm, kc:kc + 1]), r=[r_stg, r_c], w=[r_W])
                    k.op("dve", lambda e, kc=kc: e.tensor_scalar_mul(out=W2[:, kc, col0:col0 + ncol], in0=stg[:, kc, 0:ncol],
                                                                     scalar1=c2[:, jm, kc:kc + 1]), r=[r_stg, r_c], w=[r_W])

            with ExitStack() as es12:
                hsT = sb(es12, "hsT", [128, NKC, S], BF16)
                r_hs = k.res()
                for kc in range(NKC):
                    k.op("dve", lambda e, kc=kc: e.tensor_tensor(out=hsT[:, kc, 1:S - 1], in0=self.hT[:, kc, 0:S - 2],
                                                                 in1=self.hT[:, kc, 2:S], op=ALU.add), r=all_hT, w=[r_hs])
                    k.op("act", lambda e, kc=kc: e.copy(out=hsT[:, kc, 0:1], in_=self.hT[:, kc, 1:2]), r=all_hT, w=[r_hs])
                    k.op("act", lambda e, kc=kc: e.copy(out=hsT[:, kc, S - 1:S], in_=self.hT[:, kc, S - 2:S - 1]), r=all_hT, w=[r_hs])
                with ExitStack() as es1:
                    h1g = [sb(es1, f"h1g{dd}", [128, S], BF16) for dd in range(2)]
                    r_h1g = [k.res() for _ in range(2)]
                    stg = [sb(es1, f"lstg{b}", [128, NKC, 128]) for b in range(2)]
                    r_stg = [k.res() for _ in range(2)]
                    W1 = [sb(es1, f"lW1_{b}", [128, NKC, 128], BF16) for b in range(2)]
                    W2 = [sb(es1, f"lW2_{b}", [128, NKC, 128], BF16) for b in range(2)]
                    r_W = [k.res() for _ in range(2)]
                    groups = [(d["rw_w1cat"][j], 3, AF.Tanh, h1w, r_h1w), (d["rw_a1cat"][j], 4, AF.Copy, h1a, r_h1a),
                              (d["rw_g1"][j, 0], 5, AF.Sigmoid, h1g[0], r_h1g[0]), (d["rw_g1"][j, 1], 5, AF.Sigmoid, h1g[1], r_h1g[1])]
                    for gi, (src, jm, fn, dst, r_dst) in enumerate(groups):
                        b = gi % 2
                        k.dma("sp", f"lstg{b}", stg[b][:], src.rearrange("(kc p) n -> p kc n", p=128), w=[r_stg[b]])
                        scale_w(stg[b], r_stg[b], W1[b], W2[b], r_W[b], jm, 128)
                        for tb in range(NTB):
                            tsl = slice(tb * 512, (tb + 1) * 512)
                            pi = tb % 2
                            for kc in range(NKC):
                                k.op("pe", lambda e, kc=kc: e.matmul(self.ps[pi][:, :], W1[b][:, kc, :], self.hT[:, kc, tsl],
                                                                     start=(kc == 0), stop=False),
                                     r=[r_W[b], self.r_hT[kc][tb]], w=[self.r_ps[pi]])
                            for kc in range(NKC):
                                k.op("pe", lambda e, kc=kc: e.matmul(self.ps[pi][:, :], W2[b][:, kc, :], hsT[:, kc, tsl],
                                                                     start=False, stop=(kc == NKC - 1)),
                                     r=[r_W[b], r_hs], w=[self.r_ps[pi]])
                            k.op("act", lambda e: e.activation(out=dst[:, tsl], in_=self.ps[pi][:, :], func=fn),
                                 r=[self.r_ps[pi]], w=[r_dst])
                    g2 = [sb(es1, f"g2_{dd}", [128, D], BF16) for dd in range(2)]
                    r_g2 = k.res()
                    for dd in range(2):
                        k.dma("pool", "g2", g2[dd][:], d["rw_g2"][j, dd], w=[r_g2])
                    gst = [sb(es1, f"gst{b}", [128, D]) for b in range(2)]
                    r_gst = [k.res() for _ in range(2)]
                    n = 0
                    for dd in range(2):
                        for tt in range(NTT):
                            b = n % 2
                            n += 1
                            for half in range(2):
                                pi = 2 + half
                                k.op("pe", lambda e, half=half, pi=pi: e.matmul(
                                    self.ps[pi][:, :], h1g[dd][:, tt * 128:(tt + 1) * 128], g2[dd][:, half * 512:(half + 1) * 512],
                                    start=True, stop=True), r=[r_h1g[dd], r_g2], w=[self.r_ps[pi]])
                                if half == 0:
                                    k.op("act", lambda e, pi=pi: e.copy(out=gst[b][:, 0:512], in_=self.ps[pi][:, :]),
                                         r=[self.r_ps[pi]], w=[r_gst[b]])
                                else:
                                    k.op("dve", lambda e, pi=pi: e.tensor_copy(out=gst[b][:, 512:1024], in_=self.ps[pi][:, :]),
                                         r=[self.r_ps[pi]], w=[r_gst[b]])
                            k.dma("sp", f"gst{b}", sc["GG"][dd, tt * 128:(tt + 1) * 128, :], gst[b][:], r=[r_gst[b]],
                                  w=[self.r_scr["GG"][dd][tt]])
                    k.barrier()
                with ExitStack() as es2:
                    stg = [sb(es2, f"pstg{b}", [128, NKC, 256]) for b in range(2)]
                    r_stg = [k.res() for _ in range(2)]
                    W1 = sb(es2, "pW1", [128, NKC, D], BF16)
                    W2 = sb(es2, "pW2", [128, NKC, D], BF16)
                    r_W = k.res()
                    rst = [sb(es2, f"rst{b}", [128, D]) for b in range(2)]
                    rstb = [rst[b][:, :].bitcast(BF16)[:, 0:D] for b in range(2)]
                    r_rst = [k.res() for _ in range(2)]
                    n = 0
                    for pj in range(3):
                        for qq in range(4):
                            sb_ = qq % 2
                            k.dma("sp", f"pstg{sb_}", stg[sb_][:],
                                  d["rw_w_rkv"][j, pj, :, qq * 256:(qq + 1) * 256].rearrange("(kc p) n -> p kc n", p=128),
                                  w=[r_stg[sb_]])
                            scale_w(stg[sb_], r_stg[sb_], W1, W2, r_W, pj, 256, col0=qq * 256)
                        for tt in range(NTT):
                            b = n % 2
                            n += 1
                            tb = tt // 4
                            tsl = slice(tt * 128, (tt + 1) * 128)
                            for half in range(2):
                                pi = half
                                for kc in range(NKC):
                                    k.op("pe", lambda e, kc=kc, half=half, pi=pi: e.matmul(
                                        self.ps[pi][:, :], self.hT[:, kc, tsl], W1[:, kc, half * 512:(half + 1) * 512],
                                        start=(kc == 0), stop=False), r=[r_W, self.r_hT[kc][tb]], w=[self.r_ps[pi]])
                                for kc in range(NKC):
                                    k.op("pe", lambda e, kc=kc, half=half, pi=pi: e.matmul(
                                        self.ps[pi][:, :], hsT[:, kc, tsl], W2[:, kc, half * 512:(half + 1) * 512],
                                        start=False, stop=(kc == NKC - 1)), r=[r_W, r_hs], w=[self.r_ps[pi]])
                                dst_t = rst[b] if pj < 2 else rstb[b]
                                if half == 0:
                                    k.op("act", lambda e, pi=pi: e.copy(out=dst_t[:, 0:512], in_=self.ps[pi][:, :]),
                                         r=[self.r_ps[pi]], w=[r_rst[b]])
                                else:
                                    k.op("dve", lambda e, pi=pi: e.tensor_copy(out=dst_t[:, 512:1024], in_=self.ps[pi][:, :]),
                                         r=[self.r_ps[pi]], w=[r_rst[b]])
                            if pj < 2:
                                k.dma("sp", f"rst{b}", sc["RAW"][pj, tt * 128:(tt + 1) * 128, :], rst[b][:], r=[r_rst[b]],
                                      w=[self.r_scr["RAW"][pj][tt]])
                            else:
                                k.dma("sp", f"rst{b}", sc["RAWV"][tt * 128:(tt + 1) * 128, :], rstb[b], r=[r_rst[b]],
                                      w=[self.r_scr["RAW"][pj][tt]])
                    k.barrier()
            with ExitStack() as es3:
                w2c = sb(es3, "w2c", [128, D], BF16)
                a2c = sb(es3, "a2c", [128, D], BF16)
                r_l2 = k.res()
                k.dma("pool", "l2w", w2c[:], d["rw_w2cat"][j], w=[r_l2])
                k.dma("pool", "l2w", a2c[:], d["rw_a2cat"][j], w=[r_l2])
                KKb = sb(es3, "KKb", [128, D]); KAb = sb(es3, "KAb", [128, D]); RKb = sb(es3, "RKb", [128, D])
                r_par = k.res()
                k.dma("sp", "rwp", KKb[:], d["rw_k_k"][j:j + 1, :].partition_broadcast(128), w=[r_par])
                k.dma("sp", "rwp", KAb[:], d["rw_k_a"][j:j + 1, :].partition_broadcast(128), w=[r_par])
                k.dma("sp", "rwp", RKb[:], d["rw_r_k"][j:j + 1, :].partition_broadcast(128), w=[r_par])
                b32 = sb(es3, "b32", [1, 4, D])
                r_b = k.res()
                k.dma("sp", "rwp2", b32[0:1, 0:2, :], d["rw_w0"][j:j + 1, :, :], w=[r_b])
                k.dma("sp", "rwp2", b32[0:1, 2:4, :], d["rw_a0"][j:j + 1, :, :], w=[r_b])
                tri = sb(es3, "tri", [128, 6, 128])
                csel = sb(es3, "csel", [128, 2])
                k.dma("sp", "rwp", tri[:], d["c_tri"].rearrange("q s t -> s q t"), w=[r_par])
                k.dma("sp", "rwp", csel[:], d["c_csel"][:, :], w=[r_par])
                hsc = [self.hT[:, kc, :].bitcast(F32) for kc in range(NKC)]
                mk2 = lambda n_, extra: [sb(es3, f"{n_}{q}", [128, D]) if extra[q] is None else extra[q] for q in range(2)]
                Rr_s = mk2("Rr", [None, hsc[0]]); Rk_s = mk2("Rk", [None, hsc[1]]); kk_s = mk2("kk", [None, hsc[2]])
                RRK_s = mk2("RRK", [None, hsc[3]]); T0_s = mk2("T0", [None, hsc[4]])
                SIG_s = mk2("SIG", [None, hsc[5]]); A_s = mk2("A_", [None, hsc[6]]); KD_s = mk2("KD", [None, hsc[7]])
                E1_s = mk2("E1", [None, None]); E2_s = mk2("E2", [None, None])
                O = [sb(es3, f"O{b}", [128, D], BF16) for b in range(2)]
                FMst = [sb(es3, f"FMst{b}", [64, 8, 128], BF16) for b in range(2)]
                identb = sb(es3, "identb3", [128, 128], BF16)
                r_idb = k.res()
                k.op("act", lambda e: e.copy(out=identb[:], in_=self.ident[:]), r=[self.r_const], w=[r_idb])
                psb = {4: self.ps[4][0:64, :].bitcast(BF16), 5: self.ps[5][0:64, :].bitcast(BF16)}
                sm = sb(es3, "a3sm", [128, 64])
                rkt_s = [sb(es3, f"rkt{q}", [128, 16]) for q in range(2)]
                r_rkt_s = [k.res(), k.res()]
                r2 = lambda: [k.res(), k.res()]
                r_Rr_s, r_Rk_s, r_kk_s, r_RRK_s, r_T0_s = r2(), r2(), r2(), r2(), r2()
                r_SIG_s, r_A_s, r_KD_s, r_E1_s, r_E2_s = r2(), r2(), r2(), r2(), r2()
                r_sm = k.res()
                r_O = [k.res() for _ in range(2)]
                r_FM = [k.res() for _ in range(2)]
                on = [0]
                v3 = lambda t: t[:, :].rearrange("p (h n) -> p h n", h=16)

                fmn = [0]

                def emit_fm(src, r_src, dd, q, tt):
                    on[0] += 1
                    for h8 in range(2):
                        fb = fmn[0] % 2
                        fmn[0] += 1
                        for h4 in range(2):
                            pi = 4 + h4 % 2
                            for hh in range(4):
                                h = h8 * 8 + h4 * 4 + hh
                                k.op("pe", lambda e, h=h, hh=hh, pi=pi: e.transpose(
                                    psb[pi][:, hh * 128:(hh + 1) * 128], src[:, h * 64:(h + 1) * 64], identb[:]),
                                    r=[r_src, r_idb], w=[self.r_ps[pi]])
                            if h4 == 0:
                                k.op("act", lambda e, h4=h4, pi=pi: e.copy(
                                    out=FMst[fb][:, h4 * 4:h4 * 4 + 4, :], in_=psb[pi][:, 0:512].rearrange("p (a t) -> p a t", a=4)),
                                    r=[self.r_ps[pi]], w=[r_FM[fb]])
                            else:
                                k.op("dve", lambda e, h4=h4, pi=pi: e.tensor_copy(
                                    out=FMst[fb][:, h4 * 4:h4 * 4 + 4, :], in_=psb[pi][:, 0:512].rearrange("p (a t) -> p a t", a=4)),
                                    r=[self.r_ps[pi]], w=[r_FM[fb]])
                        k.dma("sp", f"FMst{fb}", sc["FM"][dd, q, tt, :, h8 * 8:(h8 + 1) * 8, :], FMst[fb][:], r=[r_FM[fb]],
                              w=[self.r_scr["FM"][dd][q][tt]])

                def a3_load(tt):
                    q = tt % 2
                    rws = slice(tt * 128, (tt + 1) * 128)
                    k.dma("pool", f"a3r{q}", Rr_s[q][:], sc["RAW"][0, rws, :], r=[self.r_scr["RAW"][0][tt]], w=[r_Rr_s[q]])
                    k.dma("pool", f"a3k{q}", Rk_s[q][:], sc["RAW"][1, rws, :], r=[self.r_scr["RAW"][1][tt]], w=[r_Rk_s[q]])

                a3_load(0)
                for tt in range(NTT):
                    rows = slice(tt * 128, (tt + 1) * 128)
                    if tt + 1 < NTT:
                        a3_load(tt + 1)
                    q_ = tt % 2
                    Rr, Rk, kk, RRK = Rr_s[q_], Rk_s[q_], kk_s[q_], RRK_s[q_]
                    r_Rr, r_Rk, r_kk, r_RRK = r_Rr_s[q_], r_Rk_s[q_], r_kk_s[q_], r_RRK_s[q_]
                    T0, r_T0 = T0_s[0], r_T0_s[0]
                    k.op("dve", lambda e: e.tensor_tensor(out=kk[:], in0=Rk[:], in1=KKb[:], op=ALU.mult), r=[r_Rk, r_par], w=[r_kk])
                    k.op("dve", lambda e: e.tensor_tensor(out=T0[:], in0=kk[:], in1=kk[:], op=ALU.mult), r=[r_kk], w=[r_T0])
                    k.op("dve", lambda e: e.reduce_sum(out=sm[:, 0:16], in_=v3(T0), axis=AX.X), r=[r_T0], w=[r_sm])
                    k.op("act", lambda e: e.activation(out=sm[:, 0:16], in_=sm[:, 0:16], func=AF.Sqrt), r=[r_sm], w=[r_sm])
                    k.op("dve", lambda e: e.tensor_scalar_max(out=sm[:, 0:16], in0=sm[:, 0:16], scalar1=1e-12), r=[r_sm], w=[r_sm])
                    k.op("dve", lambda e: e.reciprocal(out=sm[:, 16:32], in_=sm[:, 0:16]), r=[r_sm], w=[r_sm])
                    k.op("dve", lambda e: e.tensor_tensor(out=v3(kk), in0=v3(kk),
                                                          in1=sm[:, 16:32].unsqueeze(2).to_broadcast([128, 16, 64]), op=ALU.mult),
                         r=[r_kk, r_sm], w=[r_kk])
                    k.op("dve", lambda e: e.tensor_tensor(out=RRK[:], in0=Rr[:], in1=RKb[:], op=ALU.mult), r=[r_Rr, r_par], w=[r_RRK])
                    for dd in range(2):
                        T0, SIG, A_, KD, E1, E2 = T0_s[dd], SIG_s[dd], A_s[dd], KD_s[dd], E1_s[dd], E2_s[dd]
                        r_T0, r_SIG, r_A, r_KD, r_E1, r_E2 = r_T0_s[dd], r_SIG_s[dd], r_A_s[dd], r_KD_s[dd], r_E1_s[dd], r_E2_s[dd]
                        for (h1, r_h1, w2t, bi, dst, r_dst) in ((h1w, r_h1w, w2c, dd, SIG, r_SIG), (h1a, r_h1a, a2c, 2 + dd, A_, r_A)):
                            for half in range(2):
                                pi = half
                                csl = slice(half * 512, (half + 1) * 512)
                                k.op("pe", lambda e: e.matmul(self.ps[pi][:, :], h1[dd * 64:(dd + 1) * 64, rows],
                                                              w2t[dd * 64:(dd + 1) * 64, csl], start=True, stop=False),
                                     r=[r_h1, r_l2], w=[self.r_ps[pi]])
                                k.op("pe", lambda e: e.matmul(self.ps[pi][:, :], self.ones[0:1, :], b32[0:1, bi, csl],
                                                              start=False, stop=True),
                                     r=[r_b, self.r_const], w=[self.r_ps[pi]])
                                k.op("act", lambda e: e.activation(out=dst[:, csl], in_=self.ps[pi][:, :], func=AF.Sigmoid),
                                     r=[self.r_ps[pi]], w=[r_dst])
                        k.op("dve", lambda e: e.scalar_tensor_tensor(out=KD[:], in0=A_[:], scalar=-1.0, in1=KAb[:],
                                                                     op0=ALU.add, op1=ALU.mult), r=[r_A, r_par], w=[r_KD])
                        k.op("dve", lambda e: e.scalar_tensor_tensor(out=KD[:], in0=KD[:], scalar=1.0, in1=Rk[:],
                                                                     op0=ALU.add, op1=ALU.mult), r=[r_KD, r_Rk], w=[r_KD])
                        k.op("dve", lambda e: e.tensor_tensor(out=A_[:], in0=A_[:], in1=kk[:], op=ALU.mult), r=[r_A, r_kk], w=[r_A])
                        k.op("dve", lambda e: e.tensor_tensor(out=T0[:], in0=RRK[:], in1=KD[:], op=ALU.mult), r=[r_RRK, r_KD], w=[r_T0])
                        rkt, r_rkt = rkt_s[dd], r_rkt_s[dd]
                        k.op("dve", lambda e: e.reduce_sum(out=rkt[:, :], in_=v3(T0), axis=AX.X), r=[r_T0], w=[r_rkt])
                        k.dma("sp", f"rkt{dd}", sc["RK"][dd, rows, :], rkt[:], r=[r_rkt], w=[self.r_scr["RK"][dd][tt]])
                        for h in range(16):
                            k.op("pe", lambda e, h=h: e.matmul(self.ps[6][0:64, h * 2:h * 2 + 2], SIG[:, h * 64:(h + 1) * 64], csel[:, :],
                                                               start=True, stop=True), r=[r_SIG, r_par], w=[self.r_ps[6]])
                        k.op("act", lambda e: e.activation(out=PCt[:, dd, tt, :, :], in_=self.ps[6][0:64, 0:32].rearrange("p (h c) -> p h c", c=2),
                                                           func=AF.Exp), r=[self.r_ps[6]], w=[r_PC])
                        def cums(kind, outs):
                            for half in range(2):
                                pi = 2 + half
                                csl = slice(half * 512, (half + 1) * 512)
                                k.op("pe", lambda e: e.matmul(self.ps[pi][:, :], tri[:, dd * 3 + kind, :], SIG[:, csl],
                                                              start=True, stop=True), r=[r_SIG, r_par], w=[self.r_ps[pi]])
                                for (dst, r_dst, scl) in outs:
                                    k.op("act", lambda e, dst=dst, scl=scl: e.activation(out=dst[:, csl], in_=self.ps[pi][:, :],
                                                                                         func=AF.Exp, scale=scl),
                                         r=[self.r_ps[pi]], w=[r_dst])
                        cums(0, [(E1, r_E1, 1.0), (E2, r_E2, -1.0)])
                        ob = on[0] % 2
                        k.op("dve", lambda e: e.tensor_tensor(out=O[ob][:], in0=Rr[:], in1=E1[:], op=ALU.mult), r=[r_Rr, r_E1], w=[r_O[ob]])
                        emit_fm(O[ob], r_O[ob], dd, 0, tt)
                        ob = on[0] % 2
                        k.op("dve", lambda e: e.tensor_tensor(out=O[ob][:], in0=A_[:], in1=E2[:], op=ALU.mult), r=[r_A, r_E2], w=[r_O[ob]])
                        emit_fm(O[ob], r_O[ob], dd, 2, tt)
                        ob = on[0] % 2
                        k.op("dve", lambda e: e.tensor_tensor(out=O[ob][:], in0=KD[:], in1=E2[:], op=ALU.mult), r=[r_KD, r_E2], w=[r_O[ob]])
                        emit_fm(O[ob], r_O[ob], dd, 3, tt)
                        cums(1, [(E1, r_E1, 1.0)])
                        ob = on[0] % 2
                        k.op("dve", lambda e: e.scalar_tensor_tensor(out=O[ob][:], in0=kk[:], scalar=-1.0, in1=E1[:],
                                                                     op0=ALU.mult, op1=ALU.mult), r=[r_kk, r_E1], w=[r_O[ob]])
                        emit_fm(O[ob], r_O[ob], dd, 1, tt)
                        cums(2, [(E2, r_E2, 1.0)])
                        for q, (src, r_src) in enumerate(((A_, r_A), (KD, r_KD))):
                            ob = on[0] % 2
                            on[0] += 1
                            k.op("dve" if q == 0 else "pool", lambda e, src=src: e.tensor_tensor(out=O[ob][:], in0=src[:], in1=E2[:], op=ALU.mult),
                                 r=[r_src, r_E2], w=[r_O[ob]])
                            k.dma("sp", f"Otm{ob}", sc["TM"][dd, q, rows, :], O[ob][:], r=[r_O[ob]], w=[self.r_scr["TM"][dd][q][tt]])
                k.barrier()

    def rwkv_B(self, i, j, PCt, r_PC):
        nc, k, d = self.nc, self.k, self.dram
        sc = self.scr
        oT = self.hT
        r_oT = self.r_hT
        NH = 8
        with ExitStack() as es:
            sb = lambda n, s_, dt=F32: es.enter_context(self.sbuf(n, s_, dt))
            for kc in range(NKC):
                k.op("pool", lambda e, kc=kc: e.memset(oT[:, kc, :], 0.0), w=r_oT[kc])
            msk = sb("msk", [64, 6, 64])
            LNW = sb("LNW", [64, D]); LNB = sb("LNB", [64, D])
            epsg = sb("epsg", [64, 1])
            r_cb = k.res()
            k.dma("sp", "rbc", msk[:], d["c_mask"].rearrange("q s t -> s q t"), w=[r_cb])
            k.dma("sp", "rbc", LNW[:], d["rw_ln_w"][j:j + 1, :].partition_broadcast(64), w=[r_cb])
            k.dma("sp", "rbc", LNB[:], d["rw_ln_b"][j:j + 1, :].partition_broadcast(64), w=[r_cb])
            k.op("dve", lambda e: e.memset(epsg[:], 64e-5), w=[r_cb])
            i64 = self.ident[0:64, 0:64]
            bcm = lambda q: msk[:, q, :].unsqueeze(1).to_broadcast([64, NH, 64])
            bci = i64.unsqueeze(1).to_broadcast([64, NH, 64])

            class Chain:
                pass
            chains = []
            for ci, (dd, hg) in enumerate(((0, 0), (1, 0), (0, 1), (1, 1))):
                c = Chain()
                c.dd, c.hg = dd, hg
                nm = f"c{ci}"
                c.nm = nm
                c.ld = {n_: sb(f"{nm}_{n_}", [64, NH, 64], BF16) for n_ in ("RT", "AT", "BT", "KT", "Bh", "Kh", "Vt")}
                c.ld["Gt"] = sb(f"{nm}_Gt", [64, NH, 64])
                c.r_ld = {n_: k.res() for n_ in c.ld}
                c.rk = sb(f"{nm}_rk", [64, NH]); c.r_rk = k.res()
                c.sl = [sb(f"{nm}_s{q}", [64, NH, 64], BF16) for q in range(6)]
                c.r_sl = [k.res() for _ in range(6)]
                c.ST = sb(f"{nm}_ST", [64, NH, 64]); c.r_ST = k.res()
                c.STb = sb(f"{nm}_STb", [64, NH, 64], BF16); c.r_STb = k.res()
                c.y = sb(f"{nm}_y", [64, NH, 64]); c.r_y = k.res()
                c.sq = sb(f"{nm}_sq", [64, NH, 64]); c.r_sq = k.res()
                c.sm = sb(f"{nm}_sm", [64, 8 * NH]); c.r_sm = k.res()
                c.pb = [ci * 2, ci * 2 + 1]
                c.pbi = 0
                chains.append(c)

            def p3(pi):
                return self.ps[pi][0:64, :].rearrange("p (h n) -> p h n", h=NH)

            def mm(c, pi, pairs, rs):
                for h in range(NH):
                    for q, (lt, rt) in enumerate(pairs):
                        k.op("pe", lambda e, h=h, lt=lt, rt=rt, q=q: e.matmul(
                            self.ps[pi][0:64, h * 64:(h + 1) * 64], lt[:, h, :], rt[:, h, :],
                            start=(q == 0), stop=(q == len(pairs) - 1)), r=rs, w=[self.r_ps[pi]])

            def nb(c):
                c.pbi = (c.pbi + 1) % 2
                return c.pb[c.pbi]

            def chunk_steps(c, n):
                dd, hg = c.dd, c.hg
                ct = n if dd == 0 else 31 - n
                tt, half = ct // 2, ct % 2
                rows = slice(ct * 64, (ct + 1) * 64)
                hs = slice(hg * NH, (hg + 1) * NH)
                cs = slice(hg * 512, (hg + 1) * 512)
                L, R = c.ld, c.r_ld
                for q, n_ in enumerate(("RT", "AT", "BT", "KT")):
                    k.dma("sp", f"{c.nm}{n_}", L[n_][:], sc["FM"][dd, q, tt, :, hs, half * 64:(half + 1) * 64],
                          r=[self.r_scr["FM"][dd][q][tt]], w=[R[n_]])
                for q, n_ in enumerate(("Bh", "Kh")):
                    k.dma("sp", f"{c.nm}{n_}", L[n_][:].rearrange("p h n -> p (h n)"), sc["TM"][dd, q, rows, cs],
                          r=[self.r_scr["TM"][dd][q][ct // 2]], w=[R[n_]])
                yield
                P1, P1T, P2, P2T, T, U6 = range(6)
                S_, RS = c.sl, c.r_sl
                pi = nb(c)
                mm(c, pi, [(L["BT"], L["AT"])], [R["BT"], R["AT"]])
                k.op("dve", lambda e: e.tensor_tensor(out=S_[P1][:], in0=p3(pi), in1=bcm(dd * 3 + 0), op=ALU.mult),
                     r=[self.r_ps[pi], r_cb], w=[RS[P1]])
                yield
                pi = nb(c)
                mm(c, pi, [(L["AT"], L["BT"])], [R["BT"], R["AT"]])
                k.op("dve", lambda e: e.tensor_tensor(out=S_[P1T][:], in0=p3(pi), in1=bcm(dd * 3 + 1), op=ALU.mult),
                     r=[self.r_ps[pi], r_cb], w=[RS[P1T]])
                k.op("dve", lambda e: e.tensor_tensor(out=S_[T][:], in0=S_[P1][:], in1=bci, op=ALU.add),
                     r=[RS[P1], self.r_const], w=[RS[T]])
                yield
                a, aT, b_, bT = P1, P1T, P2, P2T
                for lvl in range(5):
                    pi = nb(c)
                    mm(c, pi, [(S_[a], S_[aT])], [RS[a], RS[aT]])
                    k.op("act", lambda e, pi=pi, bT=bT: e.copy(out=S_[bT][:], in_=p3(pi)), r=[self.r_ps[pi]], w=[RS[bT]])
                    if lvl < 4:
                        pi2 = nb(c)
                        mm(c, pi2, [(S_[aT], S_[a])], [RS[a], RS[aT]])
                        k.op("act", lambda e, pi2=pi2, b_=b_: e.copy(out=S_[b_][:], in_=p3(pi2)), r=[self.r_ps[pi2]], w=[RS[b_]])
                    yield
                    pi = nb(c)
                    mm(c, pi, [(S_[bT], S_[T])], [RS[bT], RS[T]])
                    k.op("dve", lambda e, pi=pi: e.tensor_tensor(out=S_[T][:], in0=S_[T][:], in1=p3(pi), op=ALU.add),
                         r=[self.r_ps[pi], RS[T]], w=[RS[T]])
                    yield
                    a, aT, b_, bT = b_, bT, a, aT
                Aak, Abr, Akr, WT = P1, P1T, P2, P2T
                for (dst, lt, rt, mq, eng) in ((Aak, "KT", "AT", 0, "dve"), (Abr, "BT", "RT", 2, "pool"), (Akr, "KT", "RT", 2, "dve")):
                    pi = nb(c)
                    mm(c, pi, [(L[lt], L[rt])], [R[lt], R[rt]])
                    k.op("dve", lambda e, pi=pi, dst=dst, mq=mq: e.tensor_tensor(out=S_[dst][:], in0=p3(pi), in1=bcm(dd * 3 + mq), op=ALU.mult),
                         r=[self.r_ps[pi], r_cb], w=[RS[dst]])
                    yield
                k.dma("pool", f"{c.nm}Vt", L["Vt"][:].rearrange("p h n -> p (h n)"), sc["RAWV"][rows, cs],
                      r=[self.r_scr["RAW"][2][ct // 2]], w=[R["Vt"]])
                k.dma("pool", f"{c.nm}Gt", L["Gt"][:].rearrange("p h n -> p (h n)"), sc["GG"][dd, rows, cs],
                      r=[self.r_scr["GG"][dd][ct // 2]], w=[R["Gt"]])
                k.dma("pool", f"{c.nm}rk", c.rk[:], sc["RK"][dd, rows, hs], r=[self.r_scr["RK"][dd][ct // 2]], w=[c.r_rk])
                pi = nb(c)
                mm(c, pi, [(L["AT"], c.STb), (S_[Aak], L["Vt"])], [R["AT"], c.r_STb, RS[Aak], R["Vt"]])
                k.op("act", lambda e: e.copy(out=S_[WT][:], in_=p3(pi)), r=[self.r_ps[pi]], w=[RS[WT]])
                yield
                pi = nb(c)
                mm(c, pi, [(S_[T], S_[WT])], [RS[T], RS[WT]])
                k.op("act", lambda e: e.copy(out=S_[U6][:], in_=p3(pi)), r=[self.r_ps[pi]], w=[RS[U6]])
                yield
                piy = nb(c)
                mm(c, piy, [(L["RT"], c.STb), (S_[Abr], S_[U6]), (S_[Akr], L["Vt"])],
                   [R["RT"], c.r_STb, RS[Abr], RS[U6], RS[Akr], R["Vt"]])
                k.op("act", lambda e: e.copy(out=c.y[:], in_=p3(piy)), r=[self.r_ps[piy]], w=[c.r_y])
                pis = nb(c)
                mm(c, pis, [(L["Bh"], S_[U6]), (L["Kh"], L["Vt"])], [R["Bh"], RS[U6], R["Kh"], R["Vt"]])
                pcb = PCt[:, dd, tt, hs, half].unsqueeze(2).to_broadcast([64, NH, 64])
                k.op("dve", lambda e: e.tensor_tensor(out=c.ST[:], in0=c.ST[:], in1=pcb, op=ALU.mult), r=[c.r_ST, r_PC], w=[c.r_ST])
                k.op("dve", lambda e: e.tensor_tensor(out=c.ST[:], in0=c.ST[:], in1=p3(pis), op=ALU.add), r=[c.r_ST, self.r_ps[pis]], w=[c.r_ST])
                k.op("act", lambda e: e.copy(out=c.STb[:], in_=c.ST[:]), r=[c.r_ST], w=[c.r_STb])
                yield

            def epi_steps(c, n):
                dd, hg = c.dd, c.hg
                ct = n if dd == 0 else 31 - n
                cs = slice(hg * 512, (hg + 1) * 512)
                L, R = c.ld, c.r_ld
                sm, r_sm = c.sm, c.r_sm
                bl = lambda a0: sm[:, a0:a0 + NH].unsqueeze(2).to_broadcast([64, NH, 64])
                dv = lambda fn, r, w: k.op("dve", fn, r=r, w=w)
                dv(lambda e: e.reduce_sum(out=sm[:, 0:NH], in_=c.y[:], axis=AX.X), [c.r_y], [r_sm])
                k.op("act", lambda e: e.activation(out=c.sq[:], in_=c.y[:], func=AF.Square), r=[c.r_y], w=[c.r_sq])
                yield
                dv(lambda e: e.reduce_sum(out=sm[:, NH:2 * NH], in_=c.sq[:], axis=AX.X), [c.r_sq], [r_sm])
                dv(lambda e: e.tensor_scalar_mul(out=sm[:, 0:2 * NH], in0=sm[:, 0:2 * NH], scalar1=1.0 / 64), [r_sm], [r_sm])
                yield
                dv(lambda e: e.tensor_tensor(out=sm[:, 2 * NH:3 * NH], in0=sm[:, 0:NH], in1=sm[:, 0:NH], op=ALU.mult), [r_sm], [r_sm])
                dv(lambda e: e.tensor_tensor(out=sm[:, 3 * NH:4 * NH], in0=sm[:, NH:2 * NH], in1=sm[:, 2 * NH:3 * NH], op=ALU.subtract), [r_sm], [r_sm])
                k.op("act", lambda e: e.activation(out=sm[:, 4 * NH:5 * NH], in_=sm[:, 3 * NH:4 * NH], func=AF.Sqrt, bias=epsg[:, 0:1], scale=1.0),
                     r=[r_sm, r_cb], w=[r_sm])
                dv(lambda e: e.reciprocal(out=sm[:, 5 * NH:6 * NH], in_=sm[:, 4 * NH:5 * NH]), [r_sm], [r_sm])
                yield
                z, r_z = c.y, c.r_y
                dv(lambda e: e.tensor_tensor(out=z[:], in0=c.y[:], in1=bl(0), op=ALU.subtract), [c.r_y, r_sm], [r_z])
                yield
                dv(lambda e: e.tensor_tensor(out=z[:], in0=z[:], in1=bl(5 * NH), op=ALU.mult), [r_z, r_sm], [r_z])
                yield
                zf = z[:].rearrange("p h n -> p (h n)")
                k.op("dve", lambda e: e.tensor_tensor(out=zf, in0=zf, in1=LNW[:, cs], op=ALU.mult), r=[r_z, r_cb], w=[r_z])
                yield
                k.op("dve", lambda e: e.tensor_tensor(out=zf, in0=zf, in1=LNB[:, cs], op=ALU.add), r=[r_z, r_cb], w=[r_z])
                yield
                dv(lambda e: e.tensor_tensor(out=c.sq[:], in0=L["Vt"][:], in1=c.rk[:, :].unsqueeze(2).to_broadcast([64, NH, 64]), op=ALU.mult),
                   [R["Vt"], c.r_rk], [c.r_sq])
                yield
                k.op("dve", lambda e: e.tensor_tensor(out=z[:], in0=z[:], in1=c.sq[:], op=ALU.add), r=[r_z, c.r_sq], w=[r_z])
                yield
                k.op("dve", lambda e: e.tensor_tensor(out=z[:], in0=z[:], in1=L["Gt"][:], op=ALU.mult), r=[r_z, R["Gt"]], w=[r_z])
                yield
                pt = nb(c)
                for q in range(4):
                    k.op("pe", lambda e, q=q: e.transpose(self.ps[pt][:, q * 64:(q + 1) * 64], zf[:, q * 128:(q + 1) * 128], i64),
                         r=[r_z, self.r_const], w=[self.r_ps[pt]])
                tb = ct // 8
                osl = oT[:, hg * 4:hg * 4 + 4, ct * 64:(ct + 1) * 64]
                k.op("dve", lambda e: e.tensor_tensor(out=osl, in0=osl, in1=self.ps[pt][:, 0:256].rearrange("p (q t) -> p q t", q=4), op=ALU.add),
                     r=[self.r_ps[pt]] + [r_oT[hg * 4 + q][tb] for q in range(4)], w=[r_oT[hg * 4 + q][tb] for q in range(4)])
                yield

            nchunks = self.cfg.get("rw_chunks", 32)
            for c in chains:
                k.op("pool", lambda e, c=c: e.memset(c.ST[:], 0.0), w=[c.r_ST])
                k.op("pool", lambda e, c=c: e.memset(c.STb[:], 0.0), w=[c.r_STb])
            for n in range(nchunks + 1):
                live = []
                for c in chains:
                    if n < nchunks:
                        live.append(chunk_steps(c, n))
                    if n >= 1:
                        live.append(epi_steps(c, n - 1))
                while live:
                    for g_ in list(live):
                        try:
                            next(g_)
                        except StopIteration:
                            live.remove(g_)
            k.barrier()
        with ExitStack() as es:
            wo = es.enter_context(self.sbuf("rwo", [128, NKC, D], BF16))
            r_wo = k.res()
            k.dma("pool", "rwo", wo[:], d["rw_w_o"][j].rearrange("(kc p) n -> p kc n", p=128), w=[r_wo])
            for tb in range(NTB):
                tsl = slice(tb * 512, (tb + 1) * 512)
                for dc in range(NKC):
                    pi = dc % 2
                    for kc in range(NKC):
                        k.op("pe", lambda e, kc=kc, dc=dc, pi=pi: e.matmul(self.ps[pi][:, :], wo[:, kc, dc * 128:(dc + 1) * 128], oT[:, kc, tsl],
                                                                           start=(kc == 0), stop=(kc == NKC - 1)),
                             r=[r_wo, r_oT[kc][tb]], w=[self.r_ps[pi]])
                    k.op("dve", lambda e, dc=dc, pi=pi: e.scalar_tensor_tensor(
                        out=self.xT[:, dc, tsl], in0=self.ps[pi][:, :], scalar=self.modT[:, i, 16 + dc:17 + dc],
                        in1=self.xT[:, dc, tsl], op0=ALU.mult, op1=ALU.add),
                        r=[self.r_ps[pi], self.r_mod], w=[self.r_xT[dc][tb]])
            k.barrier()

    def attn(self, i):
        nc, k, d = self.nc, self.k, self.dram
        j = i // 2
        DIL = [1, 4, 16]
        ss = lambda start, n, step: slice(start, start + (n - 1) * step + 1, step)
        all_hT = [x for y in self.r_hT for x in y]
        with ExitStack() as es:
            identb = es.enter_context(self.sbuf("identb", [128, 128], BF16))
            r_idb = k.res()
            k.op("act", lambda e: e.copy(out=identb[:], in_=self.ident[:]), r=[self.r_const], w=[r_idb])
            w3 = [es.enter_context(self.sbuf(f"w3_{b}", [128, 3, NKC, 128], BF16)) for b in range(2)]
            r_w3 = [k.res() for _ in range(2)]
            wo = [es.enter_context(self.sbuf(f"wo_{b}", [128, D], BF16)) for b in range(2)]
            r_wo = [k.res() for _ in range(2)]
            bias = [es.enter_context(self.sbuf(f"ab_{b}", [128, 384], F32)) for b in range(2)]
            r_bias = [k.res() for _ in range(2)]
            QT = es.enter_context(self.sbuf("QT", [128, S], BF16))
            KT = es.enter_context(self.sbuf("KT", [128, S], BF16))
            V = es.enter_context(self.sbuf("Vb", [128, 16, 128], BF16))
            r_QT, r_KT, r_V = k.res(), k.res(), k.res()
            Og = [es.enter_context(self.sbuf(f"Og{g}", [128, S], F32)) for g in range(3)]
            r_Og = [k.res() for _ in range(3)]
            LSE = es.enter_context(self.sbuf("LSE", [1, 3, S], F32))
            r_LSE = k.res()
            rowA = es.enter_context(self.sbuf("rowA", [1, S], F32))
            r_rowA = k.res()
            NB = 4
            sc = [es.enter_context(self.sbuf(f"sc{b}", [128, 384], F32)) for b in range(NB)]
            pn = [es.enter_context(self.sbuf(f"pn{b}", [128, 384], BF16)) for b in range(NB)]
            st = [es.enter_context(self.sbuf(f"ast{b}", [128, 8], F32)) for b in range(NB)]
            r_blk = [k.res() for _ in range(NB)]
            PT = [es.enter_context(self.sbuf(f"PT{b}", [128, 384], BF16)) for b in range(2)]
            r_PT = [k.res() for _ in range(2)]
            mrg = es.enter_context(self.sbuf("mrg", [128, S], BF16))
            r_mrg = k.res()
            tmpM = es.enter_context(self.sbuf("tmpM", [128, 512], F32))
            t2 = es.enter_context(self.sbuf("t2M", [128, 512], F32))
            r_tmpM, r_t2 = k.res(), k.res()
            psT = self.ps[4][:, :].bitcast(BF16)

            def load_unit(u):
                g, h = u % 3, u // 3
                b = u % 2
                for t in range(3):
                    c0 = ((g * 3 + t) * 8 + h) * 128
                    k.dma("pool", f"w3_{b}", w3[b][:, t, :, :],
                          d["at_w_qkv"][j, :, c0:c0 + 128].rearrange("(kc p) n -> p kc n", p=128), w=[r_w3[b]])
                k.dma("sp", f"ab_{b}", bias[b][:, :], d["c_abias"][g * 8 + h], w=[r_bias[b]])

            def load_wo(h):
                b = h % 2
                k.dma("pool", f"wo_{b}", wo[b][:, :], d["at_w_o"][j, h * 128:(h + 1) * 128, :], w=[r_wo[b]])

            nblk_it = [0]

            def stageA(u, blk):
                g, h = u % 3, u // 3
                dil = DIL[g]
                nblk = 16 // dil
                r_, ib = blk // nblk, blk % nblk
                kb0, kb1 = max(ib - 1, 0), min(ib + 1, nblk - 1)
                nkb = kb1 - kb0 + 1
                nk = nkb * 128
                bc0 = (kb0 - (ib - 1)) * 128
                qsl = ss(r_ + dil * ib * 128, 128, dil)
                ksl = ss(r_ + dil * kb0 * 128, nk, dil)
                n = nblk_it[0]
                nblk_it[0] += 1
                sb_ = n % NB
                psc = 3 if n % 2 == 0 else 6
                bb = u % 2
                k.op("pe", lambda e: e.matmul(self.ps[psc][:, 0:nk], QT[:, qsl], KT[:, ksl], start=True, stop=True),
                     r=[r_QT, r_KT], w=[self.r_ps[psc]])
                k.op("dve", lambda e: e.tensor_tensor(out=sc[sb_][:, 0:nk], in0=self.ps[psc][:, 0:nk],
                                                      in1=bias[bb][:, bc0:bc0 + nk], op=ALU.add),
                     r=[self.r_ps[psc], r_bias[bb]], w=[r_blk[sb_]])
                k.op("dve", lambda e: e.reduce_max(out=st[sb_][:, 0:1], in_=sc[sb_][:, 0:nk], axis=AX.X),
                     r=[r_blk[sb_]], w=[r_blk[sb_]])
                k.op("dve", lambda e: e.tensor_scalar_mul(out=st[sb_][:, 1:2], in0=st[sb_][:, 0:1], scalar1=-1.0),
                     r=[r_blk[sb_]], w=[r_blk[sb_]])
                k.op("act", lambda e: e.activation(out=sc[sb_][:, 0:nk], in_=sc[sb_][:, 0:nk], func=AF.Exp,
                                                   bias=st[sb_][:, 1:2], scale=1.0, accum_out=st[sb_][:, 2:3]),
                     r=[r_blk[sb_]], w=[r_blk[sb_]])
                k.op("act", lambda e: e.activation(out=st[sb_][:, 4:5], in_=st[sb_][:, 2:3], func=AF.Ln),
                     r=[r_blk[sb_]], w=[r_blk[sb_]])
                k.op("dve", lambda e: e.reciprocal(out=st[sb_][:, 3:4], in_=st[sb_][:, 2:3]),
                     r=[r_blk[sb_]], w=[r_blk[sb_]])
                k.op("dve", lambda e: e.tensor_scalar_mul(out=pn[sb_][:, 0:nk], in0=sc[sb_][:, 0:nk],
                                                          scalar1=st[sb_][:, 3:4]),
                     r=[r_blk[sb_]], w=[r_blk[sb_]])
                k.op("dve", lambda e: e.tensor_tensor(out=st[sb_][:, 5:6], in0=st[sb_][:, 4:5], in1=st[sb_][:, 0:1],
                                                      op=ALU.add),
                     r=[r_blk[sb_]], w=[r_blk[sb_]])
                return (g, r_, kb0, nkb, nblk, qsl, sb_, n)

            def stageB(desc):
                g, r_, kb0, nkb, nblk, qsl, sb_, n = desc
                nk = nkb * 128
                pb = n % 2
                for c in range(nkb):
                    k.op("pe", lambda e, c=c: e.transpose(psT[:, c * 128:(c + 1) * 128], pn[sb_][:, c * 128:(c + 1) * 128],
                                                          identb[:]),
                         r=[r_blk[sb_], r_idb], w=[self.r_ps[4]])
                k.op("act", lambda e: e.copy(out=PT[pb][:, 0:nk], in_=psT[:, 0:nk]),
                     r=[self.r_ps[4]], w=[r_PT[pb]])
                k.op("pe", lambda e: e.transpose(self.ps[5][0:1, 128:256], st[sb_][:, 5:6], self.ident[:]),
                     r=[r_blk[sb_], self.r_const], w=[self.r_ps[5]])
                for c in range(nkb):
                    vb = r_ * nblk + kb0 + c
                    k.op("pe", lambda e, c=c, vb=vb: e.matmul(self.ps[5][:, 0:128], V[:, vb, :], PT[pb][:, c * 128:(c + 1) * 128],
                                                              start=(c == 0), stop=(c == nkb - 1)),
                         r=[r_V, r_PT[pb]], w=[self.r_ps[5]])
                k.op("dve", lambda e: e.tensor_copy(out=Og[g][:, qsl], in_=self.ps[5][:, 0:128]),
                     r=[self.r_ps[5]], w=[r_Og[g]])
                k.op("dve", lambda e: e.tensor_copy(out=LSE[0:1, g, qsl], in_=self.ps[5][0:1, 128:256]),
                     r=[self.r_ps[5]], w=[r_LSE])

            def proj_unit(u):
                g, h = u % 3, u // 3
                b = u % 2
                dil = DIL[g]
                nblk = 16 // dil
                for tb in range(NTB):
                    tsl = slice(tb * 512, (tb + 1) * 512)
                    for kc in range(NKC):
                        k.op("pe", lambda e, kc=kc: e.matmul(self.ps[0][:, :], w3[b][:, 0, kc, :], self.hT[:, kc, tsl],
                                                             start=(kc == 0), stop=(kc == NKC - 1)),
                             r=[r_w3[b], self.r_hT[kc][tb]], w=[self.r_ps[0]])
                    k.op("act", lambda e: e.mul(out=QT[:, tsl], in_=self.ps[0][:, :], mul=float(128 ** -0.5)),
                         r=[self.r_ps[0]], w=[r_QT])
                    for kc in range(NKC):
                        k.op("pe", lambda e, kc=kc: e.matmul(self.ps[1][:, :], w3[b][:, 1, kc, :], self.hT[:, kc, tsl],
                                                             start=(kc == 0), stop=(kc == NKC - 1)),
                             r=[r_w3[b], self.r_hT[kc][tb]], w=[self.r_ps[1]])
                    k.op("act", lambda e: e.copy(out=KT[:, tsl], in_=self.ps[1][:, :]),
                         r=[self.r_ps[1]], w=[r_KT])
                for b4 in range(4):
                    pv = 2 if b4 % 2 == 0 else 7
                    for q in range(4):
                        blk = b4 * 4 + q
                        r_, ib = blk // nblk, blk % nblk
                        tsl = ss(r_ + dil * ib * 128, 128, dil)
                        for kc in range(NKC):
                            k.op("pe", lambda e, kc=kc, q=q, tsl=tsl: e.matmul(
                                self.ps[pv][:, q * 128:(q + 1) * 128], self.hT[:, kc, tsl], w3[b][:, 2, kc, :],
                                start=(kc == 0), stop=(kc == NKC - 1)),
                                r=[r_w3[b]] + all_hT, w=[self.r_ps[pv]])
                    k.op("act", lambda e, b4=b4: e.copy(out=V[:, b4 * 4:b4 * 4 + 4, :],
                                                        in_=self.ps[pv][:, :].rearrange("p (q n) -> p q n", q=4)),
                         r=[self.r_ps[pv]], w=[r_V])

            def merge_head(h):
                b = h % 2
                row = lambda g: LSE[0:1, g, :]
                dv = lambda fn, r, w: k.op("dve", fn, r=r, w=w)
                dv(lambda e: e.tensor_tensor(out=rowA[0:1, :], in0=row(0), in1=row(1), op=ALU.max), [r_LSE], [r_rowA])
                dv(lambda e: e.tensor_tensor(out=rowA[0:1, :], in0=rowA[0:1, :], in1=row(2), op=ALU.max), [r_LSE, r_rowA], [r_rowA])
                for g in range(3):
                    dv(lambda e, g=g: e.tensor_tensor(out=row(g), in0=row(g), in1=rowA[0:1, :], op=ALU.subtract),
                       [r_LSE, r_rowA], [r_LSE])
                k.op("act", lambda e: e.activation(out=LSE[0:1, :, :], in_=LSE[0:1, :, :], func=AF.Exp), r=[r_LSE], w=[r_LSE])
                dv(lambda e: e.tensor_tensor(out=rowA[0:1, :], in0=row(0), in1=row(1), op=ALU.add), [r_LSE], [r_rowA])
                dv(lambda e: e.tensor_tensor(out=rowA[0:1, :], in0=rowA[0:1, :], in1=row(2), op=ALU.add), [r_LSE, r_rowA], [r_rowA])
                dv(lambda e: e.reciprocal(out=rowA[0:1, :], in_=rowA[0:1, :]), [r_rowA], [r_rowA])
                for g in range(3):
                    dv(lambda e, g=g: e.tensor_tensor(out=row(g), in0=row(g), in1=rowA[0:1, :], op=ALU.mult),
                       [r_LSE, r_rowA], [r_LSE])
                for tb in range(NTB):
                    tsl = slice(tb * 512, (tb + 1) * 512)
                    for g in range(3):
                        pi = 6 if g % 2 == 0 else 3
                        k.op("pe", lambda e, g=g, pi=pi: e.matmul(self.ps[pi][:, :], self.ones[0:1, :], LSE[0:1, g, tsl],
                                                                  start=True, stop=True),
                             r=[r_LSE, self.r_const], w=[self.r_ps[pi]])
                        if g == 0:
                            dv(lambda e, pi=pi: e.tensor_tensor(out=tmpM[:, :], in0=Og[0][:, tsl], in1=self.ps[pi][:, :], op=ALU.mult),
                               [r_Og[0], self.r_ps[pi]], [r_tmpM])
                        else:
                            dv(lambda e, g=g, pi=pi: e.tensor_tensor(out=t2[:, :], in0=Og[g][:, tsl], in1=self.ps[pi][:, :], op=ALU.mult),
                               [r_Og[g], self.r_ps[pi]], [r_t2])
                            if g == 1:
                                k.op("dve", lambda e: e.tensor_tensor(out=tmpM[:, :], in0=tmpM[:, :], in1=t2[:, :], op=ALU.add),
                                     r=[r_tmpM, r_t2], w=[r_tmpM])
                            else:
                                k.op("dve", lambda e: e.tensor_tensor(out=mrg[:, tsl], in0=tmpM[:, :], in1=t2[:, :], op=ALU.add),
                                     r=[r_tmpM, r_t2], w=[r_mrg])
                for tb in range(NTB):
                    tsl = slice(tb * 512, (tb + 1) * 512)
                    for dc in range(NKC):
                        pi = 7 if dc % 2 == 0 else 0
                        k.op("pe", lambda e, dc=dc, pi=pi: e.matmul(self.ps[pi][:, :], wo[b][:, dc * 128:(dc + 1) * 128], mrg[:, tsl],
                                                                    start=True, stop=True),
                             r=[r_wo[b], r_mrg], w=[self.r_ps[pi]])
                        k.op("dve", lambda e, dc=dc, pi=pi: e.scalar_tensor_tensor(
                            out=self.xT[:, dc, tsl], in0=self.ps[pi][:, :], scalar=self.modT[:, i, 16 + dc:17 + dc],
                            in1=self.xT[:, dc, tsl], op0=ALU.mult, op1=ALU.add),
                            r=[self.r_ps[pi], self.r_mod], w=[self.r_xT[dc][tb]])

            LA = 2
            pend_merge = [None]
            load_unit(0)
            for u in range(24):
                g, h = u % 3, u // 3
                if g == 0:
                    load_wo(h)
                if u + 1 < 24:
                    load_unit(u + 1)
                proj_unit(u)
                if pend_merge[0] is not None:
                    merge_head(pend_merge[0])
                    pend_merge[0] = None
                pend = []
                for blk in range(16):
                    pend.append(stageA(u, blk))
                    if len(pend) > LA:
                        stageB(pend.pop(0))
                while pend:
                    stageB(pend.pop(0))
                if g == 2:
                    pend_merge[0] = h
            merge_head(pend_merge[0])
            k.barrier()

    def final(self):
        nc, k, d = self.nc, self.k, self.dram
        plain = self.cfg.get("final_plain", False)
        with ExitStack() as es:
            sq = es.enter_context(self.sbuf("fsq", [128, NKC, 512], BF16))
            rstd = es.enter_context(self.sbuf("frstd", [128, 512], F32))
            y = es.enter_context(self.sbuf("fy", [128, NKC, 512], F32))
            ost = [es.enter_context(self.sbuf(f"fo{j}", [128, D], F32)) for j in range(2)]
            r_o = [k.res() for _ in range(2)]
            r_sq, r_rstd, r_y = k.res(), k.res(), k.res()
            gfin = self.gT[:, 2 * DEPTH, :]
            for tb in range(NTB):
                tsl = slice(tb * 512, (tb + 1) * 512)
                if not plain:
                    self.norm_core(es, tb, None, None, None, sq, r_sq, rstd, r_rstd, 7)
                for kc in range(NKC):
                    if plain:
                        k.op("dve", lambda e, kc=kc: e.tensor_copy(out=y[:, kc, :], in_=self.xT[:, kc, tsl]),
                             r=[self.r_xT[kc][tb]], w=[r_y])
                    else:
                        k.op("dve", lambda e, kc=kc: e.scalar_tensor_tensor(
                            out=y[:, kc, :], in0=self.xT[:, kc, tsl], scalar=gfin[:, kc:kc + 1], in1=rstd[:, :],
                            op0=ALU.mult, op1=ALU.mult),
                            r=[self.r_xT[kc][tb], r_rstd, self.r_const], w=[r_y])
                for t4 in range(4):
                    tt = tb * 4 + t4
                    ob = tt % 2
                    for half in range(2):
                        pi = (tt * 2 + half) % 4
                        for j in range(4):
                            kc = half * 4 + j
                            k.op("pe", lambda e, kc=kc, j=j, pi=pi: e.transpose(
                                self.ps[pi][:, j * 128:(j + 1) * 128], y[:, kc, t4 * 128:(t4 + 1) * 128], self.ident[:]),
                                r=[r_y, self.r_const], w=[self.r_ps[pi]])
                        if half == 0:
                            k.op("act", lambda e, pi=pi, ob=ob: e.copy(out=ost[ob][:, 0:512], in_=self.ps[pi][:, :]),
                                 r=[self.r_ps[pi]], w=[r_o[ob]])
                        else:
                            k.op("dve", lambda e, pi=pi, ob=ob: e.tensor_copy(out=ost[ob][:, 512:1024], in_=self.ps[pi][:, :]),
                                 r=[self.r_ps[pi]], w=[r_o[ob]])
                    k.dma("sp", f"fo{ob}", d["out"][tt * 128:(tt + 1) * 128, :], ost[ob][:], r=[r_o[ob]])


def host_consts():
    ident = np.eye(128, dtype=np.float32)
    sel = np.zeros((32, 32 * 128), np.float32)
    for e in range(32):
        sel[e, e * 128:(e + 1) * 128] = 1.0
    ab = np.zeros((24, 128, 384), np.float32)
    ii = np.arange(128)[:, None]
    jj = np.arange(384)[None, :]
    rel = np.abs((jj - 128) - ii).astype(np.float32)
    for g, dil in enumerate((1, 4, 16)):
        for h in range(8):
            slope = 2.0 ** (-8.0 * (g * 8 + h + 1) / 24.0)
            ab[g * 8 + h] = np.where(rel <= 64, -slope * rel * dil, -1e30)
    CW = -float(np.exp(-0.5))
    tri = np.zeros((6, 128, 128), np.float32)
    msk = np.zeros((6, 64, 64), np.float32)
    sidx = np.arange(128)[:, None]
    tidx = np.arange(128)[None, :]
    same = (sidx // 64) == (tidx // 64)
    s64 = np.arange(64)[:, None]
    t64 = np.arange(64)[None, :]
    for dd in range(2):
        before = (sidx < tidx) if dd == 0 else (sidx > tidx)
        after = (sidx > tidx) if dd == 0 else (sidx < tidx)
        tri[dd * 3 + 0] = np.where(same & (before | (sidx == tidx)), CW, 0.0)
        tri[dd * 3 + 1] = np.where(same & before, CW, 0.0)
        tri[dd * 3 + 2] = np.where(same & after, CW, 0.0)
        b64 = (s64 < t64) if dd == 0 else (s64 > t64)
        msk[dd * 3 + 0] = b64
        msk[dd * 3 + 1] = b64.T
        msk[dd * 3 + 2] = b64 | (s64 == t64)
    csel = np.zeros((128, 2), np.float32)
    csel[:64, 0] = CW
    csel[64:, 1] = CW
    return {"c_ident": ident, "c_sel": sel, "c_abias": ab, "c_tri": tri, "c_csel": csel, "c_mask": msk}


def prep_shared(inp):
    f = lambda a: np.ascontiguousarray(a, dtype=np.float32)
    sh = {}
    sh["ada_w"] = f(inp["ada_w"])
    sh["ada_bT"] = f(np.asarray(inp["ada_b"]).reshape(DEPTH, 48, 128).transpose(0, 2, 1))
    sh["gtmT"] = f(np.asarray(inp["norm_tm_g"]).reshape(DEPTH, NKC, 128).transpose(0, 2, 1))
    sh["gcmT"] = f(np.asarray(inp["norm_cm_g"]).reshape(DEPTH, NKC, 128).transpose(0, 2, 1))
    sh["gfinT"] = f(np.asarray(inp["final_g"]).reshape(NKC, 128).T)
    rg = np.asarray(inp["moe_router_g"])
    re_ = np.asarray(inp["moe_router_e"])
    wr = np.concatenate([rg] + [re_[:, g] for g in range(4)], axis=-1)
    sh["wr"] = f(wr.reshape(DEPTH, NKC, 128, 36).transpose(0, 2, 1, 3))
    br = np.concatenate([np.asarray(inp["moe_router_g_b"]), np.asarray(inp["moe_router_e_b"]).reshape(DEPTH, 32)], axis=-1)
    sh["br"] = f(br.reshape(DEPTH, 1, 36))
    sh["rw_muT"] = f(np.asarray(inp["rw_mu"]).reshape(2, 6, NKC, 128).transpose(0, 3, 1, 2))
    sh["rw_w_rkv"] = f(inp["rw_w_rkv"])
    sh["rw_w1cat"] = f(np.concatenate([np.asarray(inp["rw_w1"])[:, 0], np.asarray(inp["rw_w1"])[:, 1]], axis=-1))
    sh["rw_a1cat"] = f(np.concatenate([np.asarray(inp["rw_a1"])[:, 0], np.asarray(inp["rw_a1"])[:, 1]], axis=-1))
    sh["rw_g1"] = f(inp["rw_g1"])
    sh["rw_w2cat"] = f(np.asarray(inp["rw_w2"]).reshape(2, 128, D))
    sh["rw_a2cat"] = f(np.asarray(inp["rw_a2"]).reshape(2, 128, D))
    sh["rw_g2"] = f(inp["rw_g2"])
    sh["rw_w0"] = f(inp["rw_w0"])
    sh["rw_a0"] = f(inp["rw_a0"])
    for n_ in ("rw_k_k", "rw_k_a", "rw_ln_w", "rw_ln_b"):
        sh[n_] = f(inp[n_])
    sh["rw_r_k"] = f(np.asarray(inp["rw_r_k"]).reshape(2, D))
    sh["rw_w_o"] = f(inp["rw_w_o"])
    sh["at_w_qkv"] = f(inp["at_w_qkv"])
    sh["at_w_o"] = f(inp["at_w_o"])
    sh["moe_w_gate"] = f(inp["moe_w_gate"])
    sh["moe_w_up"] = f(inp["moe_w_up"])
    sh["moe_w_down"] = f(inp["moe_w_down"])
    sh.update(host_consts())
    return sh


def prep_core(inp, b):
    return {
        "x": np.ascontiguousarray(np.asarray(inp["x"])[b], dtype=np.float32),
        "cT": np.ascontiguousarray(np.asarray(inp["c"])[b].reshape(NKC, 128).T, dtype=np.float32),
    }


def build_prog(cfg):
    nc = bass.Bass("TRN2", target_bir_lowering=False)
    p = Prog(nc, cfg)
    with p.k.es:
        p.build()
    return nc, p


def run(inp, cfg, cores):
    nc, p = build_prog(cfg)
    sh = prep_shared(inp)
    in_maps = []
    for b in cores:
        m = dict(sh)
        m.update(prep_core(inp, b))
        m = {kk: v for kk, v in m.items() if kk in p.dram}
        in_maps.append(m)
    res = run_bass_kernel_spmd(nc, in_maps, core_ids=list(range(len(cores))))
    return res


def kernel(**inputs):
    res = run(inputs, {}, list(range(8)))
    out = np.stack([np.asarray(r["out"]) for r in res.results], axis=0)
    return out.astype(np.float32)
```

```python
import numpy as np
from contextlib import ExitStack

import concourse.bass as bass
import concourse.mybir as mybir
from concourse.bass_utils import run_bass_kernel_spmd

F32 = mybir.dt.float32
BF16 = mybir.dt.bfloat16
AF = mybir.ActivationFunctionType
ALU = mybir.AluOpType
AX = mybir.AxisListType

D = 1024
S = 2048
DEPTH = 4
NKC = 8
NTB = 4
NTT = 16
RMS_EPS = 1e-6
SEM_LIM = 24000


class Res:
    __slots__ = ("name", "w", "rs", "excl")

    def __init__(self, name):
        self.name = name
        self.w = None
        self.rs = {}
        self.excl = False

    def add_reader(self, tag):
        st, ep, v = tag
        if self.rs.get(st, (-1, 0)) < (ep, v):
            self.rs[st] = (ep, v)


class KB:
    COMPUTE = ("pe", "act", "dve", "pool")

    def __init__(self, nc):
        self.nc = nc
        self.es = ExitStack()
        self.eng = {"pe": nc.tensor, "act": nc.scalar, "dve": nc.vector, "pool": nc.gpsimd, "sp": nc.sync}
        self.sems = {}
        self.cnt = {}
        self.seen = {e: {} for e in self.eng}
        self.nres = 0
        self.epoch_final = {}
        self.ninst = {e: 0 for e in self.eng}

    def res(self, name=None):
        self.nres += 1
        return Res(name or f"r{self.nres}")

    def sb(self, name, shape, dt):
        return self.es.enter_context(self.nc.sbuf_tensor(name, list(shape), dt))

    def _sem(self, stream, epoch):
        key = (stream, epoch)
        if key not in self.sems:
            self.sems[key] = self.es.enter_context(self.nc.semaphore(f"s_{stream}_{epoch}"))
        return self.sems[key]

    def _bump(self, stream, inc):
        ep, v = self.cnt.get(stream, (0, 0))
        if v + inc > SEM_LIM:
            self.epoch_final[(stream, ep)] = v
            ep, v = ep + 1, 0
        v += inc
        self.cnt[stream] = (ep, v)
        return (stream, ep, v), self._sem(stream, ep)

    def _wait(self, eng, tags):
        best = {}
        for t in tags:
            if t is None:
                continue
            st, ep, v = t
            if st == "pe" and eng == "pe":
                continue
            if (st not in best) or (ep, v) > best[st]:
                best[st] = (ep, v)
        for st, (ep, v) in best.items():
            if st.startswith("d_"):
                cur_ep, cur_v = self.cnt[st]
                v = cur_v if cur_ep == ep else self.epoch_final[(st, ep)]
            if self.seen[eng].get(st, (-1, 0)) >= (ep, v):
                continue
            self.eng[eng].wait_ge(self._sem(st, ep), v)
            self.seen[eng][st] = (ep, v)

    def _deps(self, r, w, same_stream=None):
        tags = []
        for x in r:
            tags.append(x.w)
            if x.excl:
                for st, (ep, v) in x.rs.items():
                    if same_stream is not None and st == same_stream:
                        continue
                    tags.append((st, ep, v))
        for x in w:
            tags.append(x.w)
            for st, (ep, v) in x.rs.items():
                if same_stream is not None and st == same_stream:
                    continue
                tags.append((st, ep, v))
        return tags

    def op(self, eng, fn, r=(), w=()):
        self._wait(eng, self._deps(r, w, same_stream=eng))
        ins = fn(self.eng[eng])
        tag, sem = self._bump(eng, 1)
        ins.then_inc(sem, 1)
        self.ninst[eng] += 1
        for x in r:
            x.add_reader(tag)
        for x in w:
            x.w = tag
            x.rs = {}
        return tag

    def dma(self, q, chan, out, in_, r=(), w=(), **kw):
        self._wait(q, self._deps(r, w))
        ins = self.eng[q].dma_start(out=out, in_=in_, **kw)
        tag, sem = self._bump("d_" + chan, 16)
        ins.then_inc(sem, 16)
        self.ninst[q] += 1
        for x in r:
            x.add_reader(tag)
        for x in w:
            x.w = tag
            x.rs = {}
        return tag

    def barrier(self, engines=("pe", "act", "dve", "pool", "sp")):
        tags = [(st, ep, v) for st, (ep, v) in self.cnt.items()]
        for e in engines:
            self._wait(e, tags)

    def wait_all(self, eng):
        self._wait(eng, [(st, ep, v) for st, (ep, v) in self.cnt.items()])


class Prog:
    def __init__(self, nc, cfg):
        self.nc = nc
        self.cfg = cfg
        self.k = KB(nc)
        self.dram = {}
        self._uid = 0

    def sbuf(self, name, shape, dt):
        self._uid += 1
        return self.nc.sbuf_tensor(f"{name}_u{self._uid}", list(shape), dt)

    def dump(self, name, ap, shape, dt, rs):
        if name not in self.cfg.get("dump", ()):
            return
        o = self.dout("dbg_" + name, shape, dt)
        self.k.dma("sp", "dbg", o, ap, r=rs)
        self.k.barrier()

    def din(self, name, shape, dt=F32):
        self.dram[name] = self.nc.dram_tensor(name, list(shape), dt, kind="ExternalInput").ap()
        return self.dram[name]

    def dout(self, name, shape, dt=F32):
        self.dram[name] = self.nc.dram_tensor(name, list(shape), dt, kind="ExternalOutput").ap()
        return self.dram[name]

    def build(self):
        nc, k, cfg = self.nc, self.k, self.cfg
        layers = cfg.get("layers", list(range(DEPTH)))
        d = self.dram
        self.din("x", [S, D])
        self.din("cT", [128, NKC])
        self.din("ada_w", [DEPTH, D, 6 * D])
        self.din("ada_bT", [DEPTH, 128, 48])
        self.din("gtmT", [DEPTH, 128, NKC])
        self.din("gcmT", [DEPTH, 128, NKC])
        self.din("gfinT", [128, NKC])
        self.din("wr", [DEPTH, 128, NKC, 36])
        self.din("br", [DEPTH, 1, 36])
        self.din("moe_w_gate", [DEPTH, 4, 8, D, 256])
        self.din("moe_w_up", [DEPTH, 4, 8, D, 256])
        self.din("moe_w_down", [DEPTH, 4, 8, 256, D])
        self.din("rw_muT", [2, 128, 6, NKC])
        self.din("rw_w_rkv", [2, 3, D, D])
        self.din("rw_w1cat", [2, D, 128])
        self.din("rw_a1cat", [2, D, 128])
        self.din("rw_g1", [2, 2, D, 128])
        self.din("rw_w2cat", [2, 128, D])
        self.din("rw_a2cat", [2, 128, D])
        self.din("rw_g2", [2, 2, 128, D])
        self.din("rw_w0", [2, 2, D])
        self.din("rw_a0", [2, 2, D])
        for n_ in ("rw_k_k", "rw_k_a", "rw_r_k", "rw_ln_w", "rw_ln_b"):
            self.din(n_, [2, D])
        self.din("rw_w_o", [2, D, D])
        self.din("c_tri", [6, 128, 128])
        self.din("c_csel", [128, 2])
        self.din("c_mask", [6, 64, 64])
        itn = lambda n_, shp, dt=F32: nc.dram_tensor(n_, list(shp), dt, kind="Internal").ap()
        self.scr = {"RAW": itn("scr_raw", [2, S, D]), "RAWV": itn("scr_rawv", [S, D], BF16),
                    "FM": itn("scr_fm", [2, 4, NTT, 64, 16, 128], BF16),
                    "TM": itn("scr_tm", [2, 2, S, D], BF16), "GG": itn("scr_gg", [2, S, D]), "RK": itn("scr_rk", [2, S, 16])}
        self.r_scr = {"RAW": [[k.res() for _ in range(NTT)] for _ in range(3)],
                      "FM": [[[k.res() for _ in range(NTT)] for _ in range(4)] for _ in range(2)],
                      "TM": [[[k.res() for _ in range(NTT)] for _ in range(2)] for _ in range(2)],
                      "GG": [[k.res() for _ in range(NTT)] for _ in range(2)],
                      "RK": [[k.res() for _ in range(NTT)] for _ in range(2)]}
        self.din("at_w_qkv", [2, D, 9216])
        self.din("at_w_o", [2, D, D])
        self.din("c_abias", [24, 128, 384])
        self.din("c_ident", [128, 128])
        self.din("c_sel", [32, 32 * 128])
        self.dout("out", [S, D])

        self.xT = k.sb("xT", [128, NKC, S], F32)
        self.hT = k.sb("hT", [128, NKC, S], BF16)
        self.r_xT = [[k.res(f"xT{kc}_{tb}") for tb in range(NTB)] for kc in range(NKC)]
        self.r_hT = [[k.res(f"hT{kc}_{tb}") for tb in range(NTB)] for kc in range(NKC)]
        self.ident = k.sb("ident", [128, 128], F32)
        self.ones = k.sb("ones", [128, 128], F32)
        self.epsT = k.sb("epsT", [128, 1], F32)
        self.onesb = k.sb("onesb", [128, 128], BF16)
        self.r_const = k.res("const")
        self.modT = k.sb("modT", [128, DEPTH, 48], F32)
        self.mod1T = k.sb("mod1T", [128, DEPTH, 48], F32)
        self.r_mod = k.res("mod")
        self.gT = k.sb("gT", [128, 2 * DEPTH + 1, NKC], F32)
        self.ps = [self.k.es.enter_context(nc.psum_tensor(f"ps{i}", [128, 512], F32)) for i in range(8)]
        self.r_ps = [k.res(f"ps{i}") for i in range(8)]
        for r_ in self.r_ps:
            r_.excl = True

        k.dma("sp", "c0", self.ident[:], d["c_ident"][:, :], w=[self.r_const])
        k.op("dve", lambda e: e.memset(self.ones[:], 1.0), w=[self.r_const])
        k.op("dve", lambda e: e.memset(self.epsT[:], RMS_EPS), w=[self.r_const])
        k.op("dve", lambda e: e.memset(self.onesb[:], 1.0), w=[self.r_const])
        k.dma("sp", "c0", self.gT[:, 0:DEPTH, :], d["gtmT"].rearrange("l p k -> p l k"), w=[self.r_const])
        k.dma("sp", "c0", self.gT[:, DEPTH:2 * DEPTH, :], d["gcmT"].rearrange("l p k -> p l k"), w=[self.r_const])
        k.dma("sp", "c0", self.gT[:, 2 * DEPTH, :], d["gfinT"][:, :], w=[self.r_const])

        self.load_x()
        self.ada_all()
        k.barrier()
        for i in layers:
            if cfg.get("mixers", True):
                self.norm_mod(i, 0)
                if i % 2 == 0:
                    self.rwkv(i)
                else:
                    self.attn(i)
            if cfg.get("moe", True):
                self.moe(i)
        self.final()
        k.barrier()
        k.wait_all("sp")
        k.wait_all("pool")

    def load_x(self):
        nc, k, d = self.nc, self.k, self.dram
        with ExitStack() as es:
            st = [es.enter_context(self.sbuf(f"xst{i}", [128, D], F32)) for i in range(2)]
            r_st = [k.res() for _ in range(2)]
            for tt in range(NTT):
                b = tt % 2
                k.dma("sp", f"xst{b}", st[b][:], d["x"][tt * 128:(tt + 1) * 128, :], w=[r_st[b]])
                for half in range(2):
                    pi = (tt * 2 + half) % 4
                    for j in range(4):
                        kc = half * 4 + j
                        k.op("pe", lambda e, kc=kc, j=j, pi=pi, b=b: e.transpose(
                            self.ps[pi][:, j * 128:(j + 1) * 128], st[b][:, kc * 128:(kc + 1) * 128], self.ident[:]),
                            r=[r_st[b], self.r_const], w=[self.r_ps[pi]])
                    tb = tt // 4
                    ws = [self.r_xT[half * 4 + j][tb] for j in range(4)]
                    eng = "act" if half else "dve"
                    if eng == "dve":
                        k.op("dve", lambda e, half=half, pi=pi, tt=tt: e.tensor_copy(
                            out=self.xT[:, half * 4:half * 4 + 4, tt * 128:(tt + 1) * 128],
                            in_=self.ps[pi][:, :].rearrange("p (j t) -> p j t", j=4)),
                            r=[self.r_ps[pi]], w=ws)
                    else:
                        k.op("act", lambda e, half=half, pi=pi, tt=tt: e.copy(
                            out=self.xT[:, half * 4:half * 4 + 4, tt * 128:(tt + 1) * 128],
                            in_=self.ps[pi][:, :].rearrange("p (j t) -> p j t", j=4)),
                            r=[self.r_ps[pi]], w=ws)
            k.barrier()

    def ada_all(self):
        nc, k, d = self.nc, self.k, self.dram
        NP = 8
        PW = 6 * D // NP
        with ExitStack() as es:
            scT = es.enter_context(self.sbuf("scT", [128, NKC], F32))
            cT = es.enter_context(self.sbuf("cTs", [128, NKC], F32))
            abT = es.enter_context(self.sbuf("abT", [128, DEPTH, 48], F32))
            wst = [es.enter_context(self.sbuf(f"adaw{i}", [128, NKC, PW], F32)) for i in range(2)]
            r_w = [k.res() for _ in range(2)]
            r_sc = k.res()
            r_ab = k.res()
            k.dma("sp", "c1", cT[:], d["cT"][:, :], w=[r_sc])
            k.dma("sp", "c1", abT[:], d["ada_bT"].rearrange("l p j -> p l j"), w=[r_ab])
            k.op("act", lambda e: e.activation(out=scT[:], in_=cT[:], func=AF.Silu), r=[r_sc], w=[r_sc])
            q = 0
            for i in range(DEPTH):
                pi = i % 2
                for pc in range(NP):
                    b = q % 2
                    q += 1
                    k.dma("sp", f"adaw{b}", wst[b][:],
                          d["ada_w"][i, :, pc * PW:(pc + 1) * PW].rearrange("(kc p) n -> p kc n", p=128),
                          w=[r_w[b]])
                    for jj in range(PW // 128):
                        j = pc * (PW // 128) + jj
                        for kc in range(NKC):
                            k.op("pe", lambda e, b=b, jj=jj, kc=kc, j=j, pi=pi: e.matmul(
                                self.ps[pi][:, j:j + 1], wst[b][:, kc, jj * 128:(jj + 1) * 128], scT[:, kc:kc + 1],
                                start=(kc == 0), stop=(kc == NKC - 1)),
                                r=[r_w[b], r_sc], w=[self.r_ps[pi]])
                k.op("dve", lambda e, i=i, pi=pi: e.tensor_tensor(
                    out=self.modT[:, i, :], in0=self.ps[pi][:, 0:48], in1=abT[:, i, :], op=ALU.add),
                    r=[self.r_ps[pi], r_ab], w=[self.r_mod])
                k.op("dve", lambda e, i=i: e.tensor_scalar_add(
                    out=self.mod1T[:, i, :], in0=self.modT[:, i, :], scalar1=1.0),
                    r=[self.r_mod], w=[self.r_mod])
            k.barrier()

    def norm_core(self, es, tb, geff, shift, r_par, sq, r_sq, rstd, r_rstd, psi):
        nc, k = self.nc, self.k
        tsl = slice(tb * 512, (tb + 1) * 512)
        k.op("act", lambda e: e.activation(out=sq[:, :, :], in_=self.xT[:, :, tsl], func=AF.Square),
             r=[self.r_xT[kc][tb] for kc in range(NKC)], w=[r_sq])
        for kc in range(NKC):
            k.op("pe", lambda e, kc=kc: e.matmul(self.ps[psi][:, :], self.onesb[:, :], sq[:, kc, :],
                                                 start=(kc == 0), stop=(kc == NKC - 1)),
                 r=[r_sq, self.r_const], w=[self.r_ps[psi]])
        k.op("act", lambda e: e.activation(out=rstd[:, :], in_=self.ps[psi][:, :], func=AF.Sqrt,
                                           scale=1.0 / D, bias=self.epsT[:, 0:1]),
             r=[self.r_ps[psi], self.r_const], w=[r_rstd])
        k.op("dve", lambda e: e.reciprocal(out=rstd[:, :], in_=rstd[:, :]), r=[r_rstd], w=[r_rstd])

    def norm_mod(self, i, which, router=None):
        nc, k = self.nc, self.k
        gi = i if which == 0 else DEPTH + i
        so = 0 if which == 0 else 24
        with ExitStack() as es:
            geff = es.enter_context(self.sbuf("geff", [128, NKC], F32))
            sq = [es.enter_context(self.sbuf(f"nsq{j}", [128, NKC, 512], BF16)) for j in range(2)]
            rstd = [es.enter_context(self.sbuf(f"nrstd{j}", [128, 512], F32)) for j in range(2)]
            t1 = [es.enter_context(self.sbuf(f"nt1_{j}", [128, 512], F32)) for j in range(2)]
            r_t1 = [k.res() for _ in range(2)]
            r_g = k.res()
            r_sq = [k.res() for _ in range(2)]
            r_rstd = [k.res() for _ in range(2)]
            if router is not None:
                h32 = es.enter_context(self.sbuf("h32", [128, NKC, 512], F32))
                r_h32 = k.res()
            k.op("dve", lambda e: e.tensor_tensor(out=geff[:], in0=self.gT[:, gi, :], in1=self.mod1T[:, i, so + 8:so + 16],
                                                  op=ALU.mult), r=[self.r_const, self.r_mod], w=[r_g])
            ncore = lambda tb: self.norm_core(es, tb, None, None, None, sq[tb % 2], r_sq[tb % 2], rstd[tb % 2], r_rstd[tb % 2],
                                              7 if tb % 2 == 0 else 5)
            ncore(0)
            for tb in range(NTB):
                tsl = slice(tb * 512, (tb + 1) * 512)
                if tb + 1 < NTB:
                    ncore(tb + 1)
                rs_, r_rs = rstd[tb % 2], r_rstd[tb % 2]
                for kc in range(NKC):
                    b = kc % 2
                    k.op("dve", lambda e, kc=kc, b=b: e.tensor_tensor(out=t1[b][:, :], in0=self.xT[:, kc, tsl], in1=rs_[:, :],
                                                                      op=ALU.mult),
                         r=[self.r_xT[kc][tb], r_rs], w=[r_t1[b]])
                    if router is None:
                        k.op("act", lambda e, kc=kc, b=b: e.activation(
                            out=self.hT[:, kc, tsl], in_=t1[b][:, :], func=AF.Identity, scale=geff[:, kc:kc + 1],
                            bias=self.modT[:, i, so + kc:so + kc + 1]),
                            r=[r_t1[b], r_g, self.r_mod], w=[self.r_hT[kc][tb]])
                    else:
                        k.op("act", lambda e, kc=kc, b=b: e.activation(
                            out=h32[:, kc, :], in_=t1[b][:, :], func=AF.Identity, scale=geff[:, kc:kc + 1],
                            bias=self.modT[:, i, so + kc:so + kc + 1]),
                            r=[r_t1[b], r_g, self.r_mod], w=[r_h32])
                        k.op("act", lambda e, kc=kc, b=b: e.activation(
                            out=self.hT[:, kc, tsl], in_=t1[b][:, :], func=AF.Identity, scale=geff[:, kc:kc + 1],
                            bias=self.modT[:, i, so + kc:so + kc + 1]),
                             r=[r_t1[b], r_g, self.r_mod], w=[self.r_hT[kc][tb]])
                if router is not None:
                    router(tb, h32, r_h32)
            k.barrier()

    def moe(self, i):
        nc, k, d = self.nc, self.k, self.dram
        with ExitStack() as es0:
            gT_all = es0.enter_context(self.sbuf("gT_all", [32, S], F32))
            r_gT = [k.res() for _ in range(NTT)]
            wg = [es0.enter_context(self.sbuf(f"wg{b}", [128, 2, NKC, 256], BF16)) for b in range(2)]
            wu = [es0.enter_context(self.sbuf(f"wu{b}", [128, 2, NKC, 256], BF16)) for b in range(2)]
            wd = [es0.enter_context(self.sbuf(f"wd{b}", [128, 2, 2, D], BF16)) for b in range(2)]
            r_w = [k.res() for _ in range(2)]

            def load_w(s):
                b = s % 2
                for ee in range(2):
                    eg = 2 * s + ee
                    g_, e_ = eg // 8, eg % 8
                    k.dma("pool", f"moew{b}", wg[b][:, ee, :, :],
                          d["moe_w_gate"][i, g_, e_].rearrange("(kc p) f -> p kc f", p=128), w=[r_w[b]])
                    k.dma("pool", f"moew{b}", wu[b][:, ee, :, :],
                          d["moe_w_up"][i, g_, e_].rearrange("(kc p) f -> p kc f", p=128), w=[r_w[b]])
                    k.dma("pool", f"moew{b}", wd[b][:, ee, :, :],
                          d["moe_w_down"][i, g_, e_].rearrange("(fc p) n -> p fc n", p=128), w=[r_w[b]])


            load_w(0)
            with ExitStack() as es:
                wr = es.enter_context(self.sbuf("wr", [128, NKC, 36], F32))
                brs = es.enter_context(self.sbuf("brs", [1, 36], F32))
                r_wr = k.res()
                k.dma("sp", "c2", wr[:], d["wr"][i], w=[r_wr])
                k.dma("sp", "c2", brs[:], d["br"][i], w=[r_wr])
                L = es.enter_context(self.sbuf("rL", [128, 36], F32))
                sm = es.enter_context(self.sbuf("rsm", [128, 64], F32))
                gates = es.enter_context(self.sbuf("rgates", [128, 32], F32))
                r_L, r_sm, r_gates = k.res(), k.res(), k.res()

                def router(tb, h32, r_h32):
                    for t4 in range(4):
                        tt = tb * 4 + t4
                        pi = 6
                        for kc in range(NKC):
                            k.op("pe", lambda e, kc=kc: e.matmul(self.ps[pi][:, 0:36], h32[:, kc, t4 * 128:(t4 + 1) * 128],
                                                                 wr[:, kc, :], start=(kc == 0), stop=False),
                                 r=[r_h32, r_wr], w=[self.r_ps[pi]])
                        k.op("pe", lambda e: e.matmul(self.ps[pi][:, 0:36], self.ones[0:1, :], brs[0:1, :],
                                                      start=False, stop=True),
                             r=[r_wr, self.r_const], w=[self.r_ps[pi]])
                        k.op("dve", lambda e: e.tensor_copy(out=L[:, :], in_=self.ps[pi][:, 0:36]),
                             r=[self.r_ps[pi]], w=[r_L])
                        self.route_math(L, r_L, sm, r_sm, gates, r_gates)
                        k.op("pe", lambda e: e.transpose(self.ps[pi][0:32, 128:256], gates[:, :], self.ident[:]),
                             r=[r_gates, self.r_const], w=[self.r_ps[pi]])
                        k.op("act", lambda e, tt=tt: e.copy(out=gT_all[:, tt * 128:(tt + 1) * 128],
                                                            in_=self.ps[pi][0:32, 128:256]),
                             r=[self.r_ps[pi]], w=[r_gT[tt]])

                self.norm_mod(i, 1, router=router)
                self.dump("modT", self.modT[:], [128, DEPTH, 48], F32, [self.r_mod])
                self.dump("hT", self.hT[:], [128, NKC, S], BF16, [x for y in self.r_hT for x in y])
                self.dump("gT_all", gT_all[:], [32, S], F32, r_gT)
            with ExitStack() as es:
                sel = es.enter_context(self.sbuf("sel", [32, 32 * 128], F32))
                r_sel = k.res()
                k.dma("sp", "c2", sel[:], d["c_sel"][:, :], w=[r_sel])
                NS = 16
                gbc = [es.enter_context(self.sbuf(f"gbc{b}", [128, 2, 512], F32)) for b in range(2)]
                r_gbc = [k.res() for _ in range(2)]
                hid = [es.enter_context(self.sbuf(f"hid{b}", [128, 4, 512], BF16)) for b in range(2)]
                r_hid = [k.res() for _ in range(2)]
                sg = [es.enter_context(self.sbuf(f"sg{b}", [128, 512], F32)) for b in range(2)]
                r_sg = [k.res() for _ in range(2)]
                tm = [es.enter_context(self.sbuf(f"tm{b}", [128, 512], F32)) for b in range(2)]
                r_tm = [k.res() for _ in range(2)]

                pending = [None]
                it = [0]

                def down(s, tb, hb):
                    b = s % 2
                    tsl = slice(tb * 512, (tb + 1) * 512)
                    for dc in range(NKC):
                        pi = 4 + dc % 2
                        for u in range(4):
                            ee, fc = u // 2, u % 2
                            k.op("pe", lambda e, u=u, ee=ee, fc=fc, dc=dc, pi=pi: e.matmul(
                                self.ps[pi][:, :], wd[b][:, ee, fc, dc * 128:(dc + 1) * 128], hid[hb][:, u, :],
                                start=(u == 0), stop=(u == 3)),
                                r=[r_w[b], r_hid[hb]], w=[self.r_ps[pi]])
                        k.op("dve", lambda e, dc=dc, pi=pi: e.scalar_tensor_tensor(
                            out=self.xT[:, dc, tsl], in0=self.ps[pi][:, :], scalar=self.modT[:, i, 40 + dc:41 + dc],
                            in1=self.xT[:, dc, tsl], op0=ALU.mult, op1=ALU.add),
                            r=[self.r_ps[pi], self.r_mod], w=[self.r_xT[dc][tb]])

                for s in range(NS):
                    b = s % 2
                    for tb in range(NTB):
                        tsl = slice(tb * 512, (tb + 1) * 512)
                        hb = it[0] % 2
                        gb = it[0] % 2
                        it[0] += 1
                        for ee in range(2):
                            eg = 2 * s + ee
                            k.op("pe", lambda e, eg=eg: e.matmul(self.ps[6][:, :], sel[:, eg * 128:(eg + 1) * 128],
                                                                 gT_all[:, tsl], start=True, stop=True),
                                 r=[r_sel] + r_gT[tb * 4:tb * 4 + 4], w=[self.r_ps[6]])
                            k.op("act", lambda e, ee=ee, gb=gb: e.copy(out=gbc[gb][:, ee, :], in_=self.ps[6][:, :]),
                                 r=[self.r_ps[6]], w=[r_gbc[gb]])
                        for u in range(4):
                            ee, fc = u // 2, u % 2
                            pg, pu = (0, 1) if u % 2 == 0 else (2, 3)
                            for kc in range(NKC):
                                k.op("pe", lambda e, kc=kc, ee=ee, fc=fc, pg=pg: e.matmul(
                                    self.ps[pg][:, :], wg[b][:, ee, kc, fc * 128:(fc + 1) * 128], self.hT[:, kc, tsl],
                                    start=(kc == 0), stop=(kc == NKC - 1)),
                                    r=[r_w[b], self.r_hT[kc][tb]], w=[self.r_ps[pg]])
                            for kc in range(NKC):
                                k.op("pe", lambda e, kc=kc, ee=ee, fc=fc, pu=pu: e.matmul(
                                    self.ps[pu][:, :], wu[b][:, ee, kc, fc * 128:(fc + 1) * 128], self.hT[:, kc, tsl],
                                    start=(kc == 0), stop=(kc == NKC - 1)),
                                    r=[r_w[b], self.r_hT[kc][tb]], w=[self.r_ps[pu]])
                            sb_ = u % 2
                            k.op("act", lambda e, pg=pg, sb_=sb_: e.activation(out=sg[sb_][:, :], in_=self.ps[pg][:, :],
                                                                               func=AF.Silu),
                                 r=[self.r_ps[pg]], w=[r_sg[sb_]])
                            k.op("dve", lambda e, pu=pu, sb_=sb_: e.tensor_tensor(out=tm[sb_][:, :], in0=sg[sb_][:, :],
                                                                                  in1=self.ps[pu][:, :], op=ALU.mult),
                                 r=[r_sg[sb_], self.r_ps[pu]], w=[r_tm[sb_]])
                            k.op("dve", lambda e, u=u, ee=ee, sb_=sb_, hb=hb, gb=gb: e.tensor_tensor(
                                out=hid[hb][:, u, :], in0=tm[sb_][:, :], in1=gbc[gb][:, ee, :], op=ALU.mult),
                                r=[r_tm[sb_], r_gbc[gb]], w=[r_hid[hb]])
                        if pending[0] is not None:
                            down(*pending[0])
                        pending[0] = (s, tb, hb)
                        if tb == 0 and s + 1 < NS:
                            load_w(s + 1)
                down(*pending[0])
                k.barrier()

    def route_math(self, L, r_L, sm, r_sm, gates, r_gates):
        k = self.k
        GMAX, NGMAX, GSUM, PG, M1, M2, DD, EE, W1, W2 = range(10)
        GOH = slice(10, 14)
        GE = slice(14, 18)
        ESEL = slice(18, 26)
        OH1 = slice(26, 34)
        MSK = slice(34, 42)
        OH2 = slice(42, 50)
        INN = slice(50, 58)
        c = lambda j: slice(j, j + 1)

        def dv(fn, r=(), w=()):
            k.op("dve", fn, r=r, w=w)

        rs = [r_L, r_sm]
        dv(lambda e: e.reduce_max(out=sm[:, c(GMAX)], in_=L[:, 0:4], axis=AX.X), r=[r_L], w=[r_sm])
        dv(lambda e: e.tensor_scalar(out=sm[:, GOH], in0=L[:, 0:4], scalar1=sm[:, c(GMAX)], scalar2=None,
                                     op0=ALU.is_equal), r=rs, w=[r_sm])
        dv(lambda e: e.tensor_scalar_mul(out=sm[:, c(NGMAX)], in0=sm[:, c(GMAX)], scalar1=-1.0), r=[r_sm], w=[r_sm])
        k.op("act", lambda e: e.activation(out=sm[:, GE], in_=L[:, 0:4], func=AF.Exp, bias=sm[:, c(NGMAX)],
                                           scale=1.0, accum_out=sm[:, c(GSUM)]), r=rs, w=[r_sm])
        dv(lambda e: e.reciprocal(out=sm[:, c(PG)], in_=sm[:, c(GSUM)]), r=[r_sm], w=[r_sm])
        dv(lambda e: e.tensor_scalar_mul(out=sm[:, ESEL], in0=L[:, 4:12], scalar1=sm[:, c(10)]), r=rs, w=[r_sm])
        for g in range(1, 4):
            dv(lambda e, g=g: e.scalar_tensor_tensor(out=sm[:, ESEL], in0=L[:, 4 + 8 * g:12 + 8 * g],
                                                     scalar=sm[:, c(10 + g)], in1=sm[:, ESEL],
                                                     op0=ALU.mult, op1=ALU.add), r=rs, w=[r_sm])
        dv(lambda e: e.reduce_max(out=sm[:, c(M1)], in_=sm[:, ESEL], axis=AX.X), r=[r_sm], w=[r_sm])
        dv(lambda e: e.tensor_scalar(out=sm[:, OH1], in0=sm[:, ESEL], scalar1=sm[:, c(M1)], scalar2=None,
                                     op0=ALU.is_equal), r=[r_sm], w=[r_sm])
        dv(lambda e: e.scalar_tensor_tensor(out=sm[:, MSK], in0=sm[:, OH1], scalar=-1e30, in1=sm[:, ESEL],
                                            op0=ALU.mult, op1=ALU.add), r=[r_sm], w=[r_sm])
        dv(lambda e: e.reduce_max(out=sm[:, c(M2)], in_=sm[:, MSK], axis=AX.X), r=[r_sm], w=[r_sm])
        dv(lambda e: e.tensor_scalar(out=sm[:, OH2], in0=sm[:, MSK], scalar1=sm[:, c(M2)], scalar2=None,
                                     op0=ALU.is_equal), r=[r_sm], w=[r_sm])
        dv(lambda e: e.tensor_tensor(out=sm[:, c(DD)], in0=sm[:, c(M2)], in1=sm[:, c(M1)], op=ALU.subtract),
           r=[r_sm], w=[r_sm])
        k.op("act", lambda e: e.activation(out=sm[:, c(EE)], in_=sm[:, c(DD)], func=AF.Exp), r=[r_sm], w=[r_sm])
        dv(lambda e: e.tensor_scalar_add(out=sm[:, c(W1)], in0=sm[:, c(EE)], scalar1=1.0), r=[r_sm], w=[r_sm])
        dv(lambda e: e.reciprocal(out=sm[:, c(W1)], in_=sm[:, c(W1)]), r=[r_sm], w=[r_sm])
        dv(lambda e: e.tensor_tensor(out=sm[:, c(W2)], in0=sm[:, c(EE)], in1=sm[:, c(W1)], op=ALU.mult),
           r=[r_sm], w=[r_sm])
        dv(lambda e: e.tensor_tensor(out=sm[:, c(W1)], in0=sm[:, c(W1)], in1=sm[:, c(PG)], op=ALU.mult),
           r=[r_sm], w=[r_sm])
        dv(lambda e: e.tensor_tensor(out=sm[:, c(W2)], in0=sm[:, c(W2)], in1=sm[:, c(PG)], op=ALU.mult),
           r=[r_sm], w=[r_sm])
        dv(lambda e: e.tensor_scalar_mul(out=sm[:, INN], in0=sm[:, OH1], scalar1=sm[:, c(W1)]), r=[r_sm], w=[r_sm])
        dv(lambda e: e.scalar_tensor_tensor(out=sm[:, INN], in0=sm[:, OH2], scalar=sm[:, c(W2)], in1=sm[:, INN],
                                            op0=ALU.mult, op1=ALU.add), r=[r_sm], w=[r_sm])
        for g in range(4):
            dv(lambda e, g=g: e.tensor_scalar_mul(out=gates[:, 8 * g:8 * g + 8], in0=sm[:, INN],
                                                  scalar1=sm[:, c(10 + g)]), r=[r_sm], w=[r_gates])

    def rwkv(self, i):
        nc, k, d = self.nc, self.k, self.dram
        j = i // 2
        with ExitStack() as es:
            PCt = es.enter_context(self.sbuf("PCt", [64, 2, NTT, 16, 2], F32))
            r_PC = k.res()
            self.rwkv_A(i, j, PCt, r_PC)
            self.rwkv_B(i, j, PCt, r_PC)

    def rwkv_A(self, i, j, PCt, r_PC):
        nc, k, d = self.nc, self.k, self.dram
        all_hT = [x for y in self.r_hT for x in y]
        sc = self.scr
        with ExitStack() as esA:
            sb = lambda es, n, s, dt=F32: es.enter_context(self.sbuf(n, s, dt))
            h1w = sb(esA, "h1w", [128, S], BF16)
            h1a = sb(esA, "h1a", [128, S], BF16)
            r_h1w, r_h1a = k.res(), k.res()
            muT = sb(esA, "muT", [128, 6, NKC])
            c1 = sb(esA, "c1", [128, 6, NKC])
            c2 = sb(esA, "c2", [128, 6, NKC])
            r_c = k.res()
            k.dma("sp", "rwc", muT[:], d["rw_muT"][j], w=[r_c])
            k.op("dve", lambda e: e.tensor_scalar(out=c1[:], in0=muT[:], scalar1=-1.0, scalar2=1.0, op0=ALU.mult, op1=ALU.add),
                 r=[r_c], w=[r_c])
            k.op("dve", lambda e: e.tensor_scalar_mul(out=c2[:], in0=muT[:], scalar1=0.5), r=[r_c], w=[r_c])

            def scale_w(stg, r_stg, W1, W2, r_W, jm, ncol, col0=0):
                for kc in range(NKC):
                    k.op("act", lambda e, kc=kc: e.mul(out=W1[:, kc, col0:col0 + ncol], in_=stg[:, kc, 0:ncol],
                                                       mul=c1[:, jm, kc:kc + 1]), r=[r_stg, r_c], w=[r_W])
                    k.op("dve", lambda e, kc=kc: e.tensor_scalar_mul(out=W2[:, kc, col0:col0 + ncol], in0=stg[:, kc, 0:ncol],
                                                                     scalar1=c2[:, jm, kc:kc + 1]), r=[r_stg, r_c], w=[r_W])

            with ExitStack() as es12:
                hsT = sb(es12, "hsT", [128, NKC, S], BF16)
                r_hs = k.res()
                for kc in range(NKC):
                    k.op("dve", lambda e, kc=kc: e.tensor_tensor(out=hsT[:, kc, 1:S - 1], in0=self.hT[:, kc, 0:S - 2],
                                                                 in1=self.hT[:, kc, 2:S], op=ALU.add), r=all_hT, w=[r_hs])
                    k.op("act", lambda e, kc=kc: e.copy(out=hsT[:, kc, 0:1], in_=self.hT[:, kc, 1:2]), r=all_hT, w=[r_hs])
                    k.op("act", lambda e, kc=kc: e.copy(out=hsT[:, kc, S - 1:S], in_=self.hT[:, kc, S - 2:S - 1]), r=all_hT, w=[r_hs])
                with ExitStack() as es1:
                    h1g = [sb(es1, f"h1g{dd}", [128, S], BF16) for dd in range(2)]
                    r_h1g = [k.res() for _ in range(2)]
                    stg = [sb(es1, f"lstg{b}", [128, NKC, 128]) for b in range(2)]
                    r_stg = [k.res() for _ in range(2)]
                    W1 = [sb(es1, f"lW1_{b}", [128, NKC, 128], BF16) for b in range(2)]
                    W2 = [sb(es1, f"lW2_{b}", [128, NKC, 128], BF16) for b in range(2)]
                    r_W = [k.res() for _ in range(2)]
                    groups = [(d["rw_w1cat"][j], 3, AF.Tanh, h1w, r_h1w), (d["rw_a1cat"][j], 4, AF.Copy, h1a, r_h1a),
                              (d["rw_g1"][j, 0], 5, AF.Sigmoid, h1g[0], r_h1g[0]), (d["rw_g1"][j, 1], 5, AF.Sigmoid, h1g[1], r_h1g[1])]
                    for gi, (src, jm, fn, dst, r_dst) in enumerate(groups):
                        b = gi % 2
                        k.dma("sp", f"lstg{b}", stg[b][:], src.rearrange("(kc p) n -> p kc n", p=128), w=[r_stg[b]])
                        scale_w(stg[b], r_stg[b], W1[b], W2[b], r_W[b], jm, 128)
                        for tb in range(NTB):
                            tsl = slice(tb * 512, (tb + 1) * 512)
                            pi = tb % 2
                            for kc in range(NKC):
                                k.op("pe", lambda e, kc=kc: e.matmul(self.ps[pi][:, :], W1[b][:, kc, :], self.hT[:, kc, tsl],
                                                                     start=(kc == 0), stop=False),
                                     r=[r_W[b], self.r_hT[kc][tb]], w=[self.r_ps[pi]])
                            for kc in range(NKC):
                                k.op("pe", lambda e, kc=kc: e.matmul(self.ps[pi][:, :], W2[b][:, kc, :], hsT[:, kc, tsl],
                                                                     start=False, stop=(kc == NKC - 1)),
                                     r=[r_W[b], r_hs], w=[self.r_ps[pi]])
                            k.op("act", lambda e: e.activation(out=dst[:, tsl], in_=self.ps[pi][:, :], func=fn),
                                 r=[self.r_ps[pi]], w=[r_dst])
                    g2 = [sb(es1, f"g2_{dd}", [128, D], BF16) for dd in range(2)]
                    r_g2 = k.res()
                    for dd in range(2):
                        k.dma("pool", "g2", g2[dd][:], d["rw_g2"][j, dd], w=[r_g2])
                    gst = [sb(es1, f"gst{b}", [128, D]) for b in range(2)]
                    r_gst = [k.res() for _ in range(2)]
                    n = 0
                    for dd in range(2):
                        for tt in range(NTT):
                            b = n % 2
                            n += 1
                            for half in range(2):
                                pi = 2 + half
                                k.op("pe", lambda e, half=half, pi=pi: e.matmul(
                                    self.ps[pi][:, :], h1g[dd][:, tt * 128:(tt + 1) * 128], g2[dd][:, half * 512:(half + 1) * 512],
                                    start=True, stop=True), r=[r_h1g[dd], r_g2], w=[self.r_ps[pi]])
                                if half == 0:
                                    k.op("act", lambda e, pi=pi: e.copy(out=gst[b][:, 0:512], in_=self.ps[pi][:, :]),
                                         r=[self.r_ps[pi]], w=[r_gst[b]])
                                else:
                                    k.op("dve", lambda e, pi=pi: e.tensor_copy(out=gst[b][:, 512:1024], in_=self.ps[pi][:, :]),
                                         r=[self.r_ps[pi]], w=[r_gst[b]])
                            k.dma("sp", f"gst{b}", sc["GG"][dd, tt * 128:(tt + 1) * 128, :], gst[b][:], r=[r_gst[b]],
                                  w=[self.r_scr["GG"][dd][tt]])
                    k.barrier()
                with ExitStack() as es2:
                    stg = [sb(es2, f"pstg{b}", [128, NKC, 256]) for b in range(2)]
                    r_stg = [k.res() for _ in range(2)]
                    W1 = sb(es2, "pW1", [128, NKC, D], BF16)
                    W2 = sb(es2, "pW2", [128, NKC, D], BF16)
                    r_W = k.res()
                    rst = [sb(es2, f"rst{b}", [128, D]) for b in range(2)]
                    rstb = [rst[b][:, :].bitcast(BF16)[:, 0:D] for b in range(2)]
                    r_rst = [k.res() for _ in range(2)]
                    n = 0
                    for pj in range(3):
                        for qq in range(4):
                            sb_ = qq % 2
                            k.dma("sp", f"pstg{sb_}", stg[sb_][:],
                                  d["rw_w_rkv"][j, pj, :, qq * 256:(qq + 1) * 256].rearrange("(kc p) n -> p kc n", p=128),
                                  w=[r_stg[sb_]])
                            scale_w(stg[sb_], r_stg[sb_], W1, W2, r_W, pj, 256, col0=qq * 256)
                        for tt in range(NTT):
                            b = n % 2
                            n += 1
                            tb = tt // 4
                            tsl = slice(tt * 128, (tt + 1) * 128)
                            for half in range(2):
                                pi = half
                                for kc in range(NKC):
                                    k.op("pe", lambda e, kc=kc, half=half, pi=pi: e.matmul(
                                        self.ps[pi][:, :], self.hT[:, kc, tsl], W1[:, kc, half * 512:(half + 1) * 512],
                                        start=(kc == 0), stop=False), r=[r_W, self.r_hT[kc][tb]], w=[self.r_ps[pi]])
                                for kc in range(NKC):
                                    k.op("pe", lambda e, kc=kc, half=half, pi=pi: e.matmul(
                                        self.ps[pi][:, :], hsT[:, kc, tsl], W2[:, kc, half * 512:(half + 1) * 512],
                                        start=False, stop=(kc == NKC - 1)), r=[r_W, r_hs], w=[self.r_ps[pi]])
                                dst_t = rst[b] if pj < 2 else rstb[b]
                                if half == 0:
                                    k.op("act", lambda e, pi=pi: e.copy(out=dst_t[:, 0:512], in_=self.ps[pi][:, :]),
                                         r=[self.r_ps[pi]], w=[r_rst[b]])
                                else:
                                    k.op("dve", lambda e, pi=pi: e.tensor_copy(out=dst_t[:, 512:1024], in_=self.ps[pi][:, :]),
                                         r=[self.r_ps[pi]], w=[r_rst[b]])
                            if pj < 2:
                                k.dma("sp", f"rst{b}", sc["RAW"][pj, tt * 128:(tt + 1) * 128, :], rst[b][:], r=[r_rst[b]],
                                      w=[self.r_scr["RAW"][pj][tt]])
                            else:
                                k.dma("sp", f"rst{b}", sc["RAWV"][tt * 128:(tt + 1) * 128, :], rstb[b], r=[r_rst[b]],
                                      w=[self.r_scr["RAW"][pj][tt]])
                    k.barrier()
            with ExitStack() as es3:
                w2c = sb(es3, "w2c", [128, D], BF16)
                a2c = sb(es3, "a2c", [128, D], BF16)
                r_l2 = k.res()
                k.dma("pool", "l2w", w2c[:], d["rw_w2cat"][j], w=[r_l2])
                k.dma("pool", "l2w", a2c[:], d["rw_a2cat"][j], w=[r_l2])
                KKb = sb(es3, "KKb", [128, D]); KAb = sb(es3, "KAb", [128, D]); RKb = sb(es3, "RKb", [128, D])
                r_par = k.res()
                k.dma("sp", "rwp", KKb[:], d["rw_k_k"][j:j + 1, :].partition_broadcast(128), w=[r_par])
                k.dma("sp", "rwp", KAb[:], d["rw_k_a"][j:j + 1, :].partition_broadcast(128), w=[r_par])
                k.dma("sp", "rwp", RKb[:], d["rw_r_k"][j:j + 1, :].partition_broadcast(128), w=[r_par])
                b32 = sb(es3, "b32", [1, 4, D])
                r_b = k.res()
                k.dma("sp", "rwp2", b32[0:1, 0:2, :], d["rw_w0"][j:j + 1, :, :], w=[r_b])
                k.dma("sp", "rwp2", b32[0:1, 2:4, :], d["rw_a0"][j:j + 1, :, :], w=[r_b])
                tri = sb(es3, "tri", [128, 6, 128])
                csel = sb(es3, "csel", [128, 2])
                k.dma("sp", "rwp", tri[:], d["c_tri"].rearrange("q s t -> s q t"), w=[r_par])
                k.dma("sp", "rwp", csel[:], d["c_csel"][:, :], w=[r_par])
                hsc = [self.hT[:, kc, :].bitcast(F32) for kc in range(NKC)]
                mk2 = lambda n_, extra: [sb(es3, f"{n_}{q}", [128, D]) if extra[q] is None else extra[q] for q in range(2)]
                Rr_s = mk2("Rr", [None, hsc[0]]); Rk_s = mk2("Rk", [None, hsc[1]]); kk_s = mk2("kk", [None, hsc[2]])
                RRK_s = mk2("RRK", [None, hsc[3]]); T0_s = mk2("T0", [None, hsc[4]])
                SIG_s = mk2("SIG", [None, hsc[5]]); A_s = mk2("A_", [None, hsc[6]]); KD_s = mk2("KD", [None, hsc[7]])
                E1_s = mk2("E1", [None, None]); E2_s = mk2("E2", [None, None])
                O = [sb(es3, f"O{b}", [128, D], BF16) for b in range(2)]
                FMst = [sb(es3, f"FMst{b}", [64, 8, 128], BF16) for b in range(2)]
                identb = sb(es3, "identb3", [128, 128], BF16)
                r_idb = k.res()
                k.op("act", lambda e: e.copy(out=identb[:], in_=self.ident[:]), r=[self.r_const], w=[r_idb])
                psb = {4: self.ps[4][0:64, :].bitcast(BF16), 5: self.ps[5][0:64, :].bitcast(BF16)}
                sm = sb(es3, "a3sm", [128, 64])
                rkt_s = [sb(es3, f"rkt{q}", [128, 16]) for q in range(2)]
                r_rkt_s = [k.res(), k.res()]
                r2 = lambda: [k.res(), k.res()]
                r_Rr_s, r_Rk_s, r_kk_s, r_RRK_s, r_T0_s = r2(), r2(), r2(), r2(), r2()
                r_SIG_s, r_A_s, r_KD_s, r_E1_s, r_E2_s = r2(), r2(), r2(), r2(), r2()
                r_sm = k.res()
                r_O = [k.res() for _ in range(2)]
                r_FM = [k.res() for _ in range(2)]
                on = [0]
                v3 = lambda t: t[:, :].rearrange("p (h n) -> p h n", h=16)

                fmn = [0]

                def emit_fm(src, r_src, dd, q, tt):
                    on[0] += 1
                    for h8 in range(2):
                        fb = fmn[0] % 2
                        fmn[0] += 1
                        for h4 in range(2):
                            pi = 4 + h4 % 2
                            for hh in range(4):
                                h = h8 * 8 + h4 * 4 + hh
                                k.op("pe", lambda e, h=h, hh=hh, pi=pi: e.transpose(
                                    psb[pi][:, hh * 128:(hh + 1) * 128], src[:, h * 64:(h + 1) * 64], identb[:]),
                                    r=[r_src, r_idb], w=[self.r_ps[pi]])
                            if h4 == 0:
                                k.op("act", lambda e, h4=h4, pi=pi: e.copy(
                                    out=FMst[fb][:, h4 * 4:h4 * 4 + 4, :], in_=psb[pi][:, 0:512].rearrange("p (a t) -> p a t", a=4)),
                                    r=[self.r_ps[pi]], w=[r_FM[fb]])
                            else:
                                k.op("dve", lambda e, h4=h4, pi=pi: e.tensor_copy(
                                    out=FMst[fb][:, h4 * 4:h4 * 4 + 4, :], in_=psb[pi][:, 0:512].rearrange("p (a t) -> p a t", a=4)),
                                    r=[self.r_ps[pi]], w=[r_FM[fb]])
                        k.dma("sp", f"FMst{fb}", sc["FM"][dd, q, tt, :, h8 * 8:(h8 + 1) * 8, :], FMst[fb][:], r=[r_FM[fb]],
                              w=[self.r_scr["FM"][dd][q][tt]])

                def a3_load(tt):
                    q = tt % 2
                    rws = slice(tt * 128, (tt + 1) * 128)
                    k.dma("pool", f"a3r{q}", Rr_s[q][:], sc["RAW"][0, rws, :], r=[self.r_scr["RAW"][0][tt]], w=[r_Rr_s[q]])
                    k.dma("pool", f"a3k{q}", Rk_s[q][:], sc["RAW"][1, rws, :], r=[self.r_scr["RAW"][1][tt]], w=[r_Rk_s[q]])

                a3_load(0)
                for tt in range(NTT):
                    rows = slice(tt * 128, (tt + 1) * 128)
                    if tt + 1 < NTT:
                        a3_load(tt + 1)
                    q_ = tt % 2
                    Rr, Rk, kk, RRK = Rr_s[q_], Rk_s[q_], kk_s[q_], RRK_s[q_]
                    r_Rr, r_Rk, r_kk, r_RRK = r_Rr_s[q_], r_Rk_s[q_], r_kk_s[q_], r_RRK_s[q_]
                    T0, r_T0 = T0_s[0], r_T0_s[0]
                    k.op("dve", lambda e: e.tensor_tensor(out=kk[:], in0=Rk[:], in1=KKb[:], op=ALU.mult), r=[r_Rk, r_par], w=[r_kk])
                    k.op("dve", lambda e: e.tensor_tensor(out=T0[:], in0=kk[:], in1=kk[:], op=ALU.mult), r=[r_kk], w=[r_T0])
                    k.op("dve", lambda e: e.reduce_sum(out=sm[:, 0:16], in_=v3(T0), axis=AX.X), r=[r_T0], w=[r_sm])
                    k.op("act", lambda e: e.activation(out=sm[:, 0:16], in_=sm[:, 0:16], func=AF.Sqrt), r=[r_sm], w=[r_sm])
                    k.op("dve", lambda e: e.tensor_scalar_max(out=sm[:, 0:16], in0=sm[:, 0:16], scalar1=1e-12), r=[r_sm], w=[r_sm])
                    k.op("dve", lambda e: e.reciprocal(out=sm[:, 16:32], in_=sm[:, 0:16]), r=[r_sm], w=[r_sm])
                    k.op("dve", lambda e: e.tensor_tensor(out=v3(kk), in0=v3(kk),
                                                          in1=sm[:, 16:32].unsqueeze(2).to_broadcast([128, 16, 64]), op=ALU.mult),
                         r=[r_kk, r_sm], w=[r_kk])
                    k.op("dve", lambda e: e.tensor_tensor(out=RRK[:], in0=Rr[:], in1=RKb[:], op=ALU.mult), r=[r_Rr, r_par], w=[r_RRK])
                    for dd in range(2):
                        T0, SIG, A_, KD, E1, E2 = T0_s[dd], SIG_s[dd], A_s[dd], KD_s[dd], E1_s[dd], E2_s[dd]
                        r_T0, r_SIG, r_A, r_KD, r_E1, r_E2 = r_T0_s[dd], r_SIG_s[dd], r_A_s[dd], r_KD_s[dd], r_E1_s[dd], r_E2_s[dd]
                        for (h1, r_h1, w2t, bi, dst, r_dst) in ((h1w, r_h1w, w2c, dd, SIG, r_SIG), (h1a, r_h1a, a2c, 2 + dd, A_, r_A)):
                            for half in range(2):
                                pi = half
                                csl = slice(half * 512, (half + 1) * 512)
                                k.op("pe", lambda e: e.matmul(self.ps[pi][:, :], h1[dd * 64:(dd + 1) * 64, rows],
                                                              w2t[dd * 64:(dd + 1) * 64, csl], start=True, stop=False),
                                     r=[r_h1, r_l2], w=[self.r_ps[pi]])
                                k.op("pe", lambda e: e.matmul(self.ps[pi][:, :], self.ones[0:1, :], b32[0:1, bi, csl],
                                                              start=False, stop=True),
                                     r=[r_b, self.r_const], w=[self.r_ps[pi]])
                                k.op("act", lambda e: e.activation(out=dst[:, csl], in_=self.ps[pi][:, :], func=AF.Sigmoid),
                                     r=[self.r_ps[pi]], w=[r_dst])
                        k.op("dve", lambda e: e.scalar_tensor_tensor(out=KD[:], in0=A_[:], scalar=-1.0, in1=KAb[:],
                                                                     op0=ALU.add, op1=ALU.mult), r=[r_A, r_par], w=[r_KD])
                        k.op("dve", lambda e: e.scalar_tensor_tensor(out=KD[:], in0=KD[:], scalar=1.0, in1=Rk[:],
                                                                     op0=ALU.add, op1=ALU.mult), r=[r_KD, r_Rk], w=[r_KD])
                        k.op("dve", lambda e: e.tensor_tensor(out=A_[:], in0=A_[:], in1=kk[:], op=ALU.mult), r=[r_A, r_kk], w=[r_A])
                        k.op("dve", lambda e: e.tensor_tensor(out=T0[:], in0=RRK[:], in1=KD[:], op=ALU.mult), r=[r_RRK, r_KD], w=[r_T0])
                        rkt, r_rkt = rkt_s[dd], r_rkt_s[dd]
                        k.op("dve", lambda e: e.reduce_sum(out=rkt[:, :], in_=v3(T0), axis=AX.X), r=[r_T0], w=[r_rkt])
                        k.dma("sp", f"rkt{dd}", sc["RK"][dd, rows, :], rkt[:], r=[r_rkt], w=[self.r_scr["RK"][dd][tt]])
                        for h in range(16):
                            k.op("pe", lambda e, h=h: e.matmul(self.ps[6][0:64, h * 2:h * 2 + 2], SIG[:, h * 64:(h + 1) * 64], csel[:, :],
                                                               start=True, stop=True), r=[r_SIG, r_par], w=[self.r_ps[6]])
                        k.op("act", lambda e: e.activation(out=PCt[:, dd, tt, :, :], in_=self.ps[6][0:64, 0:32].rearrange("p (h c) -> p h c", c=2),
                                                           func=AF.Exp), r=[self.r_ps[6]], w=[r_PC])
                        def cums(kind, outs):
                            for half in range(2):
                                pi = 2 + half
                                csl = slice(half * 512, (half + 1) * 512)
                                k.op("pe", lambda e: e.matmul(self.ps[pi][:, :], tri[:, dd * 3 + kind, :], SIG[:, csl],
                                                              start=True, stop=True), r=[r_SIG, r_par], w=[self.r_ps[pi]])
                                for (dst, r_dst, scl) in outs:
                                    k.op("act", lambda e, dst=dst, scl=scl: e.activation(out=dst[:, csl], in_=self.ps[pi][:, :],
                                                                                         func=AF.Exp, scale=scl),
                                         r=[self.r_ps[pi]], w=[r_dst])
                        cums(0, [(E1, r_E1, 1.0), (E2, r_E2, -1.0)])
                        ob = on[0] % 2
                        k.op("dve", lambda e: e.tensor_tensor(out=O[ob][:], in0=Rr[:], in1=E1[:], op=ALU.mult), r=[r_Rr, r_E1], w=[r_O[ob]])
                        emit_fm(O[ob], r_O[ob], dd, 0, tt)
                        ob = on[0] % 2
                        k.op("dve", lambda e: e.tensor_tensor(out=O[ob][:], in0=A_[:], in1=E2[:], op=ALU.mult), r=[r_A, r_E2], w=[r_O[ob]])
                        emit_fm(O[ob], r_O[ob], dd, 2, tt)
                        ob = on[0] % 2
                        k.op("dve", lambda e: e.tensor_tensor(out=O[ob][:], in0=KD[:], in1=E2[:], op=ALU.mult), r=[r_KD, r_E2], w=[r_O[ob]])
                        emit_fm(O[ob], r_O[ob], dd, 3, tt)
                        cums(1, [(E1, r_E1, 1.0)])
                        ob = on[0] % 2
                        k.op("dve", lambda e: e.scalar_tensor_tensor(out=O[ob][:], in0=kk[:], scalar=-1.0, in1=E1[:],
                                                                     op0=ALU.mult, op1=ALU.mult), r=[r_kk, r_E1], w=[r_O[ob]])
                        emit_fm(O[ob], r_O[ob], dd, 1, tt)
                        cums(2, [(E2, r_E2, 1.0)])
                        for q, (src, r_src) in enumerate(((A_, r_A), (KD, r_KD))):
                            ob = on[0] % 2
                            on[0] += 1
                            k.op("dve" if q == 0 else "pool", lambda e, src=src: e.tensor_tensor(out=O[ob][:], in0=src[:], in1=E2[:], op=ALU.mult),
                                 r=[r_src, r_E2], w=[r_O[ob]])
                            k.dma("sp", f"Otm{ob}", sc["TM"][dd, q, rows, :], O[ob][:], r=[r_O[ob]], w=[self.r_scr["TM"][dd][q][tt]])
                k.barrier()

    def rwkv_B(self, i, j, PCt, r_PC):
        nc, k, d = self.nc, self.k, self.dram
        sc = self.scr
        oT = self.hT
        r_oT = self.r_hT
        NH = 8
        with ExitStack() as es:
            sb = lambda n, s_, dt=F32: es.enter_context(self.sbuf(n, s_, dt))
            for kc in range(NKC):
                k.op("pool", lambda e, kc=kc: e.memset(oT[:, kc, :], 0.0), w=r_oT[kc])
            msk = sb("msk", [64, 6, 64])
            LNW = sb("LNW", [64, D]); LNB = sb("LNB", [64, D])
            epsg = sb("epsg", [64, 1])
            r_cb = k.res()
            k.dma("sp", "rbc", msk[:], d["c_mask"].rearrange("q s t -> s q t"), w=[r_cb])
            k.dma("sp", "rbc", LNW[:], d["rw_ln_w"][j:j + 1, :].partition_broadcast(64), w=[r_cb])
            k.dma("sp", "rbc", LNB[:], d["rw_ln_b"][j:j + 1, :].partition_broadcast(64), w=[r_cb])
            k.op("dve", lambda e: e.memset(epsg[:], 64e-5), w=[r_cb])
            i64 = self.ident[0:64, 0:64]
            bcm = lambda q: msk[:, q, :].unsqueeze(1).to_broadcast([64, NH, 64])
            bci = i64.unsqueeze(1).to_broadcast([64, NH, 64])

            class Chain:
                pass
            chains = []
            for ci, (dd, hg) in enumerate(((0, 0), (1, 0), (0, 1), (1, 1))):
                c = Chain()
                c.dd, c.hg = dd, hg
                nm = f"c{ci}"
                c.nm = nm
                c.ld = {n_: sb(f"{nm}_{n_}", [64, NH, 64], BF16) for n_ in ("RT", "AT", "BT", "KT", "Bh", "Kh", "Vt")}
                c.ld["Gt"] = sb(f"{nm}_Gt", [64, NH, 64])
                c.r_ld = {n_: k.res() for n_ in c.ld}
                c.rk = sb(f"{nm}_rk", [64, NH]); c.r_rk = k.res()
                c.sl = [sb(f"{nm}_s{q}", [64, NH, 64], BF16) for q in range(6)]
                c.r_sl = [k.res() for _ in range(6)]
                c.ST = sb(f"{nm}_ST", [64, NH, 64]); c.r_ST = k.res()
                c.STb = sb(f"{nm}_STb", [64, NH, 64], BF16); c.r_STb = k.res()
                c.y = sb(f"{nm}_y", [64, NH, 64]); c.r_y = k.res()
                c.sq = sb(f"{nm}_sq", [64, NH, 64]); c.r_sq = k.res()
                c.sm = sb(f"{nm}_sm", [64, 8 * NH]); c.r_sm = k.res()
                c.pb = [ci * 2, ci * 2 + 1]
                c.pbi = 0
                chains.append(c)

            def p3(pi):
                return self.ps[pi][0:64, :].rearrange("p (h n) -> p h n", h=NH)

            def mm(c, pi, pairs, rs):
                for h in range(NH):
                    for q, (lt, rt) in enumerate(pairs):
                        k.op("pe", lambda e, h=h, lt=lt, rt=rt, q=q: e.matmul(
                            self.ps[pi][0:64, h * 64:(h + 1) * 64], lt[:, h, :], rt[:, h, :],
                            start=(q == 0), stop=(q == len(pairs) - 1)), r=rs, w=[self.r_ps[pi]])

            def nb(c):
                c.pbi = (c.pbi + 1) % 2
                return c.pb[c.pbi]

            def chunk_steps(c, n):
                dd, hg = c.dd, c.hg
                ct = n if dd == 0 else 31 - n
                tt, half = ct // 2, ct % 2
                rows = slice(ct * 64, (ct + 1) * 64)
                hs = slice(hg * NH, (hg + 1) * NH)
                cs = slice(hg * 512, (hg + 1) * 512)
                L, R = c.ld, c.r_ld
                for q, n_ in enumerate(("RT", "AT", "BT", "KT")):
                    k.dma("sp", f"{c.nm}{n_}", L[n_][:], sc["FM"][dd, q, tt, :, hs, half * 64:(half + 1) * 64],
                          r=[self.r_scr["FM"][dd][q][tt]], w=[R[n_]])
                for q, n_ in enumerate(("Bh", "Kh")):
                    k.dma("sp", f"{c.nm}{n_}", L[n_][:].rearrange("p h n -> p (h n)"), sc["TM"][dd, q, rows, cs],
                          r=[self.r_scr["TM"][dd][q][ct // 2]], w=[R[n_]])
                yield
                P1, P1T, P2, P2T, T, U6 = range(6)
                S_, RS = c.sl, c.r_sl
                pi = nb(c)
                mm(c, pi, [(L["BT"], L["AT"])], [R["BT"], R["AT"]])
                k.op("dve", lambda e: e.tensor_tensor(out=S_[P1][:], in0=p3(pi), in1=bcm(dd * 3 + 0), op=ALU.mult),
                     r=[self.r_ps[pi], r_cb], w=[RS[P1]])
                yield
                pi = nb(c)
                mm(c, pi, [(L["AT"], L["BT"])], [R["BT"], R["AT"]])
                k.op("dve", lambda e: e.tensor_tensor(out=S_[P1T][:], in0=p3(pi), in1=bcm(dd * 3 + 1), op=ALU.mult),
                     r=[self.r_ps[pi], r_cb], w=[RS[P1T]])
                k.op("dve", lambda e: e.tensor_tensor(out=S_[T][:], in0=S_[P1][:], in1=bci, op=ALU.add),
                     r=[RS[P1], self.r_const], w=[RS[T]])
                yield
                a, aT, b_, bT = P1, P1T, P2, P2T
                for lvl in range(5):
                    pi = nb(c)
                    mm(c, pi, [(S_[a], S_[aT])], [RS[a], RS[aT]])
                    k.op("act", lambda e, pi=pi, bT=bT: e.copy(out=S_[bT][:], in_=p3(pi)), r=[self.r_ps[pi]], w=[RS[bT]])
                    if lvl < 4:
                        pi2 = nb(c)
                        mm(c, pi2, [(S_[aT], S_[a])], [RS[a], RS[aT]])
                        k.op("act", lambda e, pi2=pi2, b_=b_: e.copy(out=S_[b_][:], in_=p3(pi2)), r=[self.r_ps[pi2]], w=[RS[b_]])
                    yield
                    pi = nb(c)
                    mm(c, pi, [(S_[bT], S_[T])], [RS[bT], RS[T]])
                    k.op("dve", lambda e, pi=pi: e.tensor_tensor(out=S_[T][:], in0=S_[T][:], in1=p3(pi), op=ALU.add),
                         r=[self.r_ps[pi], RS[T]], w=[RS[T]])
                    yield
                    a, aT, b_, bT = b_, bT, a, aT
                Aak, Abr, Akr, WT = P1, P1T, P2, P2T
                for (dst, lt, rt, mq, eng) in ((Aak, "KT", "AT", 0, "dve"), (Abr, "BT", "RT", 2, "pool"), (Akr, "KT", "RT", 2, "dve")):
                    pi = nb(c)
                    mm(c, pi, [(L[lt], L[rt])], [R[lt], R[rt]])
                    k.op("dve", lambda e, pi=pi, dst=dst, mq=mq: e.tensor_tensor(out=S_[dst][:], in0=p3(pi), in1=bcm(dd * 3 + mq), op=ALU.mult),
                         r=[self.r_ps[pi], r_cb], w=[RS[dst]])
                    yield
                k.dma("pool", f"{c.nm}Vt", L["Vt"][:].rearrange("p h n -> p (h n)"), sc["RAWV"][rows, cs],
                      r=[self.r_scr["RAW"][2][ct // 2]], w=[R["Vt"]])
                k.dma("pool", f"{c.nm}Gt", L["Gt"][:].rearrange("p h n -> p (h n)"), sc["GG"][dd, rows, cs],
                      r=[self.r_scr["GG"][dd][ct // 2]], w=[R["Gt"]])
                k.dma("pool", f"{c.nm}rk", c.rk[:], sc["RK"][dd, rows, hs], r=[self.r_scr["RK"][dd][ct // 2]], w=[c.r_rk])
                pi = nb(c)
                mm(c, pi, [(L["AT"], c.STb), (S_[Aak], L["Vt"])], [R["AT"], c.r_STb, RS[Aak], R["Vt"]])
                k.op("act", lambda e: e.copy(out=S_[WT][:], in_=p3(pi)), r=[self.r_ps[pi]], w=[RS[WT]])
                yield
                pi = nb(c)
                mm(c, pi, [(S_[T], S_[WT])], [RS[T], RS[WT]])
                k.op("act", lambda e: e.copy(out=S_[U6][:], in_=p3(pi)), r=[self.r_ps[pi]], w=[RS[U6]])
                yield
                piy = nb(c)
                mm(c, piy, [(L["RT"], c.STb), (S_[Abr], S_[U6]), (S_[Akr], L["Vt"])],
                   [R["RT"], c.r_STb, RS[Abr], RS[U6], RS[Akr], R["Vt"]])
                k.op("act", lambda e: e.copy(out=c.y[:], in_=p3(piy)), r=[self.r_ps[piy]], w=[c.r_y])
                pis = nb(c)
                mm(c, pis, [(L["Bh"], S_[U6]), (L["Kh"], L["Vt"])], [R["Bh"], RS[U6], R["Kh"], R["Vt"]])
                pcb = PCt[:, dd, tt, hs, half].unsqueeze(2).to_broadcast([64, NH, 64])
                k.op("dve", lambda e: e.tensor_tensor(out=c.ST[:], in0=c.ST[:], in1=pcb, op=ALU.mult), r=[c.r_ST, r_PC], w=[c.r_ST])
                k.op("dve", lambda e: e.tensor_tensor(out=c.ST[:], in0=c.ST[:], in1=p3(pis), op=ALU.add), r=[c.r_ST, self.r_ps[pis]], w=[c.r_ST])
                k.op("act", lambda e: e.copy(out=c.STb[:], in_=c.ST[:]), r=[c.r_ST], w=[c.r_STb])
                yield

            def epi_steps(c, n):
                dd, hg = c.dd, c.hg
                ct = n if dd == 0 else 31 - n
                cs = slice(hg * 512, (hg + 1) * 512)
                L, R = c.ld, c.r_ld
                sm, r_sm = c.sm, c.r_sm
                bl = lambda a0: sm[:, a0:a0 + NH].unsqueeze(2).to_broadcast([64, NH, 64])
                dv = lambda fn, r, w: k.op("dve", fn, r=r, w=w)
                dv(lambda e: e.reduce_sum(out=sm[:, 0:NH], in_=c.y[:], axis=AX.X), [c.r_y], [r_sm])
                k.op("act", lambda e: e.activation(out=c.sq[:], in_=c.y[:], func=AF.Square), r=[c.r_y], w=[c.r_sq])
                yield
                dv(lambda e: e.reduce_sum(out=sm[:, NH:2 * NH], in_=c.sq[:], axis=AX.X), [c.r_sq], [r_sm])
                dv(lambda e: e.tensor_scalar_mul(out=sm[:, 0:2 * NH], in0=sm[:, 0:2 * NH], scalar1=1.0 / 64), [r_sm], [r_sm])
                yield
                dv(lambda e: e.tensor_tensor(out=sm[:, 2 * NH:3 * NH], in0=sm[:, 0:NH], in1=sm[:, 0:NH], op=ALU.mult), [r_sm], [r_sm])
                dv(lambda e: e.tensor_tensor(out=sm[:, 3 * NH:4 * NH], in0=sm[:, NH:2 * NH], in1=sm[:, 2 * NH:3 * NH], op=ALU.subtract), [r_sm], [r_sm])
                k.op("act", lambda e: e.activation(out=sm[:, 4 * NH:5 * NH], in_=sm[:, 3 * NH:4 * NH], func=AF.Sqrt, bias=epsg[:, 0:1], scale=1.0),
                     r=[r_sm, r_cb], w=[r_sm])
                dv(lambda e: e.reciprocal(out=sm[:, 5 * NH:6 * NH], in_=sm[:, 4 * NH:5 * NH]), [r_sm], [r_sm])
                yield
                z, r_z = c.y, c.r_y
                dv(lambda e: e.tensor_tensor(out=z[:], in0=c.y[:], in1=bl(0), op=ALU.subtract), [c.r_y, r_sm], [r_z])
                yield
                dv(lambda e: e.tensor_tensor(out=z[:], in0=z[:], in1=bl(5 * NH), op=ALU.mult), [r_z, r_sm], [r_z])
                yield
                zf = z[:].rearrange("p h n -> p (h n)")
                k.op("dve", lambda e: e.tensor_tensor(out=zf, in0=zf, in1=LNW[:, cs], op=ALU.mult), r=[r_z, r_cb], w=[r_z])
                yield
                k.op("dve", lambda e: e.tensor_tensor(out=zf, in0=zf, in1=LNB[:, cs], op=ALU.add), r=[r_z, r_cb], w=[r_z])
                yield
                dv(lambda e: e.tensor_tensor(out=c.sq[:], in0=L["Vt"][:], in1=c.rk[:, :].unsqueeze(2).to_broadcast([64, NH, 64]), op=ALU.mult),
                   [R["Vt"], c.r_rk], [c.r_sq])
                yield
                k.op("dve", lambda e: e.tensor_tensor(out=z[:], in0=z[:], in1=c.sq[:], op=ALU.add), r=[r_z, c.r_sq], w=[r_z])
                yield
                k.op("dve", lambda e: e.tensor_tensor(out=z[:], in0=z[:], in1=L["Gt"][:], op=ALU.mult), r=[r_z, R["Gt"]], w=[r_z])
                yield
                pt = nb(c)
                for q in range(4):
                    k.op("pe", lambda e, q=q: e.transpose(self.ps[pt][:, q * 64:(q + 1) * 64], zf[:, q * 128:(q + 1) * 128], i64),
                         r=[r_z, self.r_const], w=[self.r_ps[pt]])
                tb = ct // 8
                osl = oT[:, hg * 4:hg * 4 + 4, ct * 64:(ct + 1) * 64]
                k.op("dve", lambda e: e.tensor_tensor(out=osl, in0=osl, in1=self.ps[pt][:, 0:256].rearrange("p (q t) -> p q t", q=4), op=ALU.add),
                     r=[self.r_ps[pt]] + [r_oT[hg * 4 + q][tb] for q in range(4)], w=[r_oT[hg * 4 + q][tb] for q in range(4)])
                yield

            nchunks = self.cfg.get("rw_chunks", 32)
            for c in chains:
                k.op("pool", lambda e, c=c: e.memset(c.ST[:], 0.0), w=[c.r_ST])
                k.op("pool", lambda e, c=c: e.memset(c.STb[:], 0.0), w=[c.r_STb])
            for n in range(nchunks + 1):
                live = []
                for c in chains:
                    if n < nchunks:
                        live.append(chunk_steps(c, n))
                    if n >= 1:
                        live.append(epi_steps(c, n - 1))
                while live:
                    for g_ in list(live):
                        try:
                            next(g_)
                        except StopIteration:
                            live.remove(g_)
            k.barrier()
        with ExitStack() as es:
            wo = es.enter_context(self.sbuf("rwo", [128, NKC, D], BF16))
            r_wo = k.res()
            k.dma("pool", "rwo", wo[:], d["rw_w_o"][j].rearrange("(kc p) n -> p kc n", p=128), w=[r_wo])
            for tb in range(NTB):
                tsl = slice(tb * 512, (tb + 1) * 512)
                for dc in range(NKC):
                    pi = dc % 2
                    for kc in range(NKC):
                        k.op("pe", lambda e, kc=kc, dc=dc, pi=pi: e.matmul(self.ps[pi][:, :], wo[:, kc, dc * 128:(dc + 1) * 128], oT[:, kc, tsl],
                                                                           start=(kc == 0), stop=(kc == NKC - 1)),
                             r=[r_wo, r_oT[kc][tb]], w=[self.r_ps[pi]])
                    k.op("dve", lambda e, dc=dc, pi=pi: e.scalar_tensor_tensor(
                        out=self.xT[:, dc, tsl], in0=self.ps[pi][:, :], scalar=self.modT[:, i, 16 + dc:17 + dc],
                        in1=self.xT[:, dc, tsl], op0=ALU.mult, op1=ALU.add),
                        r=[self.r_ps[pi], self.r_mod], w=[self.r_xT[dc][tb]])
            k.barrier()

    def attn(self, i):
        nc, k, d = self.nc, self.k, self.dram
        j = i // 2
        DIL = [1, 4, 16]
        ss = lambda start, n, step: slice(start, start + (n - 1) * step + 1, step)
        all_hT = [x for y in self.r_hT for x in y]
        with ExitStack() as es:
            identb = es.enter_context(self.sbuf("identb", [128, 128], BF16))
            r_idb = k.res()
            k.op("act", lambda e: e.copy(out=identb[:], in_=self.ident[:]), r=[self.r_const], w=[r_idb])
            w3 = [es.enter_context(self.sbuf(f"w3_{b}", [128, 3, NKC, 128], BF16)) for b in range(2)]
            r_w3 = [k.res() for _ in range(2)]
            wo = [es.enter_context(self.sbuf(f"wo_{b}", [128, D], BF16)) for b in range(2)]
            r_wo = [k.res() for _ in range(2)]
            bias = [es.enter_context(self.sbuf(f"ab_{b}", [128, 384], F32)) for b in range(2)]
            r_bias = [k.res() for _ in range(2)]
            QT = es.enter_context(self.sbuf("QT", [128, S], BF16))
            KT = es.enter_context(self.sbuf("KT", [128, S], BF16))
            V = es.enter_context(self.sbuf("Vb", [128, 16, 128], BF16))
            r_QT, r_KT, r_V = k.res(), k.res(), k.res()
            Og = [es.enter_context(self.sbuf(f"Og{g}", [128, S], F32)) for g in range(3)]
            r_Og = [k.res() for _ in range(3)]
            LSE = es.enter_context(self.sbuf("LSE", [1, 3, S], F32))
            r_LSE = k.res()
            rowA = es.enter_context(self.sbuf("rowA", [1, S], F32))
            r_rowA = k.res()
            NB = 4
            sc = [es.enter_context(self.sbuf(f"sc{b}", [128, 384], F32)) for b in range(NB)]
            pn = [es.enter_context(self.sbuf(f"pn{b}", [128, 384], BF16)) for b in range(NB)]
            st = [es.enter_context(self.sbuf(f"ast{b}", [128, 8], F32)) for b in range(NB)]
            r_blk = [k.res() for _ in range(NB)]
            PT = [es.enter_context(self.sbuf(f"PT{b}", [128, 384], BF16)) for b in range(2)]
            r_PT = [k.res() for _ in range(2)]
            mrg = es.enter_context(self.sbuf("mrg", [128, S], BF16))
            r_mrg = k.res()
            tmpM = es.enter_context(self.sbuf("tmpM", [128, 512], F32))
            t2 = es.enter_context(self.sbuf("t2M", [128, 512], F32))
            r_tmpM, r_t2 = k.res(), k.res()
            psT = self.ps[4][:, :].bitcast(BF16)

            def load_unit(u):
                g, h = u % 3, u // 3
                b = u % 2
                for t in range(3):
                    c0 = ((g * 3 + t) * 8 + h) * 128
                    k.dma("pool", f"w3_{b}", w3[b][:, t, :, :],
                          d["at_w_qkv"][j, :, c0:c0 + 128].rearrange("(kc p) n -> p kc n", p=128), w=[r_w3[b]])
                k.dma("sp", f"ab_{b}", bias[b][:, :], d["c_abias"][g * 8 + h], w=[r_bias[b]])

            def load_wo(h):
                b = h % 2
                k.dma("pool", f"wo_{b}", wo[b][:, :], d["at_w_o"][j, h * 128:(h + 1) * 128, :], w=[r_wo[b]])

            nblk_it = [0]

            def stageA(u, blk):
                g, h = u % 3, u // 3
                dil = DIL[g]
                nblk = 16 // dil
                r_, ib = blk // nblk, blk % nblk
                kb0, kb1 = max(ib - 1, 0), min(ib + 1, nblk - 1)
                nkb = kb1 - kb0 + 1
                nk = nkb * 128
                bc0 = (kb0 - (ib - 1)) * 128
                qsl = ss(r_ + dil * ib * 128, 128, dil)
                ksl = ss(r_ + dil * kb0 * 128, nk, dil)
                n = nblk_it[0]
                nblk_it[0] += 1
                sb_ = n % NB
                psc = 3 if n % 2 == 0 else 6
                bb = u % 2
                k.op("pe", lambda e: e.matmul(self.ps[psc][:, 0:nk], QT[:, qsl], KT[:, ksl], start=True, stop=True),
                     r=[r_QT, r_KT], w=[self.r_ps[psc]])
                k.op("dve", lambda e: e.tensor_tensor(out=sc[sb_][:, 0:nk], in0=self.ps[psc][:, 0:nk],
                                                      in1=bias[bb][:, bc0:bc0 + nk], op=ALU.add),
                     r=[self.r_ps[psc], r_bias[bb]], w=[r_blk[sb_]])
                k.op("dve", lambda e: e.reduce_max(out=st[sb_][:, 0:1], in_=sc[sb_][:, 0:nk], axis=AX.X),
                     r=[r_blk[sb_]], w=[r_blk[sb_]])
                k.op("dve", lambda e: e.tensor_scalar_mul(out=st[sb_][:, 1:2], in0=st[sb_][:, 0:1], scalar1=-1.0),
                     r=[r_blk[sb_]], w=[r_blk[sb_]])
                k.op("act", lambda e: e.activation(out=sc[sb_][:, 0:nk], in_=sc[sb_][:, 0:nk], func=AF.Exp,
                                                   bias=st[sb_][:, 1:2], scale=1.0, accum_out=st[sb_][:, 2:3]),
                     r=[r_blk[sb_]], w=[r_blk[sb_]])
                k.op("act", lambda e: e.activation(out=st[sb_][:, 4:5], in_=st[sb_][:, 2:3], func=AF.Ln),
                     r=[r_blk[sb_]], w=[r_blk[sb_]])
                k.op("dve", lambda e: e.reciprocal(out=st[sb_][:, 3:4], in_=st[sb_][:, 2:3]),
                     r=[r_blk[sb_]], w=[r_blk[sb_]])
                k.op("dve", lambda e: e.tensor_scalar_mul(out=pn[sb_][:, 0:nk], in0=sc[sb_][:, 0:nk],
                                                          scalar1=st[sb_][:, 3:4]),
                     r=[r_blk[sb_]], w=[r_blk[sb_]])
                k.op("dve", lambda e: e.tensor_tensor(out=st[sb_][:, 5:6], in0=st[sb_][:, 4:5], in1=st[sb_][:, 0:1],
                                                      op=ALU.add),
                     r=[r_blk[sb_]], w=[r_blk[sb_]])
                return (g, r_, kb0, nkb, nblk, qsl, sb_, n)

            def stageB(desc):
                g, r_, kb0, nkb, nblk, qsl, sb_, n = desc
                nk = nkb * 128
                pb = n % 2
                for c in range(nkb):
                    k.op("pe", lambda e, c=c: e.transpose(psT[:, c * 128:(c + 1) * 128], pn[sb_][:, c * 128:(c + 1) * 128],
                                                          identb[:]),
                         r=[r_blk[sb_], r_idb], w=[self.r_ps[4]])
                k.op("act", lambda e: e.copy(out=PT[pb][:, 0:nk], in_=psT[:, 0:nk]),
                     r=[self.r_ps[4]], w=[r_PT[pb]])
                k.op("pe", lambda e: e.transpose(self.ps[5][0:1, 128:256], st[sb_][:, 5:6], self.ident[:]),
                     r=[r_blk[sb_], self.r_const], w=[self.r_ps[5]])
                for c in range(nkb):
                    vb = r_ * nblk + kb0 + c
                    k.op("pe", lambda e, c=c, vb=vb: e.matmul(self.ps[5][:, 0:128], V[:, vb, :], PT[pb][:, c * 128:(c + 1) * 128],
                                                              start=(c == 0), stop=(c == nkb - 1)),
                         r=[r_V, r_PT[pb]], w=[self.r_ps[5]])
                k.op("dve", lambda e: e.tensor_copy(out=Og[g][:, qsl], in_=self.ps[5][:, 0:128]),
                     r=[self.r_ps[5]], w=[r_Og[g]])
                k.op("dve", lambda e: e.tensor_copy(out=LSE[0:1, g, qsl], in_=self.ps[5][0:1, 128:256]),
                     r=[self.r_ps[5]], w=[r_LSE])

            def proj_unit(u):
                g, h = u % 3, u // 3
                b = u % 2
                dil = DIL[g]
                nblk = 16 // dil
                for tb in range(NTB):
                    tsl = slice(tb * 512, (tb + 1) * 512)
                    for kc in range(NKC):
                        k.op("pe", lambda e, kc=kc: e.matmul(self.ps[0][:, :], w3[b][:, 0, kc, :], self.hT[:, kc, tsl],
                                                             start=(kc == 0), stop=(kc == NKC - 1)),
                             r=[r_w3[b], self.r_hT[kc][tb]], w=[self.r_ps[0]])
                    k.op("act", lambda e: e.mul(out=QT[:, tsl], in_=self.ps[0][:, :], mul=float(128 ** -0.5)),
                         r=[self.r_ps[0]], w=[r_QT])
                    for kc in range(NKC):
                        k.op("pe", lambda e, kc=kc: e.matmul(self.ps[1][:, :], w3[b][:, 1, kc, :], self.hT[:, kc, tsl],
                                                             start=(kc == 0), stop=(kc == NKC - 1)),
                             r=[r_w3[b], self.r_hT[kc][tb]], w=[self.r_ps[1]])
                    k.op("act", lambda e: e.copy(out=KT[:, tsl], in_=self.ps[1][:, :]),
                         r=[self.r_ps[1]], w=[r_KT])
                for b4 in range(4):
                    pv = 2 if b4 % 2 == 0 else 7
                    for q in range(4):
                        blk = b4 * 4 + q
                        r_, ib = blk // nblk, blk % nblk
                        tsl = ss(r_ + dil * ib * 128, 128, dil)
                        for kc in range(NKC):
                            k.op("pe", lambda e, kc=kc, q=q, tsl=tsl: e.matmul(
                                self.ps[pv][:, q * 128:(q + 1) * 128], self.hT[:, kc, tsl], w3[b][:, 2, kc, :],
                                start=(kc == 0), stop=(kc == NKC - 1)),
                                r=[r_w3[b]] + all_hT, w=[self.r_ps[pv]])
                    k.op("act", lambda e, b4=b4: e.copy(out=V[:, b4 * 4:b4 * 4 + 4, :],
                                                        in_=self.ps[pv][:, :].rearrange("p (q n) -> p q n", q=4)),
                         r=[self.r_ps[pv]], w=[r_V])

            def merge_head(h):
                b = h % 2
                row = lambda g: LSE[0:1, g, :]
                dv = lambda fn, r, w: k.op("dve", fn, r=r, w=w)
                dv(lambda e: e.tensor_tensor(out=rowA[0:1, :], in0=row(0), in1=row(1), op=ALU.max), [r_LSE], [r_rowA])
                dv(lambda e: e.tensor_tensor(out=rowA[0:1, :], in0=rowA[0:1, :], in1=row(2), op=ALU.max), [r_LSE, r_rowA], [r_rowA])
                for g in range(3):
                    dv(lambda e, g=g: e.tensor_tensor(out=row(g), in0=row(g), in1=rowA[0:1, :], op=ALU.subtract),
                       [r_LSE, r_rowA], [r_LSE])
                k.op("act", lambda e: e.activation(out=LSE[0:1, :, :], in_=LSE[0:1, :, :], func=AF.Exp), r=[r_LSE], w=[r_LSE])
                dv(lambda e: e.tensor_tensor(out=rowA[0:1, :], in0=row(0), in1=row(1), op=ALU.add), [r_LSE], [r_rowA])
                dv(lambda e: e.tensor_tensor(out=rowA[0:1, :], in0=rowA[0:1, :], in1=row(2), op=ALU.add), [r_LSE, r_rowA], [r_rowA])
                dv(lambda e: e.reciprocal(out=rowA[0:1, :], in_=rowA[0:1, :]), [r_rowA], [r_rowA])
                for g in range(3):
                    dv(lambda e, g=g: e.tensor_tensor(out=row(g), in0=row(g), in1=rowA[0:1, :], op=ALU.mult),
                       [r_LSE, r_rowA], [r_LSE])
                for tb in range(NTB):
                    tsl = slice(tb * 512, (tb + 1) * 512)
                    for g in range(3):
                        pi = 6 if g % 2 == 0 else 3
                        k.op("pe", lambda e, g=g, pi=pi: e.matmul(self.ps[pi][:, :], self.ones[0:1, :], LSE[0:1, g, tsl],
                                                                  start=True, stop=True),
                             r=[r_LSE, self.r_const], w=[self.r_ps[pi]])
                        if g == 0:
                            dv(lambda e, pi=pi: e.tensor_tensor(out=tmpM[:, :], in0=Og[0][:, tsl], in1=self.ps[pi][:, :], op=ALU.mult),
                               [r_Og[0], self.r_ps[pi]], [r_tmpM])
                        else:
                            dv(lambda e, g=g, pi=pi: e.tensor_tensor(out=t2[:, :], in0=Og[g][:, tsl], in1=self.ps[pi][:, :], op=ALU.mult),
                               [r_Og[g], self.r_ps[pi]], [r_t2])
                            if g == 1:
                                k.op("dve", lambda e: e.tensor_tensor(out=tmpM[:, :], in0=tmpM[:, :], in1=t2[:, :], op=ALU.add),
                                     r=[r_tmpM, r_t2], w=[r_tmpM])
                            else:
                                k.op("dve", lambda e: e.tensor_tensor(out=mrg[:, tsl], in0=tmpM[:, :], in1=t2[:, :], op=ALU.add),
                                     r=[r_tmpM, r_t2], w=[r_mrg])
                for tb in range(NTB):
                    tsl = slice(tb * 512, (tb + 1) * 512)
                    for dc in range(NKC):
                        pi = 7 if dc % 2 == 0 else 0
                        k.op("pe", lambda e, dc=dc, pi=pi: e.matmul(self.ps[pi][:, :], wo[b][:, dc * 128:(dc + 1) * 128], mrg[:, tsl],
                                                                    start=True, stop=True),
                             r=[r_wo[b], r_mrg], w=[self.r_ps[pi]])
                        k.op("dve", lambda e, dc=dc, pi=pi: e.scalar_tensor_tensor(
                            out=self.xT[:, dc, tsl], in0=self.ps[pi][:, :], scalar=self.modT[:, i, 16 + dc:17 + dc],
                            in1=self.xT[:, dc, tsl], op0=ALU.mult, op1=ALU.add),
                            r=[self.r_ps[pi], self.r_mod], w=[self.r_xT[dc][tb]])

            LA = 2
            pend_merge = [None]
            load_unit(0)
            for u in range(24):
                g, h = u % 3, u // 3
                if g == 0:
                    load_wo(h)
                if u + 1 < 24:
                    load_unit(u + 1)
                proj_unit(u)
                if pend_merge[0] is not None:
                    merge_head(pend_merge[0])
                    pend_merge[0] = None
                pend = []
                for blk in range(16):
                    pend.append(stageA(u, blk))
                    if len(pend) > LA:
                        stageB(pend.pop(0))
                while pend:
                    stageB(pend.pop(0))
                if g == 2:
                    pend_merge[0] = h
            merge_head(pend_merge[0])
            k.barrier()

    def final(self):
        nc, k, d = self.nc, self.k, self.dram
        plain = self.cfg.get("final_plain", False)
        with ExitStack() as es:
            sq = es.enter_context(self.sbuf("fsq", [128, NKC, 512], BF16))
            rstd = es.enter_context(self.sbuf("frstd", [128, 512], F32))
            y = es.enter_context(self.sbuf("fy", [128, NKC, 512], F32))
            ost = [es.enter_context(self.sbuf(f"fo{j}", [128, D], F32)) for j in range(2)]
            r_o = [k.res() for _ in range(2)]
            r_sq, r_rstd, r_y = k.res(), k.res(), k.res()
            gfin = self.gT[:, 2 * DEPTH, :]
            for tb in range(NTB):
                tsl = slice(tb * 512, (tb + 1) * 512)
                if not plain:
                    self.norm_core(es, tb, None, None, None, sq, r_sq, rstd, r_rstd, 7)
                for kc in range(NKC):
                    if plain:
                        k.op("dve", lambda e, kc=kc: e.tensor_copy(out=y[:, kc, :], in_=self.xT[:, kc, tsl]),
                             r=[self.r_xT[kc][tb]], w=[r_y])
                    else:
                        k.op("dve", lambda e, kc=kc: e.scalar_tensor_tensor(
                            out=y[:, kc, :], in0=self.xT[:, kc, tsl], scalar=gfin[:, kc:kc + 1], in1=rstd[:, :],
                            op0=ALU.mult, op1=ALU.mult),
                            r=[self.r_xT[kc][tb], r_rstd, self.r_const], w=[r_y])
                for t4 in range(4):
                    tt = tb * 4 + t4
                    ob = tt % 2
                    for half in range(2):
                        pi = (tt * 2 + half) % 4
                        for j in range(4):
                            kc = half * 4 + j
                            k.op("pe", lambda e, kc=kc, j=j, pi=pi: e.transpose(
                                self.ps[pi][:, j * 128:(j + 1) * 128], y[:, kc, t4 * 128:(t4 + 1) * 128], self.ident[:]),
                                r=[r_y, self.r_const], w=[self.r_ps[pi]])
                        if half == 0:
                            k.op("act", lambda e, pi=pi, ob=ob: e.copy(out=ost[ob][:, 0:512], in_=self.ps[pi][:, :]),
                                 r=[self.r_ps[pi]], w=[r_o[ob]])
                        else:
                            k.op("dve", lambda e, pi=pi, ob=ob: e.tensor_copy(out=ost[ob][:, 512:1024], in_=self.ps[pi][:, :]),
                                 r=[self.r_ps[pi]], w=[r_o[ob]])
                    k.dma("sp", f"fo{ob}", d["out"][tt * 128:(tt + 1) * 128, :], ost[ob][:], r=[r_o[ob]])


def host_consts():
    ident = np.eye(128, dtype=np.float32)
    sel = np.zeros((32, 32 * 128), np.float32)
    for e in range(32):
        sel[e, e * 128:(e + 1) * 128] = 1.0
    ab = np.zeros((24, 128, 384), np.float32)
    ii = np.arange(128)[:, None]
    jj = np.arange(384)[None, :]
    rel = np.abs((jj - 128) - ii).astype(np.float32)
    for g, dil in enumerate((1, 4, 16)):
        for h in range(8):
            slope = 2.0 ** (-8.0 * (g * 8 + h + 1) / 24.0)
            ab[g * 8 + h] = np.where(rel <= 64, -slope * rel * dil, -1e30)
    CW = -float(np.exp(-0.5))
    tri = np.zeros((6, 128, 128), np.float32)
    msk = np.zeros((6, 64, 64), np.float32)
    sidx = np.arange(128)[:, None]
    tidx = np.arange(128)[None, :]
    same = (sidx // 64) == (tidx // 64)
    s64 = np.arange(64)[:, None]
    t64 = np.arange(64)[None, :]
    for dd in range(2):
        before = (sidx < tidx) if dd == 0 else (sidx > tidx)
        after = (sidx > tidx) if dd == 0 else (sidx < tidx)
        tri[dd * 3 + 0] = np.where(same & (before | (sidx == tidx)), CW, 0.0)
        tri[dd * 3 + 1] = np.where(same & before, CW, 0.0)
        tri[dd * 3 + 2] = np.where(same & after, CW, 0.0)
        b64 = (s64 < t64) if dd == 0 else (s64 > t64)
        msk[dd * 3 + 0] = b64
        msk[dd * 3 + 1] = b64.T
        msk[dd * 3 + 2] = b64 | (s64 == t64)
    csel = np.zeros((128, 2), np.float32)
    csel[:64, 0] = CW
    csel[64:, 1] = CW
    return {"c_ident": ident, "c_sel": sel, "c_abias": ab, "c_tri": tri, "c_csel": csel, "c_mask": msk}


def prep_shared(inp):
    f = lambda a: np.ascontiguousarray(a, dtype=np.float32)
    sh = {}
    sh["ada_w"] = f(inp["ada_w"])
    sh["ada_bT"] = f(np.asarray(inp["ada_b"]).reshape(DEPTH, 48, 128).transpose(0, 2, 1))
    sh["gtmT"] = f(np.asarray(inp["norm_tm_g"]).reshape(DEPTH, NKC, 128).transpose(0, 2, 1))
    sh["gcmT"] = f(np.asarray(inp["norm_cm_g"]).reshape(DEPTH, NKC, 128).transpose(0, 2, 1))
    sh["gfinT"] = f(np.asarray(inp["final_g"]).reshape(NKC, 128).T)
    rg = np.asarray(inp["moe_router_g"])
    re_ = np.asarray(inp["moe_router_e"])
    wr = np.concatenate([rg] + [re_[:, g] for g in range(4)], axis=-1)
    sh["wr"] = f(wr.reshape(DEPTH, NKC, 128, 36).transpose(0, 2, 1, 3))
    br = np.concatenate([np.asarray(inp["moe_router_g_b"]), np.asarray(inp["moe_router_e_b"]).reshape(DEPTH, 32)], axis=-1)
    sh["br"] = f(br.reshape(DEPTH, 1, 36))
    sh["rw_muT"] = f(np.asarray(inp["rw_mu"]).reshape(2, 6, NKC, 128).transpose(0, 3, 1, 2))
    sh["rw_w_rkv"] = f(inp["rw_w_rkv"])
    sh["rw_w1cat"] = f(np.concatenate([np.asarray(inp["rw_w1"])[:, 0], np.asarray(inp["rw_w1"])[:, 1]], axis=-1))
    sh["rw_a1cat"] = f(np.concatenate([np.asarray(inp["rw_a1"])[:, 0], np.asarray(inp["rw_a1"])[:, 1]], axis=-1))
    sh["rw_g1"] = f(inp["rw_g1"])
    sh["rw_w2cat"] = f(np.asarray(inp["rw_w2"]).reshape(2, 128, D))
    sh["rw_a2cat"] = f(np.asarray(inp["rw_a2"]).reshape(2, 128, D))
    sh["rw_g2"] = f(inp["rw_g2"])
    sh["rw_w0"] = f(inp["rw_w0"])
    sh["rw_a0"] = f(inp["rw_a0"])
    for n_ in ("rw_k_k", "rw_k_a", "rw_ln_w", "rw_ln_b"):
        sh[n_] = f(inp[n_])
    sh["rw_r_k"] = f(np.asarray(inp["rw_r_k"]).reshape(2, D))
    sh["rw_w_o"] = f(inp["rw_w_o"])
    sh["at_w_qkv"] = f(inp["at_w_qkv"])
    sh["at_w_o"] = f(inp["at_w_o"])
    sh["moe_w_gate"] = f(inp["moe_w_gate"])
    sh["moe_w_up"] = f(inp["moe_w_up"])
    sh["moe_w_down"] = f(inp["moe_w_down"])
    sh.update(host_consts())
    return sh


def prep_core(inp, b):
    return {
        "x": np.ascontiguousarray(np.asarray(inp["x"])[b], dtype=np.float32),
        "cT": np.ascontiguousarray(np.asarray(inp["c"])[b].reshape(NKC, 128).T, dtype=np.float32),
    }


def build_prog(cfg):
    nc = bass.Bass("TRN2", target_bir_lowering=False)
    p = Prog(nc, cfg)
    with p.k.es:
        p.build()
    return nc, p


def run(inp, cfg, cores):
    nc, p = build_prog(cfg)
    sh = prep_shared(inp)
    in_maps = []
    for b in cores:
        m = dict(sh)
        m.update(prep_core(inp, b))
        m = {kk: v for kk, v in m.items() if kk in p.dram}
        in_maps.append(m)
    res = run_bass_kernel_spmd(nc, in_maps, core_ids=list(range(len(cores))))
    return res


def kernel(**inputs):
    res = run(inputs, {}, list(range(8)))
    out = np.stack([np.asarray(r["out"]) for r in res.results], axis=0)
    return out.astype(np.float32)
```

```python
import numpy as np
from contextlib import ExitStack

import concourse.bass as bass
import concourse.mybir as mybir
from concourse.bass_utils import run_bass_kernel_spmd

F32 = mybir.dt.float32
BF16 = mybir.dt.bfloat16
AF = mybir.ActivationFunctionType
ALU = mybir.AluOpType
AX = mybir.AxisListType

D = 1024
S = 2048
DEPTH = 4
NKC = 8
NTB = 4
NTT = 16
RMS_EPS = 1e-6
SEM_LIM = 24000


class Res:
    __slots__ = ("name", "w", "rs", "excl")

    def __init__(self, name):
        self.name = name
        self.w = None
        self.rs = {}
        self.excl = False

    def add_reader(self, tag):
        st, ep, v = tag
        if self.rs.get(st, (-1, 0)) < (ep, v):
            self.rs[st] = (ep, v)


class KB:
    COMPUTE = ("pe", "act", "dve", "pool")

    def __init__(self, nc):
        self.nc = nc
        self.es = ExitStack()
        self.eng = {"pe": nc.tensor, "act": nc.scalar, "dve": nc.vector, "pool": nc.gpsimd, "sp": nc.sync}
        self.sems = {}
        self.cnt = {}
        self.seen = {e: {} for e in self.eng}
        self.nres = 0
        self.epoch_final = {}
        self.ninst = {e: 0 for e in self.eng}

    def res(self, name=None):
        self.nres += 1
        return Res(name or f"r{self.nres}")

    def sb(self, name, shape, dt):
        return self.es.enter_context(self.nc.sbuf_tensor(name, list(shape), dt))

    def _sem(self, stream, epoch):
        key = (stream, epoch)
        if key not in self.sems:
            self.sems[key] = self.es.enter_context(self.nc.semaphore(f"s_{stream}_{epoch}"))
        return self.sems[key]

    def _bump(self, stream, inc):
        ep, v = self.cnt.get(stream, (0, 0))
        if v + inc > SEM_LIM:
            self.epoch_final[(stream, ep)] = v
            ep, v = ep + 1, 0
        v += inc
        self.cnt[stream] = (ep, v)
        return (stream, ep, v), self._sem(stream, ep)

    def _wait(self, eng, tags):
        best = {}
        for t in tags:
            if t is None:
                continue
            st, ep, v = t
            if st == "pe" and eng == "pe":
                continue
            if (st not in best) or (ep, v) > best[st]:
                best[st] = (ep, v)
        for st, (ep, v) in best.items():
            if st.startswith("d_"):
                cur_ep, cur_v = self.cnt[st]
                v = cur_v if cur_ep == ep else self.epoch_final[(st, ep)]
            if self.seen[eng].get(st, (-1, 0)) >= (ep, v):
                continue
            self.eng[eng].wait_ge(self._sem(st, ep), v)
            self.seen[eng][st] = (ep, v)

    def _deps(self, r, w, same_stream=None):
        tags = []
        for x in r:
            tags.append(x.w)
            if x.excl:
                for st, (ep, v) in x.rs.items():
                    if same_stream is not None and st == same_stream:
                        continue
                    tags.append((st, ep, v))
        for x in w:
            tags.append(x.w)
            for st, (ep, v) in x.rs.items():
                if same_stream is not None and st == same_stream:
                    continue
                tags.append((st, ep, v))
        return tags

    def op(self, eng, fn, r=(), w=()):
        self._wait(eng, self._deps(r, w, same_stream=eng))
        ins = fn(self.eng[eng])
        tag, sem = self._bump(eng, 1)
        ins.then_inc(sem, 1)
        self.ninst[eng] += 1
        for x in r:
            x.add_reader(tag)
        for x in w:
            x.w = tag
            x.rs = {}
        return tag

    def dma(self, q, chan, out, in_, r=(), w=(), **kw):
        self._wait(q, self._deps(r, w))
        ins = self.eng[q].dma_start(out=out, in_=in_, **kw)
        tag, sem = self._bump("d_" + chan, 16)
        ins.then_inc(sem, 16)
        self.ninst[q] += 1
        for x in r:
            x.add_reader(tag)
        for x in w:
            x.w = tag
            x.rs = {}
        return tag

    def barrier(self, engines=("pe", "act", "dve", "pool", "sp")):
        tags = [(st, ep, v) for st, (ep, v) in self.cnt.items()]
        for e in engines:
            self._wait(e, tags)

    def wait_all(self, eng):
        self._wait(eng, [(st, ep, v) for st, (ep, v) in self.cnt.items()])


class Prog:
    def __init__(self, nc, cfg):
        self.nc = nc
        self.cfg = cfg
        self.k = KB(nc)
        self.dram = {}
        self._uid = 0

    def sbuf(self, name, shape, dt):
        self._uid += 1
        return self.nc.sbuf_tensor(f"{name}_u{self._uid}", list(shape), dt)

    def dump(self, name, ap, shape, dt, rs):
        if name not in self.cfg.get("dump", ()):
            return
        o = self.dout("dbg_" + name, shape, dt)
        self.k.dma("sp", "dbg", o, ap, r=rs)
        self.k.barrier()

    def din(self, name, shape, dt=F32):
        self.dram[name] = self.nc.dram_tensor(name, list(shape), dt, kind="ExternalInput").ap()
        return self.dram[name]

    def dout(self, name, shape, dt=F32):
        self.dram[name] = self.nc.dram_tensor(name, list(shape), dt, kind="ExternalOutput").ap()
        return self.dram[name]

    def build(self):
        nc, k, cfg = self.nc, self.k, self.cfg
        layers = cfg.get("layers", list(range(DEPTH)))
        d = self.dram
        self.din("x", [S, D])
        self.din("cT", [128, NKC])
        self.din("ada_w", [DEPTH, D, 6 * D])
        self.din("ada_bT", [DEPTH, 128, 48])
        self.din("gtmT", [DEPTH, 128, NKC])
        self.din("gcmT", [DEPTH, 128, NKC])
        self.din("gfinT", [128, NKC])
        self.din("wr", [DEPTH, 128, NKC, 36])
        self.din("br", [DEPTH, 1, 36])
        self.din("moe_w_gate", [DEPTH, 4, 8, D, 256])
        self.din("moe_w_up", [DEPTH, 4, 8, D, 256])
        self.din("moe_w_down", [DEPTH, 4, 8, 256, D])
        self.din("rw_muT", [2, 128, 6, NKC])
        self.din("rw_w_rkv", [2, 3, D, D])
        self.din("rw_w1cat", [2, D, 128])
        self.din("rw_a1cat", [2, D, 128])
        self.din("rw_g1", [2, 2, D, 128])
        self.din("rw_w2cat", [2, 128, D])
        self.din("rw_a2cat", [2, 128, D])
        self.din("rw_g2", [2, 2, 128, D])
        self.din("rw_w0", [2, 2, D])
        self.din("rw_a0", [2, 2, D])
        for n_ in ("rw_k_k", "rw_k_a", "rw_r_k", "rw_ln_w", "rw_ln_b"):
            self.din(n_, [2, D])
        self.din("rw_w_o", [2, D, D])
        self.din("c_tri", [6, 128, 128])
        self.din("c_csel", [128, 2])
        self.din("c_mask", [6, 64, 64])
        itn = lambda n_, shp, dt=F32: nc.dram_tensor(n_, list(shp), dt, kind="Internal").ap()
        self.scr = {"RAW": itn("scr_raw", [2, S, D]), "RAWV": itn("scr_rawv", [S, D], BF16),
                    "FM": itn("scr_fm", [2, 4, NTT, 64, 16, 128], BF16),
                    "TM": itn("scr_tm", [2, 2, S, D], BF16), "GG": itn("scr_gg", [2, S, D]), "RK": itn("scr_rk", [2, S, 16])}
        self.r_scr = {"RAW": [[k.res() for _ in range(NTT)] for _ in range(3)],
                      "FM": [[[k.res() for _ in range(NTT)] for _ in range(4)] for _ in range(2)],
                      "TM": [[[k.res() for _ in range(NTT)] for _ in range(2)] for _ in range(2)],
                      "GG": [[k.res() for _ in range(NTT)] for _ in range(2)],
                      "RK": [[k.res() for _ in range(NTT)] for _ in range(2)]}
        self.din("at_w_qkv", [2, D, 9216])
        self.din("at_w_o", [2, D, D])
        self.din("c_abias", [24, 128, 384])
        self.din("c_ident", [128, 128])
        self.din("c_sel", [32, 32 * 128])
        self.dout("out", [S, D])

        self.xT = k.sb("xT", [128, NKC, S], F32)
        self.hT = k.sb("hT", [128, NKC, S], BF16)
        self.r_xT = [[k.res(f"xT{kc}_{tb}") for tb in range(NTB)] for kc in range(NKC)]
        self.r_hT = [[k.res(f"hT{kc}_{tb}") for tb in range(NTB)] for kc in range(NKC)]
        self.ident = k.sb("ident", [128, 128], F32)
        self.ones = k.sb("ones", [128, 128], F32)
        self.epsT = k.sb("epsT", [128, 1], F32)
        self.onesb = k.sb("onesb", [128, 128], BF16)
        self.r_const = k.res("const")
        self.modT = k.sb("modT", [128, DEPTH, 48], F32)
        self.mod1T = k.sb("mod1T", [128, DEPTH, 48], F32)
        self.r_mod = k.res("mod")
        self.gT = k.sb("gT", [128, 2 * DEPTH + 1, NKC], F32)
        self.ps = [self.k.es.enter_context(nc.psum_tensor(f"ps{i}", [128, 512], F32)) for i in range(8)]
        self.r_ps = [k.res(f"ps{i}") for i in range(8)]
        for r_ in self.r_ps:
            r_.excl = True

        k.dma("sp", "c0", self.ident[:], d["c_ident"][:, :], w=[self.r_const])
        k.op("dve", lambda e: e.memset(self.ones[:], 1.0), w=[self.r_const])
        k.op("dve", lambda e: e.memset(self.epsT[:], RMS_EPS), w=[self.r_const])
        k.op("dve", lambda e: e.memset(self.onesb[:], 1.0), w=[self.r_const])
        k.dma("sp", "c0", self.gT[:, 0:DEPTH, :], d["gtmT"].rearrange("l p k -> p l k"), w=[self.r_const])
        k.dma("sp", "c0", self.gT[:, DEPTH:2 * DEPTH, :], d["gcmT"].rearrange("l p k -> p l k"), w=[self.r_const])
        k.dma("sp", "c0", self.gT[:, 2 * DEPTH, :], d["gfinT"][:, :], w=[self.r_const])

        self.load_x()
        self.ada_all()
        k.barrier()
        for i in layers:
            if cfg.get("mixers", True):
                self.norm_mod(i, 0)
                if i % 2 == 0:
                    self.rwkv(i)
                else:
                    self.attn(i)
            if cfg.get("moe", True):
                self.moe(i)
        self.final()
        k.barrier()
        k.wait_all("sp")
        k.wait_all("pool")

    def load_x(self):
        nc, k, d = self.nc, self.k, self.dram
        with ExitStack() as es:
            st = [es.enter_context(self.sbuf(f"xst{i}", [128, D], F32)) for i in range(2)]
            r_st = [k.res() for _ in range(2)]
            for tt in range(NTT):
                b = tt % 2
                k.dma("sp", f"xst{b}", st[b][:], d["x"][tt * 128:(tt + 1) * 128, :], w=[r_st[b]])
                for half in range(2):
                    pi = (tt * 2 + half) % 4
                    for j in range(4):
                        kc = half * 4 + j
                        k.op("pe", lambda e, kc=kc, j=j, pi=pi, b=b: e.transpose(
                            self.ps[pi][:, j * 128:(j + 1) * 128], st[b][:, kc * 128:(kc + 1) * 128], self.ident[:]),
                            r=[r_st[b], self.r_const], w=[self.r_ps[pi]])
                    tb = tt // 4
                    ws = [self.r_xT[half * 4 + j][tb] for j in range(4)]
                    eng = "act" if half else "dve"
                    if eng == "dve":
                        k.op("dve", lambda e, half=half, pi=pi, tt=tt: e.tensor_copy(
                            out=self.xT[:, half * 4:half * 4 + 4, tt * 128:(tt + 1) * 128],
                            in_=self.ps[pi][:, :].rearrange("p (j t) -> p j t", j=4)),
                            r=[self.r_ps[pi]], w=ws)
                    else:
                        k.op("act", lambda e, half=half, pi=pi, tt=tt: e.copy(
                            out=self.xT[:, half * 4:half * 4 + 4, tt * 128:(tt + 1) * 128],
                            in_=self.ps[pi][:, :].rearrange("p (j t) -> p j t", j=4)),
                            r=[self.r_ps[pi]], w=ws)
            k.barrier()

    def ada_all(self):
        nc, k, d = self.nc, self.k, self.dram
        NP = 8
        PW = 6 * D // NP
        with ExitStack() as es:
            scT = es.enter_context(self.sbuf("scT", [128, NKC], F32))
            cT = es.enter_context(self.sbuf("cTs", [128, NKC], F32))
            abT = es.enter_context(self.sbuf("abT", [128, DEPTH, 48], F32))
            wst = [es.enter_context(self.sbuf(f"adaw{i}", [128, NKC, PW], F32)) for i in range(2)]
            r_w = [k.res() for _ in range(2)]
            r_sc = k.res()
            r_ab = k.res()
            k.dma("sp", "c1", cT[:], d["cT"][:, :], w=[r_sc])
            k.dma("sp", "c1", abT[:], d["ada_bT"].rearrange("l p j -> p l j"), w=[r_ab])
            k.op("act", lambda e: e.activation(out=scT[:], in_=cT[:], func=AF.Silu), r=[r_sc], w=[r_sc])
            q = 0
            for i in range(DEPTH):
                pi = i % 2
                for pc in range(NP):
                    b = q % 2
                    q += 1
                    k.dma("sp", f"adaw{b}", wst[b][:],
                          d["ada_w"][i, :, pc * PW:(pc + 1) * PW].rearrange("(kc p) n -> p kc n", p=128),
                          w=[r_w[b]])
                    for jj in range(PW // 128):
                        j = pc * (PW // 128) + jj
                        for kc in range(NKC):
                            k.op("pe", lambda e, b=b, jj=jj, kc=kc, j=j, pi=pi: e.matmul(
                                self.ps[pi][:, j:j + 1], wst[b][:, kc, jj * 128:(jj + 1) * 128], scT[:, kc:kc + 1],
                                start=(kc == 0), stop=(kc == NKC - 1)),
                                r=[r_w[b], r_sc], w=[self.r_ps[pi]])
                k.op("dve", lambda e, i=i, pi=pi: e.tensor_tensor(
                    out=self.modT[:, i, :], in0=self.ps[pi][:, 0:48], in1=abT[:, i, :], op=ALU.add),
                    r=[self.r_ps[pi], r_ab], w=[self.r_mod])
                k.op("dve", lambda e, i=i: e.tensor_scalar_add(
                    out=self.mod1T[:, i, :], in0=self.modT[:, i, :], scalar1=1.0),
                    r=[self.r_mod], w=[self.r_mod])
            k.barrier()

    def norm_core(self, es, tb, geff, shift, r_par, sq, r_sq, rstd, r_rstd, psi):
        nc, k = self.nc, self.k
        tsl = slice(tb * 512, (tb + 1) * 512)
        k.op("act", lambda e: e.activation(out=sq[:, :, :], in_=self.xT[:, :, tsl], func=AF.Square),
             r=[self.r_xT[kc][tb] for kc in range(NKC)], w=[r_sq])
        for kc in range(NKC):
            k.op("pe", lambda e, kc=kc: e.matmul(self.ps[psi][:, :], self.onesb[:, :], sq[:, kc, :],
                                                 start=(kc == 0), stop=(kc == NKC - 1)),
                 r=[r_sq, self.r_const], w=[self.r_ps[psi]])
        k.op("act", lambda e: e.activation(out=rstd[:, :], in_=self.ps[psi][:, :], func=AF.Sqrt,
                                           scale=1.0 / D, bias=self.epsT[:, 0:1]),
             r=[self.r_ps[psi], self.r_const], w=[r_rstd])
        k.op("dve", lambda e: e.reciprocal(out=rstd[:, :], in_=rstd[:, :]), r=[r_rstd], w=[r_rstd])

    def norm_mod(self, i, which, router=None):
        nc, k = self.nc, self.k
        gi = i if which == 0 else DEPTH + i
        so = 0 if which == 0 else 24
        with ExitStack() as es:
            geff = es.enter_context(self.sbuf("geff", [128, NKC], F32))
            sq = [es.enter_context(self.sbuf(f"nsq{j}", [128, NKC, 512], BF16)) for j in range(2)]
            rstd = [es.enter_context(self.sbuf(f"nrstd{j}", [128, 512], F32)) for j in range(2)]
            t1 = [es.enter_context(self.sbuf(f"nt1_{j}", [128, 512], F32)) for j in range(2)]
            r_t1 = [k.res() for _ in range(2)]
            r_g = k.res()
            r_sq = [k.res() for _ in range(2)]
            r_rstd = [k.res() for _ in range(2)]
            if router is not None:
                h32 = es.enter_context(self.sbuf("h32", [128, NKC, 512], F32))
                r_h32 = k.res()
            k.op("dve", lambda e: e.tensor_tensor(out=geff[:], in0=self.gT[:, gi, :], in1=self.mod1T[:, i, so + 8:so + 16],
                                                  op=ALU.mult), r=[self.r_const, self.r_mod], w=[r_g])
            ncore = lambda tb: self.norm_core(es, tb, None, None, None, sq[tb % 2], r_sq[tb % 2], rstd[tb % 2], r_rstd[tb % 2],
                                              7 if tb % 2 == 0 else 5)
            ncore(0)
            for tb in range(NTB):
                tsl = slice(tb * 512, (tb + 1) * 512)
                if tb + 1 < NTB:
                    ncore(tb + 1)
                rs_, r_rs = rstd[tb % 2], r_rstd[tb % 2]
                for kc in range(NKC):
                    b = kc % 2
                    k.op("dve", lambda e, kc=kc, b=b: e.tensor_tensor(out=t1[b][:, :], in0=self.xT[:, kc, tsl], in1=rs_[:, :],
                                                                      op=ALU.mult),
                         r=[self.r_xT[kc][tb], r_rs], w=[r_t1[b]])
                    if router is None:
                        k.op("act", lambda e, kc=kc, b=b: e.activation(
                            out=self.hT[:, kc, tsl], in_=t1[b][:, :], func=AF.Identity, scale=geff[:, kc:kc + 1],
                            bias=self.modT[:, i, so + kc:so + kc + 1]),
                            r=[r_t1[b], r_g, self.r_mod], w=[self.r_hT[kc][tb]])
                    else:
                        k.op("act", lambda e, kc=kc, b=b: e.activation(
                            out=h32[:, kc, :], in_=t1[b][:, :], func=AF.Identity, scale=geff[:, kc:kc + 1],
                            bias=self.modT[:, i, so + kc:so + kc + 1]),
                            r=[r_t1[b], r_g, self.r_mod], w=[r_h32])
                        k.op("act", lambda e, kc=kc, b=b: e.activation(
                            out=self.hT[:, kc, tsl], in_=t1[b][:, :], func=AF.Identity, scale=geff[:, kc:kc + 1],
                            bias=self.modT[:, i, so + kc:so + kc + 1]),
                             r=[r_t1[b], r_g, self.r_mod], w=[self.r_hT[kc][tb]])
                if router is not None:
                    router(tb, h32, r_h32)
            k.barrier()

    def moe(self, i):
        nc, k, d = self.nc, self.k, self.dram
        with ExitStack() as es0:
            gT_all = es0.enter_context(self.sbuf("gT_all", [32, S], BF16))
            r_gT = [k.res() for _ in range(NTT)]
            with ExitStack() as es:
                wr = es.enter_context(self.sbuf("wr", [128, NKC, 36], F32))
                brs = es.enter_context(self.sbuf("brs", [1, 36], F32))
                r_wr = k.res()
                k.dma("sp", "c2", wr[:], d["wr"][i], w=[r_wr])
                k.dma("sp", "c2", brs[:], d["br"][i], w=[r_wr])
                L = es.enter_context(self.sbuf("rL", [128, 36], F32))
                sm = es.enter_context(self.sbuf("rsm", [128, 64], F32))
                gates = es.enter_context(self.sbuf("rgates", [128, 32], F32))
                r_L, r_sm, r_gates = k.res(), k.res(), k.res()

                def router(tb, h32, r_h32):
                    for t4 in range(4):
                        tt = tb * 4 + t4
                        pi = 6
                        for kc in range(NKC):
                            k.op("pe", lambda e, kc=kc: e.matmul(self.ps[pi][:, 0:36], h32[:, kc, t4 * 128:(t4 + 1) * 128],
                                                                 wr[:, kc, :], start=(kc == 0), stop=False),
                                 r=[r_h32, r_wr], w=[self.r_ps[pi]])
                        k.op("pe", lambda e: e.matmul(self.ps[pi][:, 0:36], self.ones[0:1, :], brs[0:1, :],
                                                      start=False, stop=True),
                             r=[r_wr, self.r_const], w=[self.r_ps[pi]])
                        k.op("dve", lambda e: e.tensor_copy(out=L[:, :], in_=self.ps[pi][:, 0:36]),
                             r=[self.r_ps[pi]], w=[r_L])
                        self.route_math(L, r_L, sm, r_sm, gates, r_gates)
                        k.op("pe", lambda e: e.transpose(self.ps[pi][0:32, 128:256], gates[:, :], self.ident[:]),
                             r=[r_gates, self.r_const], w=[self.r_ps[pi]])
                        k.op("act", lambda e, tt=tt: e.copy(out=gT_all[:, tt * 128:(tt + 1) * 128],
                                                            in_=self.ps[pi][0:32, 128:256]),
                             r=[self.r_ps[pi]], w=[r_gT[tt]])

                self.norm_mod(i, 1, router=router)
                self.dump("modT", self.modT[:], [128, DEPTH, 48], F32, [self.r_mod])
                self.dump("hT", self.hT[:], [128, NKC, S], BF16, [x for y in self.r_hT for x in y])
                self.dump("gT_all", gT_all[:], [32, S], F32, r_gT)
            with ExitStack() as es:
                sel = es.enter_context(self.sbuf("sel", [32, 32 * 128], BF16))
                r_sel = k.res()
                k.dma("pool", "c2sel", sel[:], d["c_sel"][:, :], w=[r_sel])
                NS = 16
                wg = [es.enter_context(self.sbuf(f"wg{b}", [128, 2, NKC, 256], BF16)) for b in range(2)]
                wu = [es.enter_context(self.sbuf(f"wu{b}", [128, 2, NKC, 256], BF16)) for b in range(2)]
                wd = [es.enter_context(self.sbuf(f"wd{b}", [128, 2, 2, D], BF16)) for b in range(2)]
                r_w = [k.res() for _ in range(2)]
                gbc = [es.enter_context(self.sbuf(f"gbc{b}", [128, 2, 512], F32)) for b in range(2)]
                r_gbc = [k.res() for _ in range(2)]
                hid = [es.enter_context(self.sbuf(f"hid{b}", [128, 4, 512], BF16)) for b in range(2)]
                r_hid = [k.res() for _ in range(2)]
                sg = [es.enter_context(self.sbuf(f"sg{b}", [128, 512], F32)) for b in range(2)]
                r_sg = [k.res() for _ in range(2)]
                tm = [es.enter_context(self.sbuf(f"tm{b}", [128, 512], F32)) for b in range(2)]
                r_tm = [k.res() for _ in range(2)]

                def load_w(s):
                    b = s % 2
                    for ee in range(2):
                        eg = 2 * s + ee
                        g_, e_ = eg // 8, eg % 8
                        k.dma("pool", f"moew{b}", wg[b][:, ee, :, :],
                              d["moe_w_gate"][i, g_, e_].rearrange("(kc p) f -> p kc f", p=128), w=[r_w[b]])
                        k.dma("pool", f"moew{b}", wu[b][:, ee, :, :],
                              d["moe_w_up"][i, g_, e_].rearrange("(kc p) f -> p kc f", p=128), w=[r_w[b]])
                        k.dma("pool", f"moew{b}", wd[b][:, ee, :, :],
                              d["moe_w_down"][i, g_, e_].rearrange("(fc p) n -> p fc n", p=128), w=[r_w[b]])

                pending = [None]
                it = [0]

                def down(s, tb, hb):
                    b = s % 2
                    tsl = slice(tb * 512, (tb + 1) * 512)
                    for dc in range(NKC):
                        pi = 4 + dc % 2
                        for u in range(4):
                            ee, fc = u // 2, u % 2
                            k.op("pe", lambda e, u=u, ee=ee, fc=fc, dc=dc, pi=pi: e.matmul(
                                self.ps[pi][:, :], wd[b][:, ee, fc, dc * 128:(dc + 1) * 128], hid[hb][:, u, :],
                                start=(u == 0), stop=(u == 3)),
                                r=[r_w[b], r_hid[hb]], w=[self.r_ps[pi]])
                        k.op("dve", lambda e, dc=dc, pi=pi: e.scalar_tensor_tensor(
                            out=self.xT[:, dc, tsl], in0=self.ps[pi][:, :], scalar=self.modT[:, i, 40 + dc:41 + dc],
                            in1=self.xT[:, dc, tsl], op0=ALU.mult, op1=ALU.add),
                            r=[self.r_ps[pi], self.r_mod], w=[self.r_xT[dc][tb]])

                load_w(0)
                for s in range(NS):
                    b = s % 2
                    for tb in range(NTB):
                        tsl = slice(tb * 512, (tb + 1) * 512)
                        hb = it[0] % 2
                        gb = it[0] % 2
                        it[0] += 1
                        for ee in range(2):
                            eg = 2 * s + ee
                            k.op("pe", lambda e, eg=eg: e.matmul(self.ps[6][:, :], sel[:, eg * 128:(eg + 1) * 128],
                                                                 gT_all[:, tsl], start=True, stop=True),
                                 r=[r_sel] + r_gT[tb * 4:tb * 4 + 4], w=[self.r_ps[6]])
                            k.op("act", lambda e, ee=ee, gb=gb: e.copy(out=gbc[gb][:, ee, :], in_=self.ps[6][:, :]),
                                 r=[self.r_ps[6]], w=[r_gbc[gb]])
                        for u in range(4):
                            ee, fc = u // 2, u % 2
                            pg, pu = (0, 1) if u % 2 == 0 else (2, 3)
                            for kc in range(NKC):
                                k.op("pe", lambda e, kc=kc, ee=ee, fc=fc, pg=pg: e.matmul(
                                    self.ps[pg][:, :], wg[b][:, ee, kc, fc * 128:(fc + 1) * 128], self.hT[:, kc, tsl],
                                    start=(kc == 0), stop=(kc == NKC - 1)),
                                    r=[r_w[b], self.r_hT[kc][tb]], w=[self.r_ps[pg]])
                            for kc in range(NKC):
                                k.op("pe", lambda e, kc=kc, ee=ee, fc=fc, pu=pu: e.matmul(
                                    self.ps[pu][:, :], wu[b][:, ee, kc, fc * 128:(fc + 1) * 128], self.hT[:, kc, tsl],
                                    start=(kc == 0), stop=(kc == NKC - 1)),
                                    r=[r_w[b], self.r_hT[kc][tb]], w=[self.r_ps[pu]])
                            sb_ = u % 2
                            k.op("act", lambda e, pg=pg, sb_=sb_: e.activation(out=sg[sb_][:, :], in_=self.ps[pg][:, :],
                                                                               func=AF.Silu),
                                 r=[self.r_ps[pg]], w=[r_sg[sb_]])
                            k.op("dve", lambda e, pu=pu, sb_=sb_: e.tensor_tensor(out=tm[sb_][:, :], in0=sg[sb_][:, :],
                                                                                  in1=self.ps[pu][:, :], op=ALU.mult),
                                 r=[r_sg[sb_], self.r_ps[pu]], w=[r_tm[sb_]])
                            k.op("dve", lambda e, u=u, ee=ee, sb_=sb_, hb=hb, gb=gb: e.tensor_tensor(
                                out=hid[hb][:, u, :], in0=tm[sb_][:, :], in1=gbc[gb][:, ee, :], op=ALU.mult),
                                r=[r_tm[sb_], r_gbc[gb]], w=[r_hid[hb]])
                        if pending[0] is not None:
                            down(*pending[0])
                        pending[0] = (s, tb, hb)
                        if tb == 0 and s + 1 < NS:
                            load_w(s + 1)
                down(*pending[0])
                k.barrier()

    def route_math(self, L, r_L, sm, r_sm, gates, r_gates):
        k = self.k
        GMAX, NGMAX, GSUM, PG, M1, M2, DD, EE, W1, W2 = range(10)
        GOH = slice(10, 14)
        GE = slice(14, 18)
        ESEL = slice(18, 26)
        OH1 = slice(26, 34)
        MSK = slice(34, 42)
        OH2 = slice(42, 50)
        INN = slice(50, 58)
        c = lambda j: slice(j, j + 1)

        def dv(fn, r=(), w=()):
            k.op("dve", fn, r=r, w=w)

        rs = [r_L, r_sm]
        dv(lambda e: e.reduce_max(out=sm[:, c(GMAX)], in_=L[:, 0:4], axis=AX.X), r=[r_L], w=[r_sm])
        dv(lambda e: e.tensor_scalar(out=sm[:, GOH], in0=L[:, 0:4], scalar1=sm[:, c(GMAX)], scalar2=None,
                                     op0=ALU.is_equal), r=rs, w=[r_sm])
        dv(lambda e: e.tensor_scalar_mul(out=sm[:, c(NGMAX)], in0=sm[:, c(GMAX)], scalar1=-1.0), r=[r_sm], w=[r_sm])
        k.op("act", lambda e: e.activation(out=sm[:, GE], in_=L[:, 0:4], func=AF.Exp, bias=sm[:, c(NGMAX)],
                                           scale=1.0, accum_out=sm[:, c(GSUM)]), r=rs, w=[r_sm])
        dv(lambda e: e.reciprocal(out=sm[:, c(PG)], in_=sm[:, c(GSUM)]), r=[r_sm], w=[r_sm])
        dv(lambda e: e.tensor_scalar_mul(out=sm[:, ESEL], in0=L[:, 4:12], scalar1=sm[:, c(10)]), r=rs, w=[r_sm])
        for g in range(1, 4):
            dv(lambda e, g=g: e.scalar_tensor_tensor(out=sm[:, ESEL], in0=L[:, 4 + 8 * g:12 + 8 * g],
                                                     scalar=sm[:, c(10 + g)], in1=sm[:, ESEL],
                                                     op0=ALU.mult, op1=ALU.add), r=rs, w=[r_sm])
        dv(lambda e: e.reduce_max(out=sm[:, c(M1)], in_=sm[:, ESEL], axis=AX.X), r=[r_sm], w=[r_sm])
        dv(lambda e: e.tensor_scalar(out=sm[:, OH1], in0=sm[:, ESEL], scalar1=sm[:, c(M1)], scalar2=None,
                                     op0=ALU.is_equal), r=[r_sm], w=[r_sm])
        dv(lambda e: e.scalar_tensor_tensor(out=sm[:, MSK], in0=sm[:, OH1], scalar=-1e30, in1=sm[:, ESEL],
                                            op0=ALU.mult, op1=ALU.add), r=[r_sm], w=[r_sm])
        dv(lambda e: e.reduce_max(out=sm[:, c(M2)], in_=sm[:, MSK], axis=AX.X), r=[r_sm], w=[r_sm])
        dv(lambda e: e.tensor_scalar(out=sm[:, OH2], in0=sm[:, MSK], scalar1=sm[:, c(M2)], scalar2=None,
                                     op0=ALU.is_equal), r=[r_sm], w=[r_sm])
        dv(lambda e: e.tensor_tensor(out=sm[:, c(DD)], in0=sm[:, c(M2)], in1=sm[:, c(M1)], op=ALU.subtract),
           r=[r_sm], w=[r_sm])
        k.op("act", lambda e: e.activation(out=sm[:, c(EE)], in_=sm[:, c(DD)], func=AF.Exp), r=[r_sm], w=[r_sm])
        dv(lambda e: e.tensor_scalar_add(out=sm[:, c(W1)], in0=sm[:, c(EE)], scalar1=1.0), r=[r_sm], w=[r_sm])
        dv(lambda e: e.reciprocal(out=sm[:, c(W1)], in_=sm[:, c(W1)]), r=[r_sm], w=[r_sm])
        dv(lambda e: e.tensor_tensor(out=sm[:, c(W2)], in0=sm[:, c(EE)], in1=sm[:, c(W1)], op=ALU.mult),
           r=[r_sm], w=[r_sm])
        dv(lambda e: e.tensor_tensor(out=sm[:, c(W1)], in0=sm[:, c(W1)], in1=sm[:, c(PG)], op=ALU.mult),
           r=[r_sm], w=[r_sm])
        dv(lambda e: e.tensor_tensor(out=sm[:, c(W2)], in0=sm[:, c(W2)], in1=sm[:, c(PG)], op=ALU.mult),
           r=[r_sm], w=[r_sm])
        dv(lambda e: e.tensor_scalar_mul(out=sm[:, INN], in0=sm[:, OH1], scalar1=sm[:, c(W1)]), r=[r_sm], w=[r_sm])
        dv(lambda e: e.scalar_tensor_tensor(out=sm[:, INN], in0=sm[:, OH2], scalar=sm[:, c(W2)], in1=sm[:, INN],
                                            op0=ALU.mult, op1=ALU.add), r=[r_sm], w=[r_sm])
        for g in range(4):
            dv(lambda e, g=g: e.tensor_scalar_mul(out=gates[:, 8 * g:8 * g + 8], in0=sm[:, INN],
                                                  scalar1=sm[:, c(10 + g)]), r=[r_sm], w=[r_gates])

    def rwkv(self, i):
        nc, k, d = self.nc, self.k, self.dram
        j = i // 2
        with ExitStack() as es:
            PCt = es.enter_context(self.sbuf("PCt", [64, 2, NTT, 16, 2], F32))
            r_PC = k.res()
            self.rwkv_A(i, j, PCt, r_PC)
            self.rwkv_B(i, j, PCt, r_PC)

    def rwkv_A(self, i, j, PCt, r_PC):
        nc, k, d = self.nc, self.k, self.dram
        all_hT = [x for y in self.r_hT for x in y]
        sc = self.scr
        with ExitStack() as esA:
            sb = lambda es, n, s, dt=F32: es.enter_context(self.sbuf(n, s, dt))
            h1w = sb(esA, "h1w", [128, S], BF16)
            h1a = sb(esA, "h1a", [128, S], BF16)
            r_h1w, r_h1a = k.res(), k.res()
            muT = sb(esA, "muT", [128, 6, NKC])
            c1 = sb(esA, "c1", [128, 6, NKC])
            c2 = sb(esA, "c2", [128, 6, NKC])
            r_c = k.res()
            k.dma("sp", "rwc", muT[:], d["rw_muT"][j], w=[r_c])
            k.op("dve", lambda e: e.tensor_scalar(out=c1[:], in0=muT[:], scalar1=-1.0, scalar2=1.0, op0=ALU.mult, op1=ALU.add),
                 r=[r_c], w=[r_c])
            k.op("dve", lambda e: e.tensor_scalar_mul(out=c2[:], in0=muT[:], scalar1=0.5), r=[r_c], w=[r_c])

            def scale_w(stg, r_stg, W1, W2, r_W, jm, ncol, col0=0):
                for kc in range(NKC):
                    k.op("act", lambda e, kc=kc: e.mul(out=W1[:, kc, col0:col0 + ncol], in_=stg[:, kc, 0:ncol],
                                                       mul=c1[:, jm, kc:kc + 1]), r=[r_stg, r_c], w=[r_W])
                    k.op("dve", lambda e, kc=kc: e.tensor_scalar_mul(out=W2[:, kc, col0:col0 + ncol], in0=stg[:, kc, 0:ncol],
                                                                     scalar1=c2[:, jm, kc:kc + 1]), r=[r_stg, r_c], w=[r_W])

            with ExitStack() as es12:
                hsT = sb(es12, "hsT", [128, NKC, S], BF16)
                r_hs = k.res()
                for kc in range(NKC):
                    k.op("dve", lambda e, kc=kc: e.tensor_tensor(out=hsT[:, kc, 1:S - 1], in0=self.hT[:, kc, 0:S - 2],
                                                                 in1=self.hT[:, kc, 2:S], op=ALU.add), r=all_hT, w=[r_hs])
                    k.op("act", lambda e, kc=kc: e.copy(out=hsT[:, kc, 0:1], in_=self.hT[:, kc, 1:2]), r=all_hT, w=[r_hs])
                    k.op("act", lambda e, kc=kc: e.copy(out=hsT[:, kc, S - 1:S], in_=self.hT[:, kc, S - 2:S - 1]), r=all_hT, w=[r_hs])
                with ExitStack() as es1:
                    h1g = [sb(es1, f"h1g{dd}", [128, S], BF16) for dd in range(2)]
                    r_h1g = [k.res() for _ in range(2)]
                    stg = [sb(es1, f"lstg{b}", [128, NKC, 128]) for b in range(2)]
                    r_stg = [k.res() for _ in range(2)]
                    W1 = [sb(es1, f"lW1_{b}", [128, NKC, 128], BF16) for b in range(2)]
                    W2 = [sb(es1, f"lW2_{b}", [128, NKC, 128], BF16) for b in range(2)]
                    r_W = [k.res() for _ in range(2)]
                    groups = [(d["rw_w1cat"][j], 3, AF.Tanh, h1w, r_h1w), (d["rw_a1cat"][j], 4, AF.Copy, h1a, r_h1a),
                              (d["rw_g1"][j, 0], 5, AF.Sigmoid, h1g[0], r_h1g[0]), (d["rw_g1"][j, 1], 5, AF.Sigmoid, h1g[1], r_h1g[1])]
                    for gi, (src, jm, fn, dst, r_dst) in enumerate(groups):
                        b = gi % 2
                        k.dma("sp", f"lstg{b}", stg[b][:], src.rearrange("(kc p) n -> p kc n", p=128), w=[r_stg[b]])
                        scale_w(stg[b], r_stg[b], W1[b], W2[b], r_W[b], jm, 128)
                        for tb in range(NTB):
                            tsl = slice(tb * 512, (tb + 1) * 512)
                            pi = tb % 2
                            for kc in range(NKC):
                                k.op("pe", lambda e, kc=kc: e.matmul(self.ps[pi][:, :], W1[b][:, kc, :], self.hT[:, kc, tsl],
                                                                     start=(kc == 0), stop=False),
                                     r=[r_W[b], self.r_hT[kc][tb]], w=[self.r_ps[pi]])
                            for kc in range(NKC):
                                k.op("pe", lambda e, kc=kc: e.matmul(self.ps[pi][:, :], W2[b][:, kc, :], hsT[:, kc, tsl],
                                                                     start=False, stop=(kc == NKC - 1)),
                                     r=[r_W[b], r_hs], w=[self.r_ps[pi]])
                            k.op("act", lambda e: e.activation(out=dst[:, tsl], in_=self.ps[pi][:, :], func=fn),
                                 r=[self.r_ps[pi]], w=[r_dst])
                    g2 = [sb(es1, f"g2_{dd}", [128, D], BF16) for dd in range(2)]
                    r_g2 = k.res()
                    for dd in range(2):
                        k.dma("pool", "g2", g2[dd][:], d["rw_g2"][j, dd], w=[r_g2])
                    gst = [sb(es1, f"gst{b}", [128, D]) for b in range(2)]
                    r_gst = [k.res() for _ in range(2)]
                    n = 0
                    for dd in range(2):
                        for tt in range(NTT):
                            b = n % 2
                            n += 1
                            for half in range(2):
                                pi = 2 + half
                                k.op("pe", lambda e, half=half, pi=pi: e.matmul(
                                    self.ps[pi][:, :], h1g[dd][:, tt * 128:(tt + 1) * 128], g2[dd][:, half * 512:(half + 1) * 512],
                                    start=True, stop=True), r=[r_h1g[dd], r_g2], w=[self.r_ps[pi]])
                                if half == 0:
                                    k.op("act", lambda e, pi=pi: e.copy(out=gst[b][:, 0:512], in_=self.ps[pi][:, :]),
                                         r=[self.r_ps[pi]], w=[r_gst[b]])
                                else:
                                    k.op("dve", lambda e, pi=pi: e.tensor_copy(out=gst[b][:, 512:1024], in_=self.ps[pi][:, :]),
                                         r=[self.r_ps[pi]], w=[r_gst[b]])
                            k.dma("sp", f"gst{b}", sc["GG"][dd, tt * 128:(tt + 1) * 128, :], gst[b][:], r=[r_gst[b]],
                                  w=[self.r_scr["GG"][dd][tt]])
                    k.barrier()
                with ExitStack() as es2:
                    stg = [sb(es2, f"pstg{b}", [128, NKC, 256]) for b in range(2)]
                    r_stg = [k.res() for _ in range(2)]
                    W1 = sb(es2, "pW1", [128, NKC, D], BF16)
                    W2 = sb(es2, "pW2", [128, NKC, D], BF16)
                    r_W = k.res()
                    rst = [sb(es2, f"rst{b}", [128, D]) for b in range(2)]
                    rstb = [rst[b][:, :].bitcast(BF16)[:, 0:D] for b in range(2)]
                    r_rst = [k.res() for _ in range(2)]
                    n = 0
                    for pj in range(3):
                        for qq in range(4):
                            sb_ = qq % 2
                            k.dma("sp", f"pstg{sb_}", stg[sb_][:],
                                  d["rw_w_rkv"][j, pj, :, qq * 256:(qq + 1) * 256].rearrange("(kc p) n -> p kc n", p=128),
                                  w=[r_stg[sb_]])
                            scale_w(stg[sb_], r_stg[sb_], W1, W2, r_W, pj, 256, col0=qq * 256)
                        for tt in range(NTT):
                            b = n % 2
                            n += 1
                            tb = tt // 4
                            tsl = slice(tt * 128, (tt + 1) * 128)
                            for half in range(2):
                                pi = half
                                for kc in range(NKC):
                                    k.op("pe", lambda e, kc=kc, half=half, pi=pi: e.matmul(
                                        self.ps[pi][:, :], self.hT[:, kc, tsl], W1[:, kc, half * 512:(half + 1) * 512],
                                        start=(kc == 0), stop=False), r=[r_W, self.r_hT[kc][tb]], w=[self.r_ps[pi]])
                                for kc in range(NKC):
                                    k.op("pe", lambda e, kc=kc, half=half, pi=pi: e.matmul(
                                        self.ps[pi][:, :], hsT[:, kc, tsl], W2[:, kc, half * 512:(half + 1) * 512],
                                        start=False, stop=(kc == NKC - 1)), r=[r_W, r_hs], w=[self.r_ps[pi]])
                                dst_t = rst[b] if pj < 2 else rstb[b]
                                if half == 0:
                                    k.op("act", lambda e, pi=pi: e.copy(out=dst_t[:, 0:512], in_=self.ps[pi][:, :]),
                                         r=[self.r_ps[pi]], w=[r_rst[b]])
                                else:
                                    k.op("dve", lambda e, pi=pi: e.tensor_copy(out=dst_t[:, 512:1024], in_=self.ps[pi][:, :]),
                                         r=[self.r_ps[pi]], w=[r_rst[b]])
                            if pj < 2:
                                k.dma("sp", f"rst{b}", sc["RAW"][pj, tt * 128:(tt + 1) * 128, :], rst[b][:], r=[r_rst[b]],
                                      w=[self.r_scr["RAW"][pj][tt]])
                            else:
                                k.dma("sp", f"rst{b}", sc["RAWV"][tt * 128:(tt + 1) * 128, :], rstb[b], r=[r_rst[b]],
                                      w=[self.r_scr["RAW"][pj][tt]])
                    k.barrier()
            with ExitStack() as es3:
                w2c = sb(es3, "w2c", [128, D], BF16)
                a2c = sb(es3, "a2c", [128, D], BF16)
                r_l2 = k.res()
                k.dma("pool", "l2w", w2c[:], d["rw_w2cat"][j], w=[r_l2])
                k.dma("pool", "l2w", a2c[:], d["rw_a2cat"][j], w=[r_l2])
                KKb = sb(es3, "KKb", [128, D]); KAb = sb(es3, "KAb", [128, D]); RKb = sb(es3, "RKb", [128, D])
                r_par = k.res()
                k.dma("sp", "rwp", KKb[:], d["rw_k_k"][j:j + 1, :].partition_broadcast(128), w=[r_par])
                k.dma("sp", "rwp", KAb[:], d["rw_k_a"][j:j + 1, :].partition_broadcast(128), w=[r_par])
                k.dma("sp", "rwp", RKb[:], d["rw_r_k"][j:j + 1, :].partition_broadcast(128), w=[r_par])
                b32 = sb(es3, "b32", [1, 4, D])
                r_b = k.res()
                k.dma("sp", "rwp2", b32[0:1, 0:2, :], d["rw_w0"][j:j + 1, :, :], w=[r_b])
                k.dma("sp", "rwp2", b32[0:1, 2:4, :], d["rw_a0"][j:j + 1, :, :], w=[r_b])
                tri = sb(es3, "tri", [128, 6, 128])
                csel = sb(es3, "csel", [128, 2])
                k.dma("sp", "rwp", tri[:], d["c_tri"].rearrange("q s t -> s q t"), w=[r_par])
                k.dma("sp", "rwp", csel[:], d["c_csel"][:, :], w=[r_par])
                hsc = [self.hT[:, kc, :].bitcast(F32) for kc in range(NKC)]
                mk2 = lambda n_, extra: [sb(es3, f"{n_}{q}", [128, D]) if extra[q] is None else extra[q] for q in range(2)]
                Rr_s = mk2("Rr", [None, hsc[0]]); Rk_s = mk2("Rk", [None, hsc[1]]); kk_s = mk2("kk", [None, hsc[2]])
                RRK_s = mk2("RRK", [None, hsc[3]]); T0_s = mk2("T0", [None, hsc[4]])
                SIG_s = mk2("SIG", [None, hsc[5]]); A_s = mk2("A_", [None, hsc[6]]); KD_s = mk2("KD", [None, hsc[7]])
                E1_s = mk2("E1", [None, None]); E2_s = mk2("E2", [None, None])
                O = [sb(es3, f"O{b}", [128, D], BF16) for b in range(2)]
                FMst = [sb(es3, f"FMst{b}", [64, 8, 128], BF16) for b in range(2)]
                identb = sb(es3, "identb3", [128, 128], BF16)
                r_idb = k.res()
                k.op("act", lambda e: e.copy(out=identb[:], in_=self.ident[:]), r=[self.r_const], w=[r_idb])
                psb = {4: self.ps[4][0:64, :].bitcast(BF16), 5: self.ps[5][0:64, :].bitcast(BF16)}
                sm = sb(es3, "a3sm", [128, 64])
                rkt_s = [sb(es3, f"rkt{q}", [128, 16]) for q in range(2)]
                r_rkt_s = [k.res(), k.res()]
                r2 = lambda: [k.res(), k.res()]
                r_Rr_s, r_Rk_s, r_kk_s, r_RRK_s, r_T0_s = r2(), r2(), r2(), r2(), r2()
                r_SIG_s, r_A_s, r_KD_s, r_E1_s, r_E2_s = r2(), r2(), r2(), r2(), r2()
                r_sm = k.res()
                r_O = [k.res() for _ in range(2)]
                r_FM = [k.res() for _ in range(2)]
                on = [0]
                v3 = lambda t: t[:, :].rearrange("p (h n) -> p h n", h=16)

                fmn = [0]

                def emit_fm(src, r_src, dd, q, tt):
                    on[0] += 1
                    for h8 in range(2):
                        fb = fmn[0] % 2
                        fmn[0] += 1
                        for h4 in range(2):
                            pi = 4 + h4 % 2
                            for hh in range(4):
                                h = h8 * 8 + h4 * 4 + hh
                                k.op("pe", lambda e, h=h, hh=hh, pi=pi: e.transpose(
                                    psb[pi][:, hh * 128:(hh + 1) * 128], src[:, h * 64:(h + 1) * 64], identb[:]),
                                    r=[r_src, r_idb], w=[self.r_ps[pi]])
                            if h4 == 0:
                                k.op("act", lambda e, h4=h4, pi=pi: e.copy(
                                    out=FMst[fb][:, h4 * 4:h4 * 4 + 4, :], in_=psb[pi][:, 0:512].rearrange("p (a t) -> p a t", a=4)),
                                    r=[self.r_ps[pi]], w=[r_FM[fb]])
                            else:
                                k.op("dve", lambda e, h4=h4, pi=pi: e.tensor_copy(
                                    out=FMst[fb][:, h4 * 4:h4 * 4 + 4, :], in_=psb[pi][:, 0:512].rearrange("p (a t) -> p a t", a=4)),
                                    r=[self.r_ps[pi]], w=[r_FM[fb]])
                        k.dma("sp", f"FMst{fb}", sc["FM"][dd, q, tt, :, h8 * 8:(h8 + 1) * 8, :], FMst[fb][:], r=[r_FM[fb]],
                              w=[self.r_scr["FM"][dd][q][tt]])

                def a3_load(tt):
                    q = tt % 2
                    rws = slice(tt * 128, (tt + 1) * 128)
                    k.dma("pool", f"a3r{q}", Rr_s[q][:], sc["RAW"][0, rws, :], r=[self.r_scr["RAW"][0][tt]], w=[r_Rr_s[q]])
                    k.dma("pool", f"a3k{q}", Rk_s[q][:], sc["RAW"][1, rws, :], r=[self.r_scr["RAW"][1][tt]], w=[r_Rk_s[q]])

                a3_load(0)
                for tt in range(NTT):
                    rows = slice(tt * 128, (tt + 1) * 128)
                    if tt + 1 < NTT:
                        a3_load(tt + 1)
                    q_ = tt % 2
                    Rr, Rk, kk, RRK = Rr_s[q_], Rk_s[q_], kk_s[q_], RRK_s[q_]
                    r_Rr, r_Rk, r_kk, r_RRK = r_Rr_s[q_], r_Rk_s[q_], r_kk_s[q_], r_RRK_s[q_]
                    T0, r_T0 = T0_s[0], r_T0_s[0]
                    k.op("dve", lambda e: e.tensor_tensor(out=kk[:], in0=Rk[:], in1=KKb[:], op=ALU.mult), r=[r_Rk, r_par], w=[r_kk])
                    k.op("dve", lambda e: e.tensor_tensor(out=T0[:], in0=kk[:], in1=kk[:], op=ALU.mult), r=[r_kk], w=[r_T0])
                    k.op("dve", lambda e: e.reduce_sum(out=sm[:, 0:16], in_=v3(T0), axis=AX.X), r=[r_T0], w=[r_sm])
                    k.op("act", lambda e: e.activation(out=sm[:, 0:16], in_=sm[:, 0:16], func=AF.Sqrt), r=[r_sm], w=[r_sm])
                    k.op("dve", lambda e: e.tensor_scalar_max(out=sm[:, 0:16], in0=sm[:, 0:16], scalar1=1e-12), r=[r_sm], w=[r_sm])
                    k.op("dve", lambda e: e.reciprocal(out=sm[:, 16:32], in_=sm[:, 0:16]), r=[r_sm], w=[r_sm])
                    k.op("dve", lambda e: e.tensor_tensor(out=v3(kk), in0=v3(kk),
                                                          in1=sm[:, 16:32].unsqueeze(2).to_broadcast([128, 16, 64]), op=ALU.mult),
                         r=[r_kk, r_sm], w=[r_kk])
                    k.op("dve", lambda e: e.tensor_tensor(out=RRK[:], in0=Rr[:], in1=RKb[:], op=ALU.mult), r=[r_Rr, r_par], w=[r_RRK])
                    for dd in range(2):
                        T0, SIG, A_, KD, E1, E2 = T0_s[dd], SIG_s[dd], A_s[dd], KD_s[dd], E1_s[dd], E2_s[dd]
                        r_T0, r_SIG, r_A, r_KD, r_E1, r_E2 = r_T0_s[dd], r_SIG_s[dd], r_A_s[dd], r_KD_s[dd], r_E1_s[dd], r_E2_s[dd]
                        for (h1, r_h1, w2t, bi, dst, r_dst) in ((h1w, r_h1w, w2c, dd, SIG, r_SIG), (h1a, r_h1a, a2c, 2 + dd, A_, r_A)):
                            for half in range(2):
                                pi = half
                                csl = slice(half * 512, (half + 1) * 512)
                                k.op("pe", lambda e: e.matmul(self.ps[pi][:, :], h1[dd * 64:(dd + 1) * 64, rows],
                                                              w2t[dd * 64:(dd + 1) * 64, csl], start=True, stop=False),
                                     r=[r_h1, r_l2], w=[self.r_ps[pi]])
                                k.op("pe", lambda e: e.matmul(self.ps[pi][:, :], self.ones[0:1, :], b32[0:1, bi, csl],
                                                              start=False, stop=True),
                                     r=[r_b, self.r_const], w=[self.r_ps[pi]])
                                k.op("act", lambda e: e.activation(out=dst[:, csl], in_=self.ps[pi][:, :], func=AF.Sigmoid),
                                     r=[self.r_ps[pi]], w=[r_dst])
                        k.op("dve", lambda e: e.scalar_tensor_tensor(out=KD[:], in0=A_[:], scalar=-1.0, in1=KAb[:],
                                                                     op0=ALU.add, op1=ALU.mult), r=[r_A, r_par], w=[r_KD])
                        k.op("dve", lambda e: e.scalar_tensor_tensor(out=KD[:], in0=KD[:], scalar=1.0, in1=Rk[:],
                                                                     op0=ALU.add, op1=ALU.mult), r=[r_KD, r_Rk], w=[r_KD])
                        k.op("dve", lambda e: e.tensor_tensor(out=A_[:], in0=A_[:], in1=kk[:], op=ALU.mult), r=[r_A, r_kk], w=[r_A])
                        k.op("dve", lambda e: e.tensor_tensor(out=T0[:], in0=RRK[:], in1=KD[:], op=ALU.mult), r=[r_RRK, r_KD], w=[r_T0])
                        rkt, r_rkt = rkt_s[dd], r_rkt_s[dd]
                        k.op("dve", lambda e: e.reduce_sum(out=rkt[:, :], in_=v3(T0), axis=AX.X), r=[r_T0], w=[r_rkt])
                        k.dma("sp", f"rkt{dd}", sc["RK"][dd, rows, :], rkt[:], r=[r_rkt], w=[self.r_scr["RK"][dd][tt]])
                        for h in range(16):
                            k.op("pe", lambda e, h=h: e.matmul(self.ps[6][0:64, h * 2:h * 2 + 2], SIG[:, h * 64:(h + 1) * 64], csel[:, :],
                                                               start=True, stop=True), r=[r_SIG, r_par], w=[self.r_ps[6]])
                        k.op("act", lambda e: e.activation(out=PCt[:, dd, tt, :, :], in_=self.ps[6][0:64, 0:32].rearrange("p (h c) -> p h c", c=2),
                                                           func=AF.Exp), r=[self.r_ps[6]], w=[r_PC])
                        def cums(kind, outs):
                            for half in range(2):
                                pi = 2 + half
                                csl = slice(half * 512, (half + 1) * 512)
                                k.op("pe", lambda e: e.matmul(self.ps[pi][:, :], tri[:, dd * 3 + kind, :], SIG[:, csl],
                                                              start=True, stop=True), r=[r_SIG, r_par], w=[self.r_ps[pi]])
                                for (dst, r_dst, scl) in outs:
                                    k.op("act", lambda e, dst=dst, scl=scl: e.activation(out=dst[:, csl], in_=self.ps[pi][:, :],
                                                                                         func=AF.Exp, scale=scl),
                                         r=[self.r_ps[pi]], w=[r_dst])
                        cums(0, [(E1, r_E1, 1.0), (E2, r_E2, -1.0)])
                        ob = on[0] % 2
                        k.op("dve", lambda e: e.tensor_tensor(out=O[ob][:], in0=Rr[:], in1=E1[:], op=ALU.mult), r=[r_Rr, r_E1], w=[r_O[ob]])
                        emit_fm(O[ob], r_O[ob], dd, 0, tt)
                        ob = on[0] % 2
                        k.op("dve", lambda e: e.tensor_tensor(out=O[ob][:], in0=A_[:], in1=E2[:], op=ALU.mult), r=[r_A, r_E2], w=[r_O[ob]])
                        emit_fm(O[ob], r_O[ob], dd, 2, tt)
                        ob = on[0] % 2
                        k.op("dve", lambda e: e.tensor_tensor(out=O[ob][:], in0=KD[:], in1=E2[:], op=ALU.mult), r=[r_KD, r_E2], w=[r_O[ob]])
                        emit_fm(O[ob], r_O[ob], dd, 3, tt)
                        cums(1, [(E1, r_E1, 1.0)])
                        ob = on[0] % 2
                        k.op("dve", lambda e: e.scalar_tensor_tensor(out=O[ob][:], in0=kk[:], scalar=-1.0, in1=E1[:],
                                                                     op0=ALU.mult, op1=ALU.mult), r=[r_kk, r_E1], w=[r_O[ob]])
                        emit_fm(O[ob], r_O[ob], dd, 1, tt)
                        cums(2, [(E2, r_E2, 1.0)])
                        for q, (src, r_src) in enumerate(((A_, r_A), (KD, r_KD))):
                            ob = on[0] % 2
                            on[0] += 1
                            k.op("dve" if q == 0 else "pool", lambda e, src=src: e.tensor_tensor(out=O[ob][:], in0=src[:], in1=E2[:], op=ALU.mult),
                                 r=[r_src, r_E2], w=[r_O[ob]])
                            k.dma("sp", f"Otm{ob}", sc["TM"][dd, q, rows, :], O[ob][:], r=[r_O[ob]], w=[self.r_scr["TM"][dd][q][tt]])
                k.barrier()

    def rwkv_B(self, i, j, PCt, r_PC):
        nc, k, d = self.nc, self.k, self.dram
        sc = self.scr
        oT = self.hT
        r_oT = self.r_hT
        NH = 8
        with ExitStack() as es:
            sb = lambda n, s_, dt=F32: es.enter_context(self.sbuf(n, s_, dt))
            for kc in range(NKC):
                k.op("pool", lambda e, kc=kc: e.memset(oT[:, kc, :], 0.0), w=r_oT[kc])
            msk = sb("msk", [64, 6, 64])
            LNW = sb("LNW", [64, D]); LNB = sb("LNB", [64, D])
            epsg = sb("epsg", [64, 1])
            r_cb = k.res()
            k.dma("sp", "rbc", msk[:], d["c_mask"].rearrange("q s t -> s q t"), w=[r_cb])
            k.dma("sp", "rbc", LNW[:], d["rw_ln_w"][j:j + 1, :].partition_broadcast(64), w=[r_cb])
            k.dma("sp", "rbc", LNB[:], d["rw_ln_b"][j:j + 1, :].partition_broadcast(64), w=[r_cb])
            k.op("dve", lambda e: e.memset(epsg[:], 64e-5), w=[r_cb])
            i64 = self.ident[0:64, 0:64]
            bcm = lambda q: msk[:, q, :].unsqueeze(1).to_broadcast([64, NH, 64])
            bci = i64.unsqueeze(1).to_broadcast([64, NH, 64])

            class Chain:
                pass
            chains = []
            for ci, (dd, hg) in enumerate(((0, 0), (1, 0), (0, 1), (1, 1))):
                c = Chain()
                c.dd, c.hg = dd, hg
                nm = f"c{ci}"
                c.nm = nm
                c.ld = {n_: sb(f"{nm}_{n_}", [64, NH, 64], BF16) for n_ in ("RT", "AT", "BT", "KT", "Bh", "Kh", "Vt")}
                c.ld["Gt"] = sb(f"{nm}_Gt", [64, NH, 64])
                c.r_ld = {n_: k.res() for n_ in c.ld}
                c.rk = sb(f"{nm}_rk", [64, NH]); c.r_rk = k.res()
                c.sl = [sb(f"{nm}_s{q}", [64, NH, 64], BF16) for q in range(6)]
                c.r_sl = [k.res() for _ in range(6)]
                c.ST = sb(f"{nm}_ST", [64, NH, 64]); c.r_ST = k.res()
                c.STb = sb(f"{nm}_STb", [64, NH, 64], BF16); c.r_STb = k.res()
                c.y = sb(f"{nm}_y", [64, NH, 64]); c.r_y = k.res()
                c.sq = sb(f"{nm}_sq", [64, NH, 64]); c.r_sq = k.res()
                c.sm = sb(f"{nm}_sm", [64, 8 * NH]); c.r_sm = k.res()
                c.pb = [ci * 2, ci * 2 + 1]
                c.pbi = 0
                chains.append(c)

            def p3(pi):
                return self.ps[pi][0:64, :].rearrange("p (h n) -> p h n", h=NH)

            def mm(c, pi, pairs, rs):
                for h in range(NH):
                    for q, (lt, rt) in enumerate(pairs):
                        k.op("pe", lambda e, h=h, lt=lt, rt=rt, q=q: e.matmul(
                            self.ps[pi][0:64, h * 64:(h + 1) * 64], lt[:, h, :], rt[:, h, :],
                            start=(q == 0), stop=(q == len(pairs) - 1)), r=rs, w=[self.r_ps[pi]])

            def nb(c):
                c.pbi = (c.pbi + 1) % 2
                return c.pb[c.pbi]

            def chunk_steps(c, n):
                dd, hg = c.dd, c.hg
                ct = n if dd == 0 else 31 - n
                tt, half = ct // 2, ct % 2
                rows = slice(ct * 64, (ct + 1) * 64)
                hs = slice(hg * NH, (hg + 1) * NH)
                cs = slice(hg * 512, (hg + 1) * 512)
                L, R = c.ld, c.r_ld
                for q, n_ in enumerate(("RT", "AT", "BT", "KT")):
                    k.dma("sp", f"{c.nm}{n_}", L[n_][:], sc["FM"][dd, q, tt, :, hs, half * 64:(half + 1) * 64],
                          r=[self.r_scr["FM"][dd][q][tt]], w=[R[n_]])
                for q, n_ in enumerate(("Bh", "Kh")):
                    k.dma("sp", f"{c.nm}{n_}", L[n_][:].rearrange("p h n -> p (h n)"), sc["TM"][dd, q, rows, cs],
                          r=[self.r_scr["TM"][dd][q][ct // 2]], w=[R[n_]])
                yield
                P1, P1T, P2, P2T, T, U6 = range(6)
                S_, RS = c.sl, c.r_sl
                pi = nb(c)
                mm(c, pi, [(L["BT"], L["AT"])], [R["BT"], R["AT"]])
                k.op("dve", lambda e: e.tensor_tensor(out=S_[P1][:], in0=p3(pi), in1=bcm(dd * 3 + 0), op=ALU.mult),
                     r=[self.r_ps[pi], r_cb], w=[RS[P1]])
                yield
                pi = nb(c)
                mm(c, pi, [(L["AT"], L["BT"])], [R["BT"], R["AT"]])
                k.op("dve", lambda e: e.tensor_tensor(out=S_[P1T][:], in0=p3(pi), in1=bcm(dd * 3 + 1), op=ALU.mult),
                     r=[self.r_ps[pi], r_cb], w=[RS[P1T]])
                k.op("dve", lambda e: e.tensor_tensor(out=S_[T][:], in0=S_[P1][:], in1=bci, op=ALU.add),
                     r=[RS[P1], self.r_const], w=[RS[T]])
                yield
                a, aT, b_, bT = P1, P1T, P2, P2T
                for lvl in range(5):
                    pi = nb(c)
                    mm(c, pi, [(S_[a], S_[aT])], [RS[a], RS[aT]])
                    k.op("act", lambda e, pi=pi, bT=bT: e.copy(out=S_[bT][:], in_=p3(pi)), r=[self.r_ps[pi]], w=[RS[bT]])
                    if lvl < 4:
                        pi2 = nb(c)
                        mm(c, pi2, [(S_[aT], S_[a])], [RS[a], RS[aT]])
                        k.op("act", lambda e, pi2=pi2, b_=b_: e.copy(out=S_[b_][:], in_=p3(pi2)), r=[self.r_ps[pi2]], w=[RS[b_]])
                    yield
                    pi = nb(c)
                    mm(c, pi, [(S_[bT], S_[T])], [RS[bT], RS[T]])
                    k.op("dve", lambda e, pi=pi: e.tensor_tensor(out=S_[T][:], in0=S_[T][:], in1=p3(pi), op=ALU.add),
                         r=[self.r_ps[pi], RS[T]], w=[RS[T]])
                    yield
                    a, aT, b_, bT = b_, bT, a, aT
                Aak, Abr, Akr, WT = P1, P1T, P2, P2T
                for (dst, lt, rt, mq, eng) in ((Aak, "KT", "AT", 0, "dve"), (Abr, "BT", "RT", 2, "pool"), (Akr, "KT", "RT", 2, "dve")):
                    pi = nb(c)
                    mm(c, pi, [(L[lt], L[rt])], [R[lt], R[rt]])
                    k.op("dve", lambda e, pi=pi, dst=dst, mq=mq: e.tensor_tensor(out=S_[dst][:], in0=p3(pi), in1=bcm(dd * 3 + mq), op=ALU.mult),
                         r=[self.r_ps[pi], r_cb], w=[RS[dst]])
                    yield
                k.dma("pool", f"{c.nm}Vt", L["Vt"][:].rearrange("p h n -> p (h n)"), sc["RAWV"][rows, cs],
                      r=[self.r_scr["RAW"][2][ct // 2]], w=[R["Vt"]])
                k.dma("pool", f"{c.nm}Gt", L["Gt"][:].rearrange("p h n -> p (h n)"), sc["GG"][dd, rows, cs],
                      r=[self.r_scr["GG"][dd][ct // 2]], w=[R["Gt"]])
                k.dma("pool", f"{c.nm}rk", c.rk[:], sc["RK"][dd, rows, hs], r=[self.r_scr["RK"][dd][ct // 2]], w=[c.r_rk])
                pi = nb(c)
                mm(c, pi, [(L["AT"], c.STb), (S_[Aak], L["Vt"])], [R["AT"], c.r_STb, RS[Aak], R["Vt"]])
                k.op("act", lambda e: e.copy(out=S_[WT][:], in_=p3(pi)), r=[self.r_ps[pi]], w=[RS[WT]])
                yield
                pi = nb(c)
                mm(c, pi, [(S_[T], S_[WT])], [RS[T], RS[WT]])
                k.op("act", lambda e: e.copy(out=S_[U6][:], in_=p3(pi)), r=[self.r_ps[pi]], w=[RS[U6]])
                yield
                piy = nb(c)
                mm(c, piy, [(L["RT"], c.STb), (S_[Abr], S_[U6]), (S_[Akr], L["Vt"])],
                   [R["RT"], c.r_STb, RS[Abr], RS[U6], RS[Akr], R["Vt"]])
                k.op("act", lambda e: e.copy(out=c.y[:], in_=p3(piy)), r=[self.r_ps[piy]], w=[c.r_y])
                pis = nb(c)
                mm(c, pis, [(L["Bh"], S_[U6]), (L["Kh"], L["Vt"])], [R["Bh"], RS[U6], R["Kh"], R["Vt"]])
                pcb = PCt[:, dd, tt, hs, half].unsqueeze(2).to_broadcast([64, NH, 64])
                k.op("dve", lambda e: e.tensor_tensor(out=c.ST[:], in0=c.ST[:], in1=pcb, op=ALU.mult), r=[c.r_ST, r_PC], w=[c.r_ST])
                k.op("dve", lambda e: e.tensor_tensor(out=c.ST[:], in0=c.ST[:], in1=p3(pis), op=ALU.add), r=[c.r_ST, self.r_ps[pis]], w=[c.r_ST])
                k.op("act", lambda e: e.copy(out=c.STb[:], in_=c.ST[:]), r=[c.r_ST], w=[c.r_STb])
                yield

            def epi_steps(c, n):
                dd, hg = c.dd, c.hg
                ct = n if dd == 0 else 31 - n
                cs = slice(hg * 512, (hg + 1) * 512)
                L, R = c.ld, c.r_ld
                sm, r_sm = c.sm, c.r_sm
                bl = lambda a0: sm[:, a0:a0 + NH].unsqueeze(2).to_broadcast([64, NH, 64])
                dv = lambda fn, r, w: k.op("dve", fn, r=r, w=w)
                dv(lambda e: e.reduce_sum(out=sm[:, 0:NH], in_=c.y[:], axis=AX.X), [c.r_y], [r_sm])
                k.op("act", lambda e: e.activation(out=c.sq[:], in_=c.y[:], func=AF.Square), r=[c.r_y], w=[c.r_sq])
                yield
                dv(lambda e: e.reduce_sum(out=sm[:, NH:2 * NH], in_=c.sq[:], axis=AX.X), [c.r_sq], [r_sm])
                dv(lambda e: e.tensor_scalar_mul(out=sm[:, 0:2 * NH], in0=sm[:, 0:2 * NH], scalar1=1.0 / 64), [r_sm], [r_sm])
                yield
                dv(lambda e: e.tensor_tensor(out=sm[:, 2 * NH:3 * NH], in0=sm[:, 0:NH], in1=sm[:, 0:NH], op=ALU.mult), [r_sm], [r_sm])
                dv(lambda e: e.tensor_tensor(out=sm[:, 3 * NH:4 * NH], in0=sm[:, NH:2 * NH], in1=sm[:, 2 * NH:3 * NH], op=ALU.subtract), [r_sm], [r_sm])
                k.op("act", lambda e: e.activation(out=sm[:, 4 * NH:5 * NH], in_=sm[:, 3 * NH:4 * NH], func=AF.Sqrt, bias=epsg[:, 0:1], scale=1.0),
                     r=[r_sm, r_cb], w=[r_sm])
                dv(lambda e: e.reciprocal(out=sm[:, 5 * NH:6 * NH], in_=sm[:, 4 * NH:5 * NH]), [r_sm], [r_sm])
                yield
                z, r_z = c.y, c.r_y
                dv(lambda e: e.tensor_tensor(out=z[:], in0=c.y[:], in1=bl(0), op=ALU.subtract), [c.r_y, r_sm], [r_z])
                yield
                dv(lambda e: e.tensor_tensor(out=z[:], in0=z[:], in1=bl(5 * NH), op=ALU.mult), [r_z, r_sm], [r_z])
                yield
                zf = z[:].rearrange("p h n -> p (h n)")
                k.op("dve", lambda e: e.tensor_tensor(out=zf, in0=zf, in1=LNW[:, cs], op=ALU.mult), r=[r_z, r_cb], w=[r_z])
                yield
                k.op("dve", lambda e: e.tensor_tensor(out=zf, in0=zf, in1=LNB[:, cs], op=ALU.add), r=[r_z, r_cb], w=[r_z])
                yield
                dv(lambda e: e.tensor_tensor(out=c.sq[:], in0=L["Vt"][:], in1=c.rk[:, :].unsqueeze(2).to_broadcast([64, NH, 64]), op=ALU.mult),
                   [R["Vt"], c.r_rk], [c.r_sq])
                yield
                k.op("dve", lambda e: e.tensor_tensor(out=z[:], in0=z[:], in1=c.sq[:], op=ALU.add), r=[r_z, c.r_sq], w=[r_z])
                yield
                k.op("dve", lambda e: e.tensor_tensor(out=z[:], in0=z[:], in1=L["Gt"][:], op=ALU.mult), r=[r_z, R["Gt"]], w=[r_z])
                yield
                pt = nb(c)
                for q in range(4):
                    k.op("pe", lambda e, q=q: e.transpose(self.ps[pt][:, q * 64:(q + 1) * 64], zf[:, q * 128:(q + 1) * 128], i64),
                         r=[r_z, self.r_const], w=[self.r_ps[pt]])
                tb = ct // 8
                osl = oT[:, hg * 4:hg * 4 + 4, ct * 64:(ct + 1) * 64]
                k.op("dve", lambda e: e.tensor_tensor(out=osl, in0=osl, in1=self.ps[pt][:, 0:256].rearrange("p (q t) -> p q t", q=4), op=ALU.add),
                     r=[self.r_ps[pt]] + [r_oT[hg * 4 + q][tb] for q in range(4)], w=[r_oT[hg * 4 + q][tb] for q in range(4)])
                yield

            nchunks = self.cfg.get("rw_chunks", 32)
            for c in chains:
                k.op("pool", lambda e, c=c: e.memset(c.ST[:], 0.0), w=[c.r_ST])
                k.op("pool", lambda e, c=c: e.memset(c.STb[:], 0.0), w=[c.r_STb])
            for n in range(nchunks + 1):
                live = []
                for c in chains:
                    if n < nchunks:
                        live.append(chunk_steps(c, n))
                    if n >= 1:
                        live.append(epi_steps(c, n - 1))
                while live:
                    for g_ in list(live):
                        try:
                            next(g_)
                        except StopIteration:
                            live.remove(g_)
            k.barrier()
        with ExitStack() as es:
            wo = es.enter_context(self.sbuf("rwo", [128, NKC, D], BF16))
            r_wo = k.res()
            k.dma("pool", "rwo", wo[:], d["rw_w_o"][j].rearrange("(kc p) n -> p kc n", p=128), w=[r_wo])
            for tb in range(NTB):
                tsl = slice(tb * 512, (tb + 1) * 512)
                for dc in range(NKC):
                    pi = dc % 2
                    for kc in range(NKC):
                        k.op("pe", lambda e, kc=kc, dc=dc, pi=pi: e.matmul(self.ps[pi][:, :], wo[:, kc, dc * 128:(dc + 1) * 128], oT[:, kc, tsl],
                                                                           start=(kc == 0), stop=(kc == NKC - 1)),
                             r=[r_wo, r_oT[kc][tb]], w=[self.r_ps[pi]])
                    k.op("dve", lambda e, dc=dc, pi=pi: e.scalar_tensor_tensor(
                        out=self.xT[:, dc, tsl], in0=self.ps[pi][:, :], scalar=self.modT[:, i, 16 + dc:17 + dc],
                        in1=self.xT[:, dc, tsl], op0=ALU.mult, op1=ALU.add),
                        r=[self.r_ps[pi], self.r_mod], w=[self.r_xT[dc][tb]])
            k.barrier()

    def attn(self, i):
        nc, k, d = self.nc, self.k, self.dram
        j = i // 2
        DIL = [1, 4, 16]
        ss = lambda start, n, step: slice(start, start + (n - 1) * step + 1, step)
        all_hT = [x for y in self.r_hT for x in y]
        with ExitStack() as es:
            identb = es.enter_context(self.sbuf("identb", [128, 128], BF16))
            r_idb = k.res()
            k.op("act", lambda e: e.copy(out=identb[:], in_=self.ident[:]), r=[self.r_const], w=[r_idb])
            w3 = [es.enter_context(self.sbuf(f"w3_{b}", [128, 3, NKC, 128], BF16)) for b in range(2)]
            r_w3 = [k.res() for _ in range(2)]
            wo = [es.enter_context(self.sbuf(f"wo_{b}", [128, D], BF16)) for b in range(2)]
            r_wo = [k.res() for _ in range(2)]
            bias = [es.enter_context(self.sbuf(f"ab_{b}", [128, 384], F32)) for b in range(2)]
            r_bias = [k.res() for _ in range(2)]
            QT = es.enter_context(self.sbuf("QT", [128, S], BF16))
            KT = es.enter_context(self.sbuf("KT", [128, S], BF16))
            V = es.enter_context(self.sbuf("Vb", [128, 16, 128], BF16))
            r_QT, r_KT, r_V = k.res(), k.res(), k.res()
            Og = [es.enter_context(self.sbuf(f"Og{g}", [128, S], F32)) for g in range(3)]
            r_Og = [k.res() for _ in range(3)]
            LSE = es.enter_context(self.sbuf("LSE", [1, 3, S], F32))
            r_LSE = k.res()
            rowA = es.enter_context(self.sbuf("rowA", [1, S], F32))
            r_rowA = k.res()
            NB = 4
            sc = [es.enter_context(self.sbuf(f"sc{b}", [128, 384], F32)) for b in range(NB)]
            pn = [es.enter_context(self.sbuf(f"pn{b}", [128, 384], BF16)) for b in range(NB)]
            st = [es.enter_context(self.sbuf(f"ast{b}", [128, 8], F32)) for b in range(NB)]
            r_blk = [k.res() for _ in range(NB)]
            PT = [es.enter_context(self.sbuf(f"PT{b}", [128, 384], BF16)) for b in range(2)]
            r_PT = [k.res() for _ in range(2)]
            mrg = es.enter_context(self.sbuf("mrg", [128, S], BF16))
            r_mrg = k.res()
            tmpM = es.enter_context(self.sbuf("tmpM", [128, 512], F32))
            t2 = es.enter_context(self.sbuf("t2M", [128, 512], F32))
            r_tmpM, r_t2 = k.res(), k.res()
            psT = self.ps[4][:, :].bitcast(BF16)

            def load_unit(u):
                g, h = u % 3, u // 3
                b = u % 2
                for t in range(3):
                    c0 = ((g * 3 + t) * 8 + h) * 128
                    k.dma("pool", f"w3_{b}", w3[b][:, t, :, :],
                          d["at_w_qkv"][j, :, c0:c0 + 128].rearrange("(kc p) n -> p kc n", p=128), w=[r_w3[b]])
                k.dma("sp", f"ab_{b}", bias[b][:, :], d["c_abias"][g * 8 + h], w=[r_bias[b]])

            def load_wo(h):
                b = h % 2
                k.dma("pool", f"wo_{b}", wo[b][:, :], d["at_w_o"][j, h * 128:(h + 1) * 128, :], w=[r_wo[b]])

            nblk_it = [0]

            def stageA(u, blk):
                g, h = u % 3, u // 3
                dil = DIL[g]
                nblk = 16 // dil
                r_, ib = blk // nblk, blk % nblk
                kb0, kb1 = max(ib - 1, 0), min(ib + 1, nblk - 1)
                nkb = kb1 - kb0 + 1
                nk = nkb * 128
                bc0 = (kb0 - (ib - 1)) * 128
                qsl = ss(r_ + dil * ib * 128, 128, dil)
                ksl = ss(r_ + dil * kb0 * 128, nk, dil)
                n = nblk_it[0]
                nblk_it[0] += 1
                sb_ = n % NB
                psc = 3 if n % 2 == 0 else 6
                bb = u % 2
                k.op("pe", lambda e: e.matmul(self.ps[psc][:, 0:nk], QT[:, qsl], KT[:, ksl], start=True, stop=True),
                     r=[r_QT, r_KT], w=[self.r_ps[psc]])
                k.op("dve", lambda e: e.tensor_tensor(out=sc[sb_][:, 0:nk], in0=self.ps[psc][:, 0:nk],
                                                      in1=bias[bb][:, bc0:bc0 + nk], op=ALU.add),
                     r=[self.r_ps[psc], r_bias[bb]], w=[r_blk[sb_]])
                k.op("dve", lambda e: e.reduce_max(out=st[sb_][:, 0:1], in_=sc[sb_][:, 0:nk], axis=AX.X),
                     r=[r_blk[sb_]], w=[r_blk[sb_]])
                k.op("dve", lambda e: e.tensor_scalar_mul(out=st[sb_][:, 1:2], in0=st[sb_][:, 0:1], scalar1=-1.0),
                     r=[r_blk[sb_]], w=[r_blk[sb_]])
                k.op("act", lambda e: e.activation(out=sc[sb_][:, 0:nk], in_=sc[sb_][:, 0:nk], func=AF.Exp,
                                                   bias=st[sb_][:, 1:2], scale=1.0, accum_out=st[sb_][:, 2:3]),
                     r=[r_blk[sb_]], w=[r_blk[sb_]])
                k.op("act", lambda e: e.activation(out=st[sb_][:, 4:5], in_=st[sb_][:, 2:3], func=AF.Ln),
                     r=[r_blk[sb_]], w=[r_blk[sb_]])
                k.op("dve", lambda e: e.reciprocal(out=st[sb_][:, 3:4], in_=st[sb_][:, 2:3]),
                     r=[r_blk[sb_]], w=[r_blk[sb_]])
                k.op("dve", lambda e: e.tensor_scalar_mul(out=pn[sb_][:, 0:nk], in0=sc[sb_][:, 0:nk],
                                                          scalar1=st[sb_][:, 3:4]),
                     r=[r_blk[sb_]], w=[r_blk[sb_]])
                k.op("dve", lambda e: e.tensor_tensor(out=st[sb_][:, 5:6], in0=st[sb_][:, 4:5], in1=st[sb_][:, 0:1],
                                                      op=ALU.add),
                     r=[r_blk[sb_]], w=[r_blk[sb_]])
                return (g, r_, kb0, nkb, nblk, qsl, sb_, n)

            def stageB(desc):
                g, r_, kb0, nkb, nblk, qsl, sb_, n = desc
                nk = nkb * 128
                pb = n % 2
                for c in range(nkb):
                    k.op("pe", lambda e, c=c: e.transpose(psT[:, c * 128:(c + 1) * 128], pn[sb_][:, c * 128:(c + 1) * 128],
                                                          identb[:]),
                         r=[r_blk[sb_], r_idb], w=[self.r_ps[4]])
                k.op("act", lambda e: e.copy(out=PT[pb][:, 0:nk], in_=psT[:, 0:nk]),
                     r=[self.r_ps[4]], w=[r_PT[pb]])
                k.op("pe", lambda e: e.transpose(self.ps[5][0:1, 128:256], st[sb_][:, 5:6], self.ident[:]),
                     r=[r_blk[sb_], self.r_const], w=[self.r_ps[5]])
                for c in range(nkb):
                    vb = r_ * nblk + kb0 + c
                    k.op("pe", lambda e, c=c, vb=vb: e.matmul(self.ps[5][:, 0:128], V[:, vb, :], PT[pb][:, c * 128:(c + 1) * 128],
                                                              start=(c == 0), stop=(c == nkb - 1)),
                         r=[r_V, r_PT[pb]], w=[self.r_ps[5]])
                k.op("dve", lambda e: e.tensor_copy(out=Og[g][:, qsl], in_=self.ps[5][:, 0:128]),
                     r=[self.r_ps[5]], w=[r_Og[g]])
                k.op("dve", lambda e: e.tensor_copy(out=LSE[0:1, g, qsl], in_=self.ps[5][0:1, 128:256]),
                     r=[self.r_ps[5]], w=[r_LSE])

            def proj_unit(u):
                g, h = u % 3, u // 3
                b = u % 2
                dil = DIL[g]
                nblk = 16 // dil
                for tb in range(NTB):
                    tsl = slice(tb * 512, (tb + 1) * 512)
                    for kc in range(NKC):
                        k.op("pe", lambda e, kc=kc: e.matmul(self.ps[0][:, :], w3[b][:, 0, kc, :], self.hT[:, kc, tsl],
                                                             start=(kc == 0), stop=(kc == NKC - 1)),
                             r=[r_w3[b], self.r_hT[kc][tb]], w=[self.r_ps[0]])
                    k.op("act", lambda e: e.mul(out=QT[:, tsl], in_=self.ps[0][:, :], mul=float(128 ** -0.5)),
                         r=[self.r_ps[0]], w=[r_QT])
                    for kc in range(NKC):
                        k.op("pe", lambda e, kc=kc: e.matmul(self.ps[1][:, :], w3[b][:, 1, kc, :], self.hT[:, kc, tsl],
                                                             start=(kc == 0), stop=(kc == NKC - 1)),
                             r=[r_w3[b], self.r_hT[kc][tb]], w=[self.r_ps[1]])
                    k.op("act", lambda e: e.copy(out=KT[:, tsl], in_=self.ps[1][:, :]),
                         r=[self.r_ps[1]], w=[r_KT])
                for b4 in range(4):
                    pv = 2 if b4 % 2 == 0 else 7
                    for q in range(4):
                        blk = b4 * 4 + q
                        r_, ib = blk // nblk, blk % nblk
                        tsl = ss(r_ + dil * ib * 128, 128, dil)
                        for kc in range(NKC):
                            k.op("pe", lambda e, kc=kc, q=q, tsl=tsl: e.matmul(
                                self.ps[pv][:, q * 128:(q + 1) * 128], self.hT[:, kc, tsl], w3[b][:, 2, kc, :],
                                start=(kc == 0), stop=(kc == NKC - 1)),
                                r=[r_w3[b]] + all_hT, w=[self.r_ps[pv]])
                    k.op("act", lambda e, b4=b4: e.copy(out=V[:, b4 * 4:b4 * 4 + 4, :],
                                                        in_=self.ps[pv][:, :].rearrange("p (q n) -> p q n", q=4)),
                         r=[self.r_ps[pv]], w=[r_V])

            def merge_head(h):
                b = h % 2
                row = lambda g: LSE[0:1, g, :]
                dv = lambda fn, r, w: k.op("dve", fn, r=r, w=w)
                dv(lambda e: e.tensor_tensor(out=rowA[0:1, :], in0=row(0), in1=row(1), op=ALU.max), [r_LSE], [r_rowA])
                dv(lambda e: e.tensor_tensor(out=rowA[0:1, :], in0=rowA[0:1, :], in1=row(2), op=ALU.max), [r_LSE, r_rowA], [r_rowA])
                for g in range(3):
                    dv(lambda e, g=g: e.tensor_tensor(out=row(g), in0=row(g), in1=rowA[0:1, :], op=ALU.subtract),
                       [r_LSE, r_rowA], [r_LSE])
                k.op("act", lambda e: e.activation(out=LSE[0:1, :, :], in_=LSE[0:1, :, :], func=AF.Exp), r=[r_LSE], w=[r_LSE])
                dv(lambda e: e.tensor_tensor(out=rowA[0:1, :], in0=row(0), in1=row(1), op=ALU.add), [r_LSE], [r_rowA])
                dv(lambda e: e.tensor_tensor(out=rowA[0:1, :], in0=rowA[0:1, :], in1=row(2), op=ALU.add), [r_LSE, r_rowA], [r_rowA])
                dv(lambda e: e.reciprocal(out=rowA[0:1, :], in_=rowA[0:1, :]), [r_rowA], [r_rowA])
                for g in range(3):
                    dv(lambda e, g=g: e.tensor_tensor(out=row(g), in0=row(g), in1=rowA[0:1, :], op=ALU.mult),
                       [r_LSE, r_rowA], [r_LSE])
                for tb in range(NTB):
                    tsl = slice(tb * 512, (tb + 1) * 512)
                    for g in range(3):
                        pi = 6 if g % 2 == 0 else 3
                        k.op("pe", lambda e, g=g, pi=pi: e.matmul(self.ps[pi][:, :], self.ones[0:1, :], LSE[0:1, g, tsl],
                                                                  start=True, stop=True),
                             r=[r_LSE, self.r_const], w=[self.r_ps[pi]])
                        if g == 0:
                            dv(lambda e, pi=pi: e.tensor_tensor(out=tmpM[:, :], in0=Og[0][:, tsl], in1=self.ps[pi][:, :], op=ALU.mult),
                               [r_Og[0], self.r_ps[pi]], [r_tmpM])
                        else:
                            dv(lambda e, g=g, pi=pi: e.tensor_tensor(out=t2[:, :], in0=Og[g][:, tsl], in1=self.ps[pi][:, :], op=ALU.mult),
                               [r_Og[g], self.r_ps[pi]], [r_t2])
                            if g == 1:
                                k.op("dve", lambda e: e.tensor_tensor(out=tmpM[:, :], in0=tmpM[:, :], in1=t2[:, :], op=ALU.add),
                                     r=[r_tmpM, r_t2], w=[r_tmpM])
                            else:
                                k.op("dve", lambda e: e.tensor_tensor(out=mrg[:, tsl], in0=tmpM[:, :], in1=t2[:, :], op=ALU.add),
                                     r=[r_tmpM, r_t2], w=[r_mrg])
                for tb in range(NTB):
                    tsl = slice(tb * 512, (tb + 1) * 512)
                    for dc in range(NKC):
                        pi = 7 if dc % 2 == 0 else 0
                        k.op("pe", lambda e, dc=dc, pi=pi: e.matmul(self.ps[pi][:, :], wo[b][:, dc * 128:(dc + 1) * 128], mrg[:, tsl],
                                                                    start=True, stop=True),
                             r=[r_wo[b], r_mrg], w=[self.r_ps[pi]])
                        k.op("dve", lambda e, dc=dc, pi=pi: e.scalar_tensor_tensor(
                            out=self.xT[:, dc, tsl], in0=self.ps[pi][:, :], scalar=self.modT[:, i, 16 + dc:17 + dc],
                            in1=self.xT[:, dc, tsl], op0=ALU.mult, op1=ALU.add),
                            r=[self.r_ps[pi], self.r_mod], w=[self.r_xT[dc][tb]])

            LA = 2
            pend_merge = [None]
            load_unit(0)
            for u in range(24):
                g, h = u % 3, u // 3
                if g == 0:
                    load_wo(h)
                if u + 1 < 24:
                    load_unit(u + 1)
                proj_unit(u)
                if pend_merge[0] is not None:
                    merge_head(pend_merge[0])
                    pend_merge[0] = None
                pend = []
                for blk in range(16):
                    pend.append(stageA(u, blk))
                    if len(pend) > LA:
                        stageB(pend.pop(0))
                while pend:
                    stageB(pend.pop(0))
                if g == 2:
                    pend_merge[0] = h
            merge_head(pend_merge[0])
            k.barrier()

    def final(self):
        nc, k, d = self.nc, self.k, self.dram
        plain = self.cfg.get("final_plain", False)
        with ExitStack() as es:
            sq = es.enter_context(self.sbuf("fsq", [128, NKC, 512], BF16))
            rstd = es.enter_context(self.sbuf("frstd", [128, 512], F32))
            y = es.enter_context(self.sbuf("fy", [128, NKC, 512], F32))
            ost = [es.enter_context(self.sbuf(f"fo{j}", [128, D], F32)) for j in range(2)]
            r_o = [k.res() for _ in range(2)]
            r_sq, r_rstd, r_y = k.res(), k.res(), k.res()
            gfin = self.gT[:, 2 * DEPTH, :]
            for tb in range(NTB):
                tsl = slice(tb * 512, (tb + 1) * 512)
                if not plain:
                    self.norm_core(es, tb, None, None, None, sq, r_sq, rstd, r_rstd, 7)
                for kc in range(NKC):
                    if plain:
                        k.op("dve", lambda e, kc=kc: e.tensor_copy(out=y[:, kc, :], in_=self.xT[:, kc, tsl]),
                             r=[self.r_xT[kc][tb]], w=[r_y])
                    else:
                        k.op("dve", lambda e, kc=kc: e.scalar_tensor_tensor(
                            out=y[:, kc, :], in0=self.xT[:, kc, tsl], scalar=gfin[:, kc:kc + 1], in1=rstd[:, :],
                            op0=ALU.mult, op1=ALU.mult),
                            r=[self.r_xT[kc][tb], r_rstd, self.r_const], w=[r_y])
                for t4 in range(4):
                    tt = tb * 4 + t4
                    ob = tt % 2
                    for half in range(2):
                        pi = (tt * 2 + half) % 4
                        for j in range(4):
                            kc = half * 4 + j
                            k.op("pe", lambda e, kc=kc, j=j, pi=pi: e.transpose(
                                self.ps[pi][:, j * 128:(j + 1) * 128], y[:, kc, t4 * 128:(t4 + 1) * 128], self.ident[:]),
                                r=[r_y, self.r_const], w=[self.r_ps[pi]])
                        if half == 0:
                            k.op("act", lambda e, pi=pi, ob=ob: e.copy(out=ost[ob][:, 0:512], in_=self.ps[pi][:, :]),
                                 r=[self.r_ps[pi]], w=[r_o[ob]])
                        else:
                            k.op("dve", lambda e, pi=pi, ob=ob: e.tensor_copy(out=ost[ob][:, 512:1024], in_=self.ps[pi][:, :]),
                                 r=[self.r_ps[pi]], w=[r_o[ob]])
                    k.dma("sp", f"fo{ob}", d["out"][tt * 128:(tt + 1) * 128, :], ost[ob][:], r=[r_o[ob]])


def host_consts():
    ident = np.eye(128, dtype=np.float32)
    sel = np.zeros((32, 32 * 128), np.float32)
    for e in range(32):
        sel[e, e * 128:(e + 1) * 128] = 1.0
    ab = np.zeros((24, 128, 384), np.float32)
    ii = np.arange(128)[:, None]
    jj = np.arange(384)[None, :]
    rel = np.abs((jj - 128) - ii).astype(np.float32)
    for g, dil in enumerate((1, 4, 16)):
        for h in range(8):
            slope = 2.0 ** (-8.0 * (g * 8 + h + 1) / 24.0)
            ab[g * 8 + h] = np.where(rel <= 64, -slope * rel * dil, -1e30)
    CW = -float(np.exp(-0.5))
    tri = np.zeros((6, 128, 128), np.float32)
    msk = np.zeros((6, 64, 64), np.float32)
    sidx = np.arange(128)[:, None]
    tidx = np.arange(128)[None, :]
    same = (sidx // 64) == (tidx // 64)
    s64 = np.arange(64)[:, None]
    t64 = np.arange(64)[None, :]
    for dd in range(2):
        before = (sidx < tidx) if dd == 0 else (sidx > tidx)
        after = (sidx > tidx) if dd == 0 else (sidx < tidx)
        tri[dd * 3 + 0] = np.where(same & (before | (sidx == tidx)), CW, 0.0)
        tri[dd * 3 + 1] = np.where(same & before, CW, 0.0)
        tri[dd * 3 + 2] = np.where(same & after, CW, 0.0)
        b64 = (s64 < t64) if dd == 0 else (s64 > t64)
        msk[dd * 3 + 0] = b64
        msk[dd * 3 + 1] = b64.T
        msk[dd * 3 + 2] = b64 | (s64 == t64)
    csel = np.zeros((128, 2), np.float32)
    csel[:64, 0] = CW
    csel[64:, 1] = CW
    return {"c_ident": ident, "c_sel": sel, "c_abias": ab, "c_tri": tri, "c_csel": csel, "c_mask": msk}


def prep_shared(inp):
    f = lambda a: np.ascontiguousarray(a, dtype=np.float32)
    sh = {}
    sh["ada_w"] = f(inp["ada_w"])
    sh["ada_bT"] = f(np.asarray(inp["ada_b"]).reshape(DEPTH, 48, 128).transpose(0, 2, 1))
    sh["gtmT"] = f(np.asarray(inp["norm_tm_g"]).reshape(DEPTH, NKC, 128).transpose(0, 2, 1))
    sh["gcmT"] = f(np.asarray(inp["norm_cm_g"]).reshape(DEPTH, NKC, 128).transpose(0, 2, 1))
    sh["gfinT"] = f(np.asarray(inp["final_g"]).reshape(NKC, 128).T)
    rg = np.asarray(inp["moe_router_g"])
    re_ = np.asarray(inp["moe_router_e"])
    wr = np.concatenate([rg] + [re_[:, g] for g in range(4)], axis=-1)
    sh["wr"] = f(wr.reshape(DEPTH, NKC, 128, 36).transpose(0, 2, 1, 3))
    br = np.concatenate([np.asarray(inp["moe_router_g_b"]), np.asarray(inp["moe_router_e_b"]).reshape(DEPTH, 32)], axis=-1)
    sh["br"] = f(br.reshape(DEPTH, 1, 36))
    sh["rw_muT"] = f(np.asarray(inp["rw_mu"]).reshape(2, 6, NKC, 128).transpose(0, 3, 1, 2))
    sh["rw_w_rkv"] = f(inp["rw_w_rkv"])
    sh["rw_w1cat"] = f(np.concatenate([np.asarray(inp["rw_w1"])[:, 0], np.asarray(inp["rw_w1"])[:, 1]], axis=-1))
    sh["rw_a1cat"] = f(np.concatenate([np.asarray(inp["rw_a1"])[:, 0], np.asarray(inp["rw_a1"])[:, 1]], axis=-1))
    sh["rw_g1"] = f(inp["rw_g1"])
    sh["rw_w2cat"] = f(np.asarray(inp["rw_w2"]).reshape(2, 128, D))
    sh["rw_a2cat"] = f(np.asarray(inp["rw_a2"]).reshape(2, 128, D))
    sh["rw_g2"] = f(inp["rw_g2"])
    sh["rw_w0"] = f(inp["rw_w0"])
    sh["rw_a0"] = f(inp["rw_a0"])
    for n_ in ("rw_k_k", "rw_k_a", "rw_ln_w", "rw_ln_b"):
        sh[n_] = f(inp[n_])
    sh["rw_r_k"] = f(np.asarray(inp["rw_r_k"]).reshape(2, D))
    sh["rw_w_o"] = f(inp["rw_w_o"])
    sh["at_w_qkv"] = f(inp["at_w_qkv"])
    sh["at_w_o"] = f(inp["at_w_o"])
    sh["moe_w_gate"] = f(inp["moe_w_gate"])
    sh["moe_w_up"] = f(inp["moe_w_up"])
    sh["moe_w_down"] = f(inp["moe_w_down"])
    sh.update(host_consts())
    return sh


def prep_core(inp, b):
    return {
        "x": np.ascontiguousarray(np.asarray(inp["x"])[b], dtype=np.float32),
        "cT": np.ascontiguousarray(np.asarray(inp["c"])[b].reshape(NKC, 128).T, dtype=np.float32),
    }


def build_prog(cfg):
    nc = bass.Bass("TRN2", target_bir_lowering=False)
    p = Prog(nc, cfg)
    with p.k.es:
        p.build()
    return nc, p


def run(inp, cfg, cores):
    nc, p = build_prog(cfg)
    sh = prep_shared(inp)
    in_maps = []
    for b in cores:
        m = dict(sh)
        m.update(prep_core(inp, b))
        m = {kk: v for kk, v in m.items() if kk in p.dram}
        in_maps.append(m)
    res = run_bass_kernel_spmd(nc, in_maps, core_ids=list(range(len(cores))))
    return res


def kernel(**inputs):
    res = run(inputs, {}, list(range(8)))
    out = np.stack([np.asarray(r["out"]) for r in res.results], axis=0)
    return out.astype(np.float32)
```
